# Optimizing a Trainium2 kernel written in Bass

```python
import jax
import jax.numpy as jnp
from jax import lax
import numpy as np

D_MODEL = 1024
BATCH = 4
SEQ = 4096
DEPTH = 2

GRID_W = 64
CTX_LEN = 256
HEAD_DIM = 64
DN_HEADS = 6
ATTN_HEADS = 6
ATTN_KV_HEADS = 2
ATTN_GROUP = ATTN_HEADS // ATTN_KV_HEADS
MLP_GROUPS = 4
DN_WIDTH = DN_HEADS * HEAD_DIM
ATTN_WIDTH = ATTN_HEADS * HEAD_DIM
ATTN_KV_WIDTH = ATTN_KV_HEADS * HEAD_DIM
MLP_WIDTH = MLP_GROUPS * HEAD_DIM
D_MIX = DN_WIDTH + ATTN_WIDTH + MLP_WIDTH
SPLIT_SIZES = (3 * DN_WIDTH, DN_WIDTH, 2 * DN_HEADS, 2 * DN_HEADS, ATTN_WIDTH, ATTN_KV_WIDTH, ATTN_KV_WIDTH, MLP_WIDTH, MLP_WIDTH)
IN_COLS = 3 * DN_WIDTH + DN_WIDTH + 4 * DN_HEADS + ATTN_WIDTH + 2 * ATTN_KV_WIDTH + 2 * MLP_WIDTH
CONV_K = 3
DN_CHUNK = 64
ATTN_BLOCK = 128
ATTN_SCALE = HEAD_DIM ** -0.5
MLP_CHUNK = 128
ROPE_THETA = 10000.0
ROPE_AXIS_DIM = HEAD_DIM // 2
ROPE_PAIRS = ROPE_AXIS_DIM // 2
D_FF = 2816
MOE_EXPERTS = 8
MOE_TOP_K = 2
MOE_D_FF = 3584
MOE_BLOCK = 256
N_DENSE = (DEPTH + 1) // 2
N_MOE = DEPTH // 2
NORM_EPS = 1e-6

kernel_name = 'hybrid_dit_deltanet_gqa_sgu_moe'


def rms_norm(x, gain):
    xf = x.astype(jnp.float32)
    y = xf * lax.rsqrt(jnp.mean(xf * xf, axis=-1, keepdims=True) + NORM_EPS)
    return (y * gain.astype(jnp.float32)).astype(x.dtype)


def ada_rms_norm(x, gain, shift, scale):
    xf = x.astype(jnp.float32)
    y = xf * lax.rsqrt(jnp.mean(xf * xf, axis=-1, keepdims=True) + NORM_EPS) * gain.astype(jnp.float32)
    return (y * (1.0 + scale.astype(jnp.float32)) + shift.astype(jnp.float32)).astype(x.dtype)


def l2_normalize(x):
    return x * lax.rsqrt(jnp.sum(x * x, axis=-1, keepdims=True) + NORM_EPS)


def split_columns(z):
    idx = np.cumsum(SPLIT_SIZES)[:-1].tolist()
    return jnp.split(z, idx, axis=-1)


def _rev(t, flag):
    return jnp.flip(t, axis=1) if flag else t


def centred_depthwise_conv(x, w):
    return lax.conv_general_dilated(x, w[:, None, :].astype(x.dtype), window_strides=(1,),
                                    padding=[(CONV_K // 2, CONV_K // 2)],
                                    dimension_numbers=('NWC', 'WIO', 'NWC'),
                                    feature_group_count=x.shape[-1])


def axial_rope_tables(n_tokens):
    rows = n_tokens // GRID_W
    row = jnp.repeat(jnp.arange(rows, dtype=jnp.float32), GRID_W)
    col = jnp.tile(jnp.arange(GRID_W, dtype=jnp.float32), rows)
    inv = ROPE_THETA ** (-2.0 * jnp.arange(ROPE_PAIRS, dtype=jnp.float32) / ROPE_AXIS_DIM)
    ang = jnp.stack([row[:, None] * inv, col[:, None] * inv], axis=1)
    return jnp.cos(ang), jnp.sin(ang)


def apply_axial_rope(x, cos, sin):
    xr = x.reshape(x.shape[:-1] + (2, 2, ROPE_PAIRS))
    a, b = xr[..., 0, :], xr[..., 1, :]
    c = cos[None, :, None].astype(x.dtype)
    s = sin[None, :, None].astype(x.dtype)
    out = jnp.stack([a * c - b * s, a * s + b * c], axis=-2)
    return out.reshape(x.shape)


def chunk_gated_delta(q, k, v, g, beta, state):
    B, T, H, Dk = q.shape
    Dv = v.shape[-1]
    n = T // DN_CHUNK

    def chunks(t):
        t = t.reshape((B, n, DN_CHUNK, H) + t.shape[3:])
        return jnp.moveaxis(jnp.moveaxis(t, 3, 2), 1, 0)

    qc, kc, vc = chunks(q), chunks(k), chunks(v)
    gc = jnp.cumsum(chunks(g), axis=-1)
    bc = chunks(beta)[..., None]
    incl = jnp.tril(jnp.ones((DN_CHUNK, DN_CHUNK), bool))
    strict = jnp.tril(jnp.ones((DN_CHUNK, DN_CHUNK), bool), -1)
    decay = jnp.exp(jnp.where(incl, gc[..., :, None] - gc[..., None, :], -jnp.inf))
    kb = kc * bc
    a_mat = jnp.where(strict, jnp.einsum('nbhid,nbhjd->nbhij', kb, kc) * decay, 0.0)
    rhs = jnp.concatenate([vc * bc, kb * jnp.exp(gc)[..., None]], axis=-1)
    sol = lax.linalg.triangular_solve(a_mat, rhs, left_side=True, lower=True, unit_diagonal=True)
    u, w = sol[..., :Dv], sol[..., Dv:]
    attn = jnp.einsum('nbhid,nbhjd->nbhij', qc, kc) * decay

    def step(S, xs):
        q_i, k_i, u_i, w_i, g_i, a_i = xs
        v_new = u_i - jnp.einsum('bhcd,bhde->bhce', w_i, S)
        o_i = (jnp.einsum('bhcd,bhde->bhce', q_i * jnp.exp(g_i)[..., None], S)
               + jnp.einsum('bhij,bhje->bhie', a_i, v_new))
        g_last = g_i[..., -1:]
        S = (S * jnp.exp(g_last)[..., None]
             + jnp.einsum('bhcd,bhce->bhde', k_i * jnp.exp(g_last - g_i)[..., None], v_new))
        return S, o_i

    S, o = lax.scan(step, state, (qc, kc, u, w, gc, attn))
    o = jnp.swapaxes(jnp.moveaxis(o, 0, 1), 2, 3).reshape(B, T, H, Dv)
    return o, S


def dn_inputs(z_qkv, z_beta, z_alpha, conv_w, a_log, dt_bias):
    B, T, _ = z_qkv.shape
    qkv = jax.nn.silu(centred_depthwise_conv(z_qkv, conv_w)).astype(jnp.float32)
    qkv = qkv.reshape(B, T, 3, DN_HEADS, HEAD_DIM)
    q = l2_normalize(qkv[:, :, 0]) * (HEAD_DIM ** -0.5)
    k = l2_normalize(qkv[:, :, 1])
    v = qkv[:, :, 2]
    beta = jax.nn.sigmoid(z_beta.astype(jnp.float32)).reshape(B, T, 2, DN_HEADS)
    g = -jnp.exp(a_log.astype(jnp.float32)) * jax.nn.softplus(
        z_alpha.astype(jnp.float32).reshape(B, T, 2, DN_HEADS) + dt_bias.astype(jnp.float32))
    return q, k, v, g, beta


def dn_bidirectional(ctx_in, lat_in):
    qc, kc, vc, gc, bc = ctx_in
    ql, kl, vl, gl, bl = lat_in
    B, _, H, Dk = qc.shape
    o_ctx, o_lat = [], []
    for d in range(2):
        rev = d == 1
        s0 = jnp.zeros((B, H, Dk, vc.shape[-1]), jnp.float32)
        oc, s_ctx = chunk_gated_delta(_rev(qc, rev), _rev(kc, rev), _rev(vc, rev),
                                      _rev(gc[:, :, d], rev), _rev(bc[:, :, d], rev), s0)
        ol, _ = chunk_gated_delta(_rev(ql, rev), _rev(kl, rev), _rev(vl, rev),
                                  _rev(gl[:, :, d], rev), _rev(bl[:, :, d], rev), s_ctx)
        o_ctx.append(_rev(oc, rev))
        o_lat.append(_rev(ol, rev))
    return o_ctx[0] + o_ctx[1], o_lat[0] + o_lat[1]


def dn_output(o, z_gate, norm_g):
    B, T, H, D = o.shape
    gate = jax.nn.silu(z_gate.astype(jnp.float32)).reshape(B, T, H, D)
    return (rms_norm(o, norm_g) * gate).reshape(B, T, H * D).astype(z_gate.dtype)


def attn_heads(zq, zk, zv, q_gain, k_gain):
    B, T, _ = zq.shape
    q = rms_norm(zq.reshape(B, T, ATTN_HEADS, HEAD_DIM), q_gain)
    k = rms_norm(zk.reshape(B, T, ATTN_KV_HEADS, HEAD_DIM), k_gain)
    v = zv.reshape(B, T, ATTN_KV_HEADS, HEAD_DIM)
    return q, k, v


def context_attention(q, k, v):
    B, L, H, D = q.shape
    qg = q.reshape(B, L, ATTN_KV_HEADS, ATTN_GROUP, D)
    s = jnp.einsum('bqhgd,bkhd->bhgqk', qg, k, preferred_element_type=jnp.float32) * ATTN_SCALE
    p = jax.nn.softmax(s, axis=-1).astype(v.dtype)
    return jnp.einsum('bhgqk,bkhd->bqhgd', p, v).reshape(B, L, H * D)


def latent_attention(q, k, v, k_ctx, v_ctx):
    B, S, H, D = q.shape
    k_all = jnp.concatenate([k_ctx, k], axis=1)
    v_all = jnp.concatenate([v_ctx, v], axis=1)
    qb = jnp.moveaxis(q.reshape(B, S // ATTN_BLOCK, ATTN_BLOCK, ATTN_KV_HEADS, ATTN_GROUP, D), 1, 0)

    def attend(q_blk):
        s = jnp.einsum('bqhgd,bkhd->bhgqk', q_blk, k_all, preferred_element_type=jnp.float32) * ATTN_SCALE
        p = jax.nn.softmax(s, axis=-1).astype(v_all.dtype)
        return jnp.einsum('bhgqk,bkhd->bqhgd', p, v_all)

    o = lax.map(attend, qb)
    return jnp.moveaxis(o, 0, 1).reshape(B, S, H * D)


def spatial_gating(z_u, z_v, norm_g, w_s, b_s):
    B, T, _ = z_u.shape
    gd = MLP_WIDTH // MLP_GROUPS
    u = jax.nn.gelu(z_u)
    v = rms_norm(jax.nn.gelu(z_v).reshape(B, T, MLP_GROUPS, gd), norm_g.reshape(MLP_GROUPS, gd))
    v = v.reshape(B, T // MLP_CHUNK, MLP_CHUNK, MLP_GROUPS, gd)
    mixed = jnp.einsum('gij,bnjgd->bnigd', w_s.astype(v.dtype), v) + b_s.T[None, None, :, :, None].astype(v.dtype)
    return u * mixed.reshape(B, T, MLP_WIDTH)


def token_mixer(h, hc, with_ctx_out, w_in, conv_w, dn_a_log, dn_dt_bias, dn_norm_g, q_norm_g, k_norm_g,
                sgu_norm_g, sgu_w, sgu_b, w_out, rope_cos, rope_sin):
    z = split_columns(h @ w_in)
    zc = split_columns(hc @ w_in)
    dn_lat = dn_inputs(z[0], z[2], z[3], conv_w, dn_a_log, dn_dt_bias)
    dn_ctx = dn_inputs(zc[0], zc[2], zc[3], conv_w, dn_a_log, dn_dt_bias)
    o_ctx, o_lat = dn_bidirectional(dn_ctx, dn_lat)
    q, k, v = attn_heads(z[4], z[5], z[6], q_norm_g, k_norm_g)
    q = apply_axial_rope(q, rope_cos, rope_sin)
    k = apply_axial_rope(k, rope_cos, rope_sin)
    q_c, k_c, v_c = attn_heads(zc[4], zc[5], zc[6], q_norm_g, k_norm_g)
    mixed = jnp.concatenate([dn_output(o_lat, z[1], dn_norm_g),
                             latent_attention(q, k, v, k_c, v_c),
                             spatial_gating(z[7], z[8], sgu_norm_g, sgu_w, sgu_b)], axis=-1)
    y = mixed @ w_out
    if not with_ctx_out:
        return y, None
    mixed_c = jnp.concatenate([dn_output(o_ctx, zc[1], dn_norm_g),
                               context_attention(q_c, k_c, v_c),
                               spatial_gating(zc[7], zc[8], sgu_norm_g, sgu_w, sgu_b)], axis=-1)
    return y, mixed_c @ w_out


def swiglu(h, w1, w3, w2):
    return (jax.nn.silu(h @ w1) * (h @ w3)) @ w2


def moe_swiglu(h, router_w, router_b, w1, w3, w2):
    shape = h.shape
    t = h.reshape(-1, shape[-1])
    n = t.shape[0]
    logits = jnp.einsum('nd,de->ne', t, router_w, preferred_element_type=jnp.float32) + router_b.astype(jnp.float32)
    top_logit, top_idx = lax.top_k(logits, MOE_TOP_K)
    gates = jax.nn.softmax(top_logit, axis=-1)
    n_assign = n * MOE_TOP_K
    expert = top_idx.reshape(-1)
    token = jnp.repeat(jnp.arange(n, dtype=jnp.int32), MOE_TOP_K)
    order = jnp.argsort(expert)
    e_sorted, tok_sorted, gate_sorted = expert[order], token[order], gates.reshape(-1)[order]
    counts = jnp.bincount(expert, length=MOE_EXPERTS)
    padded = (counts + MOE_BLOCK - 1) // MOE_BLOCK * MOE_BLOCK
    starts = jnp.cumsum(counts) - counts
    pad_ends = jnp.cumsum(padded)
    pad_starts = pad_ends - padded
    dest = pad_starts[e_sorted] + jnp.arange(n_assign, dtype=jnp.int32) - starts[e_sorted]
    n_blocks = -(-(n_assign + MOE_EXPERTS * (MOE_BLOCK - 1)) // MOE_BLOCK)
    slot_token = jnp.full((n_blocks * MOE_BLOCK,), n, jnp.int32).at[dest].set(tok_sorted)
    block_expert = jnp.minimum(jnp.searchsorted(pad_ends, jnp.arange(n_blocks) * MOE_BLOCK, side='right'),
                               MOE_EXPERTS - 1)
    t_pad = jnp.concatenate([t, jnp.zeros((1, t.shape[1]), t.dtype)], axis=0)
    xb = t_pad[slot_token].reshape(n_blocks, MOE_BLOCK, t.shape[1])

    def expert_block(args):
        xi, e = args
        return swiglu(xi, w1[e], w3[e], w2[e])

    yb = lax.map(expert_block, (xb, block_expert)).reshape(n_blocks * MOE_BLOCK, t.shape[1])
    y = jnp.zeros_like(t).at[tok_sorted].add(yb[dest] * gate_sorted[:, None].astype(t.dtype))
    return y.reshape(shape)


def channel_mixer(h, layer, ffn_w1, ffn_w3, ffn_w2, router_w, router_b, moe_w1, moe_w3, moe_w2):
    i = layer // 2
    if layer % 2 == 0:
        return swiglu(h, ffn_w1[i], ffn_w3[i], ffn_w2[i])
    return moe_swiglu(h, router_w[i], router_b[i], moe_w1[i], moe_w3[i], moe_w2[i])


def setup_inputs(seed: int = 0) -> dict:
    key = jax.random.key(seed)
    ks = iter(jax.random.split(key, 32))

    def nrm(shape, scale):
        return jax.random.normal(next(ks), shape, jnp.float32) * scale

    def gain(shape):
        return 1.0 + nrm(shape, 0.02)

    dt = jnp.exp(jax.random.uniform(next(ks), (DEPTH, 2, DN_HEADS), jnp.float32, np.log(1e-3), np.log(1e-1)))
    return {
        'x': nrm((BATCH, SEQ, D_MODEL), 1.0),
        'c': nrm((BATCH, D_MODEL), 1.0),
        'ctx': nrm((BATCH, CTX_LEN, D_MODEL), 1.0),
        'c_ctx': nrm((D_MODEL,), 1.0),
        'mod_w': nrm((DEPTH, D_MODEL, 6 * D_MODEL), 0.5 * D_MODEL ** -0.5),
        'mod_b': nrm((DEPTH, 6 * D_MODEL), 0.02),
        'norm1_g': gain((DEPTH, D_MODEL)),
        'norm2_g': gain((DEPTH, D_MODEL)),
        'w_in': nrm((DEPTH, D_MODEL, IN_COLS), D_MODEL ** -0.5),
        'conv_w': nrm((DEPTH, CONV_K, 3 * DN_WIDTH), CONV_K ** -0.5),
        'dn_a_log': jnp.log(jax.random.uniform(next(ks), (DEPTH, 2, DN_HEADS), jnp.float32, 1.0, 16.0)),
        'dn_dt_bias': dt + jnp.log(-jnp.expm1(-dt)),
        'dn_norm_g': gain((DEPTH, HEAD_DIM)),
        'q_norm_g': gain((DEPTH, HEAD_DIM)),
        'k_norm_g': gain((DEPTH, HEAD_DIM)),
        'sgu_norm_g': gain((DEPTH, MLP_WIDTH)),
        'sgu_w': nrm((DEPTH, MLP_GROUPS, MLP_CHUNK, MLP_CHUNK), MLP_CHUNK ** -0.5),
        'sgu_b': gain((DEPTH, MLP_GROUPS, MLP_CHUNK)),
        'w_out': nrm((DEPTH, D_MIX, D_MODEL), D_MIX ** -0.5),
        'ffn_w1': nrm((N_DENSE, D_MODEL, D_FF), D_MODEL ** -0.5),
        'ffn_w3': nrm((N_DENSE, D_MODEL, D_FF), D_MODEL ** -0.5),
        'ffn_w2': nrm((N_DENSE, D_FF, D_MODEL), D_FF ** -0.5),
        'router_w': nrm((N_MOE, D_MODEL, MOE_EXPERTS), D_MODEL ** -0.5),
        'router_b': nrm((N_MOE, MOE_EXPERTS), 0.01),
        'moe_w1': nrm((N_MOE, MOE_EXPERTS, D_MODEL, MOE_D_FF), D_MODEL ** -0.5),
        'moe_w3': nrm((N_MOE, MOE_EXPERTS, D_MODEL, MOE_D_FF), D_MODEL ** -0.5),
        'moe_w2': nrm((N_MOE, MOE_EXPERTS, MOE_D_FF, D_MODEL), MOE_D_FF ** -0.5),
        'final_norm_g': gain((D_MODEL,)),
    }


def reference(x, c, ctx, c_ctx, mod_w, mod_b, norm1_g, norm2_g, w_in, conv_w, dn_a_log, dn_dt_bias, dn_norm_g,
              q_norm_g, k_norm_g, sgu_norm_g, sgu_w, sgu_b, w_out, ffn_w1, ffn_w3, ffn_w2, router_w, router_b,
              moe_w1, moe_w3, moe_w2, final_norm_g):
    rope_cos, rope_sin = axial_rope_tables(x.shape[1])
    cond = jax.nn.silu(c)[:, None, :]
    cond_ctx = jax.nn.silu(c_ctx)[None, None, :]
    xc = ctx
    for layer in range(DEPTH):
        last = layer == DEPTH - 1
        mod = jnp.split(cond @ mod_w[layer] + mod_b[layer], 6, axis=-1)
        mod_c = jnp.split(cond_ctx @ mod_w[layer] + mod_b[layer], 6, axis=-1)
        h = ada_rms_norm(x, norm1_g[layer], mod[0], mod[1])
        hc = ada_rms_norm(xc, norm1_g[layer], mod_c[0], mod_c[1])
        y, yc = token_mixer(h, hc, not last, w_in[layer], conv_w[layer], dn_a_log[layer], dn_dt_bias[layer],
                            dn_norm_g[layer], q_norm_g[layer], k_norm_g[layer], sgu_norm_g[layer], sgu_w[layer],
                            sgu_b[layer], w_out[layer], rope_cos, rope_sin)
        x = x + mod[2] * y
        h = ada_rms_norm(x, norm2_g[layer], mod[3], mod[4])
        x = x + mod[5] * channel_mixer(h, layer, ffn_w1, ffn_w3, ffn_w2, router_w, router_b, moe_w1, moe_w3, moe_w2)
        if not last:
            xc = xc + mod_c[2] * yc
            hc = ada_rms_norm(xc, norm2_g[layer], mod_c[3], mod_c[4])
            xc = xc + mod_c[5] * channel_mixer(hc, layer, ffn_w1, ffn_w3, ffn_w2, router_w, router_b,
                                               moe_w1, moe_w3, moe_w2)
    return rms_norm(x, final_norm_g)
```

```python
import numpy as np
from contextlib import ExitStack
import concourse.bass as bass
import concourse.mybir as mybir
from concourse.bass_utils import run_bass_kernel_spmd

F32 = mybir.dt.float32
BF16 = mybir.dt.bfloat16
AF = mybir.ActivationFunctionType
ALU = mybir.AluOpType
AX = mybir.AxisListType

D = 1024
NCTX = 256
NLAT = 4096
T = NCTX + NLAT
NT = T // 128
DEPTH = 2
HD = 64
IN_COLS = 2712
D_FF = 2816
MOE_FF = 3584
NEXP = 8
EPS = 1e-6
C_QKV, C_GATE, C_BETA, C_ALPHA, C_AQ, C_AK, C_AV, C_U, C_V = 0, 1152, 1536, 1548, 1560, 1944, 2072, 2200, 2456
TK_GATE, TK_BETA, TK_ALPHA, TK_AQ, TK_AK, TK_AV, TK_ZV, TK_W = 0, 384, 396, 408, 792, 920, 1048, 1304


class Buf:
    __slots__ = ("name", "ap", "last_w", "readers", "excl")

    def __init__(self, name, ap=None, excl=False):
        self.name = name
        self.ap = ap
        self.excl = excl
        self.last_w = None
        self.readers = []

    def __getitem__(self, idx):
        return self.ap[idx]


class KB:
    ENGS = ("pe", "act", "dve", "pool", "sp")
    NDMA = 12

    def __init__(self, nc, ctx, same_engine_sync=True):
        self.nc = nc
        self.ctx = ctx
        self.same = same_engine_sync
        self.ops = {e: [] for e in self.ENGS}
        self.seq = {e: 0 for e in self.ENGS}
        self.sems = {e: ctx.enter_context(nc.semaphore("c_" + e)) for e in self.ENGS}
        self.dma_sems, self.dma_cnt, self.dma_rr = {}, {}, {}
        for q in ("sp", "pool", "act"):
            self.dma_sems[q] = [ctx.enter_context(nc.semaphore("d_%s%d" % (q, i))) for i in range(self.NDMA)]
            self.dma_cnt[q] = [0] * self.NDMA
            self.dma_rr[q] = 0
        self.known = {e: {} for e in self.ENGS}
        import os as _os
        self.pool_dma = _os.environ.get("POOLDMA", "1") == "1"
        self.pool_cmp = _os.environ.get("POOLCMP", "0") == "1"
        self.n_ins = 0
        self.uid = 0

    def sbuf(self, name, shape, dt=F32, ctx=None):
        self.uid += 1
        t = (ctx or self.ctx).enter_context(self.nc.sbuf_tensor("%s_%d" % (name, self.uid), list(shape), dt))
        return Buf(name, t)

    def psum(self, name, shape, dt=F32, ctx=None):
        self.uid += 1
        t = (ctx or self.ctx).enter_context(self.nc.psum_tensor("%s_%d" % (name, self.uid), list(shape), dt))
        return Buf(name, t, excl=True)

    def dram(self, name, shape, dt=F32):
        t = self.nc.dram_tensor(name, list(shape), dt, kind="Internal")
        return Buf(name, t.ap())

    def _need(self, eng, tok, waits):
        if tok is None:
            return
        key, val = tok
        if key == eng and (eng == "pe" or not self.same):
            return
        if val > waits.get(key, 0):
            waits[key] = val

    def _sem(self, key):
        if isinstance(key, str):
            return self.sems[key]
        return self.dma_sems[key[0]][key[1]]

    def op(self, eng, fn, reads=(), writes=(), dma=False):
        if eng == "pool":
            if dma and not self.pool_dma:
                eng = "sp"
            elif not dma and not self.pool_cmp:
                eng = "dve"
        ex = [b for b in reads if b.excl]
        if ex:
            reads = [b for b in reads if not b.excl]
            writes = list(writes) + [b for b in ex if b not in writes]
        waits = {}
        for b in reads:
            self._need(eng, b.last_w, waits)
        for b in writes:
            self._need(eng, b.last_w, waits)
            for r in b.readers:
                self._need(eng, r, waits)
        if dma:
            q = eng
            i = self.dma_rr[q]
            self.dma_rr[q] = (i + 1) % self.NDMA
            key = (q, i)
            prev = self.dma_cnt[q][i]
            if prev > 0 and 16 * prev > waits.get(key, 0):
                waits[key] = 16 * prev
            self.dma_cnt[q][i] = prev + 1
            tok = (key, 16 * (prev + 1))
            inc = 16
        else:
            self.seq[eng] += 1
            tok = (eng, self.seq[eng])
            inc = 1
        kn = self.known[eng]
        wl = []
        for key, val in waits.items():
            if kn.get(key, 0) >= val:
                continue
            kn[key] = val
            wl.append((self._sem(key), val))
        self.n_ins += 1
        self.ops[eng].append((wl, fn, self._sem(tok[0]), inc))
        for b in reads:
            b.readers.append(tok)
        for b in writes:
            b.last_w = tok
            b.readers = []
        return tok

    def barrier(self):
        for e in self.ENGS:
            wl = []
            kn = self.known[e]
            for e2 in self.ENGS:
                if e2 != e and self.seq[e2] > kn.get(e2, 0):
                    kn[e2] = self.seq[e2]
                    wl.append((self.sems[e2], self.seq[e2]))
            if self.same and e != "pe" and self.seq[e] > kn.get(e, 0):
                kn[e] = self.seq[e]
                wl.append((self.sems[e], self.seq[e]))
            for q in self.dma_sems:
                for i in range(self.NDMA):
                    v = 16 * self.dma_cnt[q][i]
                    if v > kn.get((q, i), 0):
                        kn[(q, i)] = v
                        wl.append((self.dma_sems[q][i], v))
            if wl:
                self.ops[e].append((wl, None, None, 0))

    def dma(self, q, out_ap, in_ap, reads=(), writes=(), **kw):
        return self.op(q, lambda e: e.dma_start(out=out_ap, in_=in_ap, **kw), reads, writes, dma=True)

    def mm(self, out, lhsT, rhs, start, stop, reads, writes):
        return self.op("pe", lambda e: e.matmul(out, lhsT, rhs, start=start, stop=stop), reads, writes)

    def tr(self, out, in_, ident, reads, writes):
        return self.op("pe", lambda e: e.transpose(out, in_, ident), reads, writes)

    def act(self, out, in_, func, reads, writes, **kw):
        return self.op("act", lambda e: e.activation(out=out, in_=in_, func=func, **kw), reads, writes)

    def copy(self, eng, out, in_, reads, writes):
        if eng == "act":
            return self.act(out, in_, AF.Copy, reads, writes)
        return self.op(eng, lambda e: e.tensor_copy(out, in_), reads, writes)

    def tt(self, eng, out, in0, in1, op, reads, writes):
        return self.op(eng, lambda e: e.tensor_tensor(out, in0, in1, op), reads, writes)

    def ts(self, eng, out, in0, s1, s2, op0, op1, reads, writes):
        if s2 is None:
            return self.op(eng, lambda e: e.tensor_scalar(out, in0, s1, None, op0), reads, writes)
        return self.op(eng, lambda e: e.tensor_scalar(out, in0, s1, s2, op0, op1), reads, writes)

    def stt(self, eng, out, in0, scalar, in1, op0, op1, reads, writes):
        return self.op(eng, lambda e: e.scalar_tensor_tensor(out, in0, scalar, in1, op0, op1), reads, writes)

    def memset(self, eng, out, val, writes):
        return self.op(eng, lambda e: e.memset(out, val), (), writes)

    def final_wait(self, eng, bufs):
        waits = {}
        for b in bufs:
            self._need("__none__", b.last_w, waits)
        self.ops[eng].append(([(self._sem(k), v) for k, v in waits.items()], None, None, 0))

    def emit(self):
        handles = {"pe": "tensor", "act": "scalar", "dve": "vector", "pool": "gpsimd", "sp": "sync"}
        with self.nc.Block() as block:
            for e in self.ENGS:
                lst = self.ops[e]
                if not lst:
                    continue

                def body(engh, lst=lst):
                    for wl, fn, sem, inc in lst:
                        for s, v in wl:
                            engh.wait_ge(s, v)
                        if fn is not None:
                            fn(engh).then_inc(sem, inc)

                getattr(block, handles[e])(body)


class G:
    pass


def rstd_from_ssq(kb, out, ssq, n, reads, writes):
    kb.act(out, ssq, AF.Sqrt, reads, writes, scale=1.0 / n, bias=EPS)
    kb.op("dve", lambda e: e.reciprocal(out, out), writes, writes)


def phase_mod(g, L):
    kb, nc = g.kb, g.kb.nc
    with ExitStack() as sc:
        cond = kb.sbuf("cond", [128, 8, 2], ctx=sc)
        condbc = kb.sbuf("condbc", [128, 16, 128], ctx=sc)
        mbF = kb.sbuf("mbF", [128, 48], ctx=sc)
        mbrow = kb.sbuf("mbrow", [1, 6144], ctx=sc)
        gF = kb.sbuf("gF", [128, 2, 8], ctx=sc)
        modF = kb.sbuf("modF", [128, 32, 2], ctx=sc)
        grow = kb.sbuf("grow", [1, 512], ctx=sc)
        mw = [kb.sbuf("mw%d" % k, [128, 3072], ctx=sc) for k in range(8)]
        for s in range(2):
            kb.dma("sp", cond[:, :, s], g.cvec.ap[s, :].rearrange("(k p) -> p k", p=128), [g.cvec], [cond],
                   allow_slow_non_contiguous=True)
        kb.act(cond[:], cond[:], AF.Silu, [cond], [cond])
        kb.copy("dve", condbc[:], cond[:].rearrange("p k s -> p (k s)").unsqueeze(2).to_broadcast([128, 16, 128]),
                [cond], [condbc])
        kb.dma("sp", mbF[:], g.mod_b.ap[L, :].rearrange("(c p) -> p c", p=128), [g.mod_b], [mbF],
               allow_slow_non_contiguous=True)
        kb.dma("sp", mbrow[:], g.mod_b.ap[L:L + 1, :], [g.mod_b], [mbrow])
        kb.dma("sp", gF[:, 0, :], g.norm1_g.ap[L, :].rearrange("(c p) -> p c", p=128), [g.norm1_g], [gF],
               allow_slow_non_contiguous=True)
        kb.dma("sp", gF[:, 1, :], g.norm2_g.ap[L, :].rearrange("(c p) -> p c", p=128), [g.norm2_g], [gF],
               allow_slow_non_contiguous=True)
        psF, psB = g.ps[0], g.ps[1]
        for h in range(2):
            for k in range(8):
                kb.dma("sp" if k % 2 == 0 else "pool", mw[k][:],
                       g.mod_w.ap[L, 128 * k:128 * k + 128, 3072 * h:3072 * h + 3072], [g.mod_w], [mw[k]])
            for j in range(16):
                for k in range(8):
                    kb.mm(psF[:, 2 * j:2 * j + 2], mw[k][:, 128 * j:128 * j + 128], cond[:, k, :], k == 0, k == 7,
                          [mw[k], cond], [psF])
            kb.tt("dve", modF[:, 16 * h:16 * h + 16, :], psF[:, 0:32].rearrange("p (j s) -> p j s", s=2),
                  mbF[:, 24 * h:24 * h + 16].unsqueeze(2).to_broadcast([128, 16, 2]), ALU.add, [psF, mbF], [modF])
            for s in range(2):
                for cc in range(2):
                    for k in range(8):
                        kb.mm(psB[:, :], condbc[:, 2 * k + s, :], mw[k][:, 2048 + 512 * cc:2048 + 512 * cc + 512],
                              k == 0, k == 7, [mw[k], condbc], [psB])
                    c0 = 3072 * h + 2048 + 512 * cc
                    kb.tt("dve", grow[0:1, :], psB[0:1, :], mbrow[0:1, c0:c0 + 512], ALU.add, [psB, mbrow], [grow])
                    kb.dma("sp", g.gates.ap[L, h, s:s + 1, 512 * cc:512 * cc + 512], grow[0:1, :], [grow], [g.gates])
        for h in range(2):
            sc_ap = modF[:, 16 * h + 8:16 * h + 16, :]
            kb.ts("dve", g.GS[:, L, h, :, :], sc_ap, 1.0, None, ALU.add, None, [modF], [g.GSb])
            kb.tt("dve", g.GS[:, L, h, :, :], g.GS[:, L, h, :, :], gF[:, h, :].unsqueeze(2).to_broadcast([128, 8, 2]),
                  ALU.mult, [gF, g.GSb], [g.GSb])
            kb.copy("dve", g.SH[:, L, h, :, :], modF[:, 16 * h:16 * h + 8, :], [modF], [g.SHb])
    kb.barrier()


def norm_tile_to_hT(g, L, h, xt, tile_is_ctx, hT_out_aps, hT_buf, scr, also_f32=None, xap=None, f32_buf=None):
    kb = g.kb
    s = 1 if tile_is_ctx else 0
    junk, ssq, xn = scr["junk"], scr["ssq"], scr["xn"]
    if xap is None:
        xap = xt[:]
    kb.act(junk[:], xap, AF.Square, [xt, ssq], [junk, ssq], accum_out=ssq[:, 0:1])
    rstd_from_ssq(kb, ssq[:, 0:1], ssq[:, 0:1], D, [ssq], [ssq])
    kb.act(xn[:], xap, AF.Copy, [xt, ssq], [xn], scale=ssq[:, 0:1])
    pT = scr["psT"]
    for c in range(8):
        kb.tr(pT[c // 4][:, (c % 4) * 128:(c % 4) * 128 + 128], xn[:, c * 128:c * 128 + 128], g.ident[:], [xn, g.ident],
              [pT[c // 4]])
    for c in range(8):
        kb.act(hT_out_aps[c], pT[c // 4][:, (c % 4) * 128:(c % 4) * 128 + 128], AF.Identity, [pT[c // 4], g.GSb, g.SHb],
               [hT_buf], scale=g.GS[:, L, h, c, s:s + 1], bias=g.SH[:, L, h, c, s:s + 1])
        if also_f32 is not None:
            kb.act(also_f32[c], pT[c // 4][:, (c % 4) * 128:(c % 4) * 128 + 128], AF.Identity,
                   [pT[c // 4], g.GSb, g.SHb], [f32_buf], scale=g.GS[:, L, h, c, s:s + 1], bias=g.SH[:, L, h, c, s:s + 1])


def load_cast_weight(g, sc, name, dram_buf, src_ap_fn, nk, ncols, stage_bufs, dst, dst_ap_fn):
    kb = g.kb
    for k in range(nk):
        st = stage_bufs[k % len(stage_bufs)]
        kb.dma("sp" if k % 2 == 0 else "pool", st[:, 0:ncols], src_ap_fn(k), [dram_buf], [st])
        kb.copy("pool" if k % 2 == 0 else "dve", dst_ap_fn(k), st[:, 0:ncols], [st], [dst])


def phase_a(g, L, xs_tiles):
    kb = g.kb
    with ExitStack() as sc:
        winb = kb.sbuf("winb", [128, 8, IN_COLS], BF16, ctx=sc)
        stage = [kb.sbuf("wst%d" % i, [128, IN_COLS], ctx=sc) for i in range(2)]
        load_cast_weight(g, sc, "win", g.w_in, lambda k: g.w_in.ap[L, 128 * k:128 * k + 128, :], 8, IN_COLS, stage, winb,
                         lambda k: winb[:, k, :])
        scr = {"junk": kb.sbuf("junk", [128, 1024], ctx=sc), "ssq": kb.sbuf("ssq", [128, 1], ctx=sc),
               "xn": kb.sbuf("xn", [128, 1024], ctx=sc), "psT": [g.ps[0], g.ps[1]]}
        xts = [kb.sbuf("xt%d" % i, [128, 1024], ctx=sc) for i in range(2)]
        hTs = [kb.sbuf("hT%d" % i, [128, 8, 512], BF16, ctx=sc) for i in range(2)]
        toks = [kb.sbuf("tok%d" % i, [128, TK_W], ctx=sc) for i in range(2)]
        fms = [kb.sbuf("fm%d" % i, [128, 512], ctx=sc) for i in range(3)]
        psTM = [g.ps[2], g.ps[3], g.ps[4]]
        psFM = [g.ps[5], g.ps[6]]
        blocks = [(0, 2)] + [(2 + 4 * i, 4) for i in range(8)]
        ti = 0
        fi = 0
        for bi, (t0, nt) in enumerate(blocks):
            hT = hTs[bi % 2]
            ntok = nt * 128
            for j in range(nt):
                t = t0 + j
                xt = xts[ti % 2]
                tok = toks[ti % 2]
                ti += 1
                kb.dma("sp", xt[:], xs_tiles[t].ap, [xs_tiles[t]], [xt])
                norm_tile_to_hT(g, L, 0, xt, t < 2, [hT[:, c, j * 128:j * 128 + 128] for c in range(8)], hT, scr)
                segs = [(psTM[0], 0, 512, C_GATE), (psTM[1], 0, 512, C_GATE + 512), (psTM[2], 0, 24, C_GATE + 1024),
                        (psTM[2], 24, 256, C_V)]
                for ps, o0, w, c0 in segs:
                    for k in range(8):
                        kb.mm(ps[:, o0:o0 + w], hT[:, k, j * 128:j * 128 + 128], winb[:, k, c0:c0 + w], k == 0, k == 7,
                              [hT, winb], [ps])
                kb.copy("dve", tok[:, 0:512], psTM[0][:, :], [psTM[0]], [tok])
                kb.copy("act", tok[:, 512:1024], psTM[1][:, :], [psTM[1]], [tok])
                kb.copy("dve", tok[:, 1024:1304], psTM[2][:, 0:280], [psTM[2]], [tok])
                kb.dma("sp", g.TOK[t].ap, tok[:], [tok], [g.TOK[t]])
            for fc in range(11):
                c0 = C_QKV + 128 * fc if fc < 9 else C_U + 128 * (fc - 9)
                ps = psFM[fi % 2]
                fm = fms[fi % 3]
                fi += 1
                for k in range(8):
                    kb.mm(ps[:, 0:ntok], winb[:, k, c0:c0 + 128], hT[:, k, 0:ntok], k == 0, k == 7, [hT, winb], [ps])
                if fc < 9:
                    kb.copy("act" if fc % 2 == 0 else "dve", fm[:, 0:ntok], ps[:, 0:ntok], [ps], [fm])
                else:
                    kb.act(fm[:, 0:ntok], ps[:, 0:ntok], AF.Gelu, [ps], [fm])
                kb.dma("pool", g.ZQ[fc][bi].ap, fm[:, 0:ntok], [fm], [g.ZQ[fc][bi]])
    kb.barrier()


def phase_attn(g, L, with_ctx):
    kb = g.kb
    with ExitStack() as sc:
        QT = [kb.sbuf("QT%d" % p, [128, T], BF16, ctx=sc) for p in range(3)]
        KT = kb.sbuf("KT", [128, T], BF16, ctx=sc)
        VE = kb.sbuf("VE", [128, NT, 128], BF16, ctx=sc)
        ones = kb.sbuf("ones", [128, 64], BF16, ctx=sc)
        gain = kb.sbuf("gain", [128, 8, 64], ctx=sc)
        kb.memset("dve", ones[:], 1.0, [ones])
        kb.dma("sp", gain[:, 0, :], g.q_norm_g.ap[L:L + 1, :].to_broadcast([128, 64]), [g.q_norm_g], [gain])
        kb.dma("sp", gain[:, 6, :], g.k_norm_g.ap[L:L + 1, :].to_broadcast([128, 64]), [g.k_norm_g], [gain])
        kb.ts("dve", gain[:, 0, :], gain[:, 0, :], 0.125, None, ALU.mult, None, [gain], [gain])
        kb.copy("dve", gain[:, 1:6, :], gain[:, 0:1, :].to_broadcast([128, 5, 64]), [gain], [gain])
        kb.copy("dve", gain[:, 7, :], gain[:, 6, :], [gain], [gain])
        qks = [kb.sbuf("qk%d" % i, [128, 512], ctx=sc) for i in range(2)]
        v32s = [kb.sbuf("v32%d" % i, [128, 128], ctx=sc) for i in range(2)]
        css = [kb.sbuf("cs%d" % i, [128, 64], ctx=sc) for i in range(2)]
        sqt = kb.sbuf("sqt", [128, 512], ctx=sc)
        ssq = kb.sbuf("ssq8", [128, 8], ctx=sc)
        qn = kb.sbuf("qn", [128, 512], ctx=sc)
        qr = kb.sbuf("qr", [128, 512], ctx=sc)
        tm = [kb.sbuf("ropet%d" % i, [128, 256], ctx=sc) for i in range(4)]
        psTr = g.ps[0]
        import os as _os
        STG = int(_os.environ.get("ATTN_STG", "9"))
        for t in range(int(_os.environ.get("ATTN_NT", str(NT)))):
            qk, v32, cs = qks[t % 2], v32s[t % 2], css[t % 2]
            kb.dma("sp", qk[:], g.TOK[t].ap[:, TK_AQ:TK_AQ + 512], [g.TOK[t]], [qk])
            kb.dma("pool", v32[:], g.TOK[t].ap[:, TK_AV:TK_AV + 128], [g.TOK[t]], [v32])
            kb.copy("pool", VE[:, t, :], v32[:], [v32], [VE])
            if STG < 2:
                continue
            kb.tt("pool", sqt[:], qk[:], qk[:], ALU.mult, [qk], [sqt])
            kb.op("dve", lambda e, o=ssq[:, 0:8], i=sqt[:].rearrange("p (h d) -> p h d", h=8): e.reduce_sum(o, i, AX.X),
                  [sqt], [ssq])
            rstd_from_ssq(kb, ssq[:, 0:8], ssq[:, 0:8], 64, [ssq], [ssq])
            if STG < 3:
                continue
            q3 = qn[:].rearrange("p (h d) -> p h d", h=8)
            kb.tt("dve", q3, qk[:].rearrange("p (h d) -> p h d", h=8), ssq[:, 0:8].unsqueeze(2).to_broadcast([128, 8, 64]),
                  ALU.mult, [qk, ssq], [qn])
            if STG < 4:
                continue
            if t >= 2:
                kb.tt("pool", q3, q3, gain[:], ALU.mult, [qn, gain], [qn])
                if STG < 5:
                    continue
                kb.dma("sp", cs[:], g.rope.ap[128 * (t - 2):128 * (t - 2) + 128, :], [g.rope], [cs])
                q5 = qn[:].rearrange("p (h a b r) -> p h a b r", h=8, a=2, b=2, r=16)
                o5 = qr[:].rearrange("p (h a b r) -> p h a b r", h=8, a=2, b=2, r=16)
                for ax in [int(v) for v in _os.environ.get("ROPE_AX", "0,1").split(",") if v != ""]:
                    a_, b_ = q5[:, :, ax, 0, :], q5[:, :, ax, 1, :]
                    cos = cs[:, 16 * ax:16 * ax + 16].unsqueeze(1).to_broadcast([128, 8, 16])
                    sin = cs[:, 32 + 16 * ax:32 + 16 * ax + 16].unsqueeze(1).to_broadcast([128, 8, 16])
                    tv = [x[:, 128 * ax:128 * ax + 128].rearrange("p (h r) -> p h r", h=8) for x in tm]
                    kb.tt("dve", tv[0], a_, cos, ALU.mult, [qn, cs], [tm[0]])
                    kb.tt("dve", tv[1], b_, sin, ALU.mult, [qn, cs], [tm[1]])
                    kb.tt("dve", o5[:, :, ax, 0, :], tv[0], tv[1], ALU.subtract, [tm[0], tm[1]], [qr])
                    kb.tt("dve", tv[2], a_, sin, ALU.mult, [qn, cs], [tm[2]])
                    kb.tt("dve", tv[3], b_, cos, ALU.mult, [qn, cs], [tm[3]])
                    kb.tt("dve", o5[:, :, ax, 1, :], tv[2], tv[3], ALU.add, [tm[2], tm[3]], [qr])
            else:
                kb.tt("pool", qr[:].rearrange("p (h d) -> p h d", h=8), q3, gain[:], ALU.mult, [qn, gain], [qr])
            if STG < 6:
                continue
            for p in range(3):
                kb.tr(psTr[:, p * 128:p * 128 + 128], qr[:, p * 128:p * 128 + 128], g.ident[:], [qr, g.ident], [psTr])
            kb.tr(psTr[:, 384:512], qr[:, 384:512], g.ident[:], [qr, g.ident], [psTr])
            if STG < 7:
                continue
            for p in range(3):
                kb.copy("act", QT[p][:, t * 128:t * 128 + 128], psTr[:, p * 128:p * 128 + 128], [psTr], [QT[p]])
            if STG < 8:
                continue
            kb.copy("act", KT[:, t * 128:t * 128 + 128], psTr[:, 384:512], [psTr], [KT])
        import os as _os
        if _os.environ.get("ATTN_PREP_ONLY"):
            kb.barrier()
            return
        psS = [g.ps[1], g.ps[2], g.ps[3]]
        accVs, accDs = [g.ps[4], g.ps[5]], [g.ps[6], g.ps[7]]
        PTs = [kb.sbuf("PT%d" % i, [128, 512], BF16, ctx=sc) for i in range(3)]
        rcs = [kb.sbuf("rc%d" % i, [64, 512], ctx=sc) for i in range(2)]
        ots = [kb.sbuf("ot%d" % i, [64, 512], ctx=sc) for i in range(2)]
        qblocks = ([(0, 0, 256, [0, 1])] if with_ctx else []) + \
                  [(1 + i, 256 + 512 * i, 512, list(range(NT))) for i in range(8)]
        items = []
        bi_ = 0
        for h in range(6):
            for (blk, q0, nq, kts) in qblocks:
                for ii, kt in enumerate(kts):
                    items.append((h, blk, q0, nq, kt, ii == 0, ii == len(kts) - 1, bi_))
                bi_ += 1
        LA = 2

        def emit_qk(i):
            h, blk, q0, nq, kt, first, last, bn = items[i]
            kv, p = h // 3, h % 3
            pr = slice(64 * kv, 64 * kv + 64)
            ps, PT = psS[i % 3], PTs[i % 3]
            kb.mm(ps[:, 0:nq], KT[pr, kt * 128:kt * 128 + 128], QT[p][pr, q0:q0 + nq], True, True, [KT, QT[p]], [ps])
            kb.act(PT[:, 0:nq], ps[:, 0:nq], AF.Exp, [ps], [PT])

        def emit_pv(i):
            h, blk, q0, nq, kt, first, last, bn = items[i]
            kv = h // 3
            PT = PTs[i % 3]
            accV, accD = accVs[bn % 2], accDs[bn % 2]
            kb.mm(accV[0:64, 0:nq], VE[:, kt, 64 * kv:64 * kv + 64], PT[:, 0:nq], first, last, [VE, PT], [accV])
            kb.mm(accD[0:64, 0:nq], ones[:, :], PT[:, 0:nq], first, last, [ones, PT], [accD])
            if last:
                rc, ot = rcs[bn % 2], ots[bn % 2]
                kb.op("dve", lambda e, o=rc[:, 0:nq], i_=accD[0:64, 0:nq]: e.reciprocal(o, i_), [accD], [rc])
                kb.tt("dve", ot[:, 0:nq], accV[0:64, 0:nq], rc[:, 0:nq], ALU.mult, [accV, rc], [ot])
                kb.dma("sp", g.MIXat[h][blk].ap, ot[:, 0:nq], [ot], [g.MIXat[h][blk]])

        n_it = len(items)
        for i in range(n_it + LA):
            if i < n_it:
                emit_qk(i)
            if i - LA >= 0:
                emit_pv(i - LA)
    kb.barrier()


def phase_sgu(g, L, tiles):
    kb = g.kb
    with ExitStack() as sc:
        WsT = kb.sbuf("WsT", [128, 4, 128], BF16, ctx=sc)
        ws32 = kb.sbuf("ws32", [128, 4, 128], ctx=sc)
        SB = kb.sbuf("SBb", [64, 512], ctx=sc)
        sgain = kb.sbuf("sgain", [128, 256], ctx=sc)
        psW = g.ps[0]
        for gi in range(4):
            kb.dma("sp", ws32[:, gi, :], g.sgu_w.ap[L, gi, :, :], [g.sgu_w], [ws32])
        for gi in range(4):
            kb.tr(psW[:, gi * 128:gi * 128 + 128], ws32[:, gi, :], g.ident[:], [ws32, g.ident], [psW])
        kb.copy("dve", WsT[:].rearrange("p g i -> p (g i)"), psW[:, :], [psW], [WsT])
        kb.dma("sp", SB[:], g.sgu_b.ap[L:L + 1, :, :].rearrange("o g i -> o (g i)").to_broadcast([64, 512]), [g.sgu_b], [SB])
        kb.dma("sp", sgain[:], g.sgu_norm_g.ap[L:L + 1, :].to_broadcast([128, 256]), [g.sgu_norm_g], [sgain])
        zvs = [kb.sbuf("zv%d" % i, [128, 256], ctx=sc) for i in range(2)]
        uts = [kb.sbuf("ut%d" % i, [64, 4, 128], ctx=sc) for i in range(2)]
        gv = kb.sbuf("gv", [128, 256], ctx=sc)
        sq = kb.sbuf("sgsq", [128, 256], ctx=sc)
        ss = kb.sbuf("sgss", [128, 4], ctx=sc)
        vb = kb.sbuf("vb", [128, 256], BF16, ctx=sc)
        tmps = [kb.sbuf("sgt%d" % i, [64, 512], ctx=sc) for i in range(2)]
        ress = [kb.sbuf("sgr%d" % i, [64, 512], ctx=sc) for i in range(2)]
        pss = [g.ps[1], g.ps[2]]
        for n, t in enumerate(tiles):
            zv, ut, tmp, res, ps = zvs[n % 2], uts[n % 2], tmps[n % 2], ress[n % 2], pss[n % 2]
            bi, boff = g.blk_of_tile(t)
            kb.dma("sp", zv[:], g.TOK[t].ap[:, TK_ZV:TK_ZV + 256], [g.TOK[t]], [zv])
            kb.dma("pool", ut[:], g.zqd[1152:1408, 128 * t:128 * t + 128].rearrange("(g d) t -> d g t", d=64),
                   [g.ZQ[9][bi], g.ZQ[10][bi]], [ut])
            kb.act(gv[:], zv[:], AF.Gelu_apprx_tanh, [zv], [gv])
            kb.tt("pool", sq[:], gv[:], gv[:], ALU.mult, [gv], [sq])
            kb.op("dve", lambda e, o=ss[:, 0:4], i=sq[:].rearrange("p (g d) -> p g d", g=4): e.reduce_sum(o, i, AX.X),
                  [sq], [ss])
            rstd_from_ssq(kb, ss[:, 0:4], ss[:, 0:4], 64, [ss], [ss])
            kb.tt("dve", gv[:].rearrange("p (g d) -> p g d", g=4), gv[:].rearrange("p (g d) -> p g d", g=4),
                  ss[:, 0:4].unsqueeze(2).to_broadcast([128, 4, 64]), ALU.mult, [gv, ss], [gv])
            kb.tt("pool", vb[:], gv[:], sgain[:], ALU.mult, [gv, sgain], [vb])
            for gi in range(4):
                kb.mm(ps[0:64, gi * 128:gi * 128 + 128], vb[:, gi * 64:gi * 64 + 64], WsT[:, gi, :], True, True, [vb, WsT], [ps])
            kb.tt("dve", tmp[:], ps[0:64, :], SB[:], ALU.add, [ps, SB], [tmp])
            kb.tt("pool", res[:], tmp[:], ut[:].rearrange("d g t -> d (g t)"), ALU.mult, [tmp, ut], [res])
            kb.dma("sp", g.mixd[768:1024, 128 * t:128 * t + 128].rearrange("(g d) t -> d g t", d=64),
                   res[:].rearrange("d (g t) -> d g t", g=4), [res], [g.MIXsg[t]])
    kb.barrier()


class _B:
    pass


def phase_dn(g, L):
    kb = g.kb
    ident = g.ident
    with ExitStack() as sc:
        def S(name, shape, dt=F32):
            return kb.sbuf(name, shape, dt, ctx=sc)
        dnc = S("dnc", [128, 7, 128])
        kb.dma("sp", dnc[:], g.dncd.ap.rearrange("c p i -> p c i"), [g.dncd], [dnc])
        ones = S("ones32", [128, 128])
        kb.memset("dve", ones[:], 1.0, [ones])
        I4 = S("I4", [128, 4, 128])
        NM4 = [S("NM4%d" % d, [128, 4, 128]) for d in range(2)]
        SM4 = [S("SM4%d" % d, [128, 4, 128]) for d in range(2)]
        for c in range(4):
            kb.copy("dve", I4[:, c, :], ident[:], [ident], [I4])
            for d in range(2):
                kb.copy("dve", NM4[d][:, c, :], dnc[:, 3 + d, :], [dnc], [NM4[d]])
                kb.copy("dve", SM4[d][:, c, :], dnc[:, 5 + d, :], [dnc], [SM4[d]])
        BA = S("BA", [128, 68, 24])
        src = g.tokd[:, TK_BETA:TK_BETA + 24].rearrange("(c i) f -> i c f", i=64)
        kb.dma("sp", BA[0:64, :, :], src, g.TOK, [BA])
        kb.dma("pool", BA[64:128, :, :], src, g.TOK, [BA])
        ZB, ZA = S("ZB", [128, 6, 68]), S("ZA", [128, 6, 68])
        for d in range(2):
            for half in range(2):
                pr = slice(64 * half, 64 * half + 64)
                kb.copy("dve", ZB[pr, 3 * d:3 * d + 3, :], BA[pr, :, 6 * d + half:6 * d + 6:2].rearrange("i c p -> i p c"), [BA], [ZB])
                kb.copy("dve", ZA[pr, 3 * d:3 * d + 3, :],
                        BA[pr, :, 12 + 6 * d + half:12 + 6 * d + 6:2].rearrange("i c p -> i p c"), [BA], [ZA])
        AL, DTB, NEA = S("AL", [128, 6]), S("DTB", [128, 6]), S("NEA", [128, 6])
        for half in range(2):
            pr = slice(64 * half, 64 * half + 64)
            kb.dma("sp", AL[pr, :].rearrange("p (d q) -> p d q", d=2), g.dn_a_log.ap[L:L + 1, :, half::2].to_broadcast([64, 2, 3]),
                   [g.dn_a_log], [AL], allow_slow_non_contiguous=True)
            kb.dma("sp", DTB[pr, :].rearrange("p (d q) -> p d q", d=2), g.dn_dt_bias.ap[L:L + 1, :, half::2].to_broadcast([64, 2, 3]),
                   [g.dn_dt_bias], [DTB], allow_slow_non_contiguous=True)
        kb.act(NEA[:], AL[:], AF.Exp, [AL], [NEA])
        kb.ts("dve", NEA[:], NEA[:], -1.0, None, ALU.mult, None, [NEA], [NEA])
        NBETA, GT, GC, GL, E, KTS, EGL = [S(n, [128, 6, 68]) for n in ("NBETA", "GT", "GC", "GL", "E", "KTS", "EGL")]
        kb.act(NBETA[:], ZB[:], AF.Sigmoid, [ZB], [NBETA])
        kb.ts("dve", NBETA[:], NBETA[:], -1.0, None, ALU.mult, None, [NBETA], [NBETA])
        kb.tt("dve", GT[:], ZA[:], DTB[:].unsqueeze(2).to_broadcast([128, 6, 68]), ALU.add, [ZA, DTB], [GT])
        kb.act(GT[:], GT[:], AF.Exp, [GT], [GT])
        kb.act(GT[:], GT[:], AF.Ln, [GT], [GT], bias=1.0)
        kb.tt("dve", GT[:], GT[:], NEA[:].unsqueeze(2).to_broadcast([128, 6, 68]), ALU.mult, [GT, NEA], [GT])
        ps = g.ps[0]
        kb.mm(ps[:, 0:204], dnc[:, 1, :], GT[:, 0:3, :].rearrange("p a c -> p (a c)"), True, True, [dnc, GT], [ps])
        kb.mm(ps[:, 204:408], dnc[:, 2, :], GT[:, 3:6, :].rearrange("p a c -> p (a c)"), True, True, [dnc, GT], [ps])
        kb.copy("dve", GC[:].rearrange("p a c -> p (a c)"), ps[:, 0:408], [ps], [GC])
        kb.mm(ps[:, 0:408], dnc[:, 0, :], GT[:].rearrange("p a c -> p (a c)"), True, True, [dnc, GT], [ps])
        kb.copy("dve", GL[:].rearrange("p a c -> p (a c)"), ps[:, 0:408], [ps], [GL])
        kb.act(E[:], GC[:], AF.Exp, [GC], [E])
        kb.act(EGL[:], GL[:], AF.Exp, [GL], [EGL])
        kb.tt("dve", KTS[:], GL[:], GC[:], ALU.subtract, [GL, GC], [KTS])
        kb.act(KTS[:], KTS[:], AF.Exp, [KTS], [KTS])
        cw = S("cw", [128, 9, 3])
        for fc in range(9):
            kb.dma("sp", cw[:, fc, :], g.conv_w.ap[L, :, 128 * fc:128 * fc + 128].rearrange("k p -> p k"), [g.conv_w], [cw],
                   allow_slow_non_contiguous=True)
        dgain = S("dgain", [64, 1])
        kb.dma("sp", dgain[:], g.dn_norm_g.ap[L, :].rearrange("(e o) -> e o", o=1), [g.dn_norm_g], [dgain],
               allow_slow_non_contiguous=True)
        qn, kn, vn, zr = S("dqn", [128, T]), S("dkn", [128, T]), S("dvn", [128, T]), S("zraw", [128, T])
        sqb = S("dsqb", [128, 512])
        rsb = S("drsb", [128, 512])
        OB = S("OB", [128, 68, 64])
        bufs = []
        for d in range(2):
            B = _B()
            for n in ("kT", "qT", "vT", "kBD", "diag", "D", "attnT", "N", "NT", "P2", "PT2", "R", "kt", "tmp"):
                setattr(B, n, S("%s%d" % (n, d), [128, 4, 128]))
            B.vst = S("vst%d" % d, [128, 4, 64])
            B.ntmp, B.vnew, B.o1 = S("ntmp%d" % d, [128, 64]), S("vnew%d" % d, [128, 64]), S("o1%d" % d, [128, 64])
            B.S = [S("S%d_%d" % (d, i), [128, 64]) for i in range(2)]
            B.banks = g.ps[4 * d:4 * d + 4]
            for t_ in (B.kT, B.qT, B.vT):
                kb.memset("dve", t_[:], 0.0, [t_])
            bufs.append(B)
        gts = [S("dgt%d" % i, [128, 4, 64]) for i in range(2)]
        otb = [S("dot%d" % i, [64, 512]) for i in range(2)]
        oss = S("doss", [128, 4])
        f2 = lambda ap: ap.rearrange("p c i -> p (c i)")

        for pp in range(3):
            for xi, dst in enumerate((qn, kn, vn)):
                fc = 3 * xi + pp
                for bi, (b0, n) in enumerate(BLOCKS):
                    kb.dma("sp" if bi % 2 == 0 else "pool", zr[:, b0:b0 + n], g.ZQ[fc][bi].ap, [g.ZQ[fc][bi]], [zr])
                kb.act(dst[:], zr[:], AF.Copy, [zr, cw], [dst], scale=cw[:, fc, 1:2])
                for (a0, a1) in ((0, NCTX), (NCTX, T)):
                    kb.stt("dve", dst[:, a0 + 1:a1], zr[:, a0:a1 - 1], cw[:, fc, 0:1], dst[:, a0 + 1:a1], ALU.mult, ALU.add,
                           [zr, cw, dst], [dst])
                    kb.stt("dve", dst[:, a0:a1 - 1], zr[:, a0 + 1:a1], cw[:, fc, 2:3], dst[:, a0:a1 - 1], ALU.mult, ALU.add,
                           [zr, cw, dst], [dst])
                kb.act(dst[:], dst[:], AF.Silu, [dst], [dst])
                if xi < 2:
                    for (b0, n) in BLOCKS:
                        kb.tt("dve", sqb[:, 0:n], dst[:, b0:b0 + n], dst[:, b0:b0 + n], ALU.mult, [dst], [sqb])
                        kb.mm(g.ps[0][:, 0:n], dnc[:, 0, :], sqb[:, 0:n], True, True, [dnc, sqb], [g.ps[0]])
                        sc_ = 64.0 if xi == 0 else 1.0
                        kb.act(rsb[:, 0:n], g.ps[0][:, 0:n], AF.Sqrt, [g.ps[0]], [rsb], scale=sc_, bias=sc_ * EPS)
                        kb.op("dve", lambda e, o=rsb[:, 0:n]: e.reciprocal(o, o), [rsb], [rsb])
                        kb.tt("dve", dst[:, b0:b0 + n], dst[:, b0:b0 + n], rsb[:, 0:n], ALU.mult, [dst, rsb], [dst])
            for d in range(2):
                kb.memset("dve", bufs[d].S[0][:], 0.0, [bufs[d].S[0]])
            written = set()
            scnt = [0, 0]

            def prep(d, grp):
                B = bufs[d]
                dp, c0, t0 = 3 * d + pp, 4 * grp, 256 * grp
                pA, pB, pC, pD = B.banks
                for dst, src_ in ((B.kT, kn), (B.qT, qn), (B.vT, vn)):
                    for half in range(2):
                        pr = slice(64 * half, 64 * half + 64)
                        kb.copy("dve", dst[pr, :, 64 * half:64 * half + 64],
                                src_[pr, t0:t0 + 256].rearrange("p (c i) -> p c i", c=4), [src_], [dst])
                        yield
                for c in range(4):
                    kb.tr(pA[:, c * 128:c * 128 + 128], B.kT[:, c, :], ident[:], [B.kT, ident], [pA])
                    yield
                kb.copy("act", f2(B.kBD[:]), pA[:, :], [pA], [B.kBD])
                yield
                for c in range(4):
                    kb.tr(pA[:, c * 128:c * 128 + 128], B.vT[:, c, :], ident[:], [B.vT, ident], [pA])
                    yield
                kb.copy("act", f2(B.tmp[:]), pA[:, :], [pA], [B.tmp])
                yield
                kb.tt("dve", B.vst[:], B.tmp[:, :, 0:64], B.tmp[:, :, 64:128], ALU.add, [B.tmp], [B.vst])
                yield
                for c in range(4):
                    kb.mm(pB[:, c * 128:c * 128 + 128], B.kT[:, c, :], B.kT[:, c, :], True, True, [B.kT], [pB])
                    yield
                for c in range(4):
                    kb.mm(pC[:, c * 128:c * 128 + 128], B.kT[:, c, :], B.qT[:, c, :], True, True, [B.kT, B.qT], [pC])
                    yield
                for c in range(4):
                    kb.ts("dve", B.diag[:, c, :], ident[:], GC[:, dp, c0 + c:c0 + c + 1], None, ALU.mult, None, [ident, GC], [B.diag])
                    yield
                kb.mm(pA[:, :], ones[:], f2(B.diag[:]), True, True, [ones, B.diag], [pA])
                yield
                for c in range(4):
                    kb.ts("dve", B.D[:, c, :], pA[:, c * 128:c * 128 + 128], GC[:, dp, c0 + c:c0 + c + 1], 0.0, ALU.subtract, ALU.min,
                          [pA, GC], [B.D])
                    yield
                kb.tt("dve", f2(B.D[:]), f2(B.D[:]), f2(NM4[d][:]), ALU.add, [B.D, NM4[d]], [B.D])
                yield
                kb.act(f2(B.D[:]), f2(B.D[:]), AF.Exp, [B.D], [B.D])
                yield
                kb.tt("dve", f2(B.attnT[:]), pC[:, :], f2(B.D[:]), ALU.mult, [pC, B.D], [B.attnT])
                yield
                kb.tt("dve", f2(B.N[:]), pB[:, :], f2(B.D[:]), ALU.mult, [pB, B.D], [B.N])
                yield
                kb.tt("dve", f2(B.N[:]), f2(B.N[:]), f2(SM4[d][:]), ALU.mult, [B.N, SM4[d]], [B.N])
                yield
                for c in range(4):
                    kb.act(B.N[:, c, :], B.N[:, c, :], AF.Copy, [B.N, NBETA], [B.N], scale=NBETA[:, dp, c0 + c:c0 + c + 1])
                    yield
                for c in range(4):
                    kb.tr(pA[:, c * 128:c * 128 + 128], B.N[:, c, :], ident[:], [B.N, ident], [pA])
                    yield
                kb.copy("act", f2(B.NT[:]), pA[:, :], [pA], [B.NT])
                yield
                kb.tt("dve", f2(B.R[:]), f2(B.N[:]), f2(I4[:]), ALU.add, [B.N, I4], [B.R])
                yield
                P, PT = B.N, B.NT
                for k in range(5):
                    Pn, PTn = (B.P2, B.PT2) if k % 2 == 0 else (B.N, B.NT)
                    for c in range(4):
                        kb.mm(pC[:, c * 128:c * 128 + 128], P[:, c, :], PT[:, c, :], True, True, [P, PT], [pC])
                        yield
                    if k < 4:
                        for c in range(4):
                            kb.mm(pB[:, c * 128:c * 128 + 128], PT[:, c, :], P[:, c, :], True, True, [P, PT], [pB])
                            yield
                    kb.copy("act", f2(PTn[:]), pC[:, :], [pC], [PTn])
                    yield
                    if k < 4:
                        kb.copy("dve", f2(Pn[:]), pB[:, :], [pB], [Pn])
                        yield
                    for c in range(4):
                        kb.mm(pA[:, c * 128:c * 128 + 128], PTn[:, c, :], B.R[:, c, :], True, True, [PTn, B.R], [pA])
                        yield
                    kb.tt("dve", f2(B.R[:]), f2(B.R[:]), pA[:, :], ALU.add, [B.R, pA], [B.R])
                    yield
                    P, PT = Pn, PTn
                for c in range(4):
                    kb.act(B.kt[:, c, :], B.kBD[:, c, :], AF.Copy, [B.kBD, KTS], [B.kt], scale=KTS[:, dp, c0 + c:c0 + c + 1])
                    yield

            def scan(d, grp):
                B = bufs[d]
                dp, c0 = 3 * d + pp, 4 * grp
                pD = B.banks[3]
                for c in (range(4) if d == 0 else range(3, -1, -1)):
                    ch = c0 + c
                    S_old, S_new = B.S[scnt[d] % 2], B.S[(scnt[d] + 1) % 2]
                    scnt[d] += 1
                    kb.mm(pD[:, 0:64], B.kT[:, c, :], S_old[:], True, True, [B.kT, S_old], [pD])
                    yield
                    kb.stt("dve", B.ntmp[:], pD[:, 0:64], E[:, dp, ch:ch + 1], B.vst[:, c, :], ALU.mult, ALU.subtract,
                           [pD, E, B.vst], [B.ntmp])
                    yield
                    kb.mm(pD[:, 64:128], B.R[:, c, :], B.ntmp[:], True, True, [B.R, B.ntmp], [pD])
                    yield
                    kb.act(B.vnew[:], pD[:, 64:128], AF.Copy, [pD, NBETA], [B.vnew], scale=NBETA[:, dp, ch:ch + 1])
                    yield
                    kb.mm(pD[:, 128:192], B.qT[:, c, :], S_old[:], True, True, [B.qT, S_old], [pD])
                    yield
                    kb.act(B.o1[:], pD[:, 128:192], AF.Copy, [pD, E], [B.o1], scale=E[:, dp, ch:ch + 1])
                    yield
                    kb.mm(pD[:, 192:256], B.attnT[:, c, :], B.vnew[:], True, True, [B.attnT, B.vnew], [pD])
                    yield
                    if ch not in written:
                        written.add(ch)
                        kb.tt("dve", OB[:, ch, :], B.o1[:], pD[:, 192:256], ALU.add, [B.o1, pD], [OB])
                        yield
                    else:
                        kb.tt("dve", B.o1[:], B.o1[:], pD[:, 192:256], ALU.add, [B.o1, pD], [B.o1])
                        yield
                        kb.tt("dve", OB[:, ch, :], OB[:, ch, :], B.o1[:], ALU.add, [OB, B.o1], [OB])
                        yield
                    kb.mm(pD[:, 256:320], B.kt[:, c, :], B.vnew[:], True, True, [B.kt, B.vnew], [pD])
                    yield
                    kb.stt("dve", S_new[:], S_old[:], EGL[:, dp, ch:ch + 1], pD[:, 256:320], ALU.mult, ALU.add,
                           [S_old, EGL, pD], [S_new])
                    yield

            order = [list(range(17)), [0] + list(range(16, 0, -1))]
            def stream(d):
                for it in range(17):
                    yield from prep(d, order[d][it])
                    yield from scan(d, order[d][it])

            gens = [stream(0), stream(1)]
            alive = [True, True]
            while any(alive):
                for d in range(2):
                    if alive[d]:
                        try:
                            next(gens[d])
                        except StopIteration:
                            alive[d] = False
            pO = g.ps[0]
            for grp in range(17):
                gt, ot = gts[grp % 2], otb[grp % 2]
                c0, t0 = 4 * grp, 256 * grp
                for ab in range(2):
                    h = 2 * pp + ab
                    kb.dma("sp" if ab == 0 else "pool", gt[64 * ab:64 * ab + 64, :, :],
                           g.tokd[t0:t0 + 256, TK_GATE + 64 * h:TK_GATE + 64 * h + 64].rearrange("(c i) e -> i c e", i=64),
                           [g.TOK[2 * grp], g.TOK[2 * grp + 1]], [gt])
                kb.act(gt[:], gt[:], AF.Silu, [gt], [gt])
                ob = OB[:, c0:c0 + 4, :]
                tmp3 = bufs[0].tmp[:, 0:2, :].rearrange("p a (b e) -> p (a b) e", e=64)
                kb.tt("dve", tmp3, ob, ob, ALU.mult, [OB], [bufs[0].tmp])
                kb.op("dve", lambda e, o=oss[:, 0:4], i=tmp3: e.reduce_sum(o, i, AX.X), [bufs[0].tmp], [oss])
                rstd_from_ssq(kb, oss[:, 0:4], oss[:, 0:4], 64, [oss], [oss])
                kb.tt("dve", ob, ob, oss[:, 0:4].unsqueeze(2).to_broadcast([128, 4, 64]), ALU.mult, [OB, oss], [OB])
                kb.tt("dve", ob, ob, gt[:], ALU.mult, [OB, gt], [OB])
                for c in range(4):
                    kb.tr(pO[0:64, c * 128:c * 128 + 128], OB[:, c0 + c, :], ident[:], [OB, ident], [pO])
                kb.act(ot[:, :], pO[0:64, :], AF.Copy, [pO, dgain], [ot], scale=dgain[:, 0:1])
                for ab in range(2):
                    h = 2 * pp + ab
                    kb.dma("sp" if ab == 0 else "pool",
                           g.mixd[64 * h:64 * h + 64, t0:t0 + 256].rearrange("e (c i) -> e c i", c=4),
                           ot[:, :].rearrange("e (c ab i) -> e c ab i", c=4, ab=2)[:, :, ab, :], [ot],
                           [g.MIXdn[2 * grp], g.MIXdn[2 * grp + 1]])
    kb.barrier()


def prep_w13(g, wa, wb, W13, nf, sc):
    kb = g.kb
    st = [kb.sbuf("w13s%d" % i, [128, 8, 256], ctx=sc) for i in range(2)]
    sb = [kb.sbuf("w13b%d" % i, [128, 8, 256], BF16, ctx=sc) for i in range(2)]
    for f in range(nf):
        s_, b_ = st[f % 2], sb[f % 2]
        kb.dma("sp", s_[:, :, 0:128], wa[0][:, 128 * f:128 * f + 128].rearrange("(k p) j -> p k j", p=128), [wa[1]], [s_])
        kb.dma("pool", s_[:, :, 128:256], wb[0][:, 128 * f:128 * f + 128].rearrange("(k p) j -> p k j", p=128), [wb[1]], [s_])
        kb.copy("dve" if f % 2 == 0 else "pool", b_[:], s_[:], [s_], [b_])
        kb.dma("sp", W13[f].ap, b_[:], [b_], [W13[f]])


def phase_out_ffn(g, L, xs_tiles, xo_tiles, do_ctx):
    kb = g.kb
    with ExitStack() as sc:
        with ExitStack() as sc2:
            prep_w13(g, (g.ffn_w1.ap[0], g.ffn_w1), (g.ffn_w3.ap[0], g.ffn_w3), g.W13, 22, sc2)
        kb.barrier()
        woutb = kb.sbuf("woutb", [128, 8, D], BF16, ctx=sc)
        w2b = kb.sbuf("w2b", [128, 22, D], BF16, ctx=sc)
        stage = [kb.sbuf("wst%d" % i, [128, D], ctx=sc) for i in range(2)]
        load_cast_weight(g, sc, "wout", g.w_out, lambda k: g.w_out.ap[L, 128 * k:128 * k + 128, :], 8, D, stage, woutb,
                         lambda k: woutb[:, k, :])
        load_cast_weight(g, sc, "w2", g.ffn_w2, lambda k: g.ffn_w2.ap[0, 128 * k:128 * k + 128, :], 22, D, stage, w2b,
                         lambda k: w2b[:, k, :])
        gmsa = kb.sbuf("gmsa", [128, 2, D], ctx=sc)
        gmlp = kb.sbuf("gmlp", [128, 2, D], ctx=sc)
        for s in range(2):
            kb.dma("sp", gmsa[:, s, :], g.gates.ap[L, 0, s:s + 1, :].to_broadcast([128, D]), [g.gates], [gmsa])
            kb.dma("sp", gmlp[:, s, :], g.gates.ap[L, 1, s:s + 1, :].to_broadcast([128, D]), [g.gates], [gmlp])
        scr = {"junk": kb.sbuf("junk", [128, 1024], ctx=sc), "ssq": kb.sbuf("ssq", [128, 1], ctx=sc),
               "xn": kb.sbuf("xn", [128, 1024], ctx=sc), "psT": [g.ps[0], g.ps[1]]}
        mix32 = [kb.sbuf("mix32_%d" % i, [128, 8, 128], ctx=sc) for i in range(2)]
        mixb = [kb.sbuf("mixb_%d" % i, [128, 8, 128], BF16, ctx=sc) for i in range(2)]
        xts = [kb.sbuf("xt%d" % i, [128, D], ctx=sc) for i in range(2)]
        tmpy = kb.sbuf("tmpy", [128, D], ctx=sc)
        x1blk = [kb.sbuf("x1b%d" % i, [128, 4, D], ctx=sc) for i in range(1)]
        h2Ts = [kb.sbuf("h2T%d" % i, [128, 8, 512], BF16, ctx=sc) for i in range(1)]
        gT = kb.sbuf("gT", [128, 22, 512], BF16, ctx=sc)
        w13s = [kb.sbuf("w13_%d" % i, [128, 8, 256], BF16, ctx=sc) for i in range(3)]
        sas = [kb.sbuf("sa%d" % i, [128, 512], ctx=sc) for i in range(2)]
        x2s = [kb.sbuf("x2_%d" % i, [128, D], ctx=sc) for i in range(2)]
        psY = [g.ps[2], g.ps[3]]
        psA, psB = [g.ps[4], g.ps[5]], [g.ps[6], g.ps[7]]
        blocks = ([(0, 0, 2)] if do_ctx else []) + [(1 + i, 2 + 4 * i, 4) for i in range(8)]
        ti = 0
        wi = 0
        for bn, (bi, t0, nt) in enumerate(blocks):
            ntok = nt * 128
            x1b, h2T = x1blk[0], h2Ts[0]
            s = 1 if bi == 0 else 0
            for j in range(nt):
                t = t0 + j
                m32, mb, xt = mix32[ti % 2], mixb[ti % 2], xts[ti % 2]
                ti += 1
                kb.dma("sp", m32[:], g.mixd[:, 128 * t:128 * t + 128].rearrange("(c p) t -> p c t", p=128),
                       [g.MIXdn[t], g.MIXsg[t]] + [g.MIXat[h][bi] for h in range(6)], [m32])
                kb.dma("pool", xt[:], xs_tiles[t].ap, [xs_tiles[t]], [xt])
                kb.copy("pool", mb[:], m32[:], [m32], [mb])
                for half in range(2):
                    for k in range(8):
                        kb.mm(psY[half][:, :], mb[:, k, :], woutb[:, k, 512 * half:512 * half + 512], k == 0, k == 7,
                              [mb, woutb], [psY[half]])
                for half in range(2):
                    kb.tt("dve", tmpy[:, 512 * half:512 * half + 512], psY[half][:, :], gmsa[:, s, 512 * half:512 * half + 512],
                          ALU.mult, [psY[half], gmsa], [tmpy])
                kb.tt("pool", x1b[:, j, :], tmpy[:], xt[:], ALU.add, [tmpy, xt], [x1b])
                norm_tile_to_hT(g, L, 1, x1b, bi == 0, [h2T[:, c, j * 128:j * 128 + 128] for c in range(8)], h2T, scr, xap=x1b[:, j, :])
            for f in range(22):
                w13 = w13s[wi % 3]
                pa, pb, sa = psA[wi % 2], psB[wi % 2], sas[wi % 2]
                wi += 1
                kb.dma("sp" if f % 2 == 0 else "pool", w13[:], g.W13[f].ap, [g.W13[f]], [w13])
                for k in range(8):
                    kb.mm(pa[:, 0:ntok], w13[:, k, 0:128], h2T[:, k, 0:ntok], k == 0, k == 7, [w13, h2T], [pa])
                for k in range(8):
                    kb.mm(pb[:, 0:ntok], w13[:, k, 128:256], h2T[:, k, 0:ntok], k == 0, k == 7, [w13, h2T], [pb])
                kb.act(sa[:, 0:ntok], pa[:, 0:ntok], AF.Silu, [pa], [sa])
                kb.tt("dve", gT[:, f, 0:ntok], sa[:, 0:ntok], pb[:, 0:ntok], ALU.mult, [sa, pb], [gT])
            for j in range(nt):
                t = t0 + j
                x2 = x2s[j % 2]
                for half in range(2):
                    for f in range(22):
                        kb.mm(psY[half][:, :], gT[:, f, j * 128:j * 128 + 128], w2b[:, f, 512 * half:512 * half + 512],
                              f == 0, f == 21, [gT, w2b], [psY[half]])
                for half in range(2):
                    kb.tt("dve", tmpy[:, 512 * half:512 * half + 512], psY[half][:, :], gmlp[:, s, 512 * half:512 * half + 512],
                          ALU.mult, [psY[half], gmlp], [tmpy])
                kb.tt("pool", x2[:], tmpy[:], x1b[:, j, :], ALU.add, [tmpy, x1b], [x2])
                kb.dma("sp", xo_tiles[t].ap, x2[:], [x2], [xo_tiles[t]])
    kb.barrier()


def phase_out_router(g, L, xs_tiles):
    kb = g.kb
    with ExitStack() as sc:
        woutb = kb.sbuf("woutb", [128, 8, D], BF16, ctx=sc)
        stage = [kb.sbuf("wst%d" % i, [128, D], ctx=sc) for i in range(2)]
        load_cast_weight(g, sc, "wout", g.w_out, lambda k: g.w_out.ap[L, 128 * k:128 * k + 128, :], 8, D, stage, woutb,
                         lambda k: woutb[:, k, :])
        gmsa = kb.sbuf("gmsa", [128, D], ctx=sc)
        kb.dma("sp", gmsa[:], g.gates.ap[L, 0, 0:1, :].to_broadcast([128, D]), [g.gates], [gmsa])
        rw = kb.sbuf("rw", [128, 8, NEXP], ctx=sc)
        kb.dma("sp", rw[:], g.router_w.ap[0].rearrange("(k p) e -> p k e", p=128), [g.router_w], [rw])
        rb = kb.sbuf("rb", [128, NEXP], ctx=sc)
        kb.dma("sp", rb[:], g.router_b.ap[0:1, :].to_broadcast([128, NEXP]), [g.router_b], [rb])
        scr = {"junk": kb.sbuf("junk", [128, 1024], ctx=sc), "ssq": kb.sbuf("ssq", [128, 1], ctx=sc),
               "xn": kb.sbuf("xn", [128, 1024], ctx=sc), "psT": [g.ps[0], g.ps[1]]}
        mix32 = [kb.sbuf("mix32_%d" % i, [128, 8, 128], ctx=sc) for i in range(2)]
        mixb = [kb.sbuf("mixb_%d" % i, [128, 8, 128], BF16, ctx=sc) for i in range(2)]
        xts = [kb.sbuf("xt%d" % i, [128, D], ctx=sc) for i in range(2)]
        tmpy = kb.sbuf("tmpy", [128, D], ctx=sc)
        x1s = [kb.sbuf("x1_%d" % i, [128, D], ctx=sc) for i in range(2)]
        h2Ts = [kb.sbuf("h2T%d" % i, [128, 8, 512], BF16, ctx=sc) for i in range(2)]
        h32 = kb.sbuf("h32", [128, 8, 128], ctx=sc)
        lg = kb.sbuf("lg", [128, NEXP], ctx=sc)
        mx8 = kb.sbuf("mx8", [128, 8], ctx=sc)
        msk = kb.sbuf("msk", [128, NEXP], ctx=sc)
        ex = kb.sbuf("ex", [128, NEXP], ctx=sc)
        nm1 = kb.sbuf("nm1", [128, 2], ctx=sc)
        psY = [g.ps[2], g.ps[3]]
        psR = g.ps[4]
        for bi in range(8):
            h2T = h2Ts[bi % 2]
            for j in range(4):
                n = 4 * bi + j
                t = 2 + n
                m32, mb, xt, x1 = mix32[n % 2], mixb[n % 2], xts[n % 2], x1s[n % 2]
                kb.dma("sp", m32[:], g.mixd[:, 128 * t:128 * t + 128].rearrange("(c p) t -> p c t", p=128),
                       [g.MIXdn[t], g.MIXsg[t]] + [g.MIXat[h][1 + bi] for h in range(6)], [m32])
                kb.dma("pool", xt[:], xs_tiles[t].ap, [xs_tiles[t]], [xt])
                kb.copy("dve", mb[:], m32[:], [m32], [mb])
                for half in range(2):
                    for k in range(8):
                        kb.mm(psY[half][:, :], mb[:, k, :], woutb[:, k, 512 * half:512 * half + 512], k == 0, k == 7,
                              [mb, woutb], [psY[half]])
                for half in range(2):
                    kb.tt("dve", tmpy[:, 512 * half:512 * half + 512], psY[half][:, :], gmsa[:, 512 * half:512 * half + 512],
                          ALU.mult, [psY[half], gmsa], [tmpy])
                kb.tt("dve", x1[:], tmpy[:], xt[:], ALU.add, [tmpy, xt], [x1])
                kb.dma("sp", g.X1S[n].ap, x1[:], [x1], [g.X1S[n]])
                norm_tile_to_hT(g, L, 1, x1, False, [h2T[:, c, j * 128:j * 128 + 128] for c in range(8)], h2T, scr,
                                also_f32=[h32[:, c, :] for c in range(8)], f32_buf=h32)
                for k in range(8):
                    kb.mm(psR[:, 0:NEXP], h32[:, k, :], rw[:, k, :], k == 0, k == 7, [h32, rw], [psR])
                kb.tt("dve", lg[:], psR[:, 0:NEXP], rb[:], ALU.add, [psR, rb], [lg])
                kb.op("dve", lambda e, o=mx8[:], i=lg[:]: e.max(out=o, in_=i), [lg], [mx8])
                kb.ts("dve", msk[:], lg[:], mx8[:, 1:2], None, ALU.is_ge, None, [lg, mx8], [msk])
                kb.ts("dve", nm1[:, 0:1], mx8[:, 0:1], -1.0, None, ALU.mult, None, [mx8], [nm1])
                kb.act(ex[:], lg[:], AF.Exp, [lg, nm1], [ex], bias=nm1[:, 0:1])
                kb.tt("dve", ex[:], ex[:], msk[:], ALU.mult, [ex, msk], [ex])
                kb.op("dve", lambda e, o=nm1[:, 1:2], i=ex[:]: e.reduce_sum(o, i, AX.X), [ex], [nm1])
                kb.op("dve", lambda e, o=nm1[:, 1:2]: e.reciprocal(o, o), [nm1], [nm1])
                kb.ts("dve", g.GATE[:, n, :], ex[:], nm1[:, 1:2], None, ALU.mult, None, [ex, nm1], [g.GATEb])
            kb.dma("sp", g.H2T[bi].ap, h2T[:], [h2T], [g.H2T[bi]])
    kb.barrier()


def phase_moe(g, L):
    kb = g.kb
    NF = MOE_FF // 128
    with ExitStack() as sc:
        w2b = kb.sbuf("w2b", [128, NF, D], BF16, ctx=sc)
        stage = [kb.sbuf("wst%d" % i, [128, D], ctx=sc) for i in range(2)]
        gmlp = kb.sbuf("gmlp", [128, D], ctx=sc)
        kb.dma("sp", gmlp[:], g.gates.ap[L, 1, 0:1, :].to_broadcast([128, D]), [g.gates], [gmlp])
        fgain = kb.sbuf("fgain", [128, D], ctx=sc)
        kb.dma("sp", fgain[:], g.final_norm_g.ap[0:1, :].to_broadcast([128, D]), [g.final_norm_g], [fgain])
        pst = [kb.sbuf("w13s%d" % i, [128, 8, 256], ctx=sc) for i in range(2)]
        psb = [kb.sbuf("w13b%d" % i, [128, 8, 256], BF16, ctx=sc) for i in range(2)]
        h2Ts = [kb.sbuf("h2T%d" % i, [128, 8, 512], BF16, ctx=sc) for i in range(2)]
        gT = kb.sbuf("gT", [128, NF, 512], BF16, ctx=sc)
        w13s = [kb.sbuf("w13_%d" % i, [128, 8, 256], BF16, ctx=sc) for i in range(3)]
        sas = [kb.sbuf("sa%d" % i, [128, 512], ctx=sc) for i in range(2)]
        tmpy = kb.sbuf("tmpy", [128, D], ctx=sc)
        ya = kb.sbuf("ya", [128, D], ctx=sc)
        x1 = kb.sbuf("x1", [128, D], ctx=sc)
        junk = kb.sbuf("junk", [128, D], ctx=sc)
        ssq = kb.sbuf("ssq", [128, 1], ctx=sc)
        psY = [g.ps[2], g.ps[3]]
        psA, psB = [g.ps[4], g.ps[5]], [g.ps[6], g.ps[7]]
        wi = 0
        hi = 0
        for e in range(NEXP):
            for f in range(NF):
                s_, b_ = pst[f % 2], psb[f % 2]
                kb.dma("sp", s_[:, :, 0:128], g.moe_w1.ap[0, e, :, 128 * f:128 * f + 128].rearrange("(k p) j -> p k j", p=128),
                       [g.moe_w1], [s_])
                kb.dma("pool", s_[:, :, 128:256], g.moe_w3.ap[0, e, :, 128 * f:128 * f + 128].rearrange("(k p) j -> p k j", p=128),
                       [g.moe_w3], [s_])
                kb.copy("dve", b_[:], s_[:], [s_], [b_])
                kb.dma("sp", g.W13[f].ap, b_[:], [b_], [g.W13[f]])
            load_cast_weight(g, sc, "w2", g.moe_w2, lambda k: g.moe_w2.ap[0, e, 128 * k:128 * k + 128, :], NF, D, stage, w2b,
                             lambda k: w2b[:, k, :])
            for bi in range(8):
                h2T = h2Ts[hi % 2]
                hi += 1
                kb.dma("sp", h2T[:], g.H2T[bi].ap, [g.H2T[bi]], [h2T])
                for f in range(NF):
                    w13 = w13s[wi % 3]
                    pa, pb, sa = psA[wi % 2], psB[wi % 2], sas[wi % 2]
                    wi += 1
                    kb.dma("sp" if f % 2 == 0 else "pool", w13[:], g.W13[f].ap, [g.W13[f]], [w13])
                    for k in range(8):
                        kb.mm(pa[:, :], w13[:, k, 0:128], h2T[:, k, :], k == 0, k == 7, [w13, h2T], [pa])
                    for k in range(8):
                        kb.mm(pb[:, :], w13[:, k, 128:256], h2T[:, k, :], k == 0, k == 7, [w13, h2T], [pb])
                    kb.act(sa[:, :], pa[:, :], AF.Silu, [pa], [sa])
                    kb.tt("dve", gT[:, f, :], sa[:, :], pb[:, :], ALU.mult, [sa, pb], [gT])
                for j in range(4):
                    n = 4 * bi + j
                    for half in range(2):
                        for f in range(NF):
                            kb.mm(psY[half][:, :], gT[:, f, j * 128:j * 128 + 128], w2b[:, f, 512 * half:512 * half + 512],
                                  f == 0, f == NF - 1, [gT, w2b], [psY[half]])
                    if e > 0:
                        kb.dma("pool", ya[:], g.YACC[n].ap, [g.YACC[n]], [ya])
                    for half in range(2):
                        kb.act(tmpy[:, 512 * half:512 * half + 512], psY[half][:, :], AF.Copy, [psY[half], g.GATEb], [tmpy],
                               scale=g.GATE[:, n, e:e + 1])
                    if e == 0:
                        kb.dma("sp", g.YACC[n].ap, tmpy[:], [tmpy], [g.YACC[n]])
                        continue
                    kb.tt("dve", ya[:], ya[:], tmpy[:], ALU.add, [ya, tmpy], [ya])
                    if e < NEXP - 1:
                        kb.dma("sp", g.YACC[n].ap, ya[:], [ya], [g.YACC[n]])
                        continue
                    kb.dma("sp", x1[:], g.X1S[n].ap, [g.X1S[n]], [x1])
                    kb.tt("dve", ya[:], ya[:], gmlp[:], ALU.mult, [ya, gmlp], [ya])
                    kb.tt("dve", ya[:], ya[:], x1[:], ALU.add, [ya, x1], [ya])
                    kb.act(junk[:], ya[:], AF.Square, [ya, ssq], [junk, ssq], accum_out=ssq[:, 0:1])
                    rstd_from_ssq(kb, ssq[:, 0:1], ssq[:, 0:1], D, [ssq], [ssq])
                    kb.act(junk[:], ya[:], AF.Copy, [ya, ssq], [junk], scale=ssq[:, 0:1])
                    kb.tt("dve", junk[:], junk[:], fgain[:], ALU.mult, [junk, fgain], [junk])
                    kb.dma("sp", g.outb.ap[128 * n:128 * n + 128, :], junk[:], [junk], [g.outb])
    kb.barrier()


W_NAMES = [("mod_w", [DEPTH, D, 6 * D]), ("mod_b", [DEPTH, 6 * D]), ("norm1_g", [DEPTH, D]), ("norm2_g", [DEPTH, D]),
           ("w_in", [DEPTH, D, IN_COLS]), ("conv_w", [DEPTH, 3, 1152]), ("dn_a_log", [DEPTH, 2, 6]), ("dn_dt_bias", [DEPTH, 2, 6]),
           ("dn_norm_g", [DEPTH, 64]), ("q_norm_g", [DEPTH, 64]), ("k_norm_g", [DEPTH, 64]), ("sgu_norm_g", [DEPTH, 256]),
           ("sgu_w", [DEPTH, 4, 128, 128]), ("sgu_b", [DEPTH, 4, 128]), ("w_out", [DEPTH, D, D]),
           ("ffn_w1", [1, D, D_FF]), ("ffn_w3", [1, D, D_FF]), ("ffn_w2", [1, D_FF, D]),
           ("router_w", [1, D, NEXP]), ("router_b", [1, NEXP]), ("moe_w1", [1, NEXP, D, MOE_FF]),
           ("moe_w3", [1, NEXP, D, MOE_FF]), ("moe_w2", [1, NEXP, MOE_FF, D]), ("final_norm_g", [1, D])]
BLOCKS = [(0, 256)] + [(256 + 512 * i, 512) for i in range(8)]


def build_program(phases=None, debug=(), dbg_in=()):
    nc = bass.Bass("TRN2", target_bir_lowering=False)
    g = G()

    def ext_in(name, shape):
        return Buf(name, nc.dram_tensor(name, list(shape), F32, kind="ExternalInput").ap())

    g.xin = ext_in("xin", [T, D])
    g.cvec = ext_in("cvec", [2, D])
    g.rope = ext_in("rope", [NLAT, 64])
    g.identd = ext_in("ident", [128, 128])
    g.dncd = ext_in("dnc", [7, 128, 128])
    for name, shape in W_NAMES:
        setattr(g, name, ext_in(name, shape))
    out = Buf("out", nc.dram_tensor("out", [NLAT, D], F32, kind="ExternalOutput").ap())
    dbg = {}
    for name, shape in debug:
        dbg[name] = Buf(name, nc.dram_tensor(name, list(shape), F32, kind="ExternalOutput").ap())
    for name, shape in dbg_in:
        dbg[name] = Buf(name, nc.dram_tensor(name, list(shape), F32, kind="ExternalInput").ap())
    g.dbg = dbg

    def scratch(name, shape, dt=F32):
        if name in dbg:
            return dbg[name].ap
        return nc.dram_tensor(name + "_s", list(shape), dt, kind="Internal").ap()

    with ExitStack() as ctx:
        import os as _os
        kb = KB(nc, ctx, same_engine_sync=_os.environ.get("SAMEENG", "1") == "1")
        g.kb = kb
        g.ps = [kb.psum("ps%d" % i, [128, 512]) for i in range(8)]
        g.ident = kb.sbuf("ident", [128, 128])
        kb.dma("sp", g.ident[:], g.identd.ap, [g.identd], [g.ident])
        GS = kb.sbuf("GS", [128, DEPTH, 2, 8, 2])
        SH = kb.sbuf("SH", [128, DEPTH, 2, 8, 2])
        g.GS, g.SH, g.GSb, g.SHb = GS.ap, SH.ap, GS, SH
        g.gates = Buf("gates", scratch("gates", [DEPTH, 2, 2, D]))
        tokd = scratch("TOK", [T, TK_W])
        g.tokd = tokd
        g.TOK = [Buf("TOK%d" % t, tokd[128 * t:128 * t + 128, :]) for t in range(NT)]
        g.zqd = scratch("ZQ", [1408, T])
        g.ZQ = [[Buf("ZQ%d_%d" % (fc, bi), g.zqd[128 * fc:128 * fc + 128, b0:b0 + n]) for bi, (b0, n) in enumerate(BLOCKS)]
                for fc in range(11)]
        g.blk_of_tile = lambda t: (0, t * 128) if t < 2 else (1 + (t - 2) // 4, ((t - 2) % 4) * 128)
        g.mixd = scratch("MIXT", [D, T])
        g.MIXdn = [Buf("MIXdn%d" % t, None) for t in range(NT)]
        g.MIXsg = [Buf("MIXsg%d" % t, None) for t in range(NT)]
        g.MIXat = [[Buf("MIXat%d_%d" % (h, bi), g.mixd[384 + 64 * h:384 + 64 * h + 64, b0:b0 + n])
                    for bi, (b0, n) in enumerate(BLOCKS)] for h in range(6)]
        w13d = scratch("W13", [28, 128, 8, 256], BF16)
        g.W13 = [Buf("W13_%d" % f, w13d[f]) for f in range(28)]
        xs = [[Buf("xin%d" % t, g.xin.ap[128 * t:128 * t + 128, :]) for t in range(NT)]]
        for L in range(DEPTH):
            xd = scratch("XS%d" % (L + 1), [T, D])
            xs.append([Buf("xs%d_%d" % (L + 1, t), xd[128 * t:128 * t + 128, :]) for t in range(NT)])
        g.xs = xs
        g.outb = out
        x1d = scratch("X1S", [NLAT, D])
        g.X1S = [Buf("X1S%d" % n, x1d[128 * n:128 * n + 128, :]) for n in range(32)]
        yd = scratch("YACC", [NLAT, D])
        g.YACC = [Buf("YACC%d" % n, yd[128 * n:128 * n + 128, :]) for n in range(32)]
        h2d = scratch("H2T", [8, 128, 8, 512], BF16)
        g.H2T = [Buf("H2T%d" % b, h2d[b]) for b in range(8)]
        GATE = kb.sbuf("GATE", [128, 32, NEXP])
        g.GATE, g.GATEb = GATE.ap, GATE
        allp = ["mod", "a0", "attn0", "sgu0", "dn0", "ffn0", "a1", "attn1", "sgu1", "dn1", "out1", "moe1"]
        phases = allp if phases is None else phases
        if "mod" in phases:
            for L in range(DEPTH):
                phase_mod(g, L)
        if "a0" in phases:
            phase_a(g, 0, xs[0])
        if "attn0" in phases:
            phase_attn(g, 0, True)
        if "sgu0" in phases:
            phase_sgu(g, 0, list(range(NT)))
        if "dn0" in phases:
            phase_dn(g, 0)
        if "ffn0" in phases:
            phase_out_ffn(g, 0, xs[0], xs[1], True)
        if "a1" in phases:
            phase_a(g, 1, xs[1])
        if "attn1" in phases:
            phase_attn(g, 1, False)
        if "sgu1" in phases:
            phase_sgu(g, 1, list(range(2, NT)))
        if "dn1" in phases:
            phase_dn(g, 1)
        if "out1" in phases:
            phase_out_router(g, 1, xs[1])
        if "moe1" in phases:
            phase_moe(g, 1)
        kb.barrier()
        kb.emit()
        g.n_ins = kb.n_ins
    return nc, g


def make_inputs(inputs):
    x = np.asarray(inputs["x"], np.float32)
    ctxa = np.asarray(inputs["ctx"], np.float32)
    rows = NLAT // 64
    row = np.repeat(np.arange(rows, dtype=np.float32), 64)
    col = np.tile(np.arange(64, dtype=np.float32), rows)
    inv = (10000.0 ** (-2.0 * np.arange(8, dtype=np.float32) / 32)).astype(np.float32)
    inv = (np.float32(10000.0) ** (-2.0 * np.arange(16, dtype=np.float32) / np.float32(32))).astype(np.float32)
    ang = np.stack([row[:, None] * inv, col[:, None] * inv], axis=1).astype(np.float32)
    rope = np.concatenate([np.cos(ang).reshape(NLAT, 32), np.sin(ang).reshape(NLAT, 32)], axis=1).astype(np.float32)
    shared = {k: np.ascontiguousarray(np.asarray(inputs[k], np.float32)).reshape(shp) for k, shp in W_NAMES}
    perm = np.concatenate([np.arange(C_AQ + 64 * h, C_AQ + 64 * h + 64) for h in (0, 3, 1, 4, 2, 5)])
    cols = np.arange(IN_COLS)
    cols[C_AQ:C_AQ + 384] = perm
    shared["w_in"] = np.ascontiguousarray(shared["w_in"][:, :, cols])
    shared["rope"] = rope
    shared["ident"] = np.eye(128, dtype=np.float32)
    j = np.arange(128)[:, None]
    i = np.arange(128)[None, :]
    blk = (j // 64) == (i // 64)
    dnc = np.zeros((7, 128, 128), np.float32)
    dnc[0] = blk
    dnc[1] = blk & (j <= i)
    dnc[2] = blk & (j >= i)
    dnc[3] = np.where(blk & (i >= j), 0.0, -30000.0)
    dnc[4] = np.where(blk & (i <= j), 0.0, -30000.0)
    dnc[5] = blk & (i > j)
    dnc[6] = blk & (i < j)
    shared["dnc"] = dnc
    maps = []
    for b in range(4):
        m = dict(shared)
        m["xin"] = np.ascontiguousarray(np.concatenate([ctxa[b], x[b]], axis=0))
        m["cvec"] = np.ascontiguousarray(np.stack([np.asarray(inputs["c"], np.float32)[b],
                                                   np.asarray(inputs["c_ctx"], np.float32)], 0))
        maps.append(m)
    return maps


def kernel(**inputs):
    nc, g = build_program()
    maps = make_inputs(inputs)
    res = run_bass_kernel_spmd(nc, maps, core_ids=list(range(len(maps))))
    return np.stack([r["out"] for r in res.results], axis=0)
```

```python
import numpy as np
from contextlib import ExitStack
import concourse.bass as bass
import concourse.mybir as mybir
from concourse.bass_utils import run_bass_kernel_spmd

F32 = mybir.dt.float32
BF16 = mybir.dt.bfloat16
AF = mybir.ActivationFunctionType
ALU = mybir.AluOpType
AX = mybir.AxisListType

D = 1024
NCTX = 256
NLAT = 4096
T = NCTX + NLAT
NT = T // 128
DEPTH = 2
HD = 64
IN_COLS = 2712
D_FF = 2816
MOE_FF = 3584
NEXP = 8
EPS = 1e-6
C_QKV, C_GATE, C_BETA, C_ALPHA, C_AQ, C_AK, C_AV, C_U, C_V = 0, 1152, 1536, 1548, 1560, 1944, 2072, 2200, 2456
TK_GATE, TK_BETA, TK_ALPHA, TK_AQ, TK_AK, TK_AV, TK_ZV, TK_W = 0, 384, 396, 408, 792, 920, 1048, 1304


class Buf:
    __slots__ = ("name", "ap", "last_w", "readers", "excl")

    def __init__(self, name, ap=None, excl=False):
        self.name = name
        self.ap = ap
        self.excl = excl
        self.last_w = None
        self.readers = []

    def __getitem__(self, idx):
        return self.ap[idx]


class KB:
    ENGS = ("pe", "act", "dve", "pool", "sp")
    NDMA = 12

    def __init__(self, nc, ctx, same_engine_sync=True):
        self.nc = nc
        self.ctx = ctx
        self.same = same_engine_sync
        self.ops = {e: [] for e in self.ENGS}
        self.seq = {e: 0 for e in self.ENGS}
        self.sems = {e: ctx.enter_context(nc.semaphore("c_" + e)) for e in self.ENGS}
        self.dma_sems, self.dma_cnt, self.dma_rr = {}, {}, {}
        for q in ("sp", "pool", "act"):
            self.dma_sems[q] = [ctx.enter_context(nc.semaphore("d_%s%d" % (q, i))) for i in range(self.NDMA)]
            self.dma_cnt[q] = [0] * self.NDMA
            self.dma_rr[q] = 0
        self.known = {e: {} for e in self.ENGS}
        import os as _os
        self.pool_dma = _os.environ.get("POOLDMA", "0") == "1"
        self.pool_cmp = _os.environ.get("POOLCMP", "0") == "1"
        self.n_ins = 0
        self.uid = 0

    def sbuf(self, name, shape, dt=F32, ctx=None):
        self.uid += 1
        t = (ctx or self.ctx).enter_context(self.nc.sbuf_tensor("%s_%d" % (name, self.uid), list(shape), dt))
        return Buf(name, t)

    def psum(self, name, shape, dt=F32, ctx=None):
        self.uid += 1
        t = (ctx or self.ctx).enter_context(self.nc.psum_tensor("%s_%d" % (name, self.uid), list(shape), dt))
        return Buf(name, t, excl=True)

    def dram(self, name, shape, dt=F32):
        t = self.nc.dram_tensor(name, list(shape), dt, kind="Internal")
        return Buf(name, t.ap())

    def _need(self, eng, tok, waits):
        if tok is None:
            return
        key, val = tok
        if key == eng and (eng == "pe" or not self.same):
            return
        if val > waits.get(key, 0):
            waits[key] = val

    def _sem(self, key):
        if isinstance(key, str):
            return self.sems[key]
        return self.dma_sems[key[0]][key[1]]

    def op(self, eng, fn, reads=(), writes=(), dma=False):
        if eng == "pool":
            if dma and not self.pool_dma:
                eng = "sp"
            elif not dma and not self.pool_cmp:
                eng = "dve"
        ex = [b for b in reads if b.excl]
        if ex:
            reads = [b for b in reads if not b.excl]
            writes = list(writes) + [b for b in ex if b not in writes]
        waits = {}
        for b in reads:
            self._need(eng, b.last_w, waits)
        for b in writes:
            self._need(eng, b.last_w, waits)
            for r in b.readers:
                self._need(eng, r, waits)
        if dma:
            q = eng
            i = self.dma_rr[q]
            self.dma_rr[q] = (i + 1) % self.NDMA
            key = (q, i)
            prev = self.dma_cnt[q][i]
            if prev > 0 and 16 * prev > waits.get(key, 0):
                waits[key] = 16 * prev
            self.dma_cnt[q][i] = prev + 1
            tok = (key, 16 * (prev + 1))
            inc = 16
        else:
            self.seq[eng] += 1
            tok = (eng, self.seq[eng])
            inc = 1
        kn = self.known[eng]
        wl = []
        for key, val in waits.items():
            if kn.get(key, 0) >= val:
                continue
            kn[key] = val
            wl.append((self._sem(key), val))
        self.n_ins += 1
        self.ops[eng].append((wl, fn, self._sem(tok[0]), inc))
        for b in reads:
            b.readers.append(tok)
        for b in writes:
            b.last_w = tok
            b.readers = []
        return tok

    def barrier(self):
        for e in self.ENGS:
            wl = []
            kn = self.known[e]
            for e2 in self.ENGS:
                if e2 != e and self.seq[e2] > kn.get(e2, 0):
                    kn[e2] = self.seq[e2]
                    wl.append((self.sems[e2], self.seq[e2]))
            if self.same and e != "pe" and self.seq[e] > kn.get(e, 0):
                kn[e] = self.seq[e]
                wl.append((self.sems[e], self.seq[e]))
            for q in self.dma_sems:
                for i in range(self.NDMA):
                    v = 16 * self.dma_cnt[q][i]
                    if v > kn.get((q, i), 0):
                        kn[(q, i)] = v
                        wl.append((self.dma_sems[q][i], v))
            if wl:
                self.ops[e].append((wl, None, None, 0))

    def dma(self, q, out_ap, in_ap, reads=(), writes=(), **kw):
        return self.op(q, lambda e: e.dma_start(out=out_ap, in_=in_ap, **kw), reads, writes, dma=True)

    def mm(self, out, lhsT, rhs, start, stop, reads, writes):
        return self.op("pe", lambda e: e.matmul(out, lhsT, rhs, start=start, stop=stop), reads, writes)

    def tr(self, out, in_, ident, reads, writes):
        return self.op("pe", lambda e: e.transpose(out, in_, ident), reads, writes)

    def act(self, out, in_, func, reads, writes, **kw):
        return self.op("act", lambda e: e.activation(out=out, in_=in_, func=func, **kw), reads, writes)

    def copy(self, eng, out, in_, reads, writes):
        if eng == "act":
            return self.act(out, in_, AF.Copy, reads, writes)
        return self.op(eng, lambda e: e.tensor_copy(out, in_), reads, writes)

    def tt(self, eng, out, in0, in1, op, reads, writes):
        return self.op(eng, lambda e: e.tensor_tensor(out, in0, in1, op), reads, writes)

    def ts(self, eng, out, in0, s1, s2, op0, op1, reads, writes):
        if s2 is None:
            return self.op(eng, lambda e: e.tensor_scalar(out, in0, s1, None, op0), reads, writes)
        return self.op(eng, lambda e: e.tensor_scalar(out, in0, s1, s2, op0, op1), reads, writes)

    def stt(self, eng, out, in0, scalar, in1, op0, op1, reads, writes):
        return self.op(eng, lambda e: e.scalar_tensor_tensor(out, in0, scalar, in1, op0, op1), reads, writes)

    def memset(self, eng, out, val, writes):
        return self.op(eng, lambda e: e.memset(out, val), (), writes)

    def final_wait(self, eng, bufs):
        waits = {}
        for b in bufs:
            self._need("__none__", b.last_w, waits)
        self.ops[eng].append(([(self._sem(k), v) for k, v in waits.items()], None, None, 0))

    def emit(self):
        handles = {"pe": "tensor", "act": "scalar", "dve": "vector", "pool": "gpsimd", "sp": "sync"}
        with self.nc.Block() as block:
            for e in self.ENGS:
                lst = self.ops[e]
                if not lst:
                    continue

                def body(engh, lst=lst):
                    for wl, fn, sem, inc in lst:
                        for s, v in wl:
                            engh.wait_ge(s, v)
                        if fn is not None:
                            fn(engh).then_inc(sem, inc)

                getattr(block, handles[e])(body)


class G:
    pass


def rstd_from_ssq(kb, out, ssq, n, reads, writes):
    kb.act(out, ssq, AF.Sqrt, reads, writes, scale=1.0 / n, bias=EPS)
    kb.op("dve", lambda e: e.reciprocal(out, out), writes, writes)


def phase_mod(g, L):
    kb, nc = g.kb, g.kb.nc
    with ExitStack() as sc:
        cond = kb.sbuf("cond", [128, 8, 2], ctx=sc)
        condbc = kb.sbuf("condbc", [128, 16, 128], ctx=sc)
        mbF = kb.sbuf("mbF", [128, 48], ctx=sc)
        mbrow = kb.sbuf("mbrow", [1, 6144], ctx=sc)
        gF = kb.sbuf("gF", [128, 2, 8], ctx=sc)
        modF = kb.sbuf("modF", [128, 32, 2], ctx=sc)
        grow = kb.sbuf("grow", [1, 512], ctx=sc)
        mw = [kb.sbuf("mw%d" % k, [128, 3072], ctx=sc) for k in range(8)]
        for s in range(2):
            kb.dma("sp", cond[:, :, s], g.cvec.ap[s, :].rearrange("(k p) -> p k", p=128), [g.cvec], [cond],
                   allow_slow_non_contiguous=True)
        kb.act(cond[:], cond[:], AF.Silu, [cond], [cond])
        kb.copy("dve", condbc[:], cond[:].rearrange("p k s -> p (k s)").unsqueeze(2).to_broadcast([128, 16, 128]),
                [cond], [condbc])
        kb.dma("sp", mbF[:], g.mod_b.ap[L, :].rearrange("(c p) -> p c", p=128), [g.mod_b], [mbF],
               allow_slow_non_contiguous=True)
        kb.dma("sp", mbrow[:], g.mod_b.ap[L:L + 1, :], [g.mod_b], [mbrow])
        kb.dma("sp", gF[:, 0, :], g.norm1_g.ap[L, :].rearrange("(c p) -> p c", p=128), [g.norm1_g], [gF],
               allow_slow_non_contiguous=True)
        kb.dma("sp", gF[:, 1, :], g.norm2_g.ap[L, :].rearrange("(c p) -> p c", p=128), [g.norm2_g], [gF],
               allow_slow_non_contiguous=True)
        psF, psB = g.ps[0], g.ps[1]
        for h in range(2):
            for k in range(8):
                kb.dma("sp" if k % 2 == 0 else "pool", mw[k][:],
                       g.mod_w.ap[L, 128 * k:128 * k + 128, 3072 * h:3072 * h + 3072], [g.mod_w], [mw[k]])
            for j in range(16):
                for k in range(8):
                    kb.mm(psF[:, 2 * j:2 * j + 2], mw[k][:, 128 * j:128 * j + 128], cond[:, k, :], k == 0, k == 7,
                          [mw[k], cond], [psF])
            kb.tt("dve", modF[:, 16 * h:16 * h + 16, :], psF[:, 0:32].rearrange("p (j s) -> p j s", s=2),
                  mbF[:, 24 * h:24 * h + 16].unsqueeze(2).to_broadcast([128, 16, 2]), ALU.add, [psF, mbF], [modF])
            for s in range(2):
                for cc in range(2):
                    for k in range(8):
                        kb.mm(psB[:, :], condbc[:, 2 * k + s, :], mw[k][:, 2048 + 512 * cc:2048 + 512 * cc + 512],
                              k == 0, k == 7, [mw[k], condbc], [psB])
                    c0 = 3072 * h + 2048 + 512 * cc
                    kb.tt("dve", grow[0:1, :], psB[0:1, :], mbrow[0:1, c0:c0 + 512], ALU.add, [psB, mbrow], [grow])
                    kb.dma("sp", g.gates.ap[L, h, s:s + 1, 512 * cc:512 * cc + 512], grow[0:1, :], [grow], [g.gates])
        for h in range(2):
            sc_ap = modF[:, 16 * h + 8:16 * h + 16, :]
            kb.ts("dve", g.GS[:, L, h, :, :], sc_ap, 1.0, None, ALU.add, None, [modF], [g.GSb])
            kb.tt("dve", g.GS[:, L, h, :, :], g.GS[:, L, h, :, :], gF[:, h, :].unsqueeze(2).to_broadcast([128, 8, 2]),
                  ALU.mult, [gF, g.GSb], [g.GSb])
            kb.copy("dve", g.SH[:, L, h, :, :], modF[:, 16 * h:16 * h + 8, :], [modF], [g.SHb])
    kb.barrier()


def norm_tile_to_hT(g, L, h, xt, tile_is_ctx, hT_out_aps, hT_buf, scr, also_f32=None, xap=None, f32_buf=None):
    kb = g.kb
    s = 1 if tile_is_ctx else 0
    junk, ssq, xn = scr["junk"], scr["ssq"], scr["xn"]
    if xap is None:
        xap = xt[:]
    kb.act(junk[:], xap, AF.Square, [xt, ssq], [junk, ssq], accum_out=ssq[:, 0:1])
    rstd_from_ssq(kb, ssq[:, 0:1], ssq[:, 0:1], D, [ssq], [ssq])
    kb.act(xn[:], xap, AF.Copy, [xt, ssq], [xn], scale=ssq[:, 0:1])
    pT = scr["psT"]
    for c in range(8):
        kb.tr(pT[c // 4][:, (c % 4) * 128:(c % 4) * 128 + 128], xn[:, c * 128:c * 128 + 128], g.ident[:], [xn, g.ident],
              [pT[c // 4]])
    for c in range(8):
        kb.act(hT_out_aps[c], pT[c // 4][:, (c % 4) * 128:(c % 4) * 128 + 128], AF.Identity, [pT[c // 4], g.GSb, g.SHb],
               [hT_buf], scale=g.GS[:, L, h, c, s:s + 1], bias=g.SH[:, L, h, c, s:s + 1])
        if also_f32 is not None:
            kb.act(also_f32[c], pT[c // 4][:, (c % 4) * 128:(c % 4) * 128 + 128], AF.Identity,
                   [pT[c // 4], g.GSb, g.SHb], [f32_buf], scale=g.GS[:, L, h, c, s:s + 1], bias=g.SH[:, L, h, c, s:s + 1])


def load_cast_weight(g, sc, name, dram_buf, src_ap_fn, nk, ncols, stage_bufs, dst, dst_ap_fn):
    kb = g.kb
    for k in range(nk):
        st = stage_bufs[k % len(stage_bufs)]
        kb.dma("sp" if k % 2 == 0 else "pool", st[:, 0:ncols], src_ap_fn(k), [dram_buf], [st])
        kb.copy("pool" if k % 2 == 0 else "dve", dst_ap_fn(k), st[:, 0:ncols], [st], [dst])


def phase_a(g, L, xs_tiles):
    kb = g.kb
    with ExitStack() as sc:
        winb = kb.sbuf("winb", [128, 8, IN_COLS], BF16, ctx=sc)
        stage = [kb.sbuf("wst%d" % i, [128, IN_COLS], ctx=sc) for i in range(2)]
        load_cast_weight(g, sc, "win", g.w_in, lambda k: g.w_in.ap[L, 128 * k:128 * k + 128, :], 8, IN_COLS, stage, winb,
                         lambda k: winb[:, k, :])
        scr = {"junk": kb.sbuf("junk", [128, 1024], ctx=sc), "ssq": kb.sbuf("ssq", [128, 1], ctx=sc),
               "xn": kb.sbuf("xn", [128, 1024], ctx=sc), "psT": [g.ps[0], g.ps[1]]}
        xts = [kb.sbuf("xt%d" % i, [128, 1024], ctx=sc) for i in range(2)]
        hTs = [kb.sbuf("hT%d" % i, [128, 8, 512], BF16, ctx=sc) for i in range(2)]
        toks = [kb.sbuf("tok%d" % i, [128, TK_W], ctx=sc) for i in range(2)]
        fms = [kb.sbuf("fm%d" % i, [128, 512], ctx=sc) for i in range(3)]
        psTM = [g.ps[2], g.ps[3], g.ps[4]]
        psFM = [g.ps[5], g.ps[6]]
        blocks = [(0, 2)] + [(2 + 4 * i, 4) for i in range(8)]
        ti = 0
        fi = 0
        for bi, (t0, nt) in enumerate(blocks):
            hT = hTs[bi % 2]
            ntok = nt * 128
            for j in range(nt):
                t = t0 + j
                xt = xts[ti % 2]
                tok = toks[ti % 2]
                ti += 1
                kb.dma("sp", xt[:], xs_tiles[t].ap, [xs_tiles[t]], [xt])
                norm_tile_to_hT(g, L, 0, xt, t < 2, [hT[:, c, j * 128:j * 128 + 128] for c in range(8)], hT, scr)
                segs = [(psTM[0], 0, 512, C_GATE), (psTM[1], 0, 512, C_GATE + 512), (psTM[2], 0, 24, C_GATE + 1024),
                        (psTM[2], 24, 256, C_V)]
                for ps, o0, w, c0 in segs:
                    for k in range(8):
                        kb.mm(ps[:, o0:o0 + w], hT[:, k, j * 128:j * 128 + 128], winb[:, k, c0:c0 + w], k == 0, k == 7,
                              [hT, winb], [ps])
                kb.copy("dve", tok[:, 0:512], psTM[0][:, :], [psTM[0]], [tok])
                kb.copy("act", tok[:, 512:1024], psTM[1][:, :], [psTM[1]], [tok])
                kb.copy("dve", tok[:, 1024:1304], psTM[2][:, 0:280], [psTM[2]], [tok])
                kb.dma("sp", g.TOK[t].ap, tok[:], [tok], [g.TOK[t]])
            for fc in range(11):
                c0 = C_QKV + 128 * fc if fc < 9 else C_U + 128 * (fc - 9)
                ps = psFM[fi % 2]
                fm = fms[fi % 3]
                fi += 1
                for k in range(8):
                    kb.mm(ps[:, 0:ntok], winb[:, k, c0:c0 + 128], hT[:, k, 0:ntok], k == 0, k == 7, [hT, winb], [ps])
                if fc < 9:
                    kb.copy("act" if fc % 2 == 0 else "dve", fm[:, 0:ntok], ps[:, 0:ntok], [ps], [fm])
                else:
                    kb.act(fm[:, 0:ntok], ps[:, 0:ntok], AF.Gelu, [ps], [fm])
                kb.dma("pool", g.ZQ[fc][bi].ap, fm[:, 0:ntok], [fm], [g.ZQ[fc][bi]])
    kb.barrier()


def phase_attn(g, L, with_ctx, nblk=8):
    kb = g.kb
    with ExitStack() as sc:
        QT = [kb.sbuf("QT%d" % p, [128, T], BF16, ctx=sc) for p in range(3)]
        KT = kb.sbuf("KT", [128, T], BF16, ctx=sc)
        VE = kb.sbuf("VE", [128, NT, 128], BF16, ctx=sc)
        ones = kb.sbuf("ones", [128, 64], BF16, ctx=sc)
        gain = kb.sbuf("gain", [128, 8, 64], ctx=sc)
        kb.memset("dve", ones[:], 1.0, [ones])
        kb.dma("sp", gain[:, 0, :], g.q_norm_g.ap[L:L + 1, :].to_broadcast([128, 64]), [g.q_norm_g], [gain])
        kb.dma("sp", gain[:, 6, :], g.k_norm_g.ap[L:L + 1, :].to_broadcast([128, 64]), [g.k_norm_g], [gain])
        kb.ts("dve", gain[:, 0, :], gain[:, 0, :], 0.125, None, ALU.mult, None, [gain], [gain])
        kb.copy("dve", gain[:, 1:6, :], gain[:, 0:1, :].to_broadcast([128, 5, 64]), [gain], [gain])
        kb.copy("dve", gain[:, 7, :], gain[:, 6, :], [gain], [gain])
        qks = [kb.sbuf("qk%d" % i, [128, 512], ctx=sc) for i in range(2)]
        v32s = [kb.sbuf("v32%d" % i, [128, 128], ctx=sc) for i in range(2)]
        css = [kb.sbuf("cs%d" % i, [128, 64], ctx=sc) for i in range(2)]
        sqt = kb.sbuf("sqt", [128, 512], ctx=sc)
        ssq = kb.sbuf("ssq8", [128, 8], ctx=sc)
        qn = kb.sbuf("qn", [128, 512], ctx=sc)
        qr = kb.sbuf("qr", [128, 512], ctx=sc)
        tm = [kb.sbuf("ropet%d" % i, [128, 256], ctx=sc) for i in range(4)]
        psTr = g.ps[0]
        import os as _os
        STG = int(_os.environ.get("ATTN_STG", "9"))
        for t in range(int(_os.environ.get("ATTN_NT", str(NT)))):
            qk, v32, cs = qks[t % 2], v32s[t % 2], css[t % 2]
            kb.dma("sp", qk[:], g.TOK[t].ap[:, TK_AQ:TK_AQ + 512], [g.TOK[t]], [qk])
            kb.dma("pool", v32[:], g.TOK[t].ap[:, TK_AV:TK_AV + 128], [g.TOK[t]], [v32])
            kb.copy("pool", VE[:, t, :], v32[:], [v32], [VE])
            if STG < 2:
                continue
            kb.tt("pool", sqt[:], qk[:], qk[:], ALU.mult, [qk], [sqt])
            kb.op("dve", lambda e, o=ssq[:, 0:8], i=sqt[:].rearrange("p (h d) -> p h d", h=8): e.reduce_sum(o, i, AX.X),
                  [sqt], [ssq])
            rstd_from_ssq(kb, ssq[:, 0:8], ssq[:, 0:8], 64, [ssq], [ssq])
            if STG < 3:
                continue
            q3 = qn[:].rearrange("p (h d) -> p h d", h=8)
            kb.tt("dve", q3, qk[:].rearrange("p (h d) -> p h d", h=8), ssq[:, 0:8].unsqueeze(2).to_broadcast([128, 8, 64]),
                  ALU.mult, [qk, ssq], [qn])
            if STG < 4:
                continue
            if t >= 2:
                kb.tt("pool", q3, q3, gain[:], ALU.mult, [qn, gain], [qn])
                if STG < 5:
                    continue
                kb.dma("sp", cs[:], g.rope.ap[128 * (t - 2):128 * (t - 2) + 128, :], [g.rope], [cs])
                q5 = qn[:].rearrange("p (h a b r) -> p h a b r", h=8, a=2, b=2, r=16)
                o5 = qr[:].rearrange("p (h a b r) -> p h a b r", h=8, a=2, b=2, r=16)
                for ax in [int(v) for v in _os.environ.get("ROPE_AX", "0,1").split(",") if v != ""]:
                    a_, b_ = q5[:, :, ax, 0, :], q5[:, :, ax, 1, :]
                    cos = cs[:, 16 * ax:16 * ax + 16].unsqueeze(1).to_broadcast([128, 8, 16])
                    sin = cs[:, 32 + 16 * ax:32 + 16 * ax + 16].unsqueeze(1).to_broadcast([128, 8, 16])
                    tv = [x[:, 128 * ax:128 * ax + 128].rearrange("p (h r) -> p h r", h=8) for x in tm]
                    kb.tt("dve", tv[0], a_, cos, ALU.mult, [qn, cs], [tm[0]])
                    kb.tt("dve", tv[1], b_, sin, ALU.mult, [qn, cs], [tm[1]])
                    kb.tt("dve", o5[:, :, ax, 0, :], tv[0], tv[1], ALU.subtract, [tm[0], tm[1]], [qr])
                    kb.tt("dve", tv[2], a_, sin, ALU.mult, [qn, cs], [tm[2]])
                    kb.tt("dve", tv[3], b_, cos, ALU.mult, [qn, cs], [tm[3]])
                    kb.tt("dve", o5[:, :, ax, 1, :], tv[2], tv[3], ALU.add, [tm[2], tm[3]], [qr])
            else:
                kb.tt("pool", qr[:].rearrange("p (h d) -> p h d", h=8), q3, gain[:], ALU.mult, [qn, gain], [qr])
            if STG < 6:
                continue
            for p in range(3):
                kb.tr(psTr[:, p * 128:p * 128 + 128], qr[:, p * 128:p * 128 + 128], g.ident[:], [qr, g.ident], [psTr])
            kb.tr(psTr[:, 384:512], qr[:, 384:512], g.ident[:], [qr, g.ident], [psTr])
            if STG < 7:
                continue
            for p in range(3):
                kb.copy("act", QT[p][:, t * 128:t * 128 + 128], psTr[:, p * 128:p * 128 + 128], [psTr], [QT[p]])
            if STG < 8:
                continue
            kb.copy("act", KT[:, t * 128:t * 128 + 128], psTr[:, 384:512], [psTr], [KT])
        import os as _os
        if _os.environ.get("ATTN_PREP_ONLY"):
            kb.barrier()
            return
        psS = [g.ps[1], g.ps[2], g.ps[3]]
        accVs, accDs = [g.ps[4], g.ps[5]], [g.ps[6], g.ps[7]]
        PTs = [kb.sbuf("PT%d" % i, [128, 512], BF16, ctx=sc) for i in range(3)]
        rcs = [kb.sbuf("rc%d" % i, [64, 512], ctx=sc) for i in range(2)]
        ots = [kb.sbuf("ot%d" % i, [64, 512], ctx=sc) for i in range(2)]
        qblocks = ([(0, 0, 256, [0, 1])] if with_ctx else []) + \
                  [(1 + i, 256 + 512 * i, 512, list(range(NT))) for i in range(nblk)]
        items = []
        bi_ = 0
        for h in range(6):
            for (blk, q0, nq, kts) in qblocks:
                for ii, kt in enumerate(kts):
                    items.append((h, blk, q0, nq, kt, ii == 0, ii == len(kts) - 1, bi_))
                bi_ += 1
        LA = 2

        def emit_qk(i):
            h, blk, q0, nq, kt, first, last, bn = items[i]
            kv, p = h // 3, h % 3
            pr = slice(64 * kv, 64 * kv + 64)
            ps, PT = psS[i % 3], PTs[i % 3]
            kb.mm(ps[:, 0:nq], KT[pr, kt * 128:kt * 128 + 128], QT[p][pr, q0:q0 + nq], True, True, [KT, QT[p]], [ps])
            kb.act(PT[:, 0:nq], ps[:, 0:nq], AF.Exp, [ps], [PT])

        def emit_pv(i):
            h, blk, q0, nq, kt, first, last, bn = items[i]
            kv = h // 3
            PT = PTs[i % 3]
            accV, accD = accVs[bn % 2], accDs[bn % 2]
            kb.mm(accV[0:64, 0:nq], VE[:, kt, 64 * kv:64 * kv + 64], PT[:, 0:nq], first, last, [VE, PT], [accV])
            kb.mm(accD[0:64, 0:nq], ones[:, :], PT[:, 0:nq], first, last, [ones, PT], [accD])
            if last:
                rc, ot = rcs[bn % 2], ots[bn % 2]
                kb.op("dve", lambda e, o=rc[:, 0:nq], i_=accD[0:64, 0:nq]: e.reciprocal(o, i_), [accD], [rc])
                kb.tt("dve", ot[:, 0:nq], accV[0:64, 0:nq], rc[:, 0:nq], ALU.mult, [accV, rc], [ot])
                kb.dma("sp", g.MIXat[h][blk].ap, ot[:, 0:nq], [ot], [g.MIXat[h][blk]])

        n_it = len(items)
        for i in range(n_it + LA):
            if i < n_it:
                emit_qk(i)
            if i - LA >= 0:
                emit_pv(i - LA)
    kb.barrier()


def phase_sgu(g, L, tiles):
    kb = g.kb
    with ExitStack() as sc:
        WsT = kb.sbuf("WsT", [128, 4, 128], BF16, ctx=sc)
        ws32 = kb.sbuf("ws32", [128, 4, 128], ctx=sc)
        SB = kb.sbuf("SBb", [64, 512], ctx=sc)
        sgain = kb.sbuf("sgain", [128, 256], ctx=sc)
        psW = g.ps[0]
        for gi in range(4):
            kb.dma("sp", ws32[:, gi, :], g.sgu_w.ap[L, gi, :, :], [g.sgu_w], [ws32])
        for gi in range(4):
            kb.tr(psW[:, gi * 128:gi * 128 + 128], ws32[:, gi, :], g.ident[:], [ws32, g.ident], [psW])
        kb.copy("dve", WsT[:].rearrange("p g i -> p (g i)"), psW[:, :], [psW], [WsT])
        kb.dma("sp", SB[:], g.sgu_b.ap[L:L + 1, :, :].rearrange("o g i -> o (g i)").to_broadcast([64, 512]), [g.sgu_b], [SB])
        kb.dma("sp", sgain[:], g.sgu_norm_g.ap[L:L + 1, :].to_broadcast([128, 256]), [g.sgu_norm_g], [sgain])
        zvs = [kb.sbuf("zv%d" % i, [128, 256], ctx=sc) for i in range(2)]
        uts = [kb.sbuf("ut%d" % i, [64, 4, 128], ctx=sc) for i in range(2)]
        gv = kb.sbuf("gv", [128, 256], ctx=sc)
        sq = kb.sbuf("sgsq", [128, 256], ctx=sc)
        ss = kb.sbuf("sgss", [128, 4], ctx=sc)
        vb = kb.sbuf("vb", [128, 256], BF16, ctx=sc)
        tmps = [kb.sbuf("sgt%d" % i, [64, 512], ctx=sc) for i in range(2)]
        ress = [kb.sbuf("sgr%d" % i, [64, 512], ctx=sc) for i in range(2)]
        pss = [g.ps[1], g.ps[2]]
        for n, t in enumerate(tiles):
            zv, ut, tmp, res, ps = zvs[n % 2], uts[n % 2], tmps[n % 2], ress[n % 2], pss[n % 2]
            bi, boff = g.blk_of_tile(t)
            kb.dma("sp", zv[:], g.TOK[t].ap[:, TK_ZV:TK_ZV + 256], [g.TOK[t]], [zv])
            kb.dma("pool", ut[:], g.zqd[1152:1408, 128 * t:128 * t + 128].rearrange("(g d) t -> d g t", d=64),
                   [g.ZQ[9][bi], g.ZQ[10][bi]], [ut])
            kb.act(gv[:], zv[:], AF.Gelu_apprx_tanh, [zv], [gv])
            kb.tt("pool", sq[:], gv[:], gv[:], ALU.mult, [gv], [sq])
            kb.op("dve", lambda e, o=ss[:, 0:4], i=sq[:].rearrange("p (g d) -> p g d", g=4): e.reduce_sum(o, i, AX.X),
                  [sq], [ss])
            rstd_from_ssq(kb, ss[:, 0:4], ss[:, 0:4], 64, [ss], [ss])
            kb.tt("dve", gv[:].rearrange("p (g d) -> p g d", g=4), gv[:].rearrange("p (g d) -> p g d", g=4),
                  ss[:, 0:4].unsqueeze(2).to_broadcast([128, 4, 64]), ALU.mult, [gv, ss], [gv])
            kb.tt("pool", vb[:], gv[:], sgain[:], ALU.mult, [gv, sgain], [vb])
            for gi in range(4):
                kb.mm(ps[0:64, gi * 128:gi * 128 + 128], vb[:, gi * 64:gi * 64 + 64], WsT[:, gi, :], True, True, [vb, WsT], [ps])
            kb.tt("dve", tmp[:], ps[0:64, :], SB[:], ALU.add, [ps, SB], [tmp])
            kb.tt("pool", res[:], tmp[:], ut[:].rearrange("d g t -> d (g t)"), ALU.mult, [tmp, ut], [res])
            kb.dma("sp", g.mixd[768:1024, 128 * t:128 * t + 128].rearrange("(g d) t -> d g t", d=64),
                   res[:].rearrange("d (g t) -> d g t", g=4), [res], [g.MIXsg[t]])
    kb.barrier()


class _B:
    pass


def phase_dn(g, L, half_mode=False):
    kb = g.kb
    ident = g.ident
    with ExitStack() as sc:
        def S(name, shape, dt=F32):
            return kb.sbuf(name, shape, dt, ctx=sc)
        dnc = S("dnc", [128, 7, 128])
        kb.dma("sp", dnc[:], g.dncd.ap.rearrange("c p i -> p c i"), [g.dncd], [dnc])
        ones = S("ones32", [128, 128])
        kb.memset("dve", ones[:], 1.0, [ones])
        I4 = S("I4", [128, 4, 128])
        NM4 = [S("NM4%d" % d, [128, 4, 128]) for d in range(2)]
        SM4 = [S("SM4%d" % d, [128, 4, 128]) for d in range(2)]
        for c in range(4):
            kb.copy("dve", I4[:, c, :], ident[:], [ident], [I4])
            for d in range(2):
                kb.copy("dve", NM4[d][:, c, :], dnc[:, 3 + d, :], [dnc], [NM4[d]])
                kb.copy("dve", SM4[d][:, c, :], dnc[:, 5 + d, :], [dnc], [SM4[d]])
        BA = S("BA", [128, 68, 24])
        src = g.tokd[:, TK_BETA:TK_BETA + 24].rearrange("(c i) f -> i c f", i=64)
        kb.dma("sp", BA[0:64, :, :], src, g.TOK, [BA])
        kb.dma("pool", BA[64:128, :, :], src, g.TOK, [BA])
        ZB, ZA = S("ZB", [128, 6, 68]), S("ZA", [128, 6, 68])
        for d in range(2):
            for half in range(2):
                pr = slice(64 * half, 64 * half + 64)
                kb.copy("dve", ZB[pr, 3 * d:3 * d + 3, :], BA[pr, :, 6 * d + half:6 * d + 6:2].rearrange("i c p -> i p c"), [BA], [ZB])
                kb.copy("dve", ZA[pr, 3 * d:3 * d + 3, :],
                        BA[pr, :, 12 + 6 * d + half:12 + 6 * d + 6:2].rearrange("i c p -> i p c"), [BA], [ZA])
        AL, DTB, NEA = S("AL", [128, 6]), S("DTB", [128, 6]), S("NEA", [128, 6])
        for half in range(2):
            pr = slice(64 * half, 64 * half + 64)
            kb.dma("sp", AL[pr, :].rearrange("p (d q) -> p d q", d=2), g.dn_a_log.ap[L:L + 1, :, half::2].to_broadcast([64, 2, 3]),
                   [g.dn_a_log], [AL], allow_slow_non_contiguous=True)
            kb.dma("sp", DTB[pr, :].rearrange("p (d q) -> p d q", d=2), g.dn_dt_bias.ap[L:L + 1, :, half::2].to_broadcast([64, 2, 3]),
                   [g.dn_dt_bias], [DTB], allow_slow_non_contiguous=True)
        kb.act(NEA[:], AL[:], AF.Exp, [AL], [NEA])
        kb.ts("dve", NEA[:], NEA[:], -1.0, None, ALU.mult, None, [NEA], [NEA])
        NBETA, GT, GC, GL, E, KTS, EGL = [S(n, [128, 6, 68]) for n in ("NBETA", "GT", "GC", "GL", "E", "KTS", "EGL")]
        kb.act(NBETA[:], ZB[:], AF.Sigmoid, [ZB], [NBETA])
        kb.ts("dve", NBETA[:], NBETA[:], -1.0, None, ALU.mult, None, [NBETA], [NBETA])
        kb.tt("dve", GT[:], ZA[:], DTB[:].unsqueeze(2).to_broadcast([128, 6, 68]), ALU.add, [ZA, DTB], [GT])
        kb.act(GT[:], GT[:], AF.Exp, [GT], [GT])
        kb.act(GT[:], GT[:], AF.Ln, [GT], [GT], bias=1.0)
        kb.tt("dve", GT[:], GT[:], NEA[:].unsqueeze(2).to_broadcast([128, 6, 68]), ALU.mult, [GT, NEA], [GT])
        ps = g.ps[0]
        kb.mm(ps[:, 0:204], dnc[:, 1, :], GT[:, 0:3, :].rearrange("p a c -> p (a c)"), True, True, [dnc, GT], [ps])
        kb.mm(ps[:, 204:408], dnc[:, 2, :], GT[:, 3:6, :].rearrange("p a c -> p (a c)"), True, True, [dnc, GT], [ps])
        kb.copy("dve", GC[:].rearrange("p a c -> p (a c)"), ps[:, 0:408], [ps], [GC])
        kb.mm(ps[:, 0:408], dnc[:, 0, :], GT[:].rearrange("p a c -> p (a c)"), True, True, [dnc, GT], [ps])
        kb.copy("dve", GL[:].rearrange("p a c -> p (a c)"), ps[:, 0:408], [ps], [GL])
        kb.act(E[:], GC[:], AF.Exp, [GC], [E])
        kb.act(EGL[:], GL[:], AF.Exp, [GL], [EGL])
        kb.tt("dve", KTS[:], GL[:], GC[:], ALU.subtract, [GL, GC], [KTS])
        kb.act(KTS[:], KTS[:], AF.Exp, [KTS], [KTS])
        cw = S("cw", [128, 9, 3])
        for fc in range(9):
            kb.dma("sp", cw[:, fc, :], g.conv_w.ap[L, :, 128 * fc:128 * fc + 128].rearrange("k p -> p k"), [g.conv_w], [cw],
                   allow_slow_non_contiguous=True)
        dgain = S("dgain", [64, 1])
        kb.dma("sp", dgain[:], g.dn_norm_g.ap[L, :].rearrange("(e o) -> e o", o=1), [g.dn_norm_g], [dgain],
               allow_slow_non_contiguous=True)
        qn, kn, vn, zr = S("dqn", [128, T]), S("dkn", [128, T]), S("dvn", [128, T]), S("zraw", [128, T])
        sqb = S("dsqb", [128, 512])
        rsb = S("drsb", [128, 512])
        OB = S("OB", [128, 68, 64])
        bufs = []
        for d in range(2):
            B = _B()
            for n in ("kT", "qT", "vT", "kBD", "diag", "D", "attnT", "N", "NT", "P2", "PT2", "R", "kt", "tmp"):
                setattr(B, n, S("%s%d" % (n, d), [128, 4, 128]))
            B.vst = S("vst%d" % d, [128, 4, 64])
            B.ntmp, B.vnew, B.o1 = S("ntmp%d" % d, [128, 64]), S("vnew%d" % d, [128, 64]), S("o1%d" % d, [128, 64])
            B.S = [S("S%d_%d" % (d, i), [128, 64]) for i in range(2)]
            B.banks = g.ps[4 * d:4 * d + 4]
            for t_ in (B.kT, B.qT, B.vT):
                kb.memset("dve", t_[:], 0.0, [t_])
            bufs.append(B)
        gts = [S("dgt%d" % i, [128, 4, 64]) for i in range(2)]
        otb = [S("dot%d" % i, [64, 512]) for i in range(2)]
        oss = S("doss", [128, 4])
        f2 = lambda ap: ap.rearrange("p c i -> p (c i)")

        for pp in range(3):
            for xi, dst in enumerate((qn, kn, vn)):
                fc = 3 * xi + pp
                for bi, (b0, n) in enumerate(BLOCKS):
                    kb.dma("sp" if bi % 2 == 0 else "pool", zr[:, b0:b0 + n], g.ZQ[fc][bi].ap, [g.ZQ[fc][bi]], [zr])
                kb.act(dst[:], zr[:], AF.Copy, [zr, cw], [dst], scale=cw[:, fc, 1:2])
                for (a0, a1) in ((0, NCTX), (NCTX, T)):
                    kb.stt("dve", dst[:, a0 + 1:a1], zr[:, a0:a1 - 1], cw[:, fc, 0:1], dst[:, a0 + 1:a1], ALU.mult, ALU.add,
                           [zr, cw, dst], [dst])
                    kb.stt("dve", dst[:, a0:a1 - 1], zr[:, a0 + 1:a1], cw[:, fc, 2:3], dst[:, a0:a1 - 1], ALU.mult, ALU.add,
                           [zr, cw, dst], [dst])
                kb.act(dst[:], dst[:], AF.Silu, [dst], [dst])
                if xi < 2:
                    for (b0, n) in BLOCKS:
                        kb.tt("dve", sqb[:, 0:n], dst[:, b0:b0 + n], dst[:, b0:b0 + n], ALU.mult, [dst], [sqb])
                        kb.mm(g.ps[0][:, 0:n], dnc[:, 0, :], sqb[:, 0:n], True, True, [dnc, sqb], [g.ps[0]])
                        sc_ = 64.0 if xi == 0 else 1.0
                        kb.act(rsb[:, 0:n], g.ps[0][:, 0:n], AF.Sqrt, [g.ps[0]], [rsb], scale=sc_, bias=sc_ * EPS)
                        kb.op("dve", lambda e, o=rsb[:, 0:n]: e.reciprocal(o, o), [rsb], [rsb])
                        kb.tt("dve", dst[:, b0:b0 + n], dst[:, b0:b0 + n], rsb[:, 0:n], ALU.mult, [dst, rsb], [dst])
            for d in range(2):
                kb.memset("dve", bufs[d].S[0][:], 0.0, [bufs[d].S[0]])
            written = set()
            scnt = [0, 0]

            def prep(d, grp):
                B = bufs[d]
                dp, c0, t0 = 3 * d + pp, 4 * grp, 256 * grp
                pA, pB, pC, pD = B.banks
                for dst, src_ in ((B.kT, kn), (B.qT, qn), (B.vT, vn)):
                    for half in range(2):
                        pr = slice(64 * half, 64 * half + 64)
                        kb.copy("dve", dst[pr, :, 64 * half:64 * half + 64],
                                src_[pr, t0:t0 + 256].rearrange("p (c i) -> p c i", c=4), [src_], [dst])
                        yield
                for c in range(4):
                    kb.tr(pA[:, c * 128:c * 128 + 128], B.kT[:, c, :], ident[:], [B.kT, ident], [pA])
                    yield
                kb.copy("act", f2(B.kBD[:]), pA[:, :], [pA], [B.kBD])
                yield
                for c in range(4):
                    kb.tr(pA[:, c * 128:c * 128 + 128], B.vT[:, c, :], ident[:], [B.vT, ident], [pA])
                    yield
                kb.copy("act", f2(B.tmp[:]), pA[:, :], [pA], [B.tmp])
                yield
                kb.tt("dve", B.vst[:], B.tmp[:, :, 0:64], B.tmp[:, :, 64:128], ALU.add, [B.tmp], [B.vst])
                yield
                for c in range(4):
                    kb.mm(pB[:, c * 128:c * 128 + 128], B.kT[:, c, :], B.kT[:, c, :], True, True, [B.kT], [pB])
                    yield
                for c in range(4):
                    kb.mm(pC[:, c * 128:c * 128 + 128], B.kT[:, c, :], B.qT[:, c, :], True, True, [B.kT, B.qT], [pC])
                    yield
                for c in range(4):
                    kb.ts("dve", B.diag[:, c, :], ident[:], GC[:, dp, c0 + c:c0 + c + 1], None, ALU.mult, None, [ident, GC], [B.diag])
                    yield
                kb.mm(pA[:, :], ones[:], f2(B.diag[:]), True, True, [ones, B.diag], [pA])
                yield
                for c in range(4):
                    kb.ts("dve", B.D[:, c, :], pA[:, c * 128:c * 128 + 128], GC[:, dp, c0 + c:c0 + c + 1], 0.0, ALU.subtract, ALU.min,
                          [pA, GC], [B.D])
                    yield
                kb.tt("dve", f2(B.D[:]), f2(B.D[:]), f2(NM4[d][:]), ALU.add, [B.D, NM4[d]], [B.D])
                yield
                kb.act(f2(B.D[:]), f2(B.D[:]), AF.Exp, [B.D], [B.D])
                yield
                kb.tt("dve", f2(B.attnT[:]), pC[:, :], f2(B.D[:]), ALU.mult, [pC, B.D], [B.attnT])
                yield
                kb.tt("dve", f2(B.N[:]), pB[:, :], f2(B.D[:]), ALU.mult, [pB, B.D], [B.N])
                yield
                kb.tt("dve", f2(B.N[:]), f2(B.N[:]), f2(SM4[d][:]), ALU.mult, [B.N, SM4[d]], [B.N])
                yield
                for c in range(4):
                    kb.act(B.N[:, c, :], B.N[:, c, :], AF.Copy, [B.N, NBETA], [B.N], scale=NBETA[:, dp, c0 + c:c0 + c + 1])
                    yield
                for c in range(4):
                    kb.tr(pA[:, c * 128:c * 128 + 128], B.N[:, c, :], ident[:], [B.N, ident], [pA])
                    yield
                kb.copy("act", f2(B.NT[:]), pA[:, :], [pA], [B.NT])
                yield
                kb.tt("dve", f2(B.R[:]), f2(B.N[:]), f2(I4[:]), ALU.add, [B.N, I4], [B.R])
                yield
                P, PT = B.N, B.NT
                for k in range(5):
                    Pn, PTn = (B.P2, B.PT2) if k % 2 == 0 else (B.N, B.NT)
                    for c in range(4):
                        kb.mm(pC[:, c * 128:c * 128 + 128], P[:, c, :], PT[:, c, :], True, True, [P, PT], [pC])
                        yield
                    if k < 4:
                        for c in range(4):
                            kb.mm(pB[:, c * 128:c * 128 + 128], PT[:, c, :], P[:, c, :], True, True, [P, PT], [pB])
                            yield
                    kb.copy("act", f2(PTn[:]), pC[:, :], [pC], [PTn])
                    yield
                    if k < 4:
                        kb.copy("dve", f2(Pn[:]), pB[:, :], [pB], [Pn])
                        yield
                    for c in range(4):
                        kb.mm(pA[:, c * 128:c * 128 + 128], PTn[:, c, :], B.R[:, c, :], True, True, [PTn, B.R], [pA])
                        yield
                    kb.tt("dve", f2(B.R[:]), f2(B.R[:]), pA[:, :], ALU.add, [B.R, pA], [B.R])
                    yield
                    P, PT = Pn, PTn
                for c in range(4):
                    kb.act(B.kt[:, c, :], B.kBD[:, c, :], AF.Copy, [B.kBD, KTS], [B.kt], scale=KTS[:, dp, c0 + c:c0 + c + 1])
                    yield

            def scan(d, grp):
                B = bufs[d]
                dp, c0 = 3 * d + pp, 4 * grp
                pD = B.banks[3]
                for c in (range(4) if d == 0 else range(3, -1, -1)):
                    ch = c0 + c
                    S_old, S_new = B.S[scnt[d] % 2], B.S[(scnt[d] + 1) % 2]
                    scnt[d] += 1
                    kb.mm(pD[:, 0:64], B.kT[:, c, :], S_old[:], True, True, [B.kT, S_old], [pD])
                    yield
                    kb.stt("dve", B.ntmp[:], pD[:, 0:64], E[:, dp, ch:ch + 1], B.vst[:, c, :], ALU.mult, ALU.subtract,
                           [pD, E, B.vst], [B.ntmp])
                    yield
                    kb.mm(pD[:, 64:128], B.R[:, c, :], B.ntmp[:], True, True, [B.R, B.ntmp], [pD])
                    yield
                    kb.act(B.vnew[:], pD[:, 64:128], AF.Copy, [pD, NBETA], [B.vnew], scale=NBETA[:, dp, ch:ch + 1])
                    yield
                    need_o = not (half_mode and (ch >= 36 or ch < 4))
                    if need_o:
                        kb.mm(pD[:, 128:192], B.qT[:, c, :], S_old[:], True, True, [B.qT, S_old], [pD])
                        yield
                        kb.act(B.o1[:], pD[:, 128:192], AF.Copy, [pD, E], [B.o1], scale=E[:, dp, ch:ch + 1])
                        yield
                        kb.mm(pD[:, 192:256], B.attnT[:, c, :], B.vnew[:], True, True, [B.attnT, B.vnew], [pD])
                        yield
                    if not need_o:
                        pass
                    elif ch not in written:
                        written.add(ch)
                        kb.tt("dve", OB[:, ch, :], B.o1[:], pD[:, 192:256], ALU.add, [B.o1, pD], [OB])
                        yield
                    else:
                        kb.tt("dve", B.o1[:], B.o1[:], pD[:, 192:256], ALU.add, [B.o1, pD], [B.o1])
                        yield
                        kb.tt("dve", OB[:, ch, :], OB[:, ch, :], B.o1[:], ALU.add, [OB, B.o1], [OB])
                        yield
                    kb.mm(pD[:, 256:320], B.kt[:, c, :], B.vnew[:], True, True, [B.kt, B.vnew], [pD])
                    yield
                    kb.stt("dve", S_new[:], S_old[:], EGL[:, dp, ch:ch + 1], pD[:, 256:320], ALU.mult, ALU.add,
                           [S_old, EGL, pD], [S_new])
                    yield

            order = [list(range(17)), [0] + list(range(16, 0, -1))]
            def stream(d):
                for it in range(9 if (half_mode and d == 0) else 17):
                    yield from prep(d, order[d][it])
                    yield from scan(d, order[d][it])

            gens = [stream(0), stream(1)]
            alive = [True, True]
            while any(alive):
                for d in range(2):
                    if alive[d]:
                        try:
                            next(gens[d])
                        except StopIteration:
                            alive[d] = False
            pO = g.ps[0]
            for grp in (range(1, 9) if half_mode else range(17)):
                gt, ot = gts[grp % 2], otb[grp % 2]
                c0, t0 = 4 * grp, 256 * grp
                for ab in range(2):
                    h = 2 * pp + ab
                    kb.dma("sp" if ab == 0 else "pool", gt[64 * ab:64 * ab + 64, :, :],
                           g.tokd[t0:t0 + 256, TK_GATE + 64 * h:TK_GATE + 64 * h + 64].rearrange("(c i) e -> i c e", i=64),
                           [g.TOK[2 * grp], g.TOK[2 * grp + 1]], [gt])
                kb.act(gt[:], gt[:], AF.Silu, [gt], [gt])
                ob = OB[:, c0:c0 + 4, :]
                tmp3 = bufs[0].tmp[:, 0:2, :].rearrange("p a (b e) -> p (a b) e", e=64)
                kb.tt("dve", tmp3, ob, ob, ALU.mult, [OB], [bufs[0].tmp])
                kb.op("dve", lambda e, o=oss[:, 0:4], i=tmp3: e.reduce_sum(o, i, AX.X), [bufs[0].tmp], [oss])
                rstd_from_ssq(kb, oss[:, 0:4], oss[:, 0:4], 64, [oss], [oss])
                kb.tt("dve", ob, ob, oss[:, 0:4].unsqueeze(2).to_broadcast([128, 4, 64]), ALU.mult, [OB, oss], [OB])
                kb.tt("dve", ob, ob, gt[:], ALU.mult, [OB, gt], [OB])
                for c in range(4):
                    kb.tr(pO[0:64, c * 128:c * 128 + 128], OB[:, c0 + c, :], ident[:], [OB, ident], [pO])
                kb.act(ot[:, :], pO[0:64, :], AF.Copy, [pO, dgain], [ot], scale=dgain[:, 0:1])
                for ab in range(2):
                    h = 2 * pp + ab
                    kb.dma("sp" if ab == 0 else "pool",
                           g.mixd[64 * h:64 * h + 64, t0:t0 + 256].rearrange("e (c i) -> e c i", c=4),
                           ot[:, :].rearrange("e (c ab i) -> e c ab i", c=4, ab=2)[:, :, ab, :], [ot],
                           [g.MIXdn[2 * grp], g.MIXdn[2 * grp + 1]])
    kb.barrier()


def prep_w13(g, wa, wb, W13, nf, sc):
    kb = g.kb
    st = [kb.sbuf("w13s%d" % i, [128, 8, 256], ctx=sc) for i in range(2)]
    sb = [kb.sbuf("w13b%d" % i, [128, 8, 256], BF16, ctx=sc) for i in range(2)]
    for f in range(nf):
        s_, b_ = st[f % 2], sb[f % 2]
        kb.dma("sp", s_[:, :, 0:128], wa[0][:, 128 * f:128 * f + 128].rearrange("(k p) j -> p k j", p=128), [wa[1]], [s_])
        kb.dma("pool", s_[:, :, 128:256], wb[0][:, 128 * f:128 * f + 128].rearrange("(k p) j -> p k j", p=128), [wb[1]], [s_])
        kb.copy("dve" if f % 2 == 0 else "pool", b_[:], s_[:], [s_], [b_])
        kb.dma("sp", W13[f].ap, b_[:], [b_], [W13[f]])


def phase_out_ffn(g, L, xs_tiles, xo_tiles, do_ctx):
    kb = g.kb
    with ExitStack() as sc:
        with ExitStack() as sc2:
            prep_w13(g, (g.ffn_w1.ap[0], g.ffn_w1), (g.ffn_w3.ap[0], g.ffn_w3), g.W13, 22, sc2)
        kb.barrier()
        woutb = kb.sbuf("woutb", [128, 8, D], BF16, ctx=sc)
        w2b = kb.sbuf("w2b", [128, 22, D], BF16, ctx=sc)
        stage = [kb.sbuf("wst%d" % i, [128, D], ctx=sc) for i in range(2)]
        load_cast_weight(g, sc, "wout", g.w_out, lambda k: g.w_out.ap[L, 128 * k:128 * k + 128, :], 8, D, stage, woutb,
                         lambda k: woutb[:, k, :])
        load_cast_weight(g, sc, "w2", g.ffn_w2, lambda k: g.ffn_w2.ap[0, 128 * k:128 * k + 128, :], 22, D, stage, w2b,
                         lambda k: w2b[:, k, :])
        gmsa = kb.sbuf("gmsa", [128, 2, D], ctx=sc)
        gmlp = kb.sbuf("gmlp", [128, 2, D], ctx=sc)
        for s in range(2):
            kb.dma("sp", gmsa[:, s, :], g.gates.ap[L, 0, s:s + 1, :].to_broadcast([128, D]), [g.gates], [gmsa])
            kb.dma("sp", gmlp[:, s, :], g.gates.ap[L, 1, s:s + 1, :].to_broadcast([128, D]), [g.gates], [gmlp])
        scr = {"junk": kb.sbuf("junk", [128, 1024], ctx=sc), "ssq": kb.sbuf("ssq", [128, 1], ctx=sc),
               "xn": kb.sbuf("xn", [128, 1024], ctx=sc), "psT": [g.ps[0], g.ps[1]]}
        mix32 = [kb.sbuf("mix32_%d" % i, [128, 8, 128], ctx=sc) for i in range(2)]
        mixb = [kb.sbuf("mixb_%d" % i, [128, 8, 128], BF16, ctx=sc) for i in range(2)]
        xts = [kb.sbuf("xt%d" % i, [128, D], ctx=sc) for i in range(2)]
        tmpy = kb.sbuf("tmpy", [128, D], ctx=sc)
        x1blk = [kb.sbuf("x1b%d" % i, [128, 4, D], ctx=sc) for i in range(1)]
        h2Ts = [kb.sbuf("h2T%d" % i, [128, 8, 512], BF16, ctx=sc) for i in range(1)]
        gT = kb.sbuf("gT", [128, 22, 512], BF16, ctx=sc)
        w13s = [kb.sbuf("w13_%d" % i, [128, 8, 256], BF16, ctx=sc) for i in range(3)]
        sas = [kb.sbuf("sa%d" % i, [128, 512], ctx=sc) for i in range(2)]
        x2s = [kb.sbuf("x2_%d" % i, [128, D], ctx=sc) for i in range(2)]
        psY = [g.ps[2], g.ps[3]]
        psA, psB = [g.ps[4], g.ps[5]], [g.ps[6], g.ps[7]]
        blocks = ([(0, 0, 2)] if do_ctx else []) + [(1 + i, 2 + 4 * i, 4) for i in range(8)]
        ti = 0
        wi = 0
        for bn, (bi, t0, nt) in enumerate(blocks):
            ntok = nt * 128
            x1b, h2T = x1blk[0], h2Ts[0]
            s = 1 if bi == 0 else 0
            for j in range(nt):
                t = t0 + j
                m32, mb, xt = mix32[ti % 2], mixb[ti % 2], xts[ti % 2]
                ti += 1
                kb.dma("sp", m32[:], g.mixd[:, 128 * t:128 * t + 128].rearrange("(c p) t -> p c t", p=128),
                       [g.MIXdn[t], g.MIXsg[t]] + [g.MIXat[h][bi] for h in range(6)], [m32])
                kb.dma("pool", xt[:], xs_tiles[t].ap, [xs_tiles[t]], [xt])
                kb.copy("pool", mb[:], m32[:], [m32], [mb])
                for half in range(2):
                    for k in range(8):
                        kb.mm(psY[half][:, :], mb[:, k, :], woutb[:, k, 512 * half:512 * half + 512], k == 0, k == 7,
                              [mb, woutb], [psY[half]])
                for half in range(2):
                    kb.tt("dve", tmpy[:, 512 * half:512 * half + 512], psY[half][:, :], gmsa[:, s, 512 * half:512 * half + 512],
                          ALU.mult, [psY[half], gmsa], [tmpy])
                kb.tt("pool", x1b[:, j, :], tmpy[:], xt[:], ALU.add, [tmpy, xt], [x1b])
                norm_tile_to_hT(g, L, 1, x1b, bi == 0, [h2T[:, c, j * 128:j * 128 + 128] for c in range(8)], h2T, scr, xap=x1b[:, j, :])
            for f in range(22):
                w13 = w13s[wi % 3]
                pa, pb, sa = psA[wi % 2], psB[wi % 2], sas[wi % 2]
                wi += 1
                kb.dma("sp" if f % 2 == 0 else "pool", w13[:], g.W13[f].ap, [g.W13[f]], [w13])
                for k in range(8):
                    kb.mm(pa[:, 0:ntok], w13[:, k, 0:128], h2T[:, k, 0:ntok], k == 0, k == 7, [w13, h2T], [pa])
                for k in range(8):
                    kb.mm(pb[:, 0:ntok], w13[:, k, 128:256], h2T[:, k, 0:ntok], k == 0, k == 7, [w13, h2T], [pb])
                kb.act(sa[:, 0:ntok], pa[:, 0:ntok], AF.Silu, [pa], [sa])
                kb.tt("dve", gT[:, f, 0:ntok], sa[:, 0:ntok], pb[:, 0:ntok], ALU.mult, [sa, pb], [gT])
            for j in range(nt):
                t = t0 + j
                x2 = x2s[j % 2]
                for half in range(2):
                    for f in range(22):
                        kb.mm(psY[half][:, :], gT[:, f, j * 128:j * 128 + 128], w2b[:, f, 512 * half:512 * half + 512],
                              f == 0, f == 21, [gT, w2b], [psY[half]])
                for half in range(2):
                    kb.tt("dve", tmpy[:, 512 * half:512 * half + 512], psY[half][:, :], gmlp[:, s, 512 * half:512 * half + 512],
                          ALU.mult, [psY[half], gmlp], [tmpy])
                kb.tt("pool", x2[:], tmpy[:], x1b[:, j, :], ALU.add, [tmpy, x1b], [x2])
                kb.dma("sp", xo_tiles[t].ap, x2[:], [x2], [xo_tiles[t]])
    kb.barrier()


def phase_out_router(g, L, xs_tiles, nblk=8):
    kb = g.kb
    with ExitStack() as sc:
        woutb = kb.sbuf("woutb", [128, 8, D], BF16, ctx=sc)
        stage = [kb.sbuf("wst%d" % i, [128, D], ctx=sc) for i in range(2)]
        load_cast_weight(g, sc, "wout", g.w_out, lambda k: g.w_out.ap[L, 128 * k:128 * k + 128, :], 8, D, stage, woutb,
                         lambda k: woutb[:, k, :])
        gmsa = kb.sbuf("gmsa", [128, D], ctx=sc)
        kb.dma("sp", gmsa[:], g.gates.ap[L, 0, 0:1, :].to_broadcast([128, D]), [g.gates], [gmsa])
        rw = kb.sbuf("rw", [128, 8, NEXP], ctx=sc)
        kb.dma("sp", rw[:], g.router_w.ap[0].rearrange("(k p) e -> p k e", p=128), [g.router_w], [rw])
        rb = kb.sbuf("rb", [128, NEXP], ctx=sc)
        kb.dma("sp", rb[:], g.router_b.ap[0:1, :].to_broadcast([128, NEXP]), [g.router_b], [rb])
        scr = {"junk": kb.sbuf("junk", [128, 1024], ctx=sc), "ssq": kb.sbuf("ssq", [128, 1], ctx=sc),
               "xn": kb.sbuf("xn", [128, 1024], ctx=sc), "psT": [g.ps[0], g.ps[1]]}
        mix32 = [kb.sbuf("mix32_%d" % i, [128, 8, 128], ctx=sc) for i in range(2)]
        mixb = [kb.sbuf("mixb_%d" % i, [128, 8, 128], BF16, ctx=sc) for i in range(2)]
        xts = [kb.sbuf("xt%d" % i, [128, D], ctx=sc) for i in range(2)]
        tmpy = kb.sbuf("tmpy", [128, D], ctx=sc)
        x1s = [kb.sbuf("x1_%d" % i, [128, D], ctx=sc) for i in range(2)]
        h2Ts = [kb.sbuf("h2T%d" % i, [128, 8, 512], BF16, ctx=sc) for i in range(2)]
        h32 = kb.sbuf("h32", [128, 8, 128], ctx=sc)
        lg = kb.sbuf("lg", [128, NEXP], ctx=sc)
        mx8 = kb.sbuf("mx8", [128, 8], ctx=sc)
        msk = kb.sbuf("msk", [128, NEXP], ctx=sc)
        ex = kb.sbuf("ex", [128, NEXP], ctx=sc)
        nm1 = kb.sbuf("nm1", [128, 2], ctx=sc)
        psY = [g.ps[2], g.ps[3]]
        psR = g.ps[4]
        for bi in range(nblk):
            h2T = h2Ts[bi % 2]
            for j in range(4):
                n = 4 * bi + j
                t = 2 + n
                m32, mb, xt, x1 = mix32[n % 2], mixb[n % 2], xts[n % 2], x1s[n % 2]
                kb.dma("sp", m32[:], g.mixd[:, 128 * t:128 * t + 128].rearrange("(c p) t -> p c t", p=128),
                       [g.MIXdn[t], g.MIXsg[t]] + [g.MIXat[h][1 + bi] for h in range(6)], [m32])
                kb.dma("pool", xt[:], xs_tiles[t].ap, [xs_tiles[t]], [xt])
                kb.copy("dve", mb[:], m32[:], [m32], [mb])
                for half in range(2):
                    for k in range(8):
                        kb.mm(psY[half][:, :], mb[:, k, :], woutb[:, k, 512 * half:512 * half + 512], k == 0, k == 7,
                              [mb, woutb], [psY[half]])
                for half in range(2):
                    kb.tt("dve", tmpy[:, 512 * half:512 * half + 512], psY[half][:, :], gmsa[:, 512 * half:512 * half + 512],
                          ALU.mult, [psY[half], gmsa], [tmpy])
                kb.tt("dve", x1[:], tmpy[:], xt[:], ALU.add, [tmpy, xt], [x1])
                kb.dma("sp", g.X1S[n].ap, x1[:], [x1], [g.X1S[n]])
                norm_tile_to_hT(g, L, 1, x1, False, [h2T[:, c, j * 128:j * 128 + 128] for c in range(8)], h2T, scr,
                                also_f32=[h32[:, c, :] for c in range(8)], f32_buf=h32)
                for k in range(8):
                    kb.mm(psR[:, 0:NEXP], h32[:, k, :], rw[:, k, :], k == 0, k == 7, [h32, rw], [psR])
                kb.tt("dve", lg[:], psR[:, 0:NEXP], rb[:], ALU.add, [psR, rb], [lg])
                kb.op("dve", lambda e, o=mx8[:], i=lg[:]: e.max(out=o, in_=i), [lg], [mx8])
                kb.ts("dve", msk[:], lg[:], mx8[:, 1:2], None, ALU.is_ge, None, [lg, mx8], [msk])
                kb.ts("dve", nm1[:, 0:1], mx8[:, 0:1], -1.0, None, ALU.mult, None, [mx8], [nm1])
                kb.act(ex[:], lg[:], AF.Exp, [lg, nm1], [ex], bias=nm1[:, 0:1])
                kb.tt("dve", ex[:], ex[:], msk[:], ALU.mult, [ex, msk], [ex])
                kb.op("dve", lambda e, o=nm1[:, 1:2], i=ex[:]: e.reduce_sum(o, i, AX.X), [ex], [nm1])
                kb.op("dve", lambda e, o=nm1[:, 1:2]: e.reciprocal(o, o), [nm1], [nm1])
                kb.ts("dve", g.GATE[:, n, :], ex[:], nm1[:, 1:2], None, ALU.mult, None, [ex, nm1], [g.GATEb])
            kb.dma("sp", g.H2T[bi].ap, h2T[:], [h2T], [g.H2T[bi]])
    kb.barrier()


def phase_moe(g, L, nblk=8):
    kb = g.kb
    NF = MOE_FF // 128
    with ExitStack() as sc:
        w2b = kb.sbuf("w2b", [128, NF, D], BF16, ctx=sc)
        stage = [kb.sbuf("wst%d" % i, [128, D], ctx=sc) for i in range(2)]
        gmlp = kb.sbuf("gmlp", [128, D], ctx=sc)
        kb.dma("sp", gmlp[:], g.gates.ap[L, 1, 0:1, :].to_broadcast([128, D]), [g.gates], [gmlp])
        fgain = kb.sbuf("fgain", [128, D], ctx=sc)
        kb.dma("sp", fgain[:], g.final_norm_g.ap[0:1, :].to_broadcast([128, D]), [g.final_norm_g], [fgain])
        pst = [kb.sbuf("w13s%d" % i, [128, 8, 256], ctx=sc) for i in range(2)]
        psb = [kb.sbuf("w13b%d" % i, [128, 8, 256], BF16, ctx=sc) for i in range(2)]
        h2Ts = [kb.sbuf("h2T%d" % i, [128, 8, 512], BF16, ctx=sc) for i in range(2)]
        gT = kb.sbuf("gT", [128, NF, 512], BF16, ctx=sc)
        w13s = [kb.sbuf("w13_%d" % i, [128, 8, 256], BF16, ctx=sc) for i in range(3)]
        sas = [kb.sbuf("sa%d" % i, [128, 512], ctx=sc) for i in range(2)]
        tmpy = kb.sbuf("tmpy", [128, D], ctx=sc)
        ya = kb.sbuf("ya", [128, D], ctx=sc)
        x1 = kb.sbuf("x1", [128, D], ctx=sc)
        junk = kb.sbuf("junk", [128, D], ctx=sc)
        ssq = kb.sbuf("ssq", [128, 1], ctx=sc)
        psY = [g.ps[2], g.ps[3]]
        psA, psB = [g.ps[4], g.ps[5]], [g.ps[6], g.ps[7]]
        wi = 0
        hi = 0
        for e in range(NEXP):
            for f in range(NF):
                s_, b_ = pst[f % 2], psb[f % 2]
                kb.dma("sp", s_[:, :, 0:128], g.moe_w1.ap[0, e, :, 128 * f:128 * f + 128].rearrange("(k p) j -> p k j", p=128),
                       [g.moe_w1], [s_])
                kb.dma("pool", s_[:, :, 128:256], g.moe_w3.ap[0, e, :, 128 * f:128 * f + 128].rearrange("(k p) j -> p k j", p=128),
                       [g.moe_w3], [s_])
                kb.copy("dve", b_[:], s_[:], [s_], [b_])
                kb.dma("sp", g.W13[f].ap, b_[:], [b_], [g.W13[f]])
            load_cast_weight(g, sc, "w2", g.moe_w2, lambda k: g.moe_w2.ap[0, e, 128 * k:128 * k + 128, :], NF, D, stage, w2b,
                             lambda k: w2b[:, k, :])
            for bi in range(nblk):
                h2T = h2Ts[hi % 2]
                hi += 1
                kb.dma("sp", h2T[:], g.H2T[bi].ap, [g.H2T[bi]], [h2T])
                for f in range(NF):
                    w13 = w13s[wi % 3]
                    pa, pb, sa = psA[wi % 2], psB[wi % 2], sas[wi % 2]
                    wi += 1
                    kb.dma("sp" if f % 2 == 0 else "pool", w13[:], g.W13[f].ap, [g.W13[f]], [w13])
                    for k in range(8):
                        kb.mm(pa[:, :], w13[:, k, 0:128], h2T[:, k, :], k == 0, k == 7, [w13, h2T], [pa])
                    for k in range(8):
                        kb.mm(pb[:, :], w13[:, k, 128:256], h2T[:, k, :], k == 0, k == 7, [w13, h2T], [pb])
                    kb.act(sa[:, :], pa[:, :], AF.Silu, [pa], [sa])
                    kb.tt("dve", gT[:, f, :], sa[:, :], pb[:, :], ALU.mult, [sa, pb], [gT])
                for j in range(4):
                    n = 4 * bi + j
                    for half in range(2):
                        for f in range(NF):
                            kb.mm(psY[half][:, :], gT[:, f, j * 128:j * 128 + 128], w2b[:, f, 512 * half:512 * half + 512],
                                  f == 0, f == NF - 1, [gT, w2b], [psY[half]])
                    if e > 0:
                        kb.dma("pool", ya[:], g.YACC[n].ap, [g.YACC[n]], [ya])
                    for half in range(2):
                        kb.act(tmpy[:, 512 * half:512 * half + 512], psY[half][:, :], AF.Copy, [psY[half], g.GATEb], [tmpy],
                               scale=g.GATE[:, n, e:e + 1])
                    if e == 0:
                        kb.dma("sp", g.YACC[n].ap, tmpy[:], [tmpy], [g.YACC[n]])
                        continue
                    kb.tt("dve", ya[:], ya[:], tmpy[:], ALU.add, [ya, tmpy], [ya])
                    if e < NEXP - 1:
                        kb.dma("sp", g.YACC[n].ap, ya[:], [ya], [g.YACC[n]])
                        continue
                    kb.dma("sp", x1[:], g.X1S[n].ap, [g.X1S[n]], [x1])
                    kb.tt("dve", ya[:], ya[:], gmlp[:], ALU.mult, [ya, gmlp], [ya])
                    kb.tt("dve", ya[:], ya[:], x1[:], ALU.add, [ya, x1], [ya])
                    kb.act(junk[:], ya[:], AF.Square, [ya, ssq], [junk, ssq], accum_out=ssq[:, 0:1])
                    rstd_from_ssq(kb, ssq[:, 0:1], ssq[:, 0:1], D, [ssq], [ssq])
                    kb.act(junk[:], ya[:], AF.Copy, [ya, ssq], [junk], scale=ssq[:, 0:1])
                    kb.tt("dve", junk[:], junk[:], fgain[:], ALU.mult, [junk, fgain], [junk])
                    kb.dma("sp", g.outb.ap[128 * n:128 * n + 128, :], junk[:], [junk], [g.outb])
    kb.barrier()


W_NAMES = [("mod_w", [DEPTH, D, 6 * D]), ("mod_b", [DEPTH, 6 * D]), ("norm1_g", [DEPTH, D]), ("norm2_g", [DEPTH, D]),
           ("w_in", [DEPTH, D, IN_COLS]), ("conv_w", [DEPTH, 3, 1152]), ("dn_a_log", [DEPTH, 2, 6]), ("dn_dt_bias", [DEPTH, 2, 6]),
           ("dn_norm_g", [DEPTH, 64]), ("q_norm_g", [DEPTH, 64]), ("k_norm_g", [DEPTH, 64]), ("sgu_norm_g", [DEPTH, 256]),
           ("sgu_w", [DEPTH, 4, 128, 128]), ("sgu_b", [DEPTH, 4, 128]), ("w_out", [DEPTH, D, D]),
           ("ffn_w1", [1, D, D_FF]), ("ffn_w3", [1, D, D_FF]), ("ffn_w2", [1, D_FF, D]),
           ("router_w", [1, D, NEXP]), ("router_b", [1, NEXP]), ("moe_w1", [1, NEXP, D, MOE_FF]),
           ("moe_w3", [1, NEXP, D, MOE_FF]), ("moe_w2", [1, NEXP, MOE_FF, D]), ("final_norm_g", [1, D])]
BLOCKS = [(0, 256)] + [(256 + 512 * i, 512) for i in range(8)]


def build_program(phases=None, debug=(), dbg_in=()):
    nc = bass.Bass("TRN2", target_bir_lowering=False)
    g = G()

    def ext_in(name, shape):
        return Buf(name, nc.dram_tensor(name, list(shape), F32, kind="ExternalInput").ap())

    g.xin = ext_in("xin", [T, D])
    g.cvec = ext_in("cvec", [2, D])
    g.rope = ext_in("rope", [NLAT, 64])
    g.identd = ext_in("ident", [128, 128])
    g.dncd = ext_in("dnc", [7, 128, 128])
    for name, shape in W_NAMES:
        setattr(g, name, ext_in(name, shape))
    out = Buf("out", nc.dram_tensor("out", [NLAT // 2, D], F32, kind="ExternalOutput").ap())
    dbg = {}
    for name, shape in debug:
        dbg[name] = Buf(name, nc.dram_tensor(name, list(shape), F32, kind="ExternalOutput").ap())
    for name, shape in dbg_in:
        dbg[name] = Buf(name, nc.dram_tensor(name, list(shape), F32, kind="ExternalInput").ap())
    g.dbg = dbg

    def scratch(name, shape, dt=F32):
        if name in dbg:
            return dbg[name].ap
        return nc.dram_tensor(name + "_s", list(shape), dt, kind="Internal").ap()

    with ExitStack() as ctx:
        import os as _os
        kb = KB(nc, ctx, same_engine_sync=_os.environ.get("SAMEENG", "1") == "1")
        g.kb = kb
        g.ps = [kb.psum("ps%d" % i, [128, 512]) for i in range(8)]
        g.ident = kb.sbuf("ident", [128, 128])
        kb.dma("sp", g.ident[:], g.identd.ap, [g.identd], [g.ident])
        GS = kb.sbuf("GS", [128, DEPTH, 2, 8, 2])
        SH = kb.sbuf("SH", [128, DEPTH, 2, 8, 2])
        g.GS, g.SH, g.GSb, g.SHb = GS.ap, SH.ap, GS, SH
        g.gates = Buf("gates", scratch("gates", [DEPTH, 2, 2, D]))
        tokd = scratch("TOK", [T, TK_W])
        g.tokd = tokd
        g.TOK = [Buf("TOK%d" % t, tokd[128 * t:128 * t + 128, :]) for t in range(NT)]
        g.zqd = scratch("ZQ", [1408, T])
        g.ZQ = [[Buf("ZQ%d_%d" % (fc, bi), g.zqd[128 * fc:128 * fc + 128, b0:b0 + n]) for bi, (b0, n) in enumerate(BLOCKS)]
                for fc in range(11)]
        g.blk_of_tile = lambda t: (0, t * 128) if t < 2 else (1 + (t - 2) // 4, ((t - 2) % 4) * 128)
        g.mixd = scratch("MIXT", [D, T])
        g.MIXdn = [Buf("MIXdn%d" % t, None) for t in range(NT)]
        g.MIXsg = [Buf("MIXsg%d" % t, None) for t in range(NT)]
        g.MIXat = [[Buf("MIXat%d_%d" % (h, bi), g.mixd[384 + 64 * h:384 + 64 * h + 64, b0:b0 + n])
                    for bi, (b0, n) in enumerate(BLOCKS)] for h in range(6)]
        w13d = scratch("W13", [28, 128, 8, 256], BF16)
        g.W13 = [Buf("W13_%d" % f, w13d[f]) for f in range(28)]
        xs = [[Buf("xin%d" % t, g.xin.ap[128 * t:128 * t + 128, :]) for t in range(NT)]]
        for L in range(DEPTH):
            xd = scratch("XS%d" % (L + 1), [T, D])
            xs.append([Buf("xs%d_%d" % (L + 1, t), xd[128 * t:128 * t + 128, :]) for t in range(NT)])
        g.xs = xs
        g.outb = out
        x1d = scratch("X1S", [NLAT, D])
        g.X1S = [Buf("X1S%d" % n, x1d[128 * n:128 * n + 128, :]) for n in range(32)]
        yd = scratch("YACC", [NLAT, D])
        g.YACC = [Buf("YACC%d" % n, yd[128 * n:128 * n + 128, :]) for n in range(32)]
        h2d = scratch("H2T", [8, 128, 8, 512], BF16)
        g.H2T = [Buf("H2T%d" % b, h2d[b]) for b in range(8)]
        GATE = kb.sbuf("GATE", [128, 32, NEXP])
        g.GATE, g.GATEb = GATE.ap, GATE
        allp = ["mod", "a0", "attn0", "sgu0", "dn0", "ffn0", "a1", "attn1", "sgu1", "dn1", "out1", "moe1"]
        phases = allp if phases is None else phases
        if "mod" in phases:
            for L in range(DEPTH):
                phase_mod(g, L)
        if "a0" in phases:
            phase_a(g, 0, xs[0])
        if "attn0" in phases:
            phase_attn(g, 0, True)
        if "sgu0" in phases:
            phase_sgu(g, 0, list(range(NT)))
        if "dn0" in phases:
            phase_dn(g, 0)
        if "ffn0" in phases:
            phase_out_ffn(g, 0, xs[0], xs[1], True)
        if "a1" in phases:
            phase_a(g, 1, xs[1])
        if "attn1" in phases:
            phase_attn(g, 1, False, nblk=4)
        if "sgu1" in phases:
            phase_sgu(g, 1, list(range(2, 18)))
        if "dn1" in phases:
            phase_dn(g, 1, half_mode=True)
        if "out1" in phases:
            phase_out_router(g, 1, xs[1], nblk=4)
        if "moe1" in phases:
            phase_moe(g, 1, nblk=4)
        kb.barrier()
        kb.emit()
        g.n_ins = kb.n_ins
    return nc, g


def make_inputs(inputs):
    x = np.asarray(inputs["x"], np.float32)
    ctxa = np.asarray(inputs["ctx"], np.float32)
    rows = NLAT // 64
    row = np.repeat(np.arange(rows, dtype=np.float32), 64)
    col = np.tile(np.arange(64, dtype=np.float32), rows)
    inv = (10000.0 ** (-2.0 * np.arange(8, dtype=np.float32) / 32)).astype(np.float32)
    inv = (np.float32(10000.0) ** (-2.0 * np.arange(16, dtype=np.float32) / np.float32(32))).astype(np.float32)
    ang = np.stack([row[:, None] * inv, col[:, None] * inv], axis=1).astype(np.float32)
    rope = np.concatenate([np.cos(ang).reshape(NLAT, 32), np.sin(ang).reshape(NLAT, 32)], axis=1).astype(np.float32)
    shared = {k: np.ascontiguousarray(np.asarray(inputs[k], np.float32)).reshape(shp) for k, shp in W_NAMES}
    perm = np.concatenate([np.arange(C_AQ + 64 * h, C_AQ + 64 * h + 64) for h in (0, 3, 1, 4, 2, 5)])
    cols = np.arange(IN_COLS)
    cols[C_AQ:C_AQ + 384] = perm
    shared["w_in"] = np.ascontiguousarray(shared["w_in"][:, :, cols])
    shared["rope"] = rope
    shared["ident"] = np.eye(128, dtype=np.float32)
    j = np.arange(128)[:, None]
    i = np.arange(128)[None, :]
    blk = (j // 64) == (i // 64)
    dnc = np.zeros((7, 128, 128), np.float32)
    dnc[0] = blk
    dnc[1] = blk & (j <= i)
    dnc[2] = blk & (j >= i)
    dnc[3] = np.where(blk & (i >= j), 0.0, -30000.0)
    dnc[4] = np.where(blk & (i <= j), 0.0, -30000.0)
    dnc[5] = blk & (i > j)
    dnc[6] = blk & (i < j)
    shared["dnc"] = dnc
    rev = dict(shared)
    cols = np.arange(IN_COLS)
    cols[C_BETA:C_BETA + 6], cols[C_BETA + 6:C_BETA + 12] = np.arange(C_BETA + 6, C_BETA + 12), np.arange(C_BETA, C_BETA + 6)
    cols[C_ALPHA:C_ALPHA + 6], cols[C_ALPHA + 6:C_ALPHA + 12] = np.arange(C_ALPHA + 6, C_ALPHA + 12), np.arange(C_ALPHA, C_ALPHA + 6)
    rev["w_in"] = np.ascontiguousarray(shared["w_in"][:, :, cols])
    rev["conv_w"] = np.ascontiguousarray(shared["conv_w"][:, ::-1, :])
    rev["dn_a_log"] = np.ascontiguousarray(shared["dn_a_log"][:, ::-1, :])
    rev["dn_dt_bias"] = np.ascontiguousarray(shared["dn_dt_bias"][:, ::-1, :])
    rev["sgu_w"] = np.ascontiguousarray(shared["sgu_w"][:, :, ::-1, ::-1])
    rev["sgu_b"] = np.ascontiguousarray(shared["sgu_b"][:, :, ::-1])
    rev["rope"] = np.ascontiguousarray(rope[::-1])
    maps = []
    cv = np.asarray(inputs["c"], np.float32)
    cc = np.asarray(inputs["c_ctx"], np.float32)
    for b in range(4):
        for tw in range(2):
            m = dict(rev if tw else shared)
            if tw:
                m["xin"] = np.ascontiguousarray(np.concatenate([ctxa[b][::-1], x[b][::-1]], axis=0))
            else:
                m["xin"] = np.ascontiguousarray(np.concatenate([ctxa[b], x[b]], axis=0))
            m["cvec"] = np.ascontiguousarray(np.stack([cv[b], cc], 0))
            maps.append(m)
    return maps


def kernel(**inputs):
    nc, g = build_program()
    maps = make_inputs(inputs)
    res = run_bass_kernel_spmd(nc, maps, core_ids=list(range(len(maps))))
    out = np.empty((4, NLAT, D), np.float32)
    for b in range(4):
        out[b, :NLAT // 2] = res.results[2 * b]["out"]
        out[b, NLAT // 2:] = res.results[2 * b + 1]["out"][::-1]
    return out
```

```python
import numpy as np
from contextlib import ExitStack
import concourse.bass as bass
import concourse.mybir as mybir
from concourse.bass_utils import run_bass_kernel_spmd

F32 = mybir.dt.float32
BF16 = mybir.dt.bfloat16
AF = mybir.ActivationFunctionType
ALU = mybir.AluOpType
AX = mybir.AxisListType

D = 1024
NCTX = 256
NLAT = 4096
T = NCTX + NLAT
NT = T // 128
DEPTH = 2
HD = 64
IN_COLS = 2712
D_FF = 2816
MOE_FF = 3584
NEXP = 8
EPS = 1e-6
C_QKV, C_GATE, C_BETA, C_ALPHA, C_AQ, C_AK, C_AV, C_U, C_V = 0, 1152, 1536, 1548, 1560, 1944, 2072, 2200, 2456
TK_GATE, TK_BETA, TK_ALPHA, TK_AQ, TK_AK, TK_AV, TK_ZV, TK_W = 0, 384, 396, 408, 792, 920, 1048, 1304


class Buf:
    __slots__ = ("name", "ap", "last_w", "readers", "excl")

    def __init__(self, name, ap=None, excl=False):
        self.name = name
        self.ap = ap
        self.excl = excl
        self.last_w = None
        self.readers = []

    def __getitem__(self, idx):
        return self.ap[idx]


class KB:
    ENGS = ("pe", "act", "dve", "pool", "sp")
    NDMA = 12

    def __init__(self, nc, ctx, same_engine_sync=True):
        self.nc = nc
        self.ctx = ctx
        self.same = same_engine_sync
        self.ops = {e: [] for e in self.ENGS}
        self.seq = {e: 0 for e in self.ENGS}
        self.sems = {e: ctx.enter_context(nc.semaphore("c_" + e)) for e in self.ENGS}
        self.dma_sems, self.dma_cnt, self.dma_rr = {}, {}, {}
        for q in ("sp", "pool", "act"):
            self.dma_sems[q] = [ctx.enter_context(nc.semaphore("d_%s%d" % (q, i))) for i in range(self.NDMA)]
            self.dma_cnt[q] = [0] * self.NDMA
            self.dma_rr[q] = 0
        self.known = {e: {} for e in self.ENGS}
        import os as _os
        self.pool_dma = _os.environ.get("POOLDMA", "0") == "1"
        self.pool_cmp = _os.environ.get("POOLCMP", "0") == "1"
        self.n_ins = 0
        self.uid = 0

    def sbuf(self, name, shape, dt=F32, ctx=None):
        self.uid += 1
        t = (ctx or self.ctx).enter_context(self.nc.sbuf_tensor("%s_%d" % (name, self.uid), list(shape), dt))
        return Buf(name, t)

    def psum(self, name, shape, dt=F32, ctx=None):
        self.uid += 1
        t = (ctx or self.ctx).enter_context(self.nc.psum_tensor("%s_%d" % (name, self.uid), list(shape), dt))
        return Buf(name, t, excl=True)

    def dram(self, name, shape, dt=F32):
        t = self.nc.dram_tensor(name, list(shape), dt, kind="Internal")
        return Buf(name, t.ap())

    def _need(self, eng, tok, waits):
        if tok is None:
            return
        key, val = tok
        if key == eng and (eng == "pe" or not self.same):
            return
        if val > waits.get(key, 0):
            waits[key] = val

    def _sem(self, key):
        if isinstance(key, str):
            return self.sems[key]
        return self.dma_sems[key[0]][key[1]]

    def op(self, eng, fn, reads=(), writes=(), dma=False):
        if eng == "pool":
            if dma and not self.pool_dma:
                eng = "sp"
            elif not dma and not self.pool_cmp:
                eng = "dve"
        ex = [b for b in reads if b.excl]
        if ex:
            reads = [b for b in reads if not b.excl]
            writes = list(writes) + [b for b in ex if b not in writes]
        waits = {}
        for b in reads:
            self._need(eng, b.last_w, waits)
        for b in writes:
            self._need(eng, b.last_w, waits)
            for r in b.readers:
                self._need(eng, r, waits)
        if dma:
            q = eng
            i = self.dma_rr[q]
            self.dma_rr[q] = (i + 1) % self.NDMA
            key = (q, i)
            prev = self.dma_cnt[q][i]
            if prev > 0 and 16 * prev > waits.get(key, 0):
                waits[key] = 16 * prev
            self.dma_cnt[q][i] = prev + 1
            tok = (key, 16 * (prev + 1))
            inc = 16
        else:
            self.seq[eng] += 1
            tok = (eng, self.seq[eng])
            inc = 1
        kn = self.known[eng]
        wl = []
        for key, val in waits.items():
            if kn.get(key, 0) >= val:
                continue
            kn[key] = val
            wl.append((self._sem(key), val))
        self.n_ins += 1
        self.ops[eng].append((wl, fn, self._sem(tok[0]), inc))
        for b in reads:
            b.readers.append(tok)
        for b in writes:
            b.last_w = tok
            b.readers = []
        return tok

    def barrier(self):
        for e in self.ENGS:
            wl = []
            kn = self.known[e]
            for e2 in self.ENGS:
                if e2 != e and self.seq[e2] > kn.get(e2, 0):
                    kn[e2] = self.seq[e2]
                    wl.append((self.sems[e2], self.seq[e2]))
            if self.same and e != "pe" and self.seq[e] > kn.get(e, 0):
                kn[e] = self.seq[e]
                wl.append((self.sems[e], self.seq[e]))
            for q in self.dma_sems:
                for i in range(self.NDMA):
                    v = 16 * self.dma_cnt[q][i]
                    if v > kn.get((q, i), 0):
                        kn[(q, i)] = v
                        wl.append((self.dma_sems[q][i], v))
            if wl:
                self.ops[e].append((wl, None, None, 0))

    def dma(self, q, out_ap, in_ap, reads=(), writes=(), **kw):
        return self.op(q, lambda e: e.dma_start(out=out_ap, in_=in_ap, **kw), reads, writes, dma=True)

    def mm(self, out, lhsT, rhs, start, stop, reads, writes):
        return self.op("pe", lambda e: e.matmul(out, lhsT, rhs, start=start, stop=stop), reads, writes)

    def tr(self, out, in_, ident, reads, writes):
        return self.op("pe", lambda e: e.transpose(out, in_, ident), reads, writes)

    def act(self, out, in_, func, reads, writes, **kw):
        return self.op("act", lambda e: e.activation(out=out, in_=in_, func=func, **kw), reads, writes)

    def copy(self, eng, out, in_, reads, writes):
        if eng == "act":
            return self.act(out, in_, AF.Copy, reads, writes)
        return self.op(eng, lambda e: e.tensor_copy(out, in_), reads, writes)

    def tt(self, eng, out, in0, in1, op, reads, writes):
        return self.op(eng, lambda e: e.tensor_tensor(out, in0, in1, op), reads, writes)

    def ts(self, eng, out, in0, s1, s2, op0, op1, reads, writes):
        if s2 is None:
            return self.op(eng, lambda e: e.tensor_scalar(out, in0, s1, None, op0), reads, writes)
        return self.op(eng, lambda e: e.tensor_scalar(out, in0, s1, s2, op0, op1), reads, writes)

    def stt(self, eng, out, in0, scalar, in1, op0, op1, reads, writes):
        return self.op(eng, lambda e: e.scalar_tensor_tensor(out, in0, scalar, in1, op0, op1), reads, writes)

    def memset(self, eng, out, val, writes):
        return self.op(eng, lambda e: e.memset(out, val), (), writes)

    def final_wait(self, eng, bufs):
        waits = {}
        for b in bufs:
            self._need("__none__", b.last_w, waits)
        self.ops[eng].append(([(self._sem(k), v) for k, v in waits.items()], None, None, 0))

    def emit(self):
        handles = {"pe": "tensor", "act": "scalar", "dve": "vector", "pool": "gpsimd", "sp": "sync"}
        with self.nc.Block() as block:
            for e in self.ENGS:
                lst = self.ops[e]
                if not lst:
                    continue

                def body(engh, lst=lst):
                    for wl, fn, sem, inc in lst:
                        for s, v in wl:
                            engh.wait_ge(s, v)
                        if fn is not None:
                            fn(engh).then_inc(sem, inc)

                getattr(block, handles[e])(body)


class G:
    pass


def rstd_from_ssq(kb, out, ssq, n, reads, writes):
    kb.act(out, ssq, AF.Sqrt, reads, writes, scale=1.0 / n, bias=EPS)
    kb.op("dve", lambda e: e.reciprocal(out, out), writes, writes)


def phase_mod(g, L):
    kb, nc = g.kb, g.kb.nc
    with ExitStack() as sc:
        cond = kb.sbuf("cond", [128, 8, 2], ctx=sc)
        condbc = kb.sbuf("condbc", [128, 16, 128], ctx=sc)
        mbF = kb.sbuf("mbF", [128, 48], ctx=sc)
        mbrow = kb.sbuf("mbrow", [1, 6144], ctx=sc)
        gF = kb.sbuf("gF", [128, 2, 8], ctx=sc)
        modF = kb.sbuf("modF", [128, 32, 2], ctx=sc)
        grow = kb.sbuf("grow", [1, 512], ctx=sc)
        mw = [kb.sbuf("mw%d" % k, [128, 3072], ctx=sc) for k in range(8)]
        for s in range(2):
            kb.dma("sp", cond[:, :, s], g.cvec.ap[s, :].rearrange("(k p) -> p k", p=128), [g.cvec], [cond],
                   allow_slow_non_contiguous=True)
        kb.act(cond[:], cond[:], AF.Silu, [cond], [cond])
        kb.copy("dve", condbc[:], cond[:].rearrange("p k s -> p (k s)").unsqueeze(2).to_broadcast([128, 16, 128]),
                [cond], [condbc])
        kb.dma("sp", mbF[:], g.mod_b.ap[L, :].rearrange("(c p) -> p c", p=128), [g.mod_b], [mbF],
               allow_slow_non_contiguous=True)
        kb.dma("sp", mbrow[:], g.mod_b.ap[L:L + 1, :], [g.mod_b], [mbrow])
        kb.dma("sp", gF[:, 0, :], g.norm1_g.ap[L, :].rearrange("(c p) -> p c", p=128), [g.norm1_g], [gF],
               allow_slow_non_contiguous=True)
        kb.dma("sp", gF[:, 1, :], g.norm2_g.ap[L, :].rearrange("(c p) -> p c", p=128), [g.norm2_g], [gF],
               allow_slow_non_contiguous=True)
        psF, psB = g.ps[0], g.ps[1]
        for h in range(2):
            for k in range(8):
                kb.dma("sp" if k % 2 == 0 else "pool", mw[k][:],
                       g.mod_w.ap[L, 128 * k:128 * k + 128, 3072 * h:3072 * h + 3072], [g.mod_w], [mw[k]])
            for j in range(16):
                for k in range(8):
                    kb.mm(psF[:, 2 * j:2 * j + 2], mw[k][:, 128 * j:128 * j + 128], cond[:, k, :], k == 0, k == 7,
                          [mw[k], cond], [psF])
            kb.tt("dve", modF[:, 16 * h:16 * h + 16, :], psF[:, 0:32].rearrange("p (j s) -> p j s", s=2),
                  mbF[:, 24 * h:24 * h + 16].unsqueeze(2).to_broadcast([128, 16, 2]), ALU.add, [psF, mbF], [modF])
            for s in range(2):
                for cc in range(2):
                    for k in range(8):
                        kb.mm(psB[:, :], condbc[:, 2 * k + s, :], mw[k][:, 2048 + 512 * cc:2048 + 512 * cc + 512],
                              k == 0, k == 7, [mw[k], condbc], [psB])
                    c0 = 3072 * h + 2048 + 512 * cc
                    kb.tt("dve", grow[0:1, :], psB[0:1, :], mbrow[0:1, c0:c0 + 512], ALU.add, [psB, mbrow], [grow])
                    kb.dma("sp", g.gates.ap[L, h, s:s + 1, 512 * cc:512 * cc + 512], grow[0:1, :], [grow], [g.gates])
        for h in range(2):
            sc_ap = modF[:, 16 * h + 8:16 * h + 16, :]
            kb.ts("dve", g.GS[:, L, h, :, :], sc_ap, 1.0, None, ALU.add, None, [modF], [g.GSb])
            kb.tt("dve", g.GS[:, L, h, :, :], g.GS[:, L, h, :, :], gF[:, h, :].unsqueeze(2).to_broadcast([128, 8, 2]),
                  ALU.mult, [gF, g.GSb], [g.GSb])
            kb.copy("dve", g.SH[:, L, h, :, :], modF[:, 16 * h:16 * h + 8, :], [modF], [g.SHb])
    kb.barrier()


def norm_tile_to_hT(g, L, h, xt, tile_is_ctx, hT_out_aps, hT_buf, scr, also_f32=None, xap=None, f32_buf=None):
    kb = g.kb
    s = 1 if tile_is_ctx else 0
    junk, ssq, xn = scr["junk"], scr["ssq"], scr["xn"]
    if xap is None:
        xap = xt[:]
    kb.act(junk[:], xap, AF.Square, [xt, ssq], [junk, ssq], accum_out=ssq[:, 0:1])
    rstd_from_ssq(kb, ssq[:, 0:1], ssq[:, 0:1], D, [ssq], [ssq])
    kb.act(xn[:], xap, AF.Copy, [xt, ssq], [xn], scale=ssq[:, 0:1])
    pT = scr["psT"]
    for c in range(8):
        kb.tr(pT[c // 4][:, (c % 4) * 128:(c % 4) * 128 + 128], xn[:, c * 128:c * 128 + 128], g.ident[:], [xn, g.ident],
              [pT[c // 4]])
    for c in range(8):
        kb.act(hT_out_aps[c], pT[c // 4][:, (c % 4) * 128:(c % 4) * 128 + 128], AF.Identity, [pT[c // 4], g.GSb, g.SHb],
               [hT_buf], scale=g.GS[:, L, h, c, s:s + 1], bias=g.SH[:, L, h, c, s:s + 1])
        if also_f32 is not None:
            kb.act(also_f32[c], pT[c // 4][:, (c % 4) * 128:(c % 4) * 128 + 128], AF.Identity,
                   [pT[c // 4], g.GSb, g.SHb], [f32_buf], scale=g.GS[:, L, h, c, s:s + 1], bias=g.SH[:, L, h, c, s:s + 1])


def load_cast_weight(g, sc, name, dram_buf, src_ap_fn, nk, ncols, stage_bufs, dst, dst_ap_fn):
    kb = g.kb
    for k in range(nk):
        st = stage_bufs[k % len(stage_bufs)]
        kb.dma("sp" if k % 2 == 0 else "pool", st[:, 0:ncols], src_ap_fn(k), [dram_buf], [st])
        kb.copy("pool" if k % 2 == 0 else "dve", dst_ap_fn(k), st[:, 0:ncols], [st], [dst])


def phase_a(g, L, xs_tiles):
    kb = g.kb
    with ExitStack() as sc:
        winb = kb.sbuf("winb", [128, 8, IN_COLS], BF16, ctx=sc)
        stage = [kb.sbuf("wst%d" % i, [128, IN_COLS], ctx=sc) for i in range(2)]
        load_cast_weight(g, sc, "win", g.w_in, lambda k: g.w_in.ap[L, 128 * k:128 * k + 128, :], 8, IN_COLS, stage, winb,
                         lambda k: winb[:, k, :])
        scr = {"junk": kb.sbuf("junk", [128, 1024], ctx=sc), "ssq": kb.sbuf("ssq", [128, 1], ctx=sc),
               "xn": kb.sbuf("xn", [128, 1024], ctx=sc), "psT": [g.ps[0], g.ps[1]]}
        xts = [kb.sbuf("xt%d" % i, [128, 1024], ctx=sc) for i in range(2)]
        hTs = [kb.sbuf("hT%d" % i, [128, 8, 512], BF16, ctx=sc) for i in range(2)]
        toks = [kb.sbuf("tok%d" % i, [128, TK_W], ctx=sc) for i in range(2)]
        fms = [kb.sbuf("fm%d" % i, [128, 512], ctx=sc) for i in range(3)]
        psTM = [g.ps[2], g.ps[3], g.ps[4]]
        psFM = [g.ps[5], g.ps[6]]
        blocks = [(0, 2)] + [(2 + 4 * i, 4) for i in range(8)]
        ti = 0
        fi = 0
        for bi, (t0, nt) in enumerate(blocks):
            hT = hTs[bi % 2]
            ntok = nt * 128
            for j in range(nt):
                t = t0 + j
                xt = xts[ti % 2]
                tok = toks[ti % 2]
                ti += 1
                kb.dma("sp", xt[:], xs_tiles[t].ap, [xs_tiles[t]], [xt])
                norm_tile_to_hT(g, L, 0, xt, t < 2, [hT[:, c, j * 128:j * 128 + 128] for c in range(8)], hT, scr)
                segs = [(psTM[0], 0, 512, C_GATE), (psTM[1], 0, 512, C_GATE + 512), (psTM[2], 0, 24, C_GATE + 1024),
                        (psTM[2], 24, 256, C_V)]
                for ps, o0, w, c0 in segs:
                    for k in range(8):
                        kb.mm(ps[:, o0:o0 + w], hT[:, k, j * 128:j * 128 + 128], winb[:, k, c0:c0 + w], k == 0, k == 7,
                              [hT, winb], [ps])
                kb.copy("dve", tok[:, 0:512], psTM[0][:, :], [psTM[0]], [tok])
                kb.copy("act", tok[:, 512:1024], psTM[1][:, :], [psTM[1]], [tok])
                kb.copy("dve", tok[:, 1024:1304], psTM[2][:, 0:280], [psTM[2]], [tok])
                kb.dma("sp", g.TOK[t].ap, tok[:], [tok], [g.TOK[t]])
            for fc in range(11):
                c0 = C_QKV + 128 * fc if fc < 9 else C_U + 128 * (fc - 9)
                ps = psFM[fi % 2]
                fm = fms[fi % 3]
                fi += 1
                for k in range(8):
                    kb.mm(ps[:, 0:ntok], winb[:, k, c0:c0 + 128], hT[:, k, 0:ntok], k == 0, k == 7, [hT, winb], [ps])
                if fc < 9:
                    kb.copy("act" if fc % 2 == 0 else "dve", fm[:, 0:ntok], ps[:, 0:ntok], [ps], [fm])
                else:
                    kb.act(fm[:, 0:ntok], ps[:, 0:ntok], AF.Gelu, [ps], [fm])
                kb.dma("pool", g.ZQ[fc][bi].ap, fm[:, 0:ntok], [fm], [g.ZQ[fc][bi]])
    kb.barrier()


def phase_attn(g, L, with_ctx, nblk=8):
    kb = g.kb
    with ExitStack() as sc:
        QT = [kb.sbuf("QT%d" % p, [128, T], BF16, ctx=sc) for p in range(3)]
        KT = kb.sbuf("KT", [128, T], BF16, ctx=sc)
        VE = kb.sbuf("VE", [128, NT, 128], BF16, ctx=sc)
        ones = kb.sbuf("ones", [128, 64], BF16, ctx=sc)
        gain = kb.sbuf("gain", [128, 8, 64], ctx=sc)
        kb.memset("dve", ones[:], 1.0, [ones])
        kb.dma("sp", gain[:, 0, :], g.q_norm_g.ap[L:L + 1, :].to_broadcast([128, 64]), [g.q_norm_g], [gain])
        kb.dma("sp", gain[:, 6, :], g.k_norm_g.ap[L:L + 1, :].to_broadcast([128, 64]), [g.k_norm_g], [gain])
        kb.ts("dve", gain[:, 0, :], gain[:, 0, :], 0.125, None, ALU.mult, None, [gain], [gain])
        kb.copy("dve", gain[:, 1:6, :], gain[:, 0:1, :].to_broadcast([128, 5, 64]), [gain], [gain])
        kb.copy("dve", gain[:, 7, :], gain[:, 6, :], [gain], [gain])
        qks = [kb.sbuf("qk%d" % i, [128, 512], ctx=sc) for i in range(2)]
        v32s = [kb.sbuf("v32%d" % i, [128, 128], ctx=sc) for i in range(2)]
        css = [kb.sbuf("cs%d" % i, [128, 64], ctx=sc) for i in range(2)]
        sqt = kb.sbuf("sqt", [128, 512], ctx=sc)
        ssq = kb.sbuf("ssq8", [128, 8], ctx=sc)
        qn = kb.sbuf("qn", [128, 512], ctx=sc)
        qr = kb.sbuf("qr", [128, 512], ctx=sc)
        tm = [kb.sbuf("ropet%d" % i, [128, 256], ctx=sc) for i in range(4)]
        psTr = g.ps[0]
        import os as _os
        STG = int(_os.environ.get("ATTN_STG", "9"))
        for t in range(int(_os.environ.get("ATTN_NT", str(NT)))):
            qk, v32, cs = qks[t % 2], v32s[t % 2], css[t % 2]
            kb.dma("sp", qk[:], g.TOK[t].ap[:, TK_AQ:TK_AQ + 512], [g.TOK[t]], [qk])
            kb.dma("pool", v32[:], g.TOK[t].ap[:, TK_AV:TK_AV + 128], [g.TOK[t]], [v32])
            kb.copy("pool", VE[:, t, :], v32[:], [v32], [VE])
            if STG < 2:
                continue
            kb.tt("pool", sqt[:], qk[:], qk[:], ALU.mult, [qk], [sqt])
            kb.op("dve", lambda e, o=ssq[:, 0:8], i=sqt[:].rearrange("p (h d) -> p h d", h=8): e.reduce_sum(o, i, AX.X),
                  [sqt], [ssq])
            rstd_from_ssq(kb, ssq[:, 0:8], ssq[:, 0:8], 64, [ssq], [ssq])
            if STG < 3:
                continue
            q3 = qn[:].rearrange("p (h d) -> p h d", h=8)
            kb.tt("dve", q3, qk[:].rearrange("p (h d) -> p h d", h=8), ssq[:, 0:8].unsqueeze(2).to_broadcast([128, 8, 64]),
                  ALU.mult, [qk, ssq], [qn])
            if STG < 4:
                continue
            if t >= 2:
                kb.tt("pool", q3, q3, gain[:], ALU.mult, [qn, gain], [qn])
                if STG < 5:
                    continue
                kb.dma("sp", cs[:], g.rope.ap[128 * (t - 2):128 * (t - 2) + 128, :], [g.rope], [cs])
                q5 = qn[:].rearrange("p (h a b r) -> p h a b r", h=8, a=2, b=2, r=16)
                o5 = qr[:].rearrange("p (h a b r) -> p h a b r", h=8, a=2, b=2, r=16)
                for ax in [int(v) for v in _os.environ.get("ROPE_AX", "0,1").split(",") if v != ""]:
                    a_, b_ = q5[:, :, ax, 0, :], q5[:, :, ax, 1, :]
                    cos = cs[:, 16 * ax:16 * ax + 16].unsqueeze(1).to_broadcast([128, 8, 16])
                    sin = cs[:, 32 + 16 * ax:32 + 16 * ax + 16].unsqueeze(1).to_broadcast([128, 8, 16])
                    tv = [x[:, 128 * ax:128 * ax + 128].rearrange("p (h r) -> p h r", h=8) for x in tm]
                    kb.tt("dve", tv[0], a_, cos, ALU.mult, [qn, cs], [tm[0]])
                    kb.tt("dve", tv[1], b_, sin, ALU.mult, [qn, cs], [tm[1]])
                    kb.tt("dve", o5[:, :, ax, 0, :], tv[0], tv[1], ALU.subtract, [tm[0], tm[1]], [qr])
                    kb.tt("dve", tv[2], a_, sin, ALU.mult, [qn, cs], [tm[2]])
                    kb.tt("dve", tv[3], b_, cos, ALU.mult, [qn, cs], [tm[3]])
                    kb.tt("dve", o5[:, :, ax, 1, :], tv[2], tv[3], ALU.add, [tm[2], tm[3]], [qr])
            else:
                kb.tt("pool", qr[:].rearrange("p (h d) -> p h d", h=8), q3, gain[:], ALU.mult, [qn, gain], [qr])
            if STG < 6:
                continue
            for p in range(3):
                kb.tr(psTr[:, p * 128:p * 128 + 128], qr[:, p * 128:p * 128 + 128], g.ident[:], [qr, g.ident], [psTr])
            kb.tr(psTr[:, 384:512], qr[:, 384:512], g.ident[:], [qr, g.ident], [psTr])
            if STG < 7:
                continue
            for p in range(3):
                kb.copy("act", QT[p][:, t * 128:t * 128 + 128], psTr[:, p * 128:p * 128 + 128], [psTr], [QT[p]])
            if STG < 8:
                continue
            kb.copy("act", KT[:, t * 128:t * 128 + 128], psTr[:, 384:512], [psTr], [KT])
        import os as _os
        if _os.environ.get("ATTN_PREP_ONLY"):
            kb.barrier()
            return
        psS = [g.ps[1], g.ps[2], g.ps[3]]
        accVs, accDs = [g.ps[4], g.ps[5]], [g.ps[6], g.ps[7]]
        PTs = [kb.sbuf("PT%d" % i, [128, 512], BF16, ctx=sc) for i in range(3)]
        rcs = [kb.sbuf("rc%d" % i, [64, 512], ctx=sc) for i in range(2)]
        ots = [kb.sbuf("ot%d" % i, [64, 512], ctx=sc) for i in range(2)]
        qblocks = ([(0, 0, 256, [0, 1])] if with_ctx else []) + \
                  [(1 + i, 256 + 512 * i, 512, list(range(NT))) for i in range(nblk)]
        items = []
        bi_ = 0
        for h in range(6):
            for (blk, q0, nq, kts) in qblocks:
                for ii, kt in enumerate(kts):
                    items.append((h, blk, q0, nq, kt, ii == 0, ii == len(kts) - 1, bi_))
                bi_ += 1
        LA = 2

        def emit_qk(i):
            h, blk, q0, nq, kt, first, last, bn = items[i]
            kv, p = h // 3, h % 3
            pr = slice(64 * kv, 64 * kv + 64)
            ps, PT = psS[i % 3], PTs[i % 3]
            kb.mm(ps[:, 0:nq], KT[pr, kt * 128:kt * 128 + 128], QT[p][pr, q0:q0 + nq], True, True, [KT, QT[p]], [ps])
            kb.act(PT[:, 0:nq], ps[:, 0:nq], AF.Exp, [ps], [PT])

        def emit_pv(i):
            h, blk, q0, nq, kt, first, last, bn = items[i]
            kv = h // 3
            PT = PTs[i % 3]
            accV, accD = accVs[bn % 2], accDs[bn % 2]
            kb.mm(accV[0:64, 0:nq], VE[:, kt, 64 * kv:64 * kv + 64], PT[:, 0:nq], first, last, [VE, PT], [accV])
            kb.mm(accD[0:64, 0:nq], ones[:, :], PT[:, 0:nq], first, last, [ones, PT], [accD])
            if last:
                rc, ot = rcs[bn % 2], ots[bn % 2]
                kb.op("dve", lambda e, o=rc[:, 0:nq], i_=accD[0:64, 0:nq]: e.reciprocal(o, i_), [accD], [rc])
                kb.tt("dve", ot[:, 0:nq], accV[0:64, 0:nq], rc[:, 0:nq], ALU.mult, [accV, rc], [ot])
                kb.dma("sp", g.MIXat[h][blk].ap, ot[:, 0:nq], [ot], [g.MIXat[h][blk]])

        n_it = len(items)
        for i in range(n_it + LA):
            if i < n_it:
                emit_qk(i)
            if i - LA >= 0:
                emit_pv(i - LA)
    kb.barrier()


def phase_sgu(g, L, tiles):
    kb = g.kb
    with ExitStack() as sc:
        WsT = kb.sbuf("WsT", [128, 4, 128], BF16, ctx=sc)
        ws32 = kb.sbuf("ws32", [128, 4, 128], ctx=sc)
        SB = kb.sbuf("SBb", [64, 512], ctx=sc)
        sgain = kb.sbuf("sgain", [128, 256], ctx=sc)
        psW = g.ps[0]
        for gi in range(4):
            kb.dma("sp", ws32[:, gi, :], g.sgu_w.ap[L, gi, :, :], [g.sgu_w], [ws32])
        for gi in range(4):
            kb.tr(psW[:, gi * 128:gi * 128 + 128], ws32[:, gi, :], g.ident[:], [ws32, g.ident], [psW])
        kb.copy("dve", WsT[:].rearrange("p g i -> p (g i)"), psW[:, :], [psW], [WsT])
        kb.dma("sp", SB[:], g.sgu_b.ap[L:L + 1, :, :].rearrange("o g i -> o (g i)").to_broadcast([64, 512]), [g.sgu_b], [SB])
        kb.dma("sp", sgain[:], g.sgu_norm_g.ap[L:L + 1, :].to_broadcast([128, 256]), [g.sgu_norm_g], [sgain])
        zvs = [kb.sbuf("zv%d" % i, [128, 256], ctx=sc) for i in range(2)]
        uts = [kb.sbuf("ut%d" % i, [64, 4, 128], ctx=sc) for i in range(2)]
        gv = kb.sbuf("gv", [128, 256], ctx=sc)
        sq = kb.sbuf("sgsq", [128, 256], ctx=sc)
        ss = kb.sbuf("sgss", [128, 4], ctx=sc)
        vb = kb.sbuf("vb", [128, 256], BF16, ctx=sc)
        tmps = [kb.sbuf("sgt%d" % i, [64, 512], ctx=sc) for i in range(2)]
        ress = [kb.sbuf("sgr%d" % i, [64, 512], ctx=sc) for i in range(2)]
        pss = [g.ps[1], g.ps[2]]
        for n, t in enumerate(tiles):
            zv, ut, tmp, res, ps = zvs[n % 2], uts[n % 2], tmps[n % 2], ress[n % 2], pss[n % 2]
            bi, boff = g.blk_of_tile(t)
            kb.dma("sp", zv[:], g.TOK[t].ap[:, TK_ZV:TK_ZV + 256], [g.TOK[t]], [zv])
            kb.dma("pool", ut[:], g.zqd[1152:1408, 128 * t:128 * t + 128].rearrange("(g d) t -> d g t", d=64),
                   [g.ZQ[9][bi], g.ZQ[10][bi]], [ut])
            kb.act(gv[:], zv[:], AF.Gelu_apprx_tanh, [zv], [gv])
            kb.tt("pool", sq[:], gv[:], gv[:], ALU.mult, [gv], [sq])
            kb.op("dve", lambda e, o=ss[:, 0:4], i=sq[:].rearrange("p (g d) -> p g d", g=4): e.reduce_sum(o, i, AX.X),
                  [sq], [ss])
            rstd_from_ssq(kb, ss[:, 0:4], ss[:, 0:4], 64, [ss], [ss])
            kb.tt("dve", gv[:].rearrange("p (g d) -> p g d", g=4), gv[:].rearrange("p (g d) -> p g d", g=4),
                  ss[:, 0:4].unsqueeze(2).to_broadcast([128, 4, 64]), ALU.mult, [gv, ss], [gv])
            kb.tt("pool", vb[:], gv[:], sgain[:], ALU.mult, [gv, sgain], [vb])
            for gi in range(4):
                kb.mm(ps[0:64, gi * 128:gi * 128 + 128], vb[:, gi * 64:gi * 64 + 64], WsT[:, gi, :], True, True, [vb, WsT], [ps])
            kb.tt("dve", tmp[:], ps[0:64, :], SB[:], ALU.add, [ps, SB], [tmp])
            kb.tt("pool", res[:], tmp[:], ut[:].rearrange("d g t -> d (g t)"), ALU.mult, [tmp, ut], [res])
            kb.dma("sp", g.mixd[768:1024, 128 * t:128 * t + 128].rearrange("(g d) t -> d g t", d=64),
                   res[:].rearrange("d (g t) -> d g t", g=4), [res], [g.MIXsg[t]])
    kb.barrier()


class _B:
    pass


def phase_dn(g, L, half_mode=False):
    kb = g.kb
    ident = g.ident
    with ExitStack() as sc:
        def S(name, shape, dt=F32):
            return kb.sbuf(name, shape, dt, ctx=sc)
        dnc = S("dnc", [128, 7, 128])
        kb.dma("sp", dnc[:], g.dncd.ap.rearrange("c p i -> p c i"), [g.dncd], [dnc])
        ones = S("ones32", [128, 128])
        kb.memset("dve", ones[:], 1.0, [ones])
        I4 = S("I4", [128, 4, 128])
        NM4 = [S("NM4%d" % d, [128, 4, 128]) for d in range(2)]
        SM4 = [S("SM4%d" % d, [128, 4, 128]) for d in range(2)]
        for c in range(4):
            kb.copy("dve", I4[:, c, :], ident[:], [ident], [I4])
            for d in range(2):
                kb.copy("dve", NM4[d][:, c, :], dnc[:, 3 + d, :], [dnc], [NM4[d]])
                kb.copy("dve", SM4[d][:, c, :], dnc[:, 5 + d, :], [dnc], [SM4[d]])
        BA = S("BA", [128, 68, 24])
        src = g.tokd[:, TK_BETA:TK_BETA + 24].rearrange("(c i) f -> i c f", i=64)
        kb.dma("sp", BA[0:64, :, :], src, g.TOK, [BA])
        kb.dma("pool", BA[64:128, :, :], src, g.TOK, [BA])
        ZB, ZA = S("ZB", [128, 6, 68]), S("ZA", [128, 6, 68])
        for d in range(2):
            for half in range(2):
                pr = slice(64 * half, 64 * half + 64)
                kb.copy("dve", ZB[pr, 3 * d:3 * d + 3, :], BA[pr, :, 6 * d + half:6 * d + 6:2].rearrange("i c p -> i p c"), [BA], [ZB])
                kb.copy("dve", ZA[pr, 3 * d:3 * d + 3, :],
                        BA[pr, :, 12 + 6 * d + half:12 + 6 * d + 6:2].rearrange("i c p -> i p c"), [BA], [ZA])
        AL, DTB, NEA = S("AL", [128, 6]), S("DTB", [128, 6]), S("NEA", [128, 6])
        for half in range(2):
            pr = slice(64 * half, 64 * half + 64)
            kb.dma("sp", AL[pr, :].rearrange("p (d q) -> p d q", d=2), g.dn_a_log.ap[L:L + 1, :, half::2].to_broadcast([64, 2, 3]),
                   [g.dn_a_log], [AL], allow_slow_non_contiguous=True)
            kb.dma("sp", DTB[pr, :].rearrange("p (d q) -> p d q", d=2), g.dn_dt_bias.ap[L:L + 1, :, half::2].to_broadcast([64, 2, 3]),
                   [g.dn_dt_bias], [DTB], allow_slow_non_contiguous=True)
        kb.act(NEA[:], AL[:], AF.Exp, [AL], [NEA])
        kb.ts("dve", NEA[:], NEA[:], -1.0, None, ALU.mult, None, [NEA], [NEA])
        NBETA, GT, GC, GL, E, KTS, EGL = [S(n, [128, 6, 68]) for n in ("NBETA", "GT", "GC", "GL", "E", "KTS", "EGL")]
        kb.act(NBETA[:], ZB[:], AF.Sigmoid, [ZB], [NBETA])
        kb.ts("dve", NBETA[:], NBETA[:], -1.0, None, ALU.mult, None, [NBETA], [NBETA])
        kb.tt("dve", GT[:], ZA[:], DTB[:].unsqueeze(2).to_broadcast([128, 6, 68]), ALU.add, [ZA, DTB], [GT])
        kb.act(GT[:], GT[:], AF.Exp, [GT], [GT])
        kb.act(GT[:], GT[:], AF.Ln, [GT], [GT], bias=1.0)
        kb.tt("dve", GT[:], GT[:], NEA[:].unsqueeze(2).to_broadcast([128, 6, 68]), ALU.mult, [GT, NEA], [GT])
        ps = g.ps[0]
        kb.mm(ps[:, 0:204], dnc[:, 1, :], GT[:, 0:3, :].rearrange("p a c -> p (a c)"), True, True, [dnc, GT], [ps])
        kb.mm(ps[:, 204:408], dnc[:, 2, :], GT[:, 3:6, :].rearrange("p a c -> p (a c)"), True, True, [dnc, GT], [ps])
        kb.copy("dve", GC[:].rearrange("p a c -> p (a c)"), ps[:, 0:408], [ps], [GC])
        kb.mm(ps[:, 0:408], dnc[:, 0, :], GT[:].rearrange("p a c -> p (a c)"), True, True, [dnc, GT], [ps])
        kb.copy("dve", GL[:].rearrange("p a c -> p (a c)"), ps[:, 0:408], [ps], [GL])
        kb.act(E[:], GC[:], AF.Exp, [GC], [E])
        kb.act(EGL[:], GL[:], AF.Exp, [GL], [EGL])
        kb.tt("dve", KTS[:], GL[:], GC[:], ALU.subtract, [GL, GC], [KTS])
        kb.act(KTS[:], KTS[:], AF.Exp, [KTS], [KTS])
        cw = S("cw", [128, 9, 3])
        for fc in range(9):
            kb.dma("sp", cw[:, fc, :], g.conv_w.ap[L, :, 128 * fc:128 * fc + 128].rearrange("k p -> p k"), [g.conv_w], [cw],
                   allow_slow_non_contiguous=True)
        dgain = S("dgain", [64, 1])
        kb.dma("sp", dgain[:], g.dn_norm_g.ap[L, :].rearrange("(e o) -> e o", o=1), [g.dn_norm_g], [dgain],
               allow_slow_non_contiguous=True)
        qn, kn, vn, zr = S("dqn", [128, T]), S("dkn", [128, T]), S("dvn", [128, T]), S("zraw", [128, T])
        sqb = S("dsqb", [128, 512])
        rsb = S("drsb", [128, 512])
        OB = S("OB", [128, 68, 64])
        bufs = []
        for d in range(2):
            B = _B()
            for n in ("kT", "qT", "vT", "kBD", "diag", "D", "attnT", "N", "NT", "P2", "PT2", "R", "kt", "tmp"):
                setattr(B, n, S("%s%d" % (n, d), [128, 4, 128]))
            B.vst = S("vst%d" % d, [128, 4, 64])
            B.ntmp, B.vnew, B.o1 = S("ntmp%d" % d, [128, 64]), S("vnew%d" % d, [128, 64]), S("o1%d" % d, [128, 64])
            B.S = [S("S%d_%d" % (d, i), [128, 64]) for i in range(2)]
            B.banks = g.ps[4 * d:4 * d + 4]
            for t_ in (B.kT, B.qT, B.vT):
                kb.memset("dve", t_[:], 0.0, [t_])
            bufs.append(B)
        gts = [S("dgt%d" % i, [128, 4, 64]) for i in range(2)]
        otb = [S("dot%d" % i, [64, 512]) for i in range(2)]
        oss = S("doss", [128, 4])
        f2 = lambda ap: ap.rearrange("p c i -> p (c i)")

        for pp in range(3):
            for xi, dst in enumerate((qn, kn, vn)):
                fc = 3 * xi + pp
                for bi, (b0, n) in enumerate(BLOCKS):
                    kb.dma("sp" if bi % 2 == 0 else "pool", zr[:, b0:b0 + n], g.ZQ[fc][bi].ap, [g.ZQ[fc][bi]], [zr])
                kb.act(dst[:], zr[:], AF.Copy, [zr, cw], [dst], scale=cw[:, fc, 1:2])
                for (a0, a1) in ((0, NCTX), (NCTX, T)):
                    kb.stt("dve", dst[:, a0 + 1:a1], zr[:, a0:a1 - 1], cw[:, fc, 0:1], dst[:, a0 + 1:a1], ALU.mult, ALU.add,
                           [zr, cw, dst], [dst])
                    kb.stt("dve", dst[:, a0:a1 - 1], zr[:, a0 + 1:a1], cw[:, fc, 2:3], dst[:, a0:a1 - 1], ALU.mult, ALU.add,
                           [zr, cw, dst], [dst])
                kb.act(dst[:], dst[:], AF.Silu, [dst], [dst])
                if xi < 2:
                    for (b0, n) in BLOCKS:
                        kb.tt("dve", sqb[:, 0:n], dst[:, b0:b0 + n], dst[:, b0:b0 + n], ALU.mult, [dst], [sqb])
                        kb.mm(g.ps[0][:, 0:n], dnc[:, 0, :], sqb[:, 0:n], True, True, [dnc, sqb], [g.ps[0]])
                        sc_ = 64.0 if xi == 0 else 1.0
                        kb.act(rsb[:, 0:n], g.ps[0][:, 0:n], AF.Sqrt, [g.ps[0]], [rsb], scale=sc_, bias=sc_ * EPS)
                        kb.op("dve", lambda e, o=rsb[:, 0:n]: e.reciprocal(o, o), [rsb], [rsb])
                        kb.tt("dve", dst[:, b0:b0 + n], dst[:, b0:b0 + n], rsb[:, 0:n], ALU.mult, [dst, rsb], [dst])
            for d in range(2):
                kb.memset("dve", bufs[d].S[0][:], 0.0, [bufs[d].S[0]])
            written = set()
            scnt = [0, 0]

            def prep(d, grp):
                B = bufs[d]
                dp, c0, t0 = 3 * d + pp, 4 * grp, 256 * grp
                pA, pB, pC, pD = B.banks
                for dst, src_ in ((B.kT, kn), (B.qT, qn), (B.vT, vn)):
                    for half in range(2):
                        pr = slice(64 * half, 64 * half + 64)
                        kb.copy("dve", dst[pr, :, 64 * half:64 * half + 64],
                                src_[pr, t0:t0 + 256].rearrange("p (c i) -> p c i", c=4), [src_], [dst])
                        yield
                for c in range(4):
                    kb.tr(pA[:, c * 128:c * 128 + 128], B.kT[:, c, :], ident[:], [B.kT, ident], [pA])
                    yield
                kb.copy("act", f2(B.kBD[:]), pA[:, :], [pA], [B.kBD])
                yield
                for c in range(4):
                    kb.tr(pA[:, c * 128:c * 128 + 128], B.vT[:, c, :], ident[:], [B.vT, ident], [pA])
                    yield
                kb.copy("act", f2(B.tmp[:]), pA[:, :], [pA], [B.tmp])
                yield
                kb.tt("dve", B.vst[:], B.tmp[:, :, 0:64], B.tmp[:, :, 64:128], ALU.add, [B.tmp], [B.vst])
                yield
                for c in range(4):
                    kb.mm(pB[:, c * 128:c * 128 + 128], B.kT[:, c, :], B.kT[:, c, :], True, True, [B.kT], [pB])
                    yield
                for c in range(4):
                    kb.mm(pC[:, c * 128:c * 128 + 128], B.kT[:, c, :], B.qT[:, c, :], True, True, [B.kT, B.qT], [pC])
                    yield
                for c in range(4):
                    kb.ts("dve", B.diag[:, c, :], ident[:], GC[:, dp, c0 + c:c0 + c + 1], None, ALU.mult, None, [ident, GC], [B.diag])
                    yield
                kb.mm(pA[:, :], ones[:], f2(B.diag[:]), True, True, [ones, B.diag], [pA])
                yield
                for c in range(4):
                    kb.ts("dve", B.D[:, c, :], pA[:, c * 128:c * 128 + 128], GC[:, dp, c0 + c:c0 + c + 1], 0.0, ALU.subtract, ALU.min,
                          [pA, GC], [B.D])
                    yield
                kb.tt("dve", f2(B.D[:]), f2(B.D[:]), f2(NM4[d][:]), ALU.add, [B.D, NM4[d]], [B.D])
                yield
                kb.act(f2(B.D[:]), f2(B.D[:]), AF.Exp, [B.D], [B.D])
                yield
                kb.tt("dve", f2(B.attnT[:]), pC[:, :], f2(B.D[:]), ALU.mult, [pC, B.D], [B.attnT])
                yield
                kb.tt("dve", f2(B.N[:]), pB[:, :], f2(B.D[:]), ALU.mult, [pB, B.D], [B.N])
                yield
                kb.tt("dve", f2(B.N[:]), f2(B.N[:]), f2(SM4[d][:]), ALU.mult, [B.N, SM4[d]], [B.N])
                yield
                for c in range(4):
                    kb.act(B.N[:, c, :], B.N[:, c, :], AF.Copy, [B.N, NBETA], [B.N], scale=NBETA[:, dp, c0 + c:c0 + c + 1])
                    yield
                for c in range(4):
                    kb.tr(pA[:, c * 128:c * 128 + 128], B.N[:, c, :], ident[:], [B.N, ident], [pA])
                    yield
                kb.copy("act", f2(B.NT[:]), pA[:, :], [pA], [B.NT])
                yield
                kb.tt("dve", f2(B.R[:]), f2(B.N[:]), f2(I4[:]), ALU.add, [B.N, I4], [B.R])
                yield
                P, PT = B.N, B.NT
                for k in range(5):
                    Pn, PTn = (B.P2, B.PT2) if k % 2 == 0 else (B.N, B.NT)
                    for c in range(4):
                        kb.mm(pC[:, c * 128:c * 128 + 128], P[:, c, :], PT[:, c, :], True, True, [P, PT], [pC])
                        yield
                    if k < 4:
                        for c in range(4):
                            kb.mm(pB[:, c * 128:c * 128 + 128], PT[:, c, :], P[:, c, :], True, True, [P, PT], [pB])
                            yield
                    kb.copy("act", f2(PTn[:]), pC[:, :], [pC], [PTn])
                    yield
                    if k < 4:
                        kb.copy("dve", f2(Pn[:]), pB[:, :], [pB], [Pn])
                        yield
                    for c in range(4):
                        kb.mm(pA[:, c * 128:c * 128 + 128], PTn[:, c, :], B.R[:, c, :], True, True, [PTn, B.R], [pA])
                        yield
                    kb.tt("dve", f2(B.R[:]), f2(B.R[:]), pA[:, :], ALU.add, [B.R, pA], [B.R])
                    yield
                    P, PT = Pn, PTn
                for c in range(4):
                    kb.act(B.kt[:, c, :], B.kBD[:, c, :], AF.Copy, [B.kBD, KTS], [B.kt], scale=KTS[:, dp, c0 + c:c0 + c + 1])
                    yield

            def scan(d, grp):
                B = bufs[d]
                dp, c0 = 3 * d + pp, 4 * grp
                pD = B.banks[3]
                for c in (range(4) if d == 0 else range(3, -1, -1)):
                    ch = c0 + c
                    S_old, S_new = B.S[scnt[d] % 2], B.S[(scnt[d] + 1) % 2]
                    scnt[d] += 1
                    kb.mm(pD[:, 0:64], B.kT[:, c, :], S_old[:], True, True, [B.kT, S_old], [pD])
                    yield
                    kb.stt("dve", B.ntmp[:], pD[:, 0:64], E[:, dp, ch:ch + 1], B.vst[:, c, :], ALU.mult, ALU.subtract,
                           [pD, E, B.vst], [B.ntmp])
                    yield
                    kb.mm(pD[:, 64:128], B.R[:, c, :], B.ntmp[:], True, True, [B.R, B.ntmp], [pD])
                    yield
                    kb.act(B.vnew[:], pD[:, 64:128], AF.Copy, [pD, NBETA], [B.vnew], scale=NBETA[:, dp, ch:ch + 1])
                    yield
                    need_o = not (half_mode and (ch >= 36 or ch < 4))
                    if need_o:
                        kb.mm(pD[:, 128:192], B.qT[:, c, :], S_old[:], True, True, [B.qT, S_old], [pD])
                        yield
                        kb.act(B.o1[:], pD[:, 128:192], AF.Copy, [pD, E], [B.o1], scale=E[:, dp, ch:ch + 1])
                        yield
                        kb.mm(pD[:, 192:256], B.attnT[:, c, :], B.vnew[:], True, True, [B.attnT, B.vnew], [pD])
                        yield
                    if not need_o:
                        pass
                    elif ch not in written:
                        written.add(ch)
                        kb.tt("dve", OB[:, ch, :], B.o1[:], pD[:, 192:256], ALU.add, [B.o1, pD], [OB])
                        yield
                    else:
                        kb.tt("dve", B.o1[:], B.o1[:], pD[:, 192:256], ALU.add, [B.o1, pD], [B.o1])
                        yield
                        kb.tt("dve", OB[:, ch, :], OB[:, ch, :], B.o1[:], ALU.add, [OB, B.o1], [OB])
                        yield
                    kb.mm(pD[:, 256:320], B.kt[:, c, :], B.vnew[:], True, True, [B.kt, B.vnew], [pD])
                    yield
                    kb.stt("dve", S_new[:], S_old[:], EGL[:, dp, ch:ch + 1], pD[:, 256:320], ALU.mult, ALU.add,
                           [S_old, EGL, pD], [S_new])
                    yield

            order = [list(range(17)), [0] + list(range(16, 0, -1))]
            def stream(d):
                for it in range(9 if (half_mode and d == 0) else 17):
                    yield from prep(d, order[d][it])
                    yield from scan(d, order[d][it])

            gens = [stream(0), stream(1)]
            alive = [True, True]
            nstep = [1, 2] if half_mode else [1, 1]
            while any(alive):
                for d in range(2):
                    for _ in range(nstep[d]):
                        if alive[d]:
                            try:
                                next(gens[d])
                            except StopIteration:
                                alive[d] = False
            pO = g.ps[0]
            for grp in (range(1, 9) if half_mode else range(17)):
                gt, ot = gts[grp % 2], otb[grp % 2]
                c0, t0 = 4 * grp, 256 * grp
                for ab in range(2):
                    h = 2 * pp + ab
                    kb.dma("sp" if ab == 0 else "pool", gt[64 * ab:64 * ab + 64, :, :],
                           g.tokd[t0:t0 + 256, TK_GATE + 64 * h:TK_GATE + 64 * h + 64].rearrange("(c i) e -> i c e", i=64),
                           [g.TOK[2 * grp], g.TOK[2 * grp + 1]], [gt])
                kb.act(gt[:], gt[:], AF.Silu, [gt], [gt])
                ob = OB[:, c0:c0 + 4, :]
                tmp3 = bufs[0].tmp[:, 0:2, :].rearrange("p a (b e) -> p (a b) e", e=64)
                kb.tt("dve", tmp3, ob, ob, ALU.mult, [OB], [bufs[0].tmp])
                kb.op("dve", lambda e, o=oss[:, 0:4], i=tmp3: e.reduce_sum(o, i, AX.X), [bufs[0].tmp], [oss])
                rstd_from_ssq(kb, oss[:, 0:4], oss[:, 0:4], 64, [oss], [oss])
                kb.tt("dve", ob, ob, oss[:, 0:4].unsqueeze(2).to_broadcast([128, 4, 64]), ALU.mult, [OB, oss], [OB])
                kb.tt("dve", ob, ob, gt[:], ALU.mult, [OB, gt], [OB])
                for c in range(4):
                    kb.tr(pO[0:64, c * 128:c * 128 + 128], OB[:, c0 + c, :], ident[:], [OB, ident], [pO])
                kb.act(ot[:, :], pO[0:64, :], AF.Copy, [pO, dgain], [ot], scale=dgain[:, 0:1])
                for ab in range(2):
                    h = 2 * pp + ab
                    kb.dma("sp" if ab == 0 else "pool",
                           g.mixd[64 * h:64 * h + 64, t0:t0 + 256].rearrange("e (c i) -> e c i", c=4),
                           ot[:, :].rearrange("e (c ab i) -> e c ab i", c=4, ab=2)[:, :, ab, :], [ot],
                           [g.MIXdn[2 * grp], g.MIXdn[2 * grp + 1]])
    kb.barrier()


def prep_w13(g, wa, wb, W13, nf, sc):
    kb = g.kb
    st = [kb.sbuf("w13s%d" % i, [128, 8, 256], ctx=sc) for i in range(2)]
    sb = [kb.sbuf("w13b%d" % i, [128, 8, 256], BF16, ctx=sc) for i in range(2)]
    for f in range(nf):
        s_, b_ = st[f % 2], sb[f % 2]
        kb.dma("sp", s_[:, :, 0:128], wa[0][:, 128 * f:128 * f + 128].rearrange("(k p) j -> p k j", p=128), [wa[1]], [s_])
        kb.dma("pool", s_[:, :, 128:256], wb[0][:, 128 * f:128 * f + 128].rearrange("(k p) j -> p k j", p=128), [wb[1]], [s_])
        kb.copy("dve" if f % 2 == 0 else "pool", b_[:], s_[:], [s_], [b_])
        kb.dma("sp", W13[f].ap, b_[:], [b_], [W13[f]])


def phase_out_ffn(g, L, xs_tiles, xo_tiles, do_ctx):
    kb = g.kb
    with ExitStack() as sc:
        with ExitStack() as sc2:
            prep_w13(g, (g.ffn_w1.ap[0], g.ffn_w1), (g.ffn_w3.ap[0], g.ffn_w3), g.W13, 22, sc2)
        kb.barrier()
        woutb = kb.sbuf("woutb", [128, 8, D], BF16, ctx=sc)
        w2b = kb.sbuf("w2b", [128, 22, D], BF16, ctx=sc)
        stage = [kb.sbuf("wst%d" % i, [128, D], ctx=sc) for i in range(2)]
        load_cast_weight(g, sc, "wout", g.w_out, lambda k: g.w_out.ap[L, 128 * k:128 * k + 128, :], 8, D, stage, woutb,
                         lambda k: woutb[:, k, :])
        load_cast_weight(g, sc, "w2", g.ffn_w2, lambda k: g.ffn_w2.ap[0, 128 * k:128 * k + 128, :], 22, D, stage, w2b,
                         lambda k: w2b[:, k, :])
        gmsa = kb.sbuf("gmsa", [128, 2, D], ctx=sc)
        gmlp = kb.sbuf("gmlp", [128, 2, D], ctx=sc)
        for s in range(2):
            kb.dma("sp", gmsa[:, s, :], g.gates.ap[L, 0, s:s + 1, :].to_broadcast([128, D]), [g.gates], [gmsa])
            kb.dma("sp", gmlp[:, s, :], g.gates.ap[L, 1, s:s + 1, :].to_broadcast([128, D]), [g.gates], [gmlp])
        scr = {"junk": kb.sbuf("junk", [128, 1024], ctx=sc), "ssq": kb.sbuf("ssq", [128, 1], ctx=sc),
               "xn": kb.sbuf("xn", [128, 1024], ctx=sc), "psT": [g.ps[0], g.ps[1]]}
        mix32 = [kb.sbuf("mix32_%d" % i, [128, 8, 128], ctx=sc) for i in range(2)]
        mixb = [kb.sbuf("mixb_%d" % i, [128, 8, 128], BF16, ctx=sc) for i in range(2)]
        xts = [kb.sbuf("xt%d" % i, [128, D], ctx=sc) for i in range(2)]
        tmpy = kb.sbuf("tmpy", [128, D], ctx=sc)
        x1blk = [kb.sbuf("x1b%d" % i, [128, 4, D], ctx=sc) for i in range(1)]
        h2Ts = [kb.sbuf("h2T%d" % i, [128, 8, 512], BF16, ctx=sc) for i in range(1)]
        gT = kb.sbuf("gT", [128, 22, 512], BF16, ctx=sc)
        w13s = [kb.sbuf("w13_%d" % i, [128, 8, 256], BF16, ctx=sc) for i in range(3)]
        sas = [kb.sbuf("sa%d" % i, [128, 512], ctx=sc) for i in range(2)]
        x2s = [kb.sbuf("x2_%d" % i, [128, D], ctx=sc) for i in range(2)]
        psY = [g.ps[2], g.ps[3]]
        psA, psB = [g.ps[4], g.ps[5]], [g.ps[6], g.ps[7]]
        blocks = ([(0, 0, 2)] if do_ctx else []) + [(1 + i, 2 + 4 * i, 4) for i in range(8)]
        ti = 0
        wi = 0
        for bn, (bi, t0, nt) in enumerate(blocks):
            ntok = nt * 128
            x1b, h2T = x1blk[0], h2Ts[0]
            s = 1 if bi == 0 else 0
            for j in range(nt):
                t = t0 + j
                m32, mb, xt = mix32[ti % 2], mixb[ti % 2], xts[ti % 2]
                ti += 1
                kb.dma("sp", m32[:], g.mixd[:, 128 * t:128 * t + 128].rearrange("(c p) t -> p c t", p=128),
                       [g.MIXdn[t], g.MIXsg[t]] + [g.MIXat[h][bi] for h in range(6)], [m32])
                kb.dma("pool", xt[:], xs_tiles[t].ap, [xs_tiles[t]], [xt])
                kb.copy("pool", mb[:], m32[:], [m32], [mb])
                for half in range(2):
                    for k in range(8):
                        kb.mm(psY[half][:, :], mb[:, k, :], woutb[:, k, 512 * half:512 * half + 512], k == 0, k == 7,
                              [mb, woutb], [psY[half]])
                for half in range(2):
                    kb.tt("dve", tmpy[:, 512 * half:512 * half + 512], psY[half][:, :], gmsa[:, s, 512 * half:512 * half + 512],
                          ALU.mult, [psY[half], gmsa], [tmpy])
                kb.tt("pool", x1b[:, j, :], tmpy[:], xt[:], ALU.add, [tmpy, xt], [x1b])
                norm_tile_to_hT(g, L, 1, x1b, bi == 0, [h2T[:, c, j * 128:j * 128 + 128] for c in range(8)], h2T, scr, xap=x1b[:, j, :])
            for f in range(22):
                w13 = w13s[wi % 3]
                pa, pb, sa = psA[wi % 2], psB[wi % 2], sas[wi % 2]
                wi += 1
                kb.dma("sp" if f % 2 == 0 else "pool", w13[:], g.W13[f].ap, [g.W13[f]], [w13])
                for k in range(8):
                    kb.mm(pa[:, 0:ntok], w13[:, k, 0:128], h2T[:, k, 0:ntok], k == 0, k == 7, [w13, h2T], [pa])
                for k in range(8):
                    kb.mm(pb[:, 0:ntok], w13[:, k, 128:256], h2T[:, k, 0:ntok], k == 0, k == 7, [w13, h2T], [pb])
                kb.act(sa[:, 0:ntok], pa[:, 0:ntok], AF.Silu, [pa], [sa])
                kb.tt("dve", gT[:, f, 0:ntok], sa[:, 0:ntok], pb[:, 0:ntok], ALU.mult, [sa, pb], [gT])
            for j in range(nt):
                t = t0 + j
                x2 = x2s[j % 2]
                for half in range(2):
                    for f in range(22):
                        kb.mm(psY[half][:, :], gT[:, f, j * 128:j * 128 + 128], w2b[:, f, 512 * half:512 * half + 512],
                              f == 0, f == 21, [gT, w2b], [psY[half]])
                for half in range(2):
                    kb.tt("dve", tmpy[:, 512 * half:512 * half + 512], psY[half][:, :], gmlp[:, s, 512 * half:512 * half + 512],
                          ALU.mult, [psY[half], gmlp], [tmpy])
                kb.tt("pool", x2[:], tmpy[:], x1b[:, j, :], ALU.add, [tmpy, x1b], [x2])
                kb.dma("sp", xo_tiles[t].ap, x2[:], [x2], [xo_tiles[t]])
    kb.barrier()


def phase_out_router(g, L, xs_tiles, nblk=8):
    kb = g.kb
    with ExitStack() as sc:
        woutb = kb.sbuf("woutb", [128, 8, D], BF16, ctx=sc)
        stage = [kb.sbuf("wst%d" % i, [128, D], ctx=sc) for i in range(2)]
        load_cast_weight(g, sc, "wout", g.w_out, lambda k: g.w_out.ap[L, 128 * k:128 * k + 128, :], 8, D, stage, woutb,
                         lambda k: woutb[:, k, :])
        gmsa = kb.sbuf("gmsa", [128, D], ctx=sc)
        kb.dma("sp", gmsa[:], g.gates.ap[L, 0, 0:1, :].to_broadcast([128, D]), [g.gates], [gmsa])
        rw = kb.sbuf("rw", [128, 8, NEXP], ctx=sc)
        kb.dma("sp", rw[:], g.router_w.ap[0].rearrange("(k p) e -> p k e", p=128), [g.router_w], [rw])
        rb = kb.sbuf("rb", [128, NEXP], ctx=sc)
        kb.dma("sp", rb[:], g.router_b.ap[0:1, :].to_broadcast([128, NEXP]), [g.router_b], [rb])
        scr = {"junk": kb.sbuf("junk", [128, 1024], ctx=sc), "ssq": kb.sbuf("ssq", [128, 1], ctx=sc),
               "xn": kb.sbuf("xn", [128, 1024], ctx=sc), "psT": [g.ps[0], g.ps[1]]}
        mix32 = [kb.sbuf("mix32_%d" % i, [128, 8, 128], ctx=sc) for i in range(2)]
        mixb = [kb.sbuf("mixb_%d" % i, [128, 8, 128], BF16, ctx=sc) for i in range(2)]
        xts = [kb.sbuf("xt%d" % i, [128, D], ctx=sc) for i in range(2)]
        tmpy = kb.sbuf("tmpy", [128, D], ctx=sc)
        x1s = [kb.sbuf("x1_%d" % i, [128, D], ctx=sc) for i in range(2)]
        h2Ts = [kb.sbuf("h2T%d" % i, [128, 8, 512], BF16, ctx=sc) for i in range(2)]
        h32 = kb.sbuf("h32", [128, 8, 128], ctx=sc)
        lg = kb.sbuf("lg", [128, NEXP], ctx=sc)
        mx8 = kb.sbuf("mx8", [128, 8], ctx=sc)
        msk = kb.sbuf("msk", [128, NEXP], ctx=sc)
        ex = kb.sbuf("ex", [128, NEXP], ctx=sc)
        nm1 = kb.sbuf("nm1", [128, 2], ctx=sc)
        psY = [g.ps[2], g.ps[3]]
        psR = g.ps[4]
        for bi in range(nblk):
            h2T = h2Ts[bi % 2]
            for j in range(4):
                n = 4 * bi + j
                t = 2 + n
                m32, mb, xt, x1 = mix32[n % 2], mixb[n % 2], xts[n % 2], x1s[n % 2]
                kb.dma("sp", m32[:], g.mixd[:, 128 * t:128 * t + 128].rearrange("(c p) t -> p c t", p=128),
                       [g.MIXdn[t], g.MIXsg[t]] + [g.MIXat[h][1 + bi] for h in range(6)], [m32])
                kb.dma("pool", xt[:], xs_tiles[t].ap, [xs_tiles[t]], [xt])
                kb.copy("dve", mb[:], m32[:], [m32], [mb])
                for half in range(2):
                    for k in range(8):
                        kb.mm(psY[half][:, :], mb[:, k, :], woutb[:, k, 512 * half:512 * half + 512], k == 0, k == 7,
                              [mb, woutb], [psY[half]])
                for half in range(2):
                    kb.tt("dve", tmpy[:, 512 * half:512 * half + 512], psY[half][:, :], gmsa[:, 512 * half:512 * half + 512],
                          ALU.mult, [psY[half], gmsa], [tmpy])
                kb.tt("dve", x1[:], tmpy[:], xt[:], ALU.add, [tmpy, xt], [x1])
                kb.dma("sp", g.X1S[n].ap, x1[:], [x1], [g.X1S[n]])
                norm_tile_to_hT(g, L, 1, x1, False, [h2T[:, c, j * 128:j * 128 + 128] for c in range(8)], h2T, scr,
                                also_f32=[h32[:, c, :] for c in range(8)], f32_buf=h32)
                for k in range(8):
                    kb.mm(psR[:, 0:NEXP], h32[:, k, :], rw[:, k, :], k == 0, k == 7, [h32, rw], [psR])
                kb.tt("dve", lg[:], psR[:, 0:NEXP], rb[:], ALU.add, [psR, rb], [lg])
                kb.op("dve", lambda e, o=mx8[:], i=lg[:]: e.max(out=o, in_=i), [lg], [mx8])
                kb.ts("dve", msk[:], lg[:], mx8[:, 1:2], None, ALU.is_ge, None, [lg, mx8], [msk])
                kb.ts("dve", nm1[:, 0:1], mx8[:, 0:1], -1.0, None, ALU.mult, None, [mx8], [nm1])
                kb.act(ex[:], lg[:], AF.Exp, [lg, nm1], [ex], bias=nm1[:, 0:1])
                kb.tt("dve", ex[:], ex[:], msk[:], ALU.mult, [ex, msk], [ex])
                kb.op("dve", lambda e, o=nm1[:, 1:2], i=ex[:]: e.reduce_sum(o, i, AX.X), [ex], [nm1])
                kb.op("dve", lambda e, o=nm1[:, 1:2]: e.reciprocal(o, o), [nm1], [nm1])
                kb.ts("dve", g.GATE[:, n, :], ex[:], nm1[:, 1:2], None, ALU.mult, None, [ex, nm1], [g.GATEb])
            kb.dma("sp", g.H2T[bi].ap, h2T[:], [h2T], [g.H2T[bi]])
    kb.barrier()


def phase_moe(g, L, nblk=8):
    kb = g.kb
    NF = MOE_FF // 128
    with ExitStack() as sc:
        w2b = kb.sbuf("w2b", [128, NF, D], BF16, ctx=sc)
        stage = [kb.sbuf("wst%d" % i, [128, D], ctx=sc) for i in range(2)]
        gmlp = kb.sbuf("gmlp", [128, D], ctx=sc)
        kb.dma("sp", gmlp[:], g.gates.ap[L, 1, 0:1, :].to_broadcast([128, D]), [g.gates], [gmlp])
        fgain = kb.sbuf("fgain", [128, D], ctx=sc)
        kb.dma("sp", fgain[:], g.final_norm_g.ap[0:1, :].to_broadcast([128, D]), [g.final_norm_g], [fgain])
        pst = [kb.sbuf("w13s%d" % i, [128, 8, 256], ctx=sc) for i in range(2)]
        psb = [kb.sbuf("w13b%d" % i, [128, 8, 256], BF16, ctx=sc) for i in range(2)]
        h2Ts = [kb.sbuf("h2T%d" % i, [128, 8, 512], BF16, ctx=sc) for i in range(2)]
        gT = kb.sbuf("gT", [128, NF, 512], BF16, ctx=sc)
        w13s = [kb.sbuf("w13_%d" % i, [128, 8, 256], BF16, ctx=sc) for i in range(3)]
        sas = [kb.sbuf("sa%d" % i, [128, 512], ctx=sc) for i in range(2)]
        tmpy = kb.sbuf("tmpy", [128, D], ctx=sc)
        ya = kb.sbuf("ya", [128, D], ctx=sc)
        x1 = kb.sbuf("x1", [128, D], ctx=sc)
        junk = kb.sbuf("junk", [128, D], ctx=sc)
        ssq = kb.sbuf("ssq", [128, 1], ctx=sc)
        psY = [g.ps[2], g.ps[3]]
        psA, psB = [g.ps[4], g.ps[5]], [g.ps[6], g.ps[7]]
        wi = 0
        hi = 0
        def prep_chunk(e, f):
            s_, b_ = pst[f % 2], psb[f % 2]
            kb.dma("sp", s_[:, :, 0:128], g.moe_w1.ap[0, e, :, 128 * f:128 * f + 128].rearrange("(k p) j -> p k j", p=128),
                   [g.moe_w1], [s_])
            kb.dma("sp", s_[:, :, 128:256], g.moe_w3.ap[0, e, :, 128 * f:128 * f + 128].rearrange("(k p) j -> p k j", p=128),
                   [g.moe_w3], [s_])
            kb.copy("dve", b_[:], s_[:], [s_], [b_])
            kb.dma("sp", g.W13x[e % 2][f].ap, b_[:], [b_], [g.W13x[e % 2][f]])

        def w2_chunk(e, k):
            st = stage[k % 2]
            kb.dma("sp", st[:, :], g.moe_w2.ap[0, e, 128 * k:128 * k + 128, :], [g.moe_w2], [st])
            kb.copy("dve", w2b[:, k, :], st[:, :], [st], [w2b])

        for f in range(NF):
            prep_chunk(0, f)
        for e in range(NEXP):
            for bi in range(nblk):
                h2T = h2Ts[hi % 2]
                hi += 1
                kb.dma("sp", h2T[:], g.H2T[bi].ap, [g.H2T[bi]], [h2T])
                for f in range(NF):
                    if bi == 0:
                        w2_chunk(e, f)
                    if bi == 1 and e + 1 < NEXP:
                        prep_chunk(e + 1, f)
                    w13 = w13s[wi % 3]
                    pa, pb, sa = psA[wi % 2], psB[wi % 2], sas[wi % 2]
                    wi += 1
                    kb.dma("sp", w13[:], g.W13x[e % 2][f].ap, [g.W13x[e % 2][f]], [w13])
                    for k in range(8):
                        kb.mm(pa[:, :], w13[:, k, 0:128], h2T[:, k, :], k == 0, k == 7, [w13, h2T], [pa])
                    for k in range(8):
                        kb.mm(pb[:, :], w13[:, k, 128:256], h2T[:, k, :], k == 0, k == 7, [w13, h2T], [pb])
                    kb.act(sa[:, :], pa[:, :], AF.Silu, [pa], [sa])
                    kb.tt("dve", gT[:, f, :], sa[:, :], pb[:, :], ALU.mult, [sa, pb], [gT])
                for j in range(4):
                    n = 4 * bi + j
                    for half in range(2):
                        for f in range(NF):
                            kb.mm(psY[half][:, :], gT[:, f, j * 128:j * 128 + 128], w2b[:, f, 512 * half:512 * half + 512],
                                  f == 0, f == NF - 1, [gT, w2b], [psY[half]])
                    if e > 0:
                        kb.dma("pool", ya[:], g.YACC[n].ap, [g.YACC[n]], [ya])
                    for half in range(2):
                        kb.act(tmpy[:, 512 * half:512 * half + 512], psY[half][:, :], AF.Copy, [psY[half], g.GATEb], [tmpy],
                               scale=g.GATE[:, n, e:e + 1])
                    if e == 0:
                        kb.dma("sp", g.YACC[n].ap, tmpy[:], [tmpy], [g.YACC[n]])
                        continue
                    kb.tt("dve", ya[:], ya[:], tmpy[:], ALU.add, [ya, tmpy], [ya])
                    if e < NEXP - 1:
                        kb.dma("sp", g.YACC[n].ap, ya[:], [ya], [g.YACC[n]])
                        continue
                    kb.dma("sp", x1[:], g.X1S[n].ap, [g.X1S[n]], [x1])
                    kb.tt("dve", ya[:], ya[:], gmlp[:], ALU.mult, [ya, gmlp], [ya])
                    kb.tt("dve", ya[:], ya[:], x1[:], ALU.add, [ya, x1], [ya])
                    kb.act(junk[:], ya[:], AF.Square, [ya, ssq], [junk, ssq], accum_out=ssq[:, 0:1])
                    rstd_from_ssq(kb, ssq[:, 0:1], ssq[:, 0:1], D, [ssq], [ssq])
                    kb.act(junk[:], ya[:], AF.Copy, [ya, ssq], [junk], scale=ssq[:, 0:1])
                    kb.tt("dve", junk[:], junk[:], fgain[:], ALU.mult, [junk, fgain], [junk])
                    kb.dma("sp", g.outb.ap[128 * n:128 * n + 128, :], junk[:], [junk], [g.outb])
    kb.barrier()


W_NAMES = [("mod_w", [DEPTH, D, 6 * D]), ("mod_b", [DEPTH, 6 * D]), ("norm1_g", [DEPTH, D]), ("norm2_g", [DEPTH, D]),
           ("w_in", [DEPTH, D, IN_COLS]), ("conv_w", [DEPTH, 3, 1152]), ("dn_a_log", [DEPTH, 2, 6]), ("dn_dt_bias", [DEPTH, 2, 6]),
           ("dn_norm_g", [DEPTH, 64]), ("q_norm_g", [DEPTH, 64]), ("k_norm_g", [DEPTH, 64]), ("sgu_norm_g", [DEPTH, 256]),
           ("sgu_w", [DEPTH, 4, 128, 128]), ("sgu_b", [DEPTH, 4, 128]), ("w_out", [DEPTH, D, D]),
           ("ffn_w1", [1, D, D_FF]), ("ffn_w3", [1, D, D_FF]), ("ffn_w2", [1, D_FF, D]),
           ("router_w", [1, D, NEXP]), ("router_b", [1, NEXP]), ("moe_w1", [1, NEXP, D, MOE_FF]),
           ("moe_w3", [1, NEXP, D, MOE_FF]), ("moe_w2", [1, NEXP, MOE_FF, D]), ("final_norm_g", [1, D])]
BLOCKS = [(0, 256)] + [(256 + 512 * i, 512) for i in range(8)]


def build_program(phases=None, debug=(), dbg_in=()):
    nc = bass.Bass("TRN2", target_bir_lowering=False)
    g = G()

    def ext_in(name, shape):
        return Buf(name, nc.dram_tensor(name, list(shape), F32, kind="ExternalInput").ap())

    g.xin = ext_in("xin", [T, D])
    g.cvec = ext_in("cvec", [2, D])
    g.rope = ext_in("rope", [NLAT, 64])
    g.identd = ext_in("ident", [128, 128])
    g.dncd = ext_in("dnc", [7, 128, 128])
    for name, shape in W_NAMES:
        setattr(g, name, ext_in(name, shape))
    out = Buf("out", nc.dram_tensor("out", [NLAT // 2, D], F32, kind="ExternalOutput").ap())
    dbg = {}
    for name, shape in debug:
        dbg[name] = Buf(name, nc.dram_tensor(name, list(shape), F32, kind="ExternalOutput").ap())
    for name, shape in dbg_in:
        dbg[name] = Buf(name, nc.dram_tensor(name, list(shape), F32, kind="ExternalInput").ap())
    g.dbg = dbg

    def scratch(name, shape, dt=F32):
        if name in dbg:
            return dbg[name].ap
        return nc.dram_tensor(name + "_s", list(shape), dt, kind="Internal").ap()

    with ExitStack() as ctx:
        import os as _os
        kb = KB(nc, ctx, same_engine_sync=_os.environ.get("SAMEENG", "1") == "1")
        g.kb = kb
        g.ps = [kb.psum("ps%d" % i, [128, 512]) for i in range(8)]
        g.ident = kb.sbuf("ident", [128, 128])
        kb.dma("sp", g.ident[:], g.identd.ap, [g.identd], [g.ident])
        GS = kb.sbuf("GS", [128, DEPTH, 2, 8, 2])
        SH = kb.sbuf("SH", [128, DEPTH, 2, 8, 2])
        g.GS, g.SH, g.GSb, g.SHb = GS.ap, SH.ap, GS, SH
        g.gates = Buf("gates", scratch("gates", [DEPTH, 2, 2, D]))
        tokd = scratch("TOK", [T, TK_W])
        g.tokd = tokd
        g.TOK = [Buf("TOK%d" % t, tokd[128 * t:128 * t + 128, :]) for t in range(NT)]
        g.zqd = scratch("ZQ", [1408, T])
        g.ZQ = [[Buf("ZQ%d_%d" % (fc, bi), g.zqd[128 * fc:128 * fc + 128, b0:b0 + n]) for bi, (b0, n) in enumerate(BLOCKS)]
                for fc in range(11)]
        g.blk_of_tile = lambda t: (0, t * 128) if t < 2 else (1 + (t - 2) // 4, ((t - 2) % 4) * 128)
        g.mixd = scratch("MIXT", [D, T])
        g.MIXdn = [Buf("MIXdn%d" % t, None) for t in range(NT)]
        g.MIXsg = [Buf("MIXsg%d" % t, None) for t in range(NT)]
        g.MIXat = [[Buf("MIXat%d_%d" % (h, bi), g.mixd[384 + 64 * h:384 + 64 * h + 64, b0:b0 + n])
                    for bi, (b0, n) in enumerate(BLOCKS)] for h in range(6)]
        w13d = scratch("W13", [28, 128, 8, 256], BF16)
        g.W13 = [Buf("W13_%d" % f, w13d[f]) for f in range(28)]
        w13x = scratch("W13X", [2, 28, 128, 8, 256], BF16)
        g.W13x = [[Buf("W13x%d_%d" % (i, f), w13x[i, f]) for f in range(28)] for i in range(2)]
        xs = [[Buf("xin%d" % t, g.xin.ap[128 * t:128 * t + 128, :]) for t in range(NT)]]
        for L in range(DEPTH):
            xd = scratch("XS%d" % (L + 1), [T, D])
            xs.append([Buf("xs%d_%d" % (L + 1, t), xd[128 * t:128 * t + 128, :]) for t in range(NT)])
        g.xs = xs
        g.outb = out
        x1d = scratch("X1S", [NLAT, D])
        g.X1S = [Buf("X1S%d" % n, x1d[128 * n:128 * n + 128, :]) for n in range(32)]
        yd = scratch("YACC", [NLAT, D])
        g.YACC = [Buf("YACC%d" % n, yd[128 * n:128 * n + 128, :]) for n in range(32)]
        h2d = scratch("H2T", [8, 128, 8, 512], BF16)
        g.H2T = [Buf("H2T%d" % b, h2d[b]) for b in range(8)]
        GATE = kb.sbuf("GATE", [128, 32, NEXP])
        g.GATE, g.GATEb = GATE.ap, GATE
        allp = ["mod", "a0", "attn0", "sgu0", "dn0", "ffn0", "a1", "attn1", "sgu1", "dn1", "out1", "moe1"]
        phases = allp if phases is None else phases
        if "mod" in phases:
            for L in range(DEPTH):
                phase_mod(g, L)
        if "a0" in phases:
            phase_a(g, 0, xs[0])
        if "attn0" in phases:
            phase_attn(g, 0, True)
        if "sgu0" in phases:
            phase_sgu(g, 0, list(range(NT)))
        if "dn0" in phases:
            phase_dn(g, 0)
        if "ffn0" in phases:
            phase_out_ffn(g, 0, xs[0], xs[1], True)
        if "a1" in phases:
            phase_a(g, 1, xs[1])
        if "attn1" in phases:
            phase_attn(g, 1, False, nblk=4)
        if "sgu1" in phases:
            phase_sgu(g, 1, list(range(2, 18)))
        if "dn1" in phases:
            phase_dn(g, 1, half_mode=True)
        if "out1" in phases:
            phase_out_router(g, 1, xs[1], nblk=4)
        if "moe1" in phases:
            phase_moe(g, 1, nblk=4)
        kb.barrier()
        kb.emit()
        g.n_ins = kb.n_ins
    return nc, g


def make_inputs(inputs):
    x = np.asarray(inputs["x"], np.float32)
    ctxa = np.asarray(inputs["ctx"], np.float32)
    rows = NLAT // 64
    row = np.repeat(np.arange(rows, dtype=np.float32), 64)
    col = np.tile(np.arange(64, dtype=np.float32), rows)
    inv = (10000.0 ** (-2.0 * np.arange(8, dtype=np.float32) / 32)).astype(np.float32)
    inv = (np.float32(10000.0) ** (-2.0 * np.arange(16, dtype=np.float32) / np.float32(32))).astype(np.float32)
    ang = np.stack([row[:, None] * inv, col[:, None] * inv], axis=1).astype(np.float32)
    rope = np.concatenate([np.cos(ang).reshape(NLAT, 32), np.sin(ang).reshape(NLAT, 32)], axis=1).astype(np.float32)
    shared = {k: np.ascontiguousarray(np.asarray(inputs[k], np.float32)).reshape(shp) for k, shp in W_NAMES}
    perm = np.concatenate([np.arange(C_AQ + 64 * h, C_AQ + 64 * h + 64) for h in (0, 3, 1, 4, 2, 5)])
    cols = np.arange(IN_COLS)
    cols[C_AQ:C_AQ + 384] = perm
    shared["w_in"] = np.ascontiguousarray(shared["w_in"][:, :, cols])
    shared["rope"] = rope
    shared["ident"] = np.eye(128, dtype=np.float32)
    j = np.arange(128)[:, None]
    i = np.arange(128)[None, :]
    blk = (j // 64) == (i // 64)
    dnc = np.zeros((7, 128, 128), np.float32)
    dnc[0] = blk
    dnc[1] = blk & (j <= i)
    dnc[2] = blk & (j >= i)
    dnc[3] = np.where(blk & (i >= j), 0.0, -30000.0)
    dnc[4] = np.where(blk & (i <= j), 0.0, -30000.0)
    dnc[5] = blk & (i > j)
    dnc[6] = blk & (i < j)
    shared["dnc"] = dnc
    rev = dict(shared)
    cols = np.arange(IN_COLS)
    cols[C_BETA:C_BETA + 6], cols[C_BETA + 6:C_BETA + 12] = np.arange(C_BETA + 6, C_BETA + 12), np.arange(C_BETA, C_BETA + 6)
    cols[C_ALPHA:C_ALPHA + 6], cols[C_ALPHA + 6:C_ALPHA + 12] = np.arange(C_ALPHA + 6, C_ALPHA + 12), np.arange(C_ALPHA, C_ALPHA + 6)
    rev["w_in"] = np.ascontiguousarray(shared["w_in"][:, :, cols])
    rev["conv_w"] = np.ascontiguousarray(shared["conv_w"][:, ::-1, :])
    rev["dn_a_log"] = np.ascontiguousarray(shared["dn_a_log"][:, ::-1, :])
    rev["dn_dt_bias"] = np.ascontiguousarray(shared["dn_dt_bias"][:, ::-1, :])
    rev["sgu_w"] = np.ascontiguousarray(shared["sgu_w"][:, :, ::-1, ::-1])
    rev["sgu_b"] = np.ascontiguousarray(shared["sgu_b"][:, :, ::-1])
    rev["rope"] = np.ascontiguousarray(rope[::-1])
    maps = []
    cv = np.asarray(inputs["c"], np.float32)
    cc = np.asarray(inputs["c_ctx"], np.float32)
    for b in range(4):
        for tw in range(2):
            m = dict(rev if tw else shared)
            if tw:
                m["xin"] = np.ascontiguousarray(np.concatenate([ctxa[b][::-1], x[b][::-1]], axis=0))
            else:
                m["xin"] = np.ascontiguousarray(np.concatenate([ctxa[b], x[b]], axis=0))
            m["cvec"] = np.ascontiguousarray(np.stack([cv[b], cc], 0))
            maps.append(m)
    return maps


def kernel(**inputs):
    nc, g = build_program()
    maps = make_inputs(inputs)
    res = run_bass_kernel_spmd(nc, maps, core_ids=list(range(len(maps))))
    out = np.empty((4, NLAT, D), np.float32)
    for b in range(4):
        out[b, :NLAT // 2] = res.results[2 * b]["out"]
        out[b, NLAT // 2:] = res.results[2 * b + 1]["out"][::-1]
    return out
```

```python
import numpy as np
from contextlib import ExitStack
import concourse.bass as bass
import concourse.mybir as mybir
from concourse.bass_utils import run_bass_kernel_spmd

F32 = mybir.dt.float32
BF16 = mybir.dt.bfloat16
AF = mybir.ActivationFunctionType
ALU = mybir.AluOpType
AX = mybir.AxisListType

D = 1024
NCTX = 256
NLAT = 4096
T = NCTX + NLAT
NT = T // 128
DEPTH = 2
HD = 64
IN_COLS = 2712
D_FF = 2816
MOE_FF = 3584
NEXP = 8
EPS = 1e-6
C_QKV, C_GATE, C_BETA, C_ALPHA, C_AQ, C_AK, C_AV, C_U, C_V = 0, 1152, 1536, 1548, 1560, 1944, 2072, 2200, 2456
TK_GATE, TK_BETA, TK_ALPHA, TK_AQ, TK_AK, TK_AV, TK_ZV, TK_W = 0, 384, 396, 408, 792, 920, 1048, 1304


class Buf:
    __slots__ = ("name", "ap", "last_w", "readers", "excl")

    def __init__(self, name, ap=None, excl=False):
        self.name = name
        self.ap = ap
        self.excl = excl
        self.last_w = None
        self.readers = []

    def __getitem__(self, idx):
        return self.ap[idx]


class KB:
    ENGS = ("pe", "act", "dve", "pool", "sp")
    NDMA = 12

    def __init__(self, nc, ctx, same_engine_sync=True):
        self.nc = nc
        self.ctx = ctx
        self.same = same_engine_sync
        self.ops = {e: [] for e in self.ENGS}
        self.seq = {e: 0 for e in self.ENGS}
        self.sems = {e: ctx.enter_context(nc.semaphore("c_" + e)) for e in self.ENGS}
        self.dma_sems, self.dma_cnt, self.dma_rr = {}, {}, {}
        for q in ("sp", "pool", "act"):
            self.dma_sems[q] = [ctx.enter_context(nc.semaphore("d_%s%d" % (q, i))) for i in range(self.NDMA)]
            self.dma_cnt[q] = [0] * self.NDMA
            self.dma_rr[q] = 0
        self.known = {e: {} for e in self.ENGS}
        import os as _os
        self.pool_dma = _os.environ.get("POOLDMA", "0") == "1"
        self.pool_cmp = _os.environ.get("POOLCMP", "0") == "1"
        self.n_ins = 0
        self.uid = 0

    def sbuf(self, name, shape, dt=F32, ctx=None):
        self.uid += 1
        t = (ctx or self.ctx).enter_context(self.nc.sbuf_tensor("%s_%d" % (name, self.uid), list(shape), dt))
        return Buf(name, t)

    def psum(self, name, shape, dt=F32, ctx=None):
        self.uid += 1
        t = (ctx or self.ctx).enter_context(self.nc.psum_tensor("%s_%d" % (name, self.uid), list(shape), dt))
        return Buf(name, t, excl=True)

    def dram(self, name, shape, dt=F32):
        t = self.nc.dram_tensor(name, list(shape), dt, kind="Internal")
        return Buf(name, t.ap())

    def _need(self, eng, tok, waits):
        if tok is None:
            return
        key, val = tok
        if key == eng and (eng == "pe" or not self.same):
            return
        if val > waits.get(key, 0):
            waits[key] = val

    def _sem(self, key):
        if isinstance(key, str):
            return self.sems[key]
        return self.dma_sems[key[0]][key[1]]

    def op(self, eng, fn, reads=(), writes=(), dma=False):
        if eng == "pool":
            if dma and not self.pool_dma:
                eng = "sp"
            elif not dma and not self.pool_cmp:
                eng = "dve"
        ex = [b for b in reads if b.excl]
        if ex:
            reads = [b for b in reads if not b.excl]
            writes = list(writes) + [b for b in ex if b not in writes]
        waits = {}
        for b in reads:
            self._need(eng, b.last_w, waits)
        for b in writes:
            self._need(eng, b.last_w, waits)
            for r in b.readers:
                self._need(eng, r, waits)
        if dma:
            q = eng
            i = self.dma_rr[q]
            self.dma_rr[q] = (i + 1) % self.NDMA
            key = (q, i)
            prev = self.dma_cnt[q][i]
            if prev > 0 and 16 * prev > waits.get(key, 0):
                waits[key] = 16 * prev
            self.dma_cnt[q][i] = prev + 1
            tok = (key, 16 * (prev + 1))
            inc = 16
        else:
            self.seq[eng] += 1
            tok = (eng, self.seq[eng])
            inc = 1
        kn = self.known[eng]
        wl = []
        for key, val in waits.items():
            if kn.get(key, 0) >= val:
                continue
            kn[key] = val
            wl.append((self._sem(key), val))
        self.n_ins += 1
        self.ops[eng].append((wl, fn, self._sem(tok[0]), inc))
        for b in reads:
            b.readers.append(tok)
        for b in writes:
            b.last_w = tok
            b.readers = []
        return tok

    def barrier(self):
        for e in self.ENGS:
            wl = []
            kn = self.known[e]
            for e2 in self.ENGS:
                if e2 != e and self.seq[e2] > kn.get(e2, 0):
                    kn[e2] = self.seq[e2]
                    wl.append((self.sems[e2], self.seq[e2]))
            if self.same and e != "pe" and self.seq[e] > kn.get(e, 0):
                kn[e] = self.seq[e]
                wl.append((self.sems[e], self.seq[e]))
            for q in self.dma_sems:
                for i in range(self.NDMA):
                    v = 16 * self.dma_cnt[q][i]
                    if v > kn.get((q, i), 0):
                        kn[(q, i)] = v
                        wl.append((self.dma_sems[q][i], v))
            if wl:
                self.ops[e].append((wl, None, None, 0))

    def dma(self, q, out_ap, in_ap, reads=(), writes=(), **kw):
        return self.op(q, lambda e: e.dma_start(out=out_ap, in_=in_ap, **kw), reads, writes, dma=True)

    def mm(self, out, lhsT, rhs, start, stop, reads, writes):
        return self.op("pe", lambda e: e.matmul(out, lhsT, rhs, start=start, stop=stop), reads, writes)

    def tr(self, out, in_, ident, reads, writes):
        return self.op("pe", lambda e: e.transpose(out, in_, ident), reads, writes)

    def act(self, out, in_, func, reads, writes, **kw):
        return self.op("act", lambda e: e.activation(out=out, in_=in_, func=func, **kw), reads, writes)

    def copy(self, eng, out, in_, reads, writes):
        if eng == "act":
            return self.act(out, in_, AF.Copy, reads, writes)
        return self.op(eng, lambda e: e.tensor_copy(out, in_), reads, writes)

    def tt(self, eng, out, in0, in1, op, reads, writes):
        return self.op(eng, lambda e: e.tensor_tensor(out, in0, in1, op), reads, writes)

    def ts(self, eng, out, in0, s1, s2, op0, op1, reads, writes):
        if s2 is None:
            return self.op(eng, lambda e: e.tensor_scalar(out, in0, s1, None, op0), reads, writes)
        return self.op(eng, lambda e: e.tensor_scalar(out, in0, s1, s2, op0, op1), reads, writes)

    def stt(self, eng, out, in0, scalar, in1, op0, op1, reads, writes):
        return self.op(eng, lambda e: e.scalar_tensor_tensor(out, in0, scalar, in1, op0, op1), reads, writes)

    def memset(self, eng, out, val, writes):
        return self.op(eng, lambda e: e.memset(out, val), (), writes)

    def final_wait(self, eng, bufs):
        waits = {}
        for b in bufs:
            self._need("__none__", b.last_w, waits)
        self.ops[eng].append(([(self._sem(k), v) for k, v in waits.items()], None, None, 0))

    def emit(self):
        handles = {"pe": "tensor", "act": "scalar", "dve": "vector", "pool": "gpsimd", "sp": "sync"}
        with self.nc.Block() as block:
            for e in self.ENGS:
                lst = self.ops[e]
                if not lst:
                    continue

                def body(engh, lst=lst):
                    for wl, fn, sem, inc in lst:
                        for s, v in wl:
                            engh.wait_ge(s, v)
                        if fn is not None:
                            fn(engh).then_inc(sem, inc)

                getattr(block, handles[e])(body)


class G:
    pass


def rstd_from_ssq(kb, out, ssq, n, reads, writes):
    kb.act(out, ssq, AF.Sqrt, reads, writes, scale=1.0 / n, bias=EPS)
    kb.op("dve", lambda e: e.reciprocal(out, out), writes, writes)


def phase_mod(g, L):
    kb, nc = g.kb, g.kb.nc
    with ExitStack() as sc:
        cond = kb.sbuf("cond", [128, 8, 2], ctx=sc)
        condbc = kb.sbuf("condbc", [128, 16, 128], ctx=sc)
        mbF = kb.sbuf("mbF", [128, 48], ctx=sc)
        mbrow = kb.sbuf("mbrow", [1, 6144], ctx=sc)
        gF = kb.sbuf("gF", [128, 2, 8], ctx=sc)
        modF = kb.sbuf("modF", [128, 32, 2], ctx=sc)
        grow = kb.sbuf("grow", [1, 512], ctx=sc)
        mw = [kb.sbuf("mw%d" % k, [128, 3072], ctx=sc) for k in range(8)]
        for s in range(2):
            kb.dma("sp", cond[:, :, s], g.cvec.ap[s, :].rearrange("(k p) -> p k", p=128), [g.cvec], [cond],
                   allow_slow_non_contiguous=True)
        kb.act(cond[:], cond[:], AF.Silu, [cond], [cond])
        kb.copy("dve", condbc[:], cond[:].rearrange("p k s -> p (k s)").unsqueeze(2).to_broadcast([128, 16, 128]),
                [cond], [condbc])
        kb.dma("sp", mbF[:], g.mod_b.ap[L, :].rearrange("(c p) -> p c", p=128), [g.mod_b], [mbF],
               allow_slow_non_contiguous=True)
        kb.dma("sp", mbrow[:], g.mod_b.ap[L:L + 1, :], [g.mod_b], [mbrow])
        kb.dma("sp", gF[:, 0, :], g.norm1_g.ap[L, :].rearrange("(c p) -> p c", p=128), [g.norm1_g], [gF],
               allow_slow_non_contiguous=True)
        kb.dma("sp", gF[:, 1, :], g.norm2_g.ap[L, :].rearrange("(c p) -> p c", p=128), [g.norm2_g], [gF],
               allow_slow_non_contiguous=True)
        psF, psB = g.ps[0], g.ps[1]
        for h in range(2):
            for k in range(8):
                kb.dma("sp" if k % 2 == 0 else "pool", mw[k][:],
                       g.mod_w.ap[L, 128 * k:128 * k + 128, 3072 * h:3072 * h + 3072], [g.mod_w], [mw[k]])
            for j in range(16):
                for k in range(8):
                    kb.mm(psF[:, 2 * j:2 * j + 2], mw[k][:, 128 * j:128 * j + 128], cond[:, k, :], k == 0, k == 7,
                          [mw[k], cond], [psF])
            kb.tt("dve", modF[:, 16 * h:16 * h + 16, :], psF[:, 0:32].rearrange("p (j s) -> p j s", s=2),
                  mbF[:, 24 * h:24 * h + 16].unsqueeze(2).to_broadcast([128, 16, 2]), ALU.add, [psF, mbF], [modF])
            for s in range(2):
                for cc in range(2):
                    for k in range(8):
                        kb.mm(psB[:, :], condbc[:, 2 * k + s, :], mw[k][:, 2048 + 512 * cc:2048 + 512 * cc + 512],
                              k == 0, k == 7, [mw[k], condbc], [psB])
                    c0 = 3072 * h + 2048 + 512 * cc
                    kb.tt("dve", grow[0:1, :], psB[0:1, :], mbrow[0:1, c0:c0 + 512], ALU.add, [psB, mbrow], [grow])
                    kb.dma("sp", g.gates.ap[L, h, s:s + 1, 512 * cc:512 * cc + 512], grow[0:1, :], [grow], [g.gates])
        for h in range(2):
            sc_ap = modF[:, 16 * h + 8:16 * h + 16, :]
            kb.ts("dve", g.GS[:, L, h, :, :], sc_ap, 1.0, None, ALU.add, None, [modF], [g.GSb])
            kb.tt("dve", g.GS[:, L, h, :, :], g.GS[:, L, h, :, :], gF[:, h, :].unsqueeze(2).to_broadcast([128, 8, 2]),
                  ALU.mult, [gF, g.GSb], [g.GSb])
            kb.copy("dve", g.SH[:, L, h, :, :], modF[:, 16 * h:16 * h + 8, :], [modF], [g.SHb])
    kb.barrier()


def norm_tile_to_hT(g, L, h, xt, tile_is_ctx, hT_out_aps, hT_buf, scr, also_f32=None, xap=None, f32_buf=None):
    kb = g.kb
    s = 1 if tile_is_ctx else 0
    junk, ssq, xn = scr["junk"], scr["ssq"], scr["xn"]
    if xap is None:
        xap = xt[:]
    kb.act(junk[:], xap, AF.Square, [xt, ssq], [junk, ssq], accum_out=ssq[:, 0:1])
    rstd_from_ssq(kb, ssq[:, 0:1], ssq[:, 0:1], D, [ssq], [ssq])
    kb.act(xn[:], xap, AF.Copy, [xt, ssq], [xn], scale=ssq[:, 0:1])
    pT = scr["psT"]
    for c in range(8):
        kb.tr(pT[c // 4][:, (c % 4) * 128:(c % 4) * 128 + 128], xn[:, c * 128:c * 128 + 128], g.ident[:], [xn, g.ident],
              [pT[c // 4]])
    for c in range(8):
        kb.act(hT_out_aps[c], pT[c // 4][:, (c % 4) * 128:(c % 4) * 128 + 128], AF.Identity, [pT[c // 4], g.GSb, g.SHb],
               [hT_buf], scale=g.GS[:, L, h, c, s:s + 1], bias=g.SH[:, L, h, c, s:s + 1])
        if also_f32 is not None:
            kb.act(also_f32[c], pT[c // 4][:, (c % 4) * 128:(c % 4) * 128 + 128], AF.Identity,
                   [pT[c // 4], g.GSb, g.SHb], [f32_buf], scale=g.GS[:, L, h, c, s:s + 1], bias=g.SH[:, L, h, c, s:s + 1])


def load_cast_weight(g, sc, name, dram_buf, src_ap_fn, nk, ncols, stage_bufs, dst, dst_ap_fn):
    kb = g.kb
    for k in range(nk):
        st = stage_bufs[k % len(stage_bufs)]
        kb.dma("sp" if k % 2 == 0 else "pool", st[:, 0:ncols], src_ap_fn(k), [dram_buf], [st])
        kb.copy("pool" if k % 2 == 0 else "dve", dst_ap_fn(k), st[:, 0:ncols], [st], [dst])


def phase_a(g, L, xs_tiles):
    kb = g.kb
    with ExitStack() as sc:
        winb = kb.sbuf("winb", [128, 8, IN_COLS], BF16, ctx=sc)
        stage = [kb.sbuf("wst%d" % i, [128, IN_COLS], ctx=sc) for i in range(2)]
        load_cast_weight(g, sc, "win", g.w_in, lambda k: g.w_in.ap[L, 128 * k:128 * k + 128, :], 8, IN_COLS, stage, winb,
                         lambda k: winb[:, k, :])
        scr = {"junk": kb.sbuf("junk", [128, 1024], ctx=sc), "ssq": kb.sbuf("ssq", [128, 1], ctx=sc),
               "xn": kb.sbuf("xn", [128, 1024], ctx=sc), "psT": [g.ps[0], g.ps[1]]}
        xts = [kb.sbuf("xt%d" % i, [128, 1024], ctx=sc) for i in range(2)]
        hTs = [kb.sbuf("hT%d" % i, [128, 8, 512], BF16, ctx=sc) for i in range(2)]
        toks = [kb.sbuf("tok%d" % i, [128, TK_W], ctx=sc) for i in range(2)]
        fms = [kb.sbuf("fm%d" % i, [128, 512], ctx=sc) for i in range(3)]
        psTM = [g.ps[2], g.ps[3], g.ps[4]]
        psFM = [g.ps[5], g.ps[6]]
        blocks = [(0, 2)] + [(2 + 4 * i, 4) for i in range(8)]
        ti = 0
        fi = 0
        for bi, (t0, nt) in enumerate(blocks):
            hT = hTs[bi % 2]
            ntok = nt * 128
            for j in range(nt):
                t = t0 + j
                xt = xts[ti % 2]
                tok = toks[ti % 2]
                ti += 1
                kb.dma("sp", xt[:], xs_tiles[t].ap, [xs_tiles[t]], [xt])
                norm_tile_to_hT(g, L, 0, xt, t < 2, [hT[:, c, j * 128:j * 128 + 128] for c in range(8)], hT, scr)
                segs = [(psTM[0], 0, 512, C_GATE), (psTM[1], 0, 512, C_GATE + 512), (psTM[2], 0, 24, C_GATE + 1024),
                        (psTM[2], 24, 256, C_V)]
                for ps, o0, w, c0 in segs:
                    for k in range(8):
                        kb.mm(ps[:, o0:o0 + w], hT[:, k, j * 128:j * 128 + 128], winb[:, k, c0:c0 + w], k == 0, k == 7,
                              [hT, winb], [ps])
                kb.copy("dve", tok[:, 0:512], psTM[0][:, :], [psTM[0]], [tok])
                kb.copy("act", tok[:, 512:1024], psTM[1][:, :], [psTM[1]], [tok])
                kb.copy("dve", tok[:, 1024:1304], psTM[2][:, 0:280], [psTM[2]], [tok])
                kb.dma("sp", g.TOK[t].ap, tok[:], [tok], [g.TOK[t]])
            for fc in range(11):
                c0 = C_QKV + 128 * fc if fc < 9 else C_U + 128 * (fc - 9)
                ps = psFM[fi % 2]
                fm = fms[fi % 3]
                fi += 1
                for k in range(8):
                    kb.mm(ps[:, 0:ntok], winb[:, k, c0:c0 + 128], hT[:, k, 0:ntok], k == 0, k == 7, [hT, winb], [ps])
                if fc < 9:
                    kb.copy("act" if fc % 2 == 0 else "dve", fm[:, 0:ntok], ps[:, 0:ntok], [ps], [fm])
                else:
                    kb.act(fm[:, 0:ntok], ps[:, 0:ntok], AF.Gelu, [ps], [fm])
                kb.dma("pool", g.ZQ[fc][bi].ap, fm[:, 0:ntok], [fm], [g.ZQ[fc][bi]])
    kb.barrier()


def phase_attn(g, L, with_ctx, nblk=8):
    kb = g.kb
    with ExitStack() as sc:
        QT = [kb.sbuf("QT%d" % p, [128, T], BF16, ctx=sc) for p in range(3)]
        KT = kb.sbuf("KT", [128, T], BF16, ctx=sc)
        VE = kb.sbuf("VE", [128, NT, 128], BF16, ctx=sc)
        ones = kb.sbuf("ones", [128, 64], BF16, ctx=sc)
        gain = kb.sbuf("gain", [128, 8, 64], ctx=sc)
        kb.memset("dve", ones[:], 1.0, [ones])
        kb.dma("sp", gain[:, 0, :], g.q_norm_g.ap[L:L + 1, :].to_broadcast([128, 64]), [g.q_norm_g], [gain])
        kb.dma("sp", gain[:, 6, :], g.k_norm_g.ap[L:L + 1, :].to_broadcast([128, 64]), [g.k_norm_g], [gain])
        kb.ts("dve", gain[:, 0, :], gain[:, 0, :], 0.125, None, ALU.mult, None, [gain], [gain])
        kb.copy("dve", gain[:, 1:6, :], gain[:, 0:1, :].to_broadcast([128, 5, 64]), [gain], [gain])
        kb.copy("dve", gain[:, 7, :], gain[:, 6, :], [gain], [gain])
        qks = [kb.sbuf("qk%d" % i, [128, 512], ctx=sc) for i in range(2)]
        v32s = [kb.sbuf("v32%d" % i, [128, 128], ctx=sc) for i in range(2)]
        css = [kb.sbuf("cs%d" % i, [128, 64], ctx=sc) for i in range(2)]
        sqt = kb.sbuf("sqt", [128, 512], ctx=sc)
        ssq = kb.sbuf("ssq8", [128, 8], ctx=sc)
        qn = kb.sbuf("qn", [128, 512], ctx=sc)
        qr = kb.sbuf("qr", [128, 512], ctx=sc)
        tm = [kb.sbuf("ropet%d" % i, [128, 256], ctx=sc) for i in range(4)]
        psTr = g.ps[0]
        import os as _os
        STG = int(_os.environ.get("ATTN_STG", "9"))
        for t in range(int(_os.environ.get("ATTN_NT", str(NT)))):
            qk, v32, cs = qks[t % 2], v32s[t % 2], css[t % 2]
            kb.dma("sp", qk[:], g.TOK[t].ap[:, TK_AQ:TK_AQ + 512], [g.TOK[t]], [qk])
            kb.dma("pool", v32[:], g.TOK[t].ap[:, TK_AV:TK_AV + 128], [g.TOK[t]], [v32])
            kb.copy("pool", VE[:, t, :], v32[:], [v32], [VE])
            if STG < 2:
                continue
            kb.tt("pool", sqt[:], qk[:], qk[:], ALU.mult, [qk], [sqt])
            kb.op("dve", lambda e, o=ssq[:, 0:8], i=sqt[:].rearrange("p (h d) -> p h d", h=8): e.reduce_sum(o, i, AX.X),
                  [sqt], [ssq])
            rstd_from_ssq(kb, ssq[:, 0:8], ssq[:, 0:8], 64, [ssq], [ssq])
            if STG < 3:
                continue
            q3 = qn[:].rearrange("p (h d) -> p h d", h=8)
            kb.tt("dve", q3, qk[:].rearrange("p (h d) -> p h d", h=8), ssq[:, 0:8].unsqueeze(2).to_broadcast([128, 8, 64]),
                  ALU.mult, [qk, ssq], [qn])
            if STG < 4:
                continue
            if t >= 2:
                kb.tt("pool", q3, q3, gain[:], ALU.mult, [qn, gain], [qn])
                if STG < 5:
                    continue
                kb.dma("sp", cs[:], g.rope.ap[128 * (t - 2):128 * (t - 2) + 128, :], [g.rope], [cs])
                q5 = qn[:].rearrange("p (h a b r) -> p h a b r", h=8, a=2, b=2, r=16)
                o5 = qr[:].rearrange("p (h a b r) -> p h a b r", h=8, a=2, b=2, r=16)
                for ax in [int(v) for v in _os.environ.get("ROPE_AX", "0,1").split(",") if v != ""]:
                    a_, b_ = q5[:, :, ax, 0, :], q5[:, :, ax, 1, :]
                    cos = cs[:, 16 * ax:16 * ax + 16].unsqueeze(1).to_broadcast([128, 8, 16])
                    sin = cs[:, 32 + 16 * ax:32 + 16 * ax + 16].unsqueeze(1).to_broadcast([128, 8, 16])
                    tv = [x[:, 128 * ax:128 * ax + 128].rearrange("p (h r) -> p h r", h=8) for x in tm]
                    kb.tt("dve", tv[0], a_, cos, ALU.mult, [qn, cs], [tm[0]])
                    kb.tt("dve", tv[1], b_, sin, ALU.mult, [qn, cs], [tm[1]])
                    kb.tt("dve", o5[:, :, ax, 0, :], tv[0], tv[1], ALU.subtract, [tm[0], tm[1]], [qr])
                    kb.tt("dve", tv[2], a_, sin, ALU.mult, [qn, cs], [tm[2]])
                    kb.tt("dve", tv[3], b_, cos, ALU.mult, [qn, cs], [tm[3]])
                    kb.tt("dve", o5[:, :, ax, 1, :], tv[2], tv[3], ALU.add, [tm[2], tm[3]], [qr])
            else:
                kb.tt("pool", qr[:].rearrange("p (h d) -> p h d", h=8), q3, gain[:], ALU.mult, [qn, gain], [qr])
            if STG < 6:
                continue
            for p in range(3):
                kb.tr(psTr[:, p * 128:p * 128 + 128], qr[:, p * 128:p * 128 + 128], g.ident[:], [qr, g.ident], [psTr])
            kb.tr(psTr[:, 384:512], qr[:, 384:512], g.ident[:], [qr, g.ident], [psTr])
            if STG < 7:
                continue
            for p in range(3):
                kb.copy("act", QT[p][:, t * 128:t * 128 + 128], psTr[:, p * 128:p * 128 + 128], [psTr], [QT[p]])
            if STG < 8:
                continue
            kb.copy("act", KT[:, t * 128:t * 128 + 128], psTr[:, 384:512], [psTr], [KT])
        import os as _os
        if _os.environ.get("ATTN_PREP_ONLY"):
            kb.barrier()
            return
        psS = [g.ps[1], g.ps[2], g.ps[3]]
        accVs, accDs = [g.ps[4], g.ps[5]], [g.ps[6], g.ps[7]]
        PTs = [kb.sbuf("PT%d" % i, [128, 512], BF16, ctx=sc) for i in range(3)]
        rcs = [kb.sbuf("rc%d" % i, [64, 512], ctx=sc) for i in range(2)]
        ots = [kb.sbuf("ot%d" % i, [64, 512], ctx=sc) for i in range(2)]
        qblocks = ([(0, 0, 256, [0, 1])] if with_ctx else []) + \
                  [(1 + i, 256 + 512 * i, 512, list(range(NT))) for i in range(nblk)]
        items = []
        bi_ = 0
        for h in range(6):
            for (blk, q0, nq, kts) in qblocks:
                for ii, kt in enumerate(kts):
                    items.append((h, blk, q0, nq, kt, ii == 0, ii == len(kts) - 1, bi_))
                bi_ += 1
        LA = 2

        def emit_qk(i):
            h, blk, q0, nq, kt, first, last, bn = items[i]
            kv, p = h // 3, h % 3
            pr = slice(64 * kv, 64 * kv + 64)
            ps, PT = psS[i % 3], PTs[i % 3]
            kb.mm(ps[:, 0:nq], KT[pr, kt * 128:kt * 128 + 128], QT[p][pr, q0:q0 + nq], True, True, [KT, QT[p]], [ps])
            kb.act(PT[:, 0:nq], ps[:, 0:nq], AF.Exp, [ps], [PT])

        def emit_pv(i):
            h, blk, q0, nq, kt, first, last, bn = items[i]
            kv = h // 3
            PT = PTs[i % 3]
            accV, accD = accVs[bn % 2], accDs[bn % 2]
            kb.mm(accV[0:64, 0:nq], VE[:, kt, 64 * kv:64 * kv + 64], PT[:, 0:nq], first, last, [VE, PT], [accV])
            kb.mm(accD[0:64, 0:nq], ones[:, :], PT[:, 0:nq], first, last, [ones, PT], [accD])
            if last:
                rc, ot = rcs[bn % 2], ots[bn % 2]
                kb.op("dve", lambda e, o=rc[:, 0:nq], i_=accD[0:64, 0:nq]: e.reciprocal(o, i_), [accD], [rc])
                kb.tt("dve", ot[:, 0:nq], accV[0:64, 0:nq], rc[:, 0:nq], ALU.mult, [accV, rc], [ot])
                kb.dma("sp", g.MIXat[h][blk].ap, ot[:, 0:nq], [ot], [g.MIXat[h][blk]])

        n_it = len(items)
        for i in range(n_it + LA):
            if i < n_it:
                emit_qk(i)
            if i - LA >= 0:
                emit_pv(i - LA)
    kb.barrier()


def phase_sgu(g, L, tiles):
    kb = g.kb
    with ExitStack() as sc:
        WsT = kb.sbuf("WsT", [128, 4, 128], BF16, ctx=sc)
        ws32 = kb.sbuf("ws32", [128, 4, 128], ctx=sc)
        SB = kb.sbuf("SBb", [64, 512], ctx=sc)
        sgain = kb.sbuf("sgain", [128, 256], ctx=sc)
        psW = g.ps[0]
        for gi in range(4):
            kb.dma("sp", ws32[:, gi, :], g.sgu_w.ap[L, gi, :, :], [g.sgu_w], [ws32])
        for gi in range(4):
            kb.tr(psW[:, gi * 128:gi * 128 + 128], ws32[:, gi, :], g.ident[:], [ws32, g.ident], [psW])
        kb.copy("dve", WsT[:].rearrange("p g i -> p (g i)"), psW[:, :], [psW], [WsT])
        kb.dma("sp", SB[:], g.sgu_b.ap[L:L + 1, :, :].rearrange("o g i -> o (g i)").to_broadcast([64, 512]), [g.sgu_b], [SB])
        kb.dma("sp", sgain[:], g.sgu_norm_g.ap[L:L + 1, :].to_broadcast([128, 256]), [g.sgu_norm_g], [sgain])
        zvs = [kb.sbuf("zv%d" % i, [128, 256], ctx=sc) for i in range(2)]
        uts = [kb.sbuf("ut%d" % i, [64, 4, 128], ctx=sc) for i in range(2)]
        gv = kb.sbuf("gv", [128, 256], ctx=sc)
        sq = kb.sbuf("sgsq", [128, 256], ctx=sc)
        ss = kb.sbuf("sgss", [128, 4], ctx=sc)
        vb = kb.sbuf("vb", [128, 256], BF16, ctx=sc)
        tmps = [kb.sbuf("sgt%d" % i, [64, 512], ctx=sc) for i in range(2)]
        ress = [kb.sbuf("sgr%d" % i, [64, 512], ctx=sc) for i in range(2)]
        pss = [g.ps[1], g.ps[2]]
        for n, t in enumerate(tiles):
            zv, ut, tmp, res, ps = zvs[n % 2], uts[n % 2], tmps[n % 2], ress[n % 2], pss[n % 2]
            bi, boff = g.blk_of_tile(t)
            kb.dma("sp", zv[:], g.TOK[t].ap[:, TK_ZV:TK_ZV + 256], [g.TOK[t]], [zv])
            kb.dma("pool", ut[:], g.zqd[1152:1408, 128 * t:128 * t + 128].rearrange("(g d) t -> d g t", d=64),
                   [g.ZQ[9][bi], g.ZQ[10][bi]], [ut])
            kb.act(gv[:], zv[:], AF.Gelu_apprx_tanh, [zv], [gv])
            kb.tt("pool", sq[:], gv[:], gv[:], ALU.mult, [gv], [sq])
            kb.op("dve", lambda e, o=ss[:, 0:4], i=sq[:].rearrange("p (g d) -> p g d", g=4): e.reduce_sum(o, i, AX.X),
                  [sq], [ss])
            rstd_from_ssq(kb, ss[:, 0:4], ss[:, 0:4], 64, [ss], [ss])
            kb.tt("dve", gv[:].rearrange("p (g d) -> p g d", g=4), gv[:].rearrange("p (g d) -> p g d", g=4),
                  ss[:, 0:4].unsqueeze(2).to_broadcast([128, 4, 64]), ALU.mult, [gv, ss], [gv])
            kb.tt("pool", vb[:], gv[:], sgain[:], ALU.mult, [gv, sgain], [vb])
            for gi in range(4):
                kb.mm(ps[0:64, gi * 128:gi * 128 + 128], vb[:, gi * 64:gi * 64 + 64], WsT[:, gi, :], True, True, [vb, WsT], [ps])
            kb.tt("dve", tmp[:], ps[0:64, :], SB[:], ALU.add, [ps, SB], [tmp])
            kb.tt("pool", res[:], tmp[:], ut[:].rearrange("d g t -> d (g t)"), ALU.mult, [tmp, ut], [res])
            kb.dma("sp", g.mixd[768:1024, 128 * t:128 * t + 128].rearrange("(g d) t -> d g t", d=64),
                   res[:].rearrange("d (g t) -> d g t", g=4), [res], [g.MIXsg[t]])
    kb.barrier()


class _B:
    pass


def phase_dn(g, L, half_mode=False):
    kb = g.kb
    ident = g.ident
    with ExitStack() as sc:
        def S(name, shape, dt=F32):
            return kb.sbuf(name, shape, dt, ctx=sc)
        dnc = S("dnc", [128, 7, 128])
        kb.dma("sp", dnc[:], g.dncd.ap.rearrange("c p i -> p c i"), [g.dncd], [dnc])
        ones = S("ones32", [128, 128])
        kb.memset("dve", ones[:], 1.0, [ones])
        I4 = S("I4", [128, 4, 128])
        NM4 = [S("NM4%d" % d, [128, 4, 128]) for d in range(2)]
        SM4 = [S("SM4%d" % d, [128, 4, 128]) for d in range(2)]
        for c in range(4):
            kb.copy("dve", I4[:, c, :], ident[:], [ident], [I4])
            for d in range(2):
                kb.copy("dve", NM4[d][:, c, :], dnc[:, 3 + d, :], [dnc], [NM4[d]])
                kb.copy("dve", SM4[d][:, c, :], dnc[:, 5 + d, :], [dnc], [SM4[d]])
        BA = S("BA", [128, 68, 24])
        src = g.tokd[:, TK_BETA:TK_BETA + 24].rearrange("(c i) f -> i c f", i=64)
        kb.dma("sp", BA[0:64, :, :], src, g.TOK, [BA])
        kb.dma("pool", BA[64:128, :, :], src, g.TOK, [BA])
        ZB, ZA = S("ZB", [128, 6, 68]), S("ZA", [128, 6, 68])
        for d in range(2):
            for half in range(2):
                pr = slice(64 * half, 64 * half + 64)
                kb.copy("dve", ZB[pr, 3 * d:3 * d + 3, :], BA[pr, :, 6 * d + half:6 * d + 6:2].rearrange("i c p -> i p c"), [BA], [ZB])
                kb.copy("dve", ZA[pr, 3 * d:3 * d + 3, :],
                        BA[pr, :, 12 + 6 * d + half:12 + 6 * d + 6:2].rearrange("i c p -> i p c"), [BA], [ZA])
        AL, DTB, NEA = S("AL", [128, 6]), S("DTB", [128, 6]), S("NEA", [128, 6])
        for half in range(2):
            pr = slice(64 * half, 64 * half + 64)
            kb.dma("sp", AL[pr, :].rearrange("p (d q) -> p d q", d=2), g.dn_a_log.ap[L:L + 1, :, half::2].to_broadcast([64, 2, 3]),
                   [g.dn_a_log], [AL], allow_slow_non_contiguous=True)
            kb.dma("sp", DTB[pr, :].rearrange("p (d q) -> p d q", d=2), g.dn_dt_bias.ap[L:L + 1, :, half::2].to_broadcast([64, 2, 3]),
                   [g.dn_dt_bias], [DTB], allow_slow_non_contiguous=True)
        kb.act(NEA[:], AL[:], AF.Exp, [AL], [NEA])
        kb.ts("dve", NEA[:], NEA[:], -1.0, None, ALU.mult, None, [NEA], [NEA])
        NBETA, GT, GC, GL, E, KTS, EGL = [S(n, [128, 6, 68]) for n in ("NBETA", "GT", "GC", "GL", "E", "KTS", "EGL")]
        kb.act(NBETA[:], ZB[:], AF.Sigmoid, [ZB], [NBETA])
        kb.ts("dve", NBETA[:], NBETA[:], -1.0, None, ALU.mult, None, [NBETA], [NBETA])
        kb.tt("dve", GT[:], ZA[:], DTB[:].unsqueeze(2).to_broadcast([128, 6, 68]), ALU.add, [ZA, DTB], [GT])
        kb.act(GT[:], GT[:], AF.Exp, [GT], [GT])
        kb.act(GT[:], GT[:], AF.Ln, [GT], [GT], bias=1.0)
        kb.tt("dve", GT[:], GT[:], NEA[:].unsqueeze(2).to_broadcast([128, 6, 68]), ALU.mult, [GT, NEA], [GT])
        ps = g.ps[0]
        kb.mm(ps[:, 0:204], dnc[:, 1, :], GT[:, 0:3, :].rearrange("p a c -> p (a c)"), True, True, [dnc, GT], [ps])
        kb.mm(ps[:, 204:408], dnc[:, 2, :], GT[:, 3:6, :].rearrange("p a c -> p (a c)"), True, True, [dnc, GT], [ps])
        kb.copy("dve", GC[:].rearrange("p a c -> p (a c)"), ps[:, 0:408], [ps], [GC])
        kb.mm(ps[:, 0:408], dnc[:, 0, :], GT[:].rearrange("p a c -> p (a c)"), True, True, [dnc, GT], [ps])
        kb.copy("dve", GL[:].rearrange("p a c -> p (a c)"), ps[:, 0:408], [ps], [GL])
        kb.act(E[:], GC[:], AF.Exp, [GC], [E])
        kb.act(EGL[:], GL[:], AF.Exp, [GL], [EGL])
        kb.tt("dve", KTS[:], GL[:], GC[:], ALU.subtract, [GL, GC], [KTS])
        kb.act(KTS[:], KTS[:], AF.Exp, [KTS], [KTS])
        cw = S("cw", [128, 9, 3])
        for fc in range(9):
            kb.dma("sp", cw[:, fc, :], g.conv_w.ap[L, :, 128 * fc:128 * fc + 128].rearrange("k p -> p k"), [g.conv_w], [cw],
                   allow_slow_non_contiguous=True)
        dgain = S("dgain", [64, 1])
        kb.dma("sp", dgain[:], g.dn_norm_g.ap[L, :].rearrange("(e o) -> e o", o=1), [g.dn_norm_g], [dgain],
               allow_slow_non_contiguous=True)
        qn, kn, vn, zr = S("dqn", [128, T]), S("dkn", [128, T]), S("dvn", [128, T]), S("zraw", [128, T])
        sqb = S("dsqb", [128, 512])
        rsb = S("drsb", [128, 512])
        OB = S("OB", [128, 68, 64])
        bufs = []
        for d in range(2):
            B = _B()
            for n in ("kT", "qT", "vT", "kBD", "diag", "D", "attnT", "N", "NT", "P2", "PT2", "R", "kt", "tmp"):
                setattr(B, n, S("%s%d" % (n, d), [128, 4, 128]))
            B.vst = S("vst%d" % d, [128, 4, 64])
            B.ntmp, B.vnew, B.o1 = S("ntmp%d" % d, [128, 64]), S("vnew%d" % d, [128, 64]), S("o1%d" % d, [128, 64])
            B.S = [S("S%d_%d" % (d, i), [128, 64]) for i in range(2)]
            B.banks = g.ps[4 * d:4 * d + 4]
            for t_ in (B.kT, B.qT, B.vT):
                kb.memset("dve", t_[:], 0.0, [t_])
            bufs.append(B)
        gts = [S("dgt%d" % i, [128, 4, 64]) for i in range(2)]
        otb = [S("dot%d" % i, [64, 512]) for i in range(2)]
        oss = S("doss", [128, 4])
        f2 = lambda ap: ap.rearrange("p c i -> p (c i)")

        def conv_gen(pp):
                for xi, dst in enumerate((qn, kn, vn)):
                    fc = 3 * xi + pp
                    for bi, (b0, n) in enumerate(BLOCKS):
                        kb.dma("sp" if bi % 2 == 0 else "pool", zr[:, b0:b0 + n], g.ZQ[fc][bi].ap, [g.ZQ[fc][bi]], [zr])
                        yield
                    kb.act(dst[:], zr[:], AF.Copy, [zr, cw], [dst], scale=cw[:, fc, 1:2])
                    yield
                    for (a0, a1) in ((0, NCTX), (NCTX, T)):
                        kb.stt("dve", dst[:, a0 + 1:a1], zr[:, a0:a1 - 1], cw[:, fc, 0:1], dst[:, a0 + 1:a1], ALU.mult, ALU.add,
                               [zr, cw, dst], [dst])
                        yield
                        kb.stt("dve", dst[:, a0:a1 - 1], zr[:, a0 + 1:a1], cw[:, fc, 2:3], dst[:, a0:a1 - 1], ALU.mult, ALU.add,
                               [zr, cw, dst], [dst])
                        yield
                    kb.act(dst[:], dst[:], AF.Silu, [dst], [dst])
                    yield
                    if xi < 2:
                        for (b0, n) in BLOCKS:
                            kb.tt("dve", sqb[:, 0:n], dst[:, b0:b0 + n], dst[:, b0:b0 + n], ALU.mult, [dst], [sqb])
                            yield
                            kb.mm(g.ps[5][:, 0:n], dnc[:, 0, :], sqb[:, 0:n], True, True, [dnc, sqb], [g.ps[5]])
                            yield
                            sc_ = 64.0 if xi == 0 else 1.0
                            kb.act(rsb[:, 0:n], g.ps[5][:, 0:n], AF.Sqrt, [g.ps[5]], [rsb], scale=sc_, bias=sc_ * EPS)
                            yield
                            kb.op("dve", lambda e, o=rsb[:, 0:n]: e.reciprocal(o, o), [rsb], [rsb])
                            yield
                            kb.tt("dve", dst[:, b0:b0 + n], dst[:, b0:b0 + n], rsb[:, 0:n], ALU.mult, [dst, rsb], [dst])
                            yield

        def out_gen(pp):
                pO = g.ps[0]
                for grp in (range(1, 9) if half_mode else range(17)):
                    gt, ot = gts[grp % 2], otb[grp % 2]
                    c0, t0 = 4 * grp, 256 * grp
                    for ab in range(2):
                        h = 2 * pp + ab
                        kb.dma("sp" if ab == 0 else "pool", gt[64 * ab:64 * ab + 64, :, :],
                               g.tokd[t0:t0 + 256, TK_GATE + 64 * h:TK_GATE + 64 * h + 64].rearrange("(c i) e -> i c e", i=64),
                               [g.TOK[2 * grp], g.TOK[2 * grp + 1]], [gt])
                        yield
                    kb.act(gt[:], gt[:], AF.Silu, [gt], [gt])
                    yield
                    ob = OB[:, c0:c0 + 4, :]
                    tmp3 = bufs[0].tmp[:, 0:2, :].rearrange("p a (b e) -> p (a b) e", e=64)
                    kb.tt("dve", tmp3, ob, ob, ALU.mult, [OB], [bufs[0].tmp])
                    yield
                    kb.op("dve", lambda e, o=oss[:, 0:4], i=tmp3: e.reduce_sum(o, i, AX.X), [bufs[0].tmp], [oss])
                    yield
                    rstd_from_ssq(kb, oss[:, 0:4], oss[:, 0:4], 64, [oss], [oss])
                    kb.tt("dve", ob, ob, oss[:, 0:4].unsqueeze(2).to_broadcast([128, 4, 64]), ALU.mult, [OB, oss], [OB])
                    yield
                    kb.tt("dve", ob, ob, gt[:], ALU.mult, [OB, gt], [OB])
                    yield
                    for c in range(4):
                        kb.tr(pO[0:64, c * 128:c * 128 + 128], OB[:, c0 + c, :], ident[:], [OB, ident], [pO])
                        yield
                    kb.act(ot[:, :], pO[0:64, :], AF.Copy, [pO, dgain], [ot], scale=dgain[:, 0:1])
                    yield
                    for ab in range(2):
                        h = 2 * pp + ab
                        kb.dma("sp" if ab == 0 else "pool",
                               g.mixd[64 * h:64 * h + 64, t0:t0 + 256].rearrange("e (c i) -> e c i", c=4),
                               ot[:, :].rearrange("e (c ab i) -> e c ab i", c=4, ab=2)[:, :, ab, :], [ot],
                               [g.MIXdn[2 * grp], g.MIXdn[2 * grp + 1]])
                        yield

        def run_all(gen):
            for _ in gen:
                pass

        def interleave(ga, gb):
            live = [True, True]
            gs = [ga, gb]
            while any(live):
                for i_ in range(2):
                    if live[i_]:
                        try:
                            next(gs[i_])
                        except StopIteration:
                            live[i_] = False

        run_all(conv_gen(0))
        for pp in range(3):
            for d in range(2):
                kb.memset("dve", bufs[d].S[0][:], 0.0, [bufs[d].S[0]])
            written = set()
            scnt = [0, 0]

            def prep(d, grp):
                B = bufs[d]
                dp, c0, t0 = 3 * d + pp, 4 * grp, 256 * grp
                pA, pB, pC, pD = B.banks
                for dst, src_ in ((B.kT, kn), (B.qT, qn), (B.vT, vn)):
                    for half in range(2):
                        pr = slice(64 * half, 64 * half + 64)
                        kb.copy("dve", dst[pr, :, 64 * half:64 * half + 64],
                                src_[pr, t0:t0 + 256].rearrange("p (c i) -> p c i", c=4), [src_], [dst])
                        yield
                for c in range(4):
                    kb.tr(pA[:, c * 128:c * 128 + 128], B.kT[:, c, :], ident[:], [B.kT, ident], [pA])
                    yield
                kb.copy("act", f2(B.kBD[:]), pA[:, :], [pA], [B.kBD])
                yield
                for c in range(4):
                    kb.tr(pA[:, c * 128:c * 128 + 128], B.vT[:, c, :], ident[:], [B.vT, ident], [pA])
                    yield
                kb.copy("act", f2(B.tmp[:]), pA[:, :], [pA], [B.tmp])
                yield
                kb.tt("dve", B.vst[:], B.tmp[:, :, 0:64], B.tmp[:, :, 64:128], ALU.add, [B.tmp], [B.vst])
                yield
                for c in range(4):
                    kb.mm(pB[:, c * 128:c * 128 + 128], B.kT[:, c, :], B.kT[:, c, :], True, True, [B.kT], [pB])
                    yield
                for c in range(4):
                    kb.mm(pC[:, c * 128:c * 128 + 128], B.kT[:, c, :], B.qT[:, c, :], True, True, [B.kT, B.qT], [pC])
                    yield
                for c in range(4):
                    kb.ts("dve", B.diag[:, c, :], ident[:], GC[:, dp, c0 + c:c0 + c + 1], None, ALU.mult, None, [ident, GC], [B.diag])
                    yield
                kb.mm(pA[:, :], ones[:], f2(B.diag[:]), True, True, [ones, B.diag], [pA])
                yield
                for c in range(4):
                    kb.ts("dve", B.D[:, c, :], pA[:, c * 128:c * 128 + 128], GC[:, dp, c0 + c:c0 + c + 1], 0.0, ALU.subtract, ALU.min,
                          [pA, GC], [B.D])
                    yield
                kb.tt("dve", f2(B.D[:]), f2(B.D[:]), f2(NM4[d][:]), ALU.add, [B.D, NM4[d]], [B.D])
                yield
                kb.act(f2(B.D[:]), f2(B.D[:]), AF.Exp, [B.D], [B.D])
                yield
                kb.tt("dve", f2(B.attnT[:]), pC[:, :], f2(B.D[:]), ALU.mult, [pC, B.D], [B.attnT])
                yield
                kb.tt("dve", f2(B.N[:]), pB[:, :], f2(B.D[:]), ALU.mult, [pB, B.D], [B.N])
                yield
                kb.tt("dve", f2(B.N[:]), f2(B.N[:]), f2(SM4[d][:]), ALU.mult, [B.N, SM4[d]], [B.N])
                yield
                for c in range(4):
                    kb.act(B.N[:, c, :], B.N[:, c, :], AF.Copy, [B.N, NBETA], [B.N], scale=NBETA[:, dp, c0 + c:c0 + c + 1])
                    yield
                for c in range(4):
                    kb.tr(pA[:, c * 128:c * 128 + 128], B.N[:, c, :], ident[:], [B.N, ident], [pA])
                    yield
                kb.copy("act", f2(B.NT[:]), pA[:, :], [pA], [B.NT])
                yield
                kb.tt("dve", f2(B.R[:]), f2(B.N[:]), f2(I4[:]), ALU.add, [B.N, I4], [B.R])
                yield
                P, PT = B.N, B.NT
                for k in range(5):
                    Pn, PTn = (B.P2, B.PT2) if k % 2 == 0 else (B.N, B.NT)
                    for c in range(4):
                        kb.mm(pC[:, c * 128:c * 128 + 128], P[:, c, :], PT[:, c, :], True, True, [P, PT], [pC])
                        yield
                    if k < 4:
                        for c in range(4):
                            kb.mm(pB[:, c * 128:c * 128 + 128], PT[:, c, :], P[:, c, :], True, True, [P, PT], [pB])
                            yield
                    kb.copy("act", f2(PTn[:]), pC[:, :], [pC], [PTn])
                    yield
                    if k < 4:
                        kb.copy("dve", f2(Pn[:]), pB[:, :], [pB], [Pn])
                        yield
                    for c in range(4):
                        kb.mm(pA[:, c * 128:c * 128 + 128], PTn[:, c, :], B.R[:, c, :], True, True, [PTn, B.R], [pA])
                        yield
                    kb.tt("dve", f2(B.R[:]), f2(B.R[:]), pA[:, :], ALU.add, [B.R, pA], [B.R])
                    yield
                    P, PT = Pn, PTn
                for c in range(4):
                    kb.act(B.kt[:, c, :], B.kBD[:, c, :], AF.Copy, [B.kBD, KTS], [B.kt], scale=KTS[:, dp, c0 + c:c0 + c + 1])
                    yield

            def scan(d, grp):
                B = bufs[d]
                dp, c0 = 3 * d + pp, 4 * grp
                pD = B.banks[3]
                for c in (range(4) if d == 0 else range(3, -1, -1)):
                    ch = c0 + c
                    S_old, S_new = B.S[scnt[d] % 2], B.S[(scnt[d] + 1) % 2]
                    scnt[d] += 1
                    kb.mm(pD[:, 0:64], B.kT[:, c, :], S_old[:], True, True, [B.kT, S_old], [pD])
                    yield
                    kb.stt("dve", B.ntmp[:], pD[:, 0:64], E[:, dp, ch:ch + 1], B.vst[:, c, :], ALU.mult, ALU.subtract,
                           [pD, E, B.vst], [B.ntmp])
                    yield
                    kb.mm(pD[:, 64:128], B.R[:, c, :], B.ntmp[:], True, True, [B.R, B.ntmp], [pD])
                    yield
                    kb.act(B.vnew[:], pD[:, 64:128], AF.Copy, [pD, NBETA], [B.vnew], scale=NBETA[:, dp, ch:ch + 1])
                    yield
                    need_o = not (half_mode and (ch >= 36 or ch < 4))
                    if need_o:
                        kb.mm(pD[:, 128:192], B.qT[:, c, :], S_old[:], True, True, [B.qT, S_old], [pD])
                        yield
                        kb.act(B.o1[:], pD[:, 128:192], AF.Copy, [pD, E], [B.o1], scale=E[:, dp, ch:ch + 1])
                        yield
                        kb.mm(pD[:, 192:256], B.attnT[:, c, :], B.vnew[:], True, True, [B.attnT, B.vnew], [pD])
                        yield
                    if not need_o:
                        pass
                    elif ch not in written:
                        written.add(ch)
                        kb.tt("dve", OB[:, ch, :], B.o1[:], pD[:, 192:256], ALU.add, [B.o1, pD], [OB])
                        yield
                    else:
                        kb.tt("dve", B.o1[:], B.o1[:], pD[:, 192:256], ALU.add, [B.o1, pD], [B.o1])
                        yield
                        kb.tt("dve", OB[:, ch, :], OB[:, ch, :], B.o1[:], ALU.add, [OB, B.o1], [OB])
                        yield
                    kb.mm(pD[:, 256:320], B.kt[:, c, :], B.vnew[:], True, True, [B.kt, B.vnew], [pD])
                    yield
                    kb.stt("dve", S_new[:], S_old[:], EGL[:, dp, ch:ch + 1], pD[:, 256:320], ALU.mult, ALU.add,
                           [S_old, EGL, pD], [S_new])
                    yield

            order = [list(range(17)), [0] + list(range(16, 0, -1))]
            def stream(d):
                for it in range(9 if (half_mode and d == 0) else 17):
                    yield from prep(d, order[d][it])
                    yield from scan(d, order[d][it])

            gens = [stream(0), stream(1)]
            alive = [True, True]
            nstep = [1, 2] if half_mode else [1, 1]
            while any(alive):
                for d in range(2):
                    for _ in range(nstep[d]):
                        if alive[d]:
                            try:
                                next(gens[d])
                            except StopIteration:
                                alive[d] = False
            if pp < 2:
                interleave(out_gen(pp), conv_gen(pp + 1))
            else:
                run_all(out_gen(pp))
    kb.barrier()


def prep_w13(g, wa, wb, W13, nf, sc):
    kb = g.kb
    st = [kb.sbuf("w13s%d" % i, [128, 8, 256], ctx=sc) for i in range(2)]
    sb = [kb.sbuf("w13b%d" % i, [128, 8, 256], BF16, ctx=sc) for i in range(2)]
    for f in range(nf):
        s_, b_ = st[f % 2], sb[f % 2]
        kb.dma("sp", s_[:, :, 0:128], wa[0][:, 128 * f:128 * f + 128].rearrange("(k p) j -> p k j", p=128), [wa[1]], [s_])
        kb.dma("pool", s_[:, :, 128:256], wb[0][:, 128 * f:128 * f + 128].rearrange("(k p) j -> p k j", p=128), [wb[1]], [s_])
        kb.copy("dve" if f % 2 == 0 else "pool", b_[:], s_[:], [s_], [b_])
        kb.dma("sp", W13[f].ap, b_[:], [b_], [W13[f]])


def phase_out_ffn(g, L, xs_tiles, xo_tiles, do_ctx):
    kb = g.kb
    with ExitStack() as sc:
        with ExitStack() as sc2:
            prep_w13(g, (g.ffn_w1.ap[0], g.ffn_w1), (g.ffn_w3.ap[0], g.ffn_w3), g.W13, 22, sc2)
        kb.barrier()
        woutb = kb.sbuf("woutb", [128, 8, D], BF16, ctx=sc)
        w2b = kb.sbuf("w2b", [128, 22, D], BF16, ctx=sc)
        stage = [kb.sbuf("wst%d" % i, [128, D], ctx=sc) for i in range(2)]
        load_cast_weight(g, sc, "wout", g.w_out, lambda k: g.w_out.ap[L, 128 * k:128 * k + 128, :], 8, D, stage, woutb,
                         lambda k: woutb[:, k, :])
        load_cast_weight(g, sc, "w2", g.ffn_w2, lambda k: g.ffn_w2.ap[0, 128 * k:128 * k + 128, :], 22, D, stage, w2b,
                         lambda k: w2b[:, k, :])
        gmsa = kb.sbuf("gmsa", [128, 2, D], ctx=sc)
        gmlp = kb.sbuf("gmlp", [128, 2, D], ctx=sc)
        for s in range(2):
            kb.dma("sp", gmsa[:, s, :], g.gates.ap[L, 0, s:s + 1, :].to_broadcast([128, D]), [g.gates], [gmsa])
            kb.dma("sp", gmlp[:, s, :], g.gates.ap[L, 1, s:s + 1, :].to_broadcast([128, D]), [g.gates], [gmlp])
        scr = {"junk": kb.sbuf("junk", [128, 1024], ctx=sc), "ssq": kb.sbuf("ssq", [128, 1], ctx=sc),
               "xn": kb.sbuf("xn", [128, 1024], ctx=sc), "psT": [g.ps[0], g.ps[1]]}
        mix32 = [kb.sbuf("mix32_%d" % i, [128, 8, 128], ctx=sc) for i in range(2)]
        mixb = [kb.sbuf("mixb_%d" % i, [128, 8, 128], BF16, ctx=sc) for i in range(2)]
        xts = [kb.sbuf("xt%d" % i, [128, D], ctx=sc) for i in range(2)]
        tmpy = kb.sbuf("tmpy", [128, D], ctx=sc)
        x1blk = [kb.sbuf("x1b%d" % i, [128, 4, D], ctx=sc) for i in range(1)]
        h2Ts = [kb.sbuf("h2T%d" % i, [128, 8, 512], BF16, ctx=sc) for i in range(1)]
        gT = kb.sbuf("gT", [128, 22, 512], BF16, ctx=sc)
        w13s = [kb.sbuf("w13_%d" % i, [128, 8, 256], BF16, ctx=sc) for i in range(3)]
        sas = [kb.sbuf("sa%d" % i, [128, 512], ctx=sc) for i in range(2)]
        x2s = [kb.sbuf("x2_%d" % i, [128, D], ctx=sc) for i in range(2)]
        psY = [g.ps[2], g.ps[3]]
        psA, psB = [g.ps[4], g.ps[5]], [g.ps[6], g.ps[7]]
        blocks = ([(0, 0, 2)] if do_ctx else []) + [(1 + i, 2 + 4 * i, 4) for i in range(8)]
        ti = 0
        wi = 0
        for bn, (bi, t0, nt) in enumerate(blocks):
            ntok = nt * 128
            x1b, h2T = x1blk[0], h2Ts[0]
            s = 1 if bi == 0 else 0
            for j in range(nt):
                t = t0 + j
                m32, mb, xt = mix32[ti % 2], mixb[ti % 2], xts[ti % 2]
                ti += 1
                kb.dma("sp", m32[:], g.mixd[:, 128 * t:128 * t + 128].rearrange("(c p) t -> p c t", p=128),
                       [g.MIXdn[t], g.MIXsg[t]] + [g.MIXat[h][bi] for h in range(6)], [m32])
                kb.dma("pool", xt[:], xs_tiles[t].ap, [xs_tiles[t]], [xt])
                kb.copy("pool", mb[:], m32[:], [m32], [mb])
                for half in range(2):
                    for k in range(8):
                        kb.mm(psY[half][:, :], mb[:, k, :], woutb[:, k, 512 * half:512 * half + 512], k == 0, k == 7,
                              [mb, woutb], [psY[half]])
                for half in range(2):
                    kb.tt("dve", tmpy[:, 512 * half:512 * half + 512], psY[half][:, :], gmsa[:, s, 512 * half:512 * half + 512],
                          ALU.mult, [psY[half], gmsa], [tmpy])
                kb.tt("pool", x1b[:, j, :], tmpy[:], xt[:], ALU.add, [tmpy, xt], [x1b])
                norm_tile_to_hT(g, L, 1, x1b, bi == 0, [h2T[:, c, j * 128:j * 128 + 128] for c in range(8)], h2T, scr, xap=x1b[:, j, :])
            for f in range(22):
                w13 = w13s[wi % 3]
                pa, pb, sa = psA[wi % 2], psB[wi % 2], sas[wi % 2]
                wi += 1
                kb.dma("sp" if f % 2 == 0 else "pool", w13[:], g.W13[f].ap, [g.W13[f]], [w13])
                for k in range(8):
                    kb.mm(pa[:, 0:ntok], w13[:, k, 0:128], h2T[:, k, 0:ntok], k == 0, k == 7, [w13, h2T], [pa])
                for k in range(8):
                    kb.mm(pb[:, 0:ntok], w13[:, k, 128:256], h2T[:, k, 0:ntok], k == 0, k == 7, [w13, h2T], [pb])
                kb.act(sa[:, 0:ntok], pa[:, 0:ntok], AF.Silu, [pa], [sa])
                kb.tt("dve", gT[:, f, 0:ntok], sa[:, 0:ntok], pb[:, 0:ntok], ALU.mult, [sa, pb], [gT])
            for j in range(nt):
                t = t0 + j
                x2 = x2s[j % 2]
                for half in range(2):
                    for f in range(22):
                        kb.mm(psY[half][:, :], gT[:, f, j * 128:j * 128 + 128], w2b[:, f, 512 * half:512 * half + 512],
                              f == 0, f == 21, [gT, w2b], [psY[half]])
                for half in range(2):
                    kb.tt("dve", tmpy[:, 512 * half:512 * half + 512], psY[half][:, :], gmlp[:, s, 512 * half:512 * half + 512],
                          ALU.mult, [psY[half], gmlp], [tmpy])
                kb.tt("pool", x2[:], tmpy[:], x1b[:, j, :], ALU.add, [tmpy, x1b], [x2])
                kb.dma("sp", xo_tiles[t].ap, x2[:], [x2], [xo_tiles[t]])
    kb.barrier()


def phase_out_router(g, L, xs_tiles, nblk=8):
    kb = g.kb
    with ExitStack() as sc:
        woutb = kb.sbuf("woutb", [128, 8, D], BF16, ctx=sc)
        stage = [kb.sbuf("wst%d" % i, [128, D], ctx=sc) for i in range(2)]
        load_cast_weight(g, sc, "wout", g.w_out, lambda k: g.w_out.ap[L, 128 * k:128 * k + 128, :], 8, D, stage, woutb,
                         lambda k: woutb[:, k, :])
        gmsa = kb.sbuf("gmsa", [128, D], ctx=sc)
        kb.dma("sp", gmsa[:], g.gates.ap[L, 0, 0:1, :].to_broadcast([128, D]), [g.gates], [gmsa])
        rw = kb.sbuf("rw", [128, 8, NEXP], ctx=sc)
        kb.dma("sp", rw[:], g.router_w.ap[0].rearrange("(k p) e -> p k e", p=128), [g.router_w], [rw])
        rb = kb.sbuf("rb", [128, NEXP], ctx=sc)
        kb.dma("sp", rb[:], g.router_b.ap[0:1, :].to_broadcast([128, NEXP]), [g.router_b], [rb])
        scr = {"junk": kb.sbuf("junk", [128, 1024], ctx=sc), "ssq": kb.sbuf("ssq", [128, 1], ctx=sc),
               "xn": kb.sbuf("xn", [128, 1024], ctx=sc), "psT": [g.ps[0], g.ps[1]]}
        mix32 = [kb.sbuf("mix32_%d" % i, [128, 8, 128], ctx=sc) for i in range(2)]
        mixb = [kb.sbuf("mixb_%d" % i, [128, 8, 128], BF16, ctx=sc) for i in range(2)]
        xts = [kb.sbuf("xt%d" % i, [128, D], ctx=sc) for i in range(2)]
        tmpy = kb.sbuf("tmpy", [128, D], ctx=sc)
        x1s = [kb.sbuf("x1_%d" % i, [128, D], ctx=sc) for i in range(2)]
        h2Ts = [kb.sbuf("h2T%d" % i, [128, 8, 512], BF16, ctx=sc) for i in range(2)]
        h32 = kb.sbuf("h32", [128, 8, 128], ctx=sc)
        lg = kb.sbuf("lg", [128, NEXP], ctx=sc)
        mx8 = kb.sbuf("mx8", [128, 8], ctx=sc)
        msk = kb.sbuf("msk", [128, NEXP], ctx=sc)
        ex = kb.sbuf("ex", [128, NEXP], ctx=sc)
        nm1 = kb.sbuf("nm1", [128, 2], ctx=sc)
        psY = [g.ps[2], g.ps[3]]
        psR = g.ps[4]
        for bi in range(nblk):
            h2T = h2Ts[bi % 2]
            for j in range(4):
                n = 4 * bi + j
                t = 2 + n
                m32, mb, xt, x1 = mix32[n % 2], mixb[n % 2], xts[n % 2], x1s[n % 2]
                kb.dma("sp", m32[:], g.mixd[:, 128 * t:128 * t + 128].rearrange("(c p) t -> p c t", p=128),
                       [g.MIXdn[t], g.MIXsg[t]] + [g.MIXat[h][1 + bi] for h in range(6)], [m32])
                kb.dma("pool", xt[:], xs_tiles[t].ap, [xs_tiles[t]], [xt])
                kb.copy("dve", mb[:], m32[:], [m32], [mb])
                for half in range(2):
                    for k in range(8):
                        kb.mm(psY[half][:, :], mb[:, k, :], woutb[:, k, 512 * half:512 * half + 512], k == 0, k == 7,
                              [mb, woutb], [psY[half]])
                for half in range(2):
                    kb.tt("dve", tmpy[:, 512 * half:512 * half + 512], psY[half][:, :], gmsa[:, 512 * half:512 * half + 512],
                          ALU.mult, [psY[half], gmsa], [tmpy])
                kb.tt("dve", x1[:], tmpy[:], xt[:], ALU.add, [tmpy, xt], [x1])
                kb.dma("sp", g.X1S[n].ap, x1[:], [x1], [g.X1S[n]])
                norm_tile_to_hT(g, L, 1, x1, False, [h2T[:, c, j * 128:j * 128 + 128] for c in range(8)], h2T, scr,
                                also_f32=[h32[:, c, :] for c in range(8)], f32_buf=h32)
                for k in range(8):
                    kb.mm(psR[:, 0:NEXP], h32[:, k, :], rw[:, k, :], k == 0, k == 7, [h32, rw], [psR])
                kb.tt("dve", lg[:], psR[:, 0:NEXP], rb[:], ALU.add, [psR, rb], [lg])
                kb.op("dve", lambda e, o=mx8[:], i=lg[:]: e.max(out=o, in_=i), [lg], [mx8])
                kb.ts("dve", msk[:], lg[:], mx8[:, 1:2], None, ALU.is_ge, None, [lg, mx8], [msk])
                kb.ts("dve", nm1[:, 0:1], mx8[:, 0:1], -1.0, None, ALU.mult, None, [mx8], [nm1])
                kb.act(ex[:], lg[:], AF.Exp, [lg, nm1], [ex], bias=nm1[:, 0:1])
                kb.tt("dve", ex[:], ex[:], msk[:], ALU.mult, [ex, msk], [ex])
                kb.op("dve", lambda e, o=nm1[:, 1:2], i=ex[:]: e.reduce_sum(o, i, AX.X), [ex], [nm1])
                kb.op("dve", lambda e, o=nm1[:, 1:2]: e.reciprocal(o, o), [nm1], [nm1])
                kb.ts("dve", g.GATE[:, n, :], ex[:], nm1[:, 1:2], None, ALU.mult, None, [ex, nm1], [g.GATEb])
            kb.dma("sp", g.H2T[bi].ap, h2T[:], [h2T], [g.H2T[bi]])
    kb.barrier()


def phase_moe(g, L, nblk=8):
    kb = g.kb
    NF = MOE_FF // 128
    with ExitStack() as sc:
        w2b = kb.sbuf("w2b", [128, NF, D], BF16, ctx=sc)
        stage = [kb.sbuf("wst%d" % i, [128, D], ctx=sc) for i in range(2)]
        gmlp = kb.sbuf("gmlp", [128, D], ctx=sc)
        kb.dma("sp", gmlp[:], g.gates.ap[L, 1, 0:1, :].to_broadcast([128, D]), [g.gates], [gmlp])
        fgain = kb.sbuf("fgain", [128, D], ctx=sc)
        kb.dma("sp", fgain[:], g.final_norm_g.ap[0:1, :].to_broadcast([128, D]), [g.final_norm_g], [fgain])
        pst = [kb.sbuf("w13s%d" % i, [128, 8, 256], ctx=sc) for i in range(2)]
        psb = [kb.sbuf("w13b%d" % i, [128, 8, 256], BF16, ctx=sc) for i in range(2)]
        h2Ts = [kb.sbuf("h2T%d" % i, [128, 8, 512], BF16, ctx=sc) for i in range(2)]
        gT = kb.sbuf("gT", [128, NF, 512], BF16, ctx=sc)
        w13s = [kb.sbuf("w13_%d" % i, [128, 8, 256], BF16, ctx=sc) for i in range(3)]
        sas = [kb.sbuf("sa%d" % i, [128, 512], ctx=sc) for i in range(2)]
        tmpy = kb.sbuf("tmpy", [128, D], ctx=sc)
        ya = kb.sbuf("ya", [128, D], ctx=sc)
        x1 = kb.sbuf("x1", [128, D], ctx=sc)
        junk = kb.sbuf("junk", [128, D], ctx=sc)
        ssq = kb.sbuf("ssq", [128, 1], ctx=sc)
        psY = [g.ps[2], g.ps[3]]
        psA, psB = [g.ps[4], g.ps[5]], [g.ps[6], g.ps[7]]
        wi = 0
        hi = 0
        def prep_chunk(e, f):
            s_, b_ = pst[f % 2], psb[f % 2]
            kb.dma("sp", s_[:, :, 0:128], g.moe_w1.ap[0, e, :, 128 * f:128 * f + 128].rearrange("(k p) j -> p k j", p=128),
                   [g.moe_w1], [s_])
            kb.dma("sp", s_[:, :, 128:256], g.moe_w3.ap[0, e, :, 128 * f:128 * f + 128].rearrange("(k p) j -> p k j", p=128),
                   [g.moe_w3], [s_])
            kb.copy("dve", b_[:], s_[:], [s_], [b_])
            kb.dma("sp", g.W13x[e % 2][f].ap, b_[:], [b_], [g.W13x[e % 2][f]])

        def w2_chunk(e, k):
            st = stage[k % 2]
            kb.dma("sp", st[:, :], g.moe_w2.ap[0, e, 128 * k:128 * k + 128, :], [g.moe_w2], [st])
            kb.copy("dve", w2b[:, k, :], st[:, :], [st], [w2b])

        for f in range(NF):
            prep_chunk(0, f)
        for e in range(NEXP):
            for bi in range(nblk):
                h2T = h2Ts[hi % 2]
                hi += 1
                kb.dma("sp", h2T[:], g.H2T[bi].ap, [g.H2T[bi]], [h2T])
                for f in range(NF):
                    if bi == 0:
                        w2_chunk(e, f)
                    if bi == 1 and e + 1 < NEXP:
                        prep_chunk(e + 1, f)
                    w13 = w13s[wi % 3]
                    pa, pb, sa = psA[wi % 2], psB[wi % 2], sas[wi % 2]
                    wi += 1
                    kb.dma("sp", w13[:], g.W13x[e % 2][f].ap, [g.W13x[e % 2][f]], [w13])
                    for k in range(8):
                        kb.mm(pa[:, :], w13[:, k, 0:128], h2T[:, k, :], k == 0, k == 7, [w13, h2T], [pa])
                    for k in range(8):
                        kb.mm(pb[:, :], w13[:, k, 128:256], h2T[:, k, :], k == 0, k == 7, [w13, h2T], [pb])
                    kb.act(sa[:, :], pa[:, :], AF.Silu, [pa], [sa])
                    kb.tt("dve", gT[:, f, :], sa[:, :], pb[:, :], ALU.mult, [sa, pb], [gT])
                for j in range(4):
                    n = 4 * bi + j
                    for half in range(2):
                        for f in range(NF):
                            kb.mm(psY[half][:, :], gT[:, f, j * 128:j * 128 + 128], w2b[:, f, 512 * half:512 * half + 512],
                                  f == 0, f == NF - 1, [gT, w2b], [psY[half]])
                    if e > 0:
                        kb.dma("pool", ya[:], g.YACC[n].ap, [g.YACC[n]], [ya])
                    for half in range(2):
                        kb.act(tmpy[:, 512 * half:512 * half + 512], psY[half][:, :], AF.Copy, [psY[half], g.GATEb], [tmpy],
                               scale=g.GATE[:, n, e:e + 1])
                    if e == 0:
                        kb.dma("sp", g.YACC[n].ap, tmpy[:], [tmpy], [g.YACC[n]])
                        continue
                    kb.tt("dve", ya[:], ya[:], tmpy[:], ALU.add, [ya, tmpy], [ya])
                    if e < NEXP - 1:
                        kb.dma("sp", g.YACC[n].ap, ya[:], [ya], [g.YACC[n]])
                        continue
                    kb.dma("sp", x1[:], g.X1S[n].ap, [g.X1S[n]], [x1])
                    kb.tt("dve", ya[:], ya[:], gmlp[:], ALU.mult, [ya, gmlp], [ya])
                    kb.tt("dve", ya[:], ya[:], x1[:], ALU.add, [ya, x1], [ya])
                    kb.act(junk[:], ya[:], AF.Square, [ya, ssq], [junk, ssq], accum_out=ssq[:, 0:1])
                    rstd_from_ssq(kb, ssq[:, 0:1], ssq[:, 0:1], D, [ssq], [ssq])
                    kb.act(junk[:], ya[:], AF.Copy, [ya, ssq], [junk], scale=ssq[:, 0:1])
                    kb.tt("dve", junk[:], junk[:], fgain[:], ALU.mult, [junk, fgain], [junk])
                    kb.dma("sp", g.outb.ap[128 * n:128 * n + 128, :], junk[:], [junk], [g.outb])
    kb.barrier()


W_NAMES = [("mod_w", [DEPTH, D, 6 * D]), ("mod_b", [DEPTH, 6 * D]), ("norm1_g", [DEPTH, D]), ("norm2_g", [DEPTH, D]),
           ("w_in", [DEPTH, D, IN_COLS]), ("conv_w", [DEPTH, 3, 1152]), ("dn_a_log", [DEPTH, 2, 6]), ("dn_dt_bias", [DEPTH, 2, 6]),
           ("dn_norm_g", [DEPTH, 64]), ("q_norm_g", [DEPTH, 64]), ("k_norm_g", [DEPTH, 64]), ("sgu_norm_g", [DEPTH, 256]),
           ("sgu_w", [DEPTH, 4, 128, 128]), ("sgu_b", [DEPTH, 4, 128]), ("w_out", [DEPTH, D, D]),
           ("ffn_w1", [1, D, D_FF]), ("ffn_w3", [1, D, D_FF]), ("ffn_w2", [1, D_FF, D]),
           ("router_w", [1, D, NEXP]), ("router_b", [1, NEXP]), ("moe_w1", [1, NEXP, D, MOE_FF]),
           ("moe_w3", [1, NEXP, D, MOE_FF]), ("moe_w2", [1, NEXP, MOE_FF, D]), ("final_norm_g", [1, D])]
BLOCKS = [(0, 256)] + [(256 + 512 * i, 512) for i in range(8)]


def build_program(phases=None, debug=(), dbg_in=()):
    nc = bass.Bass("TRN2", target_bir_lowering=False)
    g = G()

    def ext_in(name, shape):
        return Buf(name, nc.dram_tensor(name, list(shape), F32, kind="ExternalInput").ap())

    g.xin = ext_in("xin", [T, D])
    g.cvec = ext_in("cvec", [2, D])
    g.rope = ext_in("rope", [NLAT, 64])
    g.identd = ext_in("ident", [128, 128])
    g.dncd = ext_in("dnc", [7, 128, 128])
    for name, shape in W_NAMES:
        setattr(g, name, ext_in(name, shape))
    out = Buf("out", nc.dram_tensor("out", [NLAT // 2, D], F32, kind="ExternalOutput").ap())
    dbg = {}
    for name, shape in debug:
        dbg[name] = Buf(name, nc.dram_tensor(name, list(shape), F32, kind="ExternalOutput").ap())
    for name, shape in dbg_in:
        dbg[name] = Buf(name, nc.dram_tensor(name, list(shape), F32, kind="ExternalInput").ap())
    g.dbg = dbg

    def scratch(name, shape, dt=F32):
        if name in dbg:
            return dbg[name].ap
        return nc.dram_tensor(name + "_s", list(shape), dt, kind="Internal").ap()

    with ExitStack() as ctx:
        import os as _os
        kb = KB(nc, ctx, same_engine_sync=_os.environ.get("SAMEENG", "1") == "1")
        g.kb = kb
        g.ps = [kb.psum("ps%d" % i, [128, 512]) for i in range(8)]
        g.ident = kb.sbuf("ident", [128, 128])
        kb.dma("sp", g.ident[:], g.identd.ap, [g.identd], [g.ident])
        GS = kb.sbuf("GS", [128, DEPTH, 2, 8, 2])
        SH = kb.sbuf("SH", [128, DEPTH, 2, 8, 2])
        g.GS, g.SH, g.GSb, g.SHb = GS.ap, SH.ap, GS, SH
        g.gates = Buf("gates", scratch("gates", [DEPTH, 2, 2, D]))
        tokd = scratch("TOK", [T, TK_W])
        g.tokd = tokd
        g.TOK = [Buf("TOK%d" % t, tokd[128 * t:128 * t + 128, :]) for t in range(NT)]
        g.zqd = scratch("ZQ", [1408, T])
        g.ZQ = [[Buf("ZQ%d_%d" % (fc, bi), g.zqd[128 * fc:128 * fc + 128, b0:b0 + n]) for bi, (b0, n) in enumerate(BLOCKS)]
                for fc in range(11)]
        g.blk_of_tile = lambda t: (0, t * 128) if t < 2 else (1 + (t - 2) // 4, ((t - 2) % 4) * 128)
        g.mixd = scratch("MIXT", [D, T])
        g.MIXdn = [Buf("MIXdn%d" % t, None) for t in range(NT)]
        g.MIXsg = [Buf("MIXsg%d" % t, None) for t in range(NT)]
        g.MIXat = [[Buf("MIXat%d_%d" % (h, bi), g.mixd[384 + 64 * h:384 + 64 * h + 64, b0:b0 + n])
                    for bi, (b0, n) in enumerate(BLOCKS)] for h in range(6)]
        w13d = scratch("W13", [28, 128, 8, 256], BF16)
        g.W13 = [Buf("W13_%d" % f, w13d[f]) for f in range(28)]
        w13x = scratch("W13X", [2, 28, 128, 8, 256], BF16)
        g.W13x = [[Buf("W13x%d_%d" % (i, f), w13x[i, f]) for f in range(28)] for i in range(2)]
        xs = [[Buf("xin%d" % t, g.xin.ap[128 * t:128 * t + 128, :]) for t in range(NT)]]
        for L in range(DEPTH):
            xd = scratch("XS%d" % (L + 1), [T, D])
            xs.append([Buf("xs%d_%d" % (L + 1, t), xd[128 * t:128 * t + 128, :]) for t in range(NT)])
        g.xs = xs
        g.outb = out
        x1d = scratch("X1S", [NLAT, D])
        g.X1S = [Buf("X1S%d" % n, x1d[128 * n:128 * n + 128, :]) for n in range(32)]
        yd = scratch("YACC", [NLAT, D])
        g.YACC = [Buf("YACC%d" % n, yd[128 * n:128 * n + 128, :]) for n in range(32)]
        h2d = scratch("H2T", [8, 128, 8, 512], BF16)
        g.H2T = [Buf("H2T%d" % b, h2d[b]) for b in range(8)]
        GATE = kb.sbuf("GATE", [128, 32, NEXP])
        g.GATE, g.GATEb = GATE.ap, GATE
        allp = ["mod", "a0", "attn0", "sgu0", "dn0", "ffn0", "a1", "attn1", "sgu1", "dn1", "out1", "moe1"]
        phases = allp if phases is None else phases
        if "mod" in phases:
            for L in range(DEPTH):
                phase_mod(g, L)
        if "a0" in phases:
            phase_a(g, 0, xs[0])
        if "attn0" in phases:
            phase_attn(g, 0, True)
        if "sgu0" in phases:
            phase_sgu(g, 0, list(range(NT)))
        if "dn0" in phases:
            phase_dn(g, 0)
        if "ffn0" in phases:
            phase_out_ffn(g, 0, xs[0], xs[1], True)
        if "a1" in phases:
            phase_a(g, 1, xs[1])
        if "attn1" in phases:
            phase_attn(g, 1, False, nblk=4)
        if "sgu1" in phases:
            phase_sgu(g, 1, list(range(2, 18)))
        if "dn1" in phases:
            phase_dn(g, 1, half_mode=True)
        if "out1" in phases:
            phase_out_router(g, 1, xs[1], nblk=4)
        if "moe1" in phases:
            phase_moe(g, 1, nblk=4)
        kb.barrier()
        kb.emit()
        g.n_ins = kb.n_ins
    return nc, g


def make_inputs(inputs):
    x = np.asarray(inputs["x"], np.float32)
    ctxa = np.asarray(inputs["ctx"], np.float32)
    rows = NLAT // 64
    row = np.repeat(np.arange(rows, dtype=np.float32), 64)
    col = np.tile(np.arange(64, dtype=np.float32), rows)
    inv = (10000.0 ** (-2.0 * np.arange(8, dtype=np.float32) / 32)).astype(np.float32)
    inv = (np.float32(10000.0) ** (-2.0 * np.arange(16, dtype=np.float32) / np.float32(32))).astype(np.float32)
    ang = np.stack([row[:, None] * inv, col[:, None] * inv], axis=1).astype(np.float32)
    rope = np.concatenate([np.cos(ang).reshape(NLAT, 32), np.sin(ang).reshape(NLAT, 32)], axis=1).astype(np.float32)
    shared = {k: np.ascontiguousarray(np.asarray(inputs[k], np.float32)).reshape(shp) for k, shp in W_NAMES}
    perm = np.concatenate([np.arange(C_AQ + 64 * h, C_AQ + 64 * h + 64) for h in (0, 3, 1, 4, 2, 5)])
    cols = np.arange(IN_COLS)
    cols[C_AQ:C_AQ + 384] = perm
    shared["w_in"] = np.ascontiguousarray(shared["w_in"][:, :, cols])
    shared["rope"] = rope
    shared["ident"] = np.eye(128, dtype=np.float32)
    j = np.arange(128)[:, None]
    i = np.arange(128)[None, :]
    blk = (j // 64) == (i // 64)
    dnc = np.zeros((7, 128, 128), np.float32)
    dnc[0] = blk
    dnc[1] = blk & (j <= i)
    dnc[2] = blk & (j >= i)
    dnc[3] = np.where(blk & (i >= j), 0.0, -30000.0)
    dnc[4] = np.where(blk & (i <= j), 0.0, -30000.0)
    dnc[5] = blk & (i > j)
    dnc[6] = blk & (i < j)
    shared["dnc"] = dnc
    rev = dict(shared)
    cols = np.arange(IN_COLS)
    cols[C_BETA:C_BETA + 6], cols[C_BETA + 6:C_BETA + 12] = np.arange(C_BETA + 6, C_BETA + 12), np.arange(C_BETA, C_BETA + 6)
    cols[C_ALPHA:C_ALPHA + 6], cols[C_ALPHA + 6:C_ALPHA + 12] = np.arange(C_ALPHA + 6, C_ALPHA + 12), np.arange(C_ALPHA, C_ALPHA + 6)
    rev["w_in"] = np.ascontiguousarray(shared["w_in"][:, :, cols])
    rev["conv_w"] = np.ascontiguousarray(shared["conv_w"][:, ::-1, :])
    rev["dn_a_log"] = np.ascontiguousarray(shared["dn_a_log"][:, ::-1, :])
    rev["dn_dt_bias"] = np.ascontiguousarray(shared["dn_dt_bias"][:, ::-1, :])
    rev["sgu_w"] = np.ascontiguousarray(shared["sgu_w"][:, :, ::-1, ::-1])
    rev["sgu_b"] = np.ascontiguousarray(shared["sgu_b"][:, :, ::-1])
    rev["rope"] = np.ascontiguousarray(rope[::-1])
    maps = []
    cv = np.asarray(inputs["c"], np.float32)
    cc = np.asarray(inputs["c_ctx"], np.float32)
    for b in range(4):
        for tw in range(2):
            m = dict(rev if tw else shared)
            if tw:
                m["xin"] = np.ascontiguousarray(np.concatenate([ctxa[b][::-1], x[b][::-1]], axis=0))
            else:
                m["xin"] = np.ascontiguousarray(np.concatenate([ctxa[b], x[b]], axis=0))
            m["cvec"] = np.ascontiguousarray(np.stack([cv[b], cc], 0))
            maps.append(m)
    return maps


def kernel(**inputs):
    nc, g = build_program()
    maps = make_inputs(inputs)
    res = run_bass_kernel_spmd(nc, maps, core_ids=list(range(len(maps))))
    out = np.empty((4, NLAT, D), np.float32)
    for b in range(4):
        out[b, :NLAT // 2] = res.results[2 * b]["out"]
        out[b, NLAT // 2:] = res.results[2 * b + 1]["out"][::-1]
    return out
```

```python
import numpy as np
from contextlib import ExitStack
import concourse.bass as bass
import concourse.mybir as mybir
from concourse.bass_utils import run_bass_kernel_spmd

F32 = mybir.dt.float32
BF16 = mybir.dt.bfloat16
AF = mybir.ActivationFunctionType
ALU = mybir.AluOpType
AX = mybir.AxisListType

D = 1024
NCTX = 256
NLAT = 4096
T = NCTX + NLAT
NT = T // 128
DEPTH = 2
HD = 64
IN_COLS = 2712
D_FF = 2816
MOE_FF = 3584
NEXP = 8
EPS = 1e-6
C_QKV, C_GATE, C_BETA, C_ALPHA, C_AQ, C_AK, C_AV, C_U, C_V = 0, 1152, 1536, 1548, 1560, 1944, 2072, 2200, 2456
TK_GATE, TK_BETA, TK_ALPHA, TK_AQ, TK_AK, TK_AV, TK_ZV, TK_W = 0, 384, 396, 408, 792, 920, 1048, 1304


class Buf:
    __slots__ = ("name", "ap", "last_w", "readers", "excl")

    def __init__(self, name, ap=None, excl=False):
        self.name = name
        self.ap = ap
        self.excl = excl
        self.last_w = None
        self.readers = []

    def __getitem__(self, idx):
        return self.ap[idx]


class KB:
    ENGS = ("pe", "act", "dve", "pool", "sp")
    NDMA = 12

    def __init__(self, nc, ctx, same_engine_sync=True):
        self.nc = nc
        self.ctx = ctx
        self.same = same_engine_sync
        self.ops = {e: [] for e in self.ENGS}
        self.seq = {e: 0 for e in self.ENGS}
        self.sems = {e: ctx.enter_context(nc.semaphore("c_" + e)) for e in self.ENGS}
        self.dma_sems, self.dma_cnt, self.dma_rr = {}, {}, {}
        for q in ("sp", "pool", "act"):
            self.dma_sems[q] = [ctx.enter_context(nc.semaphore("d_%s%d" % (q, i))) for i in range(self.NDMA)]
            self.dma_cnt[q] = [0] * self.NDMA
            self.dma_rr[q] = 0
        self.known = {e: {} for e in self.ENGS}
        import os as _os
        self.pool_dma = _os.environ.get("POOLDMA", "0") == "1"
        self.pool_cmp = _os.environ.get("POOLCMP", "0") == "1"
        self.n_ins = 0
        self.uid = 0

    def sbuf(self, name, shape, dt=F32, ctx=None):
        self.uid += 1
        t = (ctx or self.ctx).enter_context(self.nc.sbuf_tensor("%s_%d" % (name, self.uid), list(shape), dt))
        return Buf(name, t)

    def psum(self, name, shape, dt=F32, ctx=None):
        self.uid += 1
        t = (ctx or self.ctx).enter_context(self.nc.psum_tensor("%s_%d" % (name, self.uid), list(shape), dt))
        return Buf(name, t, excl=True)

    def dram(self, name, shape, dt=F32):
        t = self.nc.dram_tensor(name, list(shape), dt, kind="Internal")
        return Buf(name, t.ap())

    def _need(self, eng, tok, waits):
        if tok is None:
            return
        key, val = tok
        if key == eng and (eng == "pe" or not self.same):
            return
        if val > waits.get(key, 0):
            waits[key] = val

    def _sem(self, key):
        if isinstance(key, str):
            return self.sems[key]
        return self.dma_sems[key[0]][key[1]]

    def op(self, eng, fn, reads=(), writes=(), dma=False):
        if eng == "pool":
            if dma and not self.pool_dma:
                eng = "sp"
            elif not dma and not self.pool_cmp:
                eng = "dve"
        ex = [b for b in reads if b.excl]
        if ex:
            reads = [b for b in reads if not b.excl]
            writes = list(writes) + [b for b in ex if b not in writes]
        waits = {}
        for b in reads:
            self._need(eng, b.last_w, waits)
        for b in writes:
            self._need(eng, b.last_w, waits)
            for r in b.readers:
                self._need(eng, r, waits)
        if dma:
            q = eng
            i = self.dma_rr[q]
            self.dma_rr[q] = (i + 1) % self.NDMA
            key = (q, i)
            prev = self.dma_cnt[q][i]
            if prev > 0 and 16 * prev > waits.get(key, 0):
                waits[key] = 16 * prev
            self.dma_cnt[q][i] = prev + 1
            tok = (key, 16 * (prev + 1))
            inc = 16
        else:
            self.seq[eng] += 1
            tok = (eng, self.seq[eng])
            inc = 1
        kn = self.known[eng]
        wl = []
        for key, val in waits.items():
            if kn.get(key, 0) >= val:
                continue
            kn[key] = val
            wl.append((self._sem(key), val))
        self.n_ins += 1
        self.ops[eng].append((wl, fn, self._sem(tok[0]), inc))
        for b in reads:
            b.readers.append(tok)
        for b in writes:
            b.last_w = tok
            b.readers = []
        return tok

    def barrier(self):
        for e in self.ENGS:
            wl = []
            kn = self.known[e]
            for e2 in self.ENGS:
                if e2 != e and self.seq[e2] > kn.get(e2, 0):
                    kn[e2] = self.seq[e2]
                    wl.append((self.sems[e2], self.seq[e2]))
            if self.same and e != "pe" and self.seq[e] > kn.get(e, 0):
                kn[e] = self.seq[e]
                wl.append((self.sems[e], self.seq[e]))
            for q in self.dma_sems:
                for i in range(self.NDMA):
                    v = 16 * self.dma_cnt[q][i]
                    if v > kn.get((q, i), 0):
                        kn[(q, i)] = v
                        wl.append((self.dma_sems[q][i], v))
            if wl:
                self.ops[e].append((wl, None, None, 0))

    def dma(self, q, out_ap, in_ap, reads=(), writes=(), **kw):
        return self.op(q, lambda e: e.dma_start(out=out_ap, in_=in_ap, **kw), reads, writes, dma=True)

    def mm(self, out, lhsT, rhs, start, stop, reads, writes):
        return self.op("pe", lambda e: e.matmul(out, lhsT, rhs, start=start, stop=stop), reads, writes)

    def tr(self, out, in_, ident, reads, writes):
        return self.op("pe", lambda e: e.transpose(out, in_, ident), reads, writes)

    def act(self, out, in_, func, reads, writes, **kw):
        return self.op("act", lambda e: e.activation(out=out, in_=in_, func=func, **kw), reads, writes)

    def copy(self, eng, out, in_, reads, writes):
        if eng == "act":
            return self.act(out, in_, AF.Copy, reads, writes)
        return self.op(eng, lambda e: e.tensor_copy(out, in_), reads, writes)

    def tt(self, eng, out, in0, in1, op, reads, writes):
        return self.op(eng, lambda e: e.tensor_tensor(out, in0, in1, op), reads, writes)

    def ts(self, eng, out, in0, s1, s2, op0, op1, reads, writes):
        if s2 is None:
            return self.op(eng, lambda e: e.tensor_scalar(out, in0, s1, None, op0), reads, writes)
        return self.op(eng, lambda e: e.tensor_scalar(out, in0, s1, s2, op0, op1), reads, writes)

    def stt(self, eng, out, in0, scalar, in1, op0, op1, reads, writes):
        return self.op(eng, lambda e: e.scalar_tensor_tensor(out, in0, scalar, in1, op0, op1), reads, writes)

    def memset(self, eng, out, val, writes):
        return self.op(eng, lambda e: e.memset(out, val), (), writes)

    def final_wait(self, eng, bufs):
        waits = {}
        for b in bufs:
            self._need("__none__", b.last_w, waits)
        self.ops[eng].append(([(self._sem(k), v) for k, v in waits.items()], None, None, 0))

    def emit(self):
        handles = {"pe": "tensor", "act": "scalar", "dve": "vector", "pool": "gpsimd", "sp": "sync"}
        with self.nc.Block() as block:
            for e in self.ENGS:
                lst = self.ops[e]
                if not lst:
                    continue

                def body(engh, lst=lst):
                    for wl, fn, sem, inc in lst:
                        for s, v in wl:
                            engh.wait_ge(s, v)
                        if fn is not None:
                            fn(engh).then_inc(sem, inc)

                getattr(block, handles[e])(body)


class G:
    pass


def rstd_from_ssq(kb, out, ssq, n, reads, writes):
    kb.act(out, ssq, AF.Sqrt, reads, writes, scale=1.0 / n, bias=EPS)
    kb.op("dve", lambda e: e.reciprocal(out, out), writes, writes)


def phase_mod(g, L):
    kb, nc = g.kb, g.kb.nc
    with ExitStack() as sc:
        cond = kb.sbuf("cond", [128, 8, 2], ctx=sc)
        condbc = kb.sbuf("condbc", [128, 16, 128], ctx=sc)
        mbF = kb.sbuf("mbF", [128, 48], ctx=sc)
        mbrow = kb.sbuf("mbrow", [1, 6144], ctx=sc)
        gF = kb.sbuf("gF", [128, 2, 8], ctx=sc)
        modF = kb.sbuf("modF", [128, 32, 2], ctx=sc)
        grow = kb.sbuf("grow", [1, 512], ctx=sc)
        mw = [kb.sbuf("mw%d" % k, [128, 3072], ctx=sc) for k in range(8)]
        for s in range(2):
            kb.dma("sp", cond[:, :, s], g.cvec.ap[s, :].rearrange("(k p) -> p k", p=128), [g.cvec], [cond],
                   allow_slow_non_contiguous=True)
        kb.act(cond[:], cond[:], AF.Silu, [cond], [cond])
        kb.copy("dve", condbc[:], cond[:].rearrange("p k s -> p (k s)").unsqueeze(2).to_broadcast([128, 16, 128]),
                [cond], [condbc])
        kb.dma("sp", mbF[:], g.mod_b.ap[L, :].rearrange("(c p) -> p c", p=128), [g.mod_b], [mbF],
               allow_slow_non_contiguous=True)
        kb.dma("sp", mbrow[:], g.mod_b.ap[L:L + 1, :], [g.mod_b], [mbrow])
        kb.dma("sp", gF[:, 0, :], g.norm1_g.ap[L, :].rearrange("(c p) -> p c", p=128), [g.norm1_g], [gF],
               allow_slow_non_contiguous=True)
        kb.dma("sp", gF[:, 1, :], g.norm2_g.ap[L, :].rearrange("(c p) -> p c", p=128), [g.norm2_g], [gF],
               allow_slow_non_contiguous=True)
        psF, psB = g.ps[0], g.ps[1]
        for h in range(2):
            for k in range(8):
                kb.dma("sp" if k % 2 == 0 else "pool", mw[k][:],
                       g.mod_w.ap[L, 128 * k:128 * k + 128, 3072 * h:3072 * h + 3072], [g.mod_w], [mw[k]])
            for j in range(16):
                for k in range(8):
                    kb.mm(psF[:, 2 * j:2 * j + 2], mw[k][:, 128 * j:128 * j + 128], cond[:, k, :], k == 0, k == 7,
                          [mw[k], cond], [psF])
            kb.tt("dve", modF[:, 16 * h:16 * h + 16, :], psF[:, 0:32].rearrange("p (j s) -> p j s", s=2),
                  mbF[:, 24 * h:24 * h + 16].unsqueeze(2).to_broadcast([128, 16, 2]), ALU.add, [psF, mbF], [modF])
            for s in range(2):
                for cc in range(2):
                    for k in range(8):
                        kb.mm(psB[:, :], condbc[:, 2 * k + s, :], mw[k][:, 2048 + 512 * cc:2048 + 512 * cc + 512],
                              k == 0, k == 7, [mw[k], condbc], [psB])
                    c0 = 3072 * h + 2048 + 512 * cc
                    kb.tt("dve", grow[0:1, :], psB[0:1, :], mbrow[0:1, c0:c0 + 512], ALU.add, [psB, mbrow], [grow])
                    kb.dma("sp", g.gates.ap[L, h, s:s + 1, 512 * cc:512 * cc + 512], grow[0:1, :], [grow], [g.gates])
        for h in range(2):
            sc_ap = modF[:, 16 * h + 8:16 * h + 16, :]
            kb.ts("dve", g.GS[:, L, h, :, :], sc_ap, 1.0, None, ALU.add, None, [modF], [g.GSb])
            kb.tt("dve", g.GS[:, L, h, :, :], g.GS[:, L, h, :, :], gF[:, h, :].unsqueeze(2).to_broadcast([128, 8, 2]),
                  ALU.mult, [gF, g.GSb], [g.GSb])
            kb.copy("dve", g.SH[:, L, h, :, :], modF[:, 16 * h:16 * h + 8, :], [modF], [g.SHb])
    kb.barrier()


def norm_tile_to_hT(g, L, h, xt, tile_is_ctx, hT_out_aps, hT_buf, scr, also_f32=None, xap=None, f32_buf=None):
    kb = g.kb
    s = 1 if tile_is_ctx else 0
    junk, ssq, xn = scr["junk"], scr["ssq"], scr["xn"]
    if xap is None:
        xap = xt[:]
    kb.act(junk[:], xap, AF.Square, [xt, ssq], [junk, ssq], accum_out=ssq[:, 0:1])
    rstd_from_ssq(kb, ssq[:, 0:1], ssq[:, 0:1], D, [ssq], [ssq])
    kb.act(xn[:], xap, AF.Copy, [xt, ssq], [xn], scale=ssq[:, 0:1])
    pT = scr["psT"]
    for c in range(8):
        kb.tr(pT[c // 4][:, (c % 4) * 128:(c % 4) * 128 + 128], xn[:, c * 128:c * 128 + 128], g.ident[:], [xn, g.ident],
              [pT[c // 4]])
    for c in range(8):
        kb.act(hT_out_aps[c], pT[c // 4][:, (c % 4) * 128:(c % 4) * 128 + 128], AF.Identity, [pT[c // 4], g.GSb, g.SHb],
               [hT_buf], scale=g.GS[:, L, h, c, s:s + 1], bias=g.SH[:, L, h, c, s:s + 1])
        if also_f32 is not None:
            kb.act(also_f32[c], pT[c // 4][:, (c % 4) * 128:(c % 4) * 128 + 128], AF.Identity,
                   [pT[c // 4], g.GSb, g.SHb], [f32_buf], scale=g.GS[:, L, h, c, s:s + 1], bias=g.SH[:, L, h, c, s:s + 1])


def load_cast_weight(g, sc, name, dram_buf, src_ap_fn, nk, ncols, stage_bufs, dst, dst_ap_fn):
    kb = g.kb
    for k in range(nk):
        st = stage_bufs[k % len(stage_bufs)]
        kb.dma("sp" if k % 2 == 0 else "pool", st[:, 0:ncols], src_ap_fn(k), [dram_buf], [st])
        kb.copy("pool" if k % 2 == 0 else "dve", dst_ap_fn(k), st[:, 0:ncols], [st], [dst])


def phase_a(g, L, xs_tiles):
    kb = g.kb
    with ExitStack() as sc:
        winb = kb.sbuf("winb", [128, 8, IN_COLS], BF16, ctx=sc)
        stage = [kb.sbuf("wst%d" % i, [128, IN_COLS], ctx=sc) for i in range(2)]
        load_cast_weight(g, sc, "win", g.w_in, lambda k: g.w_in.ap[L, 128 * k:128 * k + 128, :], 8, IN_COLS, stage, winb,
                         lambda k: winb[:, k, :])
        scr = {"junk": kb.sbuf("junk", [128, 1024], ctx=sc), "ssq": kb.sbuf("ssq", [128, 1], ctx=sc),
               "xn": kb.sbuf("xn", [128, 1024], ctx=sc), "psT": [g.ps[0], g.ps[1]]}
        xts = [kb.sbuf("xt%d" % i, [128, 1024], ctx=sc) for i in range(2)]
        hTs = [kb.sbuf("hT%d" % i, [128, 8, 512], BF16, ctx=sc) for i in range(2)]
        toks = [kb.sbuf("tok%d" % i, [128, TK_W], ctx=sc) for i in range(2)]
        fms = [kb.sbuf("fm%d" % i, [128, 512], ctx=sc) for i in range(3)]
        psTM = [g.ps[2], g.ps[3], g.ps[4]]
        psFM = [g.ps[5], g.ps[6]]
        blocks = [(0, 2)] + [(2 + 4 * i, 4) for i in range(8)]
        ti = 0
        fi = 0
        for bi, (t0, nt) in enumerate(blocks):
            hT = hTs[bi % 2]
            ntok = nt * 128
            for j in range(nt):
                t = t0 + j
                xt = xts[ti % 2]
                tok = toks[ti % 2]
                ti += 1
                kb.dma("sp", xt[:], xs_tiles[t].ap, [xs_tiles[t]], [xt])
                norm_tile_to_hT(g, L, 0, xt, t < 2, [hT[:, c, j * 128:j * 128 + 128] for c in range(8)], hT, scr)
                segs = [(psTM[0], 0, 512, C_GATE), (psTM[1], 0, 512, C_GATE + 512), (psTM[2], 0, 24, C_GATE + 1024),
                        (psTM[2], 24, 256, C_V)]
                for ps, o0, w, c0 in segs:
                    for k in range(8):
                        kb.mm(ps[:, o0:o0 + w], hT[:, k, j * 128:j * 128 + 128], winb[:, k, c0:c0 + w], k == 0, k == 7,
                              [hT, winb], [ps])
                kb.copy("dve", tok[:, 0:512], psTM[0][:, :], [psTM[0]], [tok])
                kb.copy("act", tok[:, 512:1024], psTM[1][:, :], [psTM[1]], [tok])
                kb.copy("dve", tok[:, 1024:1304], psTM[2][:, 0:280], [psTM[2]], [tok])
                kb.dma("sp", g.TOK[t].ap, tok[:], [tok], [g.TOK[t]])
            for fc in range(11):
                c0 = C_QKV + 128 * fc if fc < 9 else C_U + 128 * (fc - 9)
                ps = psFM[fi % 2]
                fm = fms[fi % 3]
                fi += 1
                for k in range(8):
                    kb.mm(ps[:, 0:ntok], winb[:, k, c0:c0 + 128], hT[:, k, 0:ntok], k == 0, k == 7, [hT, winb], [ps])
                if fc < 9:
                    kb.copy("act" if fc % 2 == 0 else "dve", fm[:, 0:ntok], ps[:, 0:ntok], [ps], [fm])
                else:
                    kb.act(fm[:, 0:ntok], ps[:, 0:ntok], AF.Gelu, [ps], [fm])
                kb.dma("pool", g.ZQ[fc][bi].ap, fm[:, 0:ntok], [fm], [g.ZQ[fc][bi]])
    kb.barrier()


def phase_attn(g, L, with_ctx, nblk=8):
    kb = g.kb
    with ExitStack() as sc:
        QT = [kb.sbuf("QT%d" % p, [128, T], BF16, ctx=sc) for p in range(3)]
        KT = kb.sbuf("KT", [128, T], BF16, ctx=sc)
        VE = kb.sbuf("VE", [128, NT, 2, 128], BF16, ctx=sc)
        gain = kb.sbuf("gain", [128, 8, 64], ctx=sc)
        kb.memset("dve", VE[:], 1.0, [VE])
        kb.dma("sp", gain[:, 0, :], g.q_norm_g.ap[L:L + 1, :].to_broadcast([128, 64]), [g.q_norm_g], [gain])
        kb.dma("sp", gain[:, 6, :], g.k_norm_g.ap[L:L + 1, :].to_broadcast([128, 64]), [g.k_norm_g], [gain])
        kb.ts("dve", gain[:, 0, :], gain[:, 0, :], 0.125, None, ALU.mult, None, [gain], [gain])
        kb.copy("dve", gain[:, 1:6, :], gain[:, 0:1, :].to_broadcast([128, 5, 64]), [gain], [gain])
        kb.copy("dve", gain[:, 7, :], gain[:, 6, :], [gain], [gain])
        qks = [kb.sbuf("qk%d" % i, [128, 512], ctx=sc) for i in range(2)]
        v32s = [kb.sbuf("v32%d" % i, [128, 128], ctx=sc) for i in range(2)]
        css = [kb.sbuf("cs%d" % i, [128, 64], ctx=sc) for i in range(2)]
        sqt = kb.sbuf("sqt", [128, 512], ctx=sc)
        ssq = kb.sbuf("ssq8", [128, 8], ctx=sc)
        qn = kb.sbuf("qn", [128, 512], ctx=sc)
        qr = kb.sbuf("qr", [128, 512], ctx=sc)
        tm = [kb.sbuf("ropet%d" % i, [128, 256], ctx=sc) for i in range(4)]
        psTr = g.ps[0]
        import os as _os
        STG = int(_os.environ.get("ATTN_STG", "9"))
        for t in range(int(_os.environ.get("ATTN_NT", str(NT)))):
            qk, v32, cs = qks[t % 2], v32s[t % 2], css[t % 2]
            kb.dma("sp", qk[:], g.TOK[t].ap[:, TK_AQ:TK_AQ + 512], [g.TOK[t]], [qk])
            kb.dma("pool", v32[:], g.TOK[t].ap[:, TK_AV:TK_AV + 128], [g.TOK[t]], [v32])
            kb.copy("pool", VE[:, t, :, 0:64], v32[:].rearrange("p (k e) -> p k e", k=2), [v32], [VE])
            if STG < 2:
                continue
            kb.tt("pool", sqt[:], qk[:], qk[:], ALU.mult, [qk], [sqt])
            kb.op("dve", lambda e, o=ssq[:, 0:8], i=sqt[:].rearrange("p (h d) -> p h d", h=8): e.reduce_sum(o, i, AX.X),
                  [sqt], [ssq])
            rstd_from_ssq(kb, ssq[:, 0:8], ssq[:, 0:8], 64, [ssq], [ssq])
            if STG < 3:
                continue
            q3 = qn[:].rearrange("p (h d) -> p h d", h=8)
            kb.tt("dve", q3, qk[:].rearrange("p (h d) -> p h d", h=8), ssq[:, 0:8].unsqueeze(2).to_broadcast([128, 8, 64]),
                  ALU.mult, [qk, ssq], [qn])
            if STG < 4:
                continue
            if t >= 2:
                kb.tt("pool", q3, q3, gain[:], ALU.mult, [qn, gain], [qn])
                if STG < 5:
                    continue
                kb.dma("sp", cs[:], g.rope.ap[128 * (t - 2):128 * (t - 2) + 128, :], [g.rope], [cs])
                q5 = qn[:].rearrange("p (h a b r) -> p h a b r", h=8, a=2, b=2, r=16)
                o5 = qr[:].rearrange("p (h a b r) -> p h a b r", h=8, a=2, b=2, r=16)
                for ax in [int(v) for v in _os.environ.get("ROPE_AX", "0,1").split(",") if v != ""]:
                    a_, b_ = q5[:, :, ax, 0, :], q5[:, :, ax, 1, :]
                    cos = cs[:, 16 * ax:16 * ax + 16].unsqueeze(1).to_broadcast([128, 8, 16])
                    sin = cs[:, 32 + 16 * ax:32 + 16 * ax + 16].unsqueeze(1).to_broadcast([128, 8, 16])
                    tv = [x[:, 128 * ax:128 * ax + 128].rearrange("p (h r) -> p h r", h=8) for x in tm]
                    kb.tt("dve", tv[0], a_, cos, ALU.mult, [qn, cs], [tm[0]])
                    kb.tt("dve", tv[1], b_, sin, ALU.mult, [qn, cs], [tm[1]])
                    kb.tt("dve", o5[:, :, ax, 0, :], tv[0], tv[1], ALU.subtract, [tm[0], tm[1]], [qr])
                    kb.tt("dve", tv[2], a_, sin, ALU.mult, [qn, cs], [tm[2]])
                    kb.tt("dve", tv[3], b_, cos, ALU.mult, [qn, cs], [tm[3]])
                    kb.tt("dve", o5[:, :, ax, 1, :], tv[2], tv[3], ALU.add, [tm[2], tm[3]], [qr])
            else:
                kb.tt("pool", qr[:].rearrange("p (h d) -> p h d", h=8), q3, gain[:], ALU.mult, [qn, gain], [qr])
            if STG < 6:
                continue
            for p in range(3):
                kb.tr(psTr[:, p * 128:p * 128 + 128], qr[:, p * 128:p * 128 + 128], g.ident[:], [qr, g.ident], [psTr])
            kb.tr(psTr[:, 384:512], qr[:, 384:512], g.ident[:], [qr, g.ident], [psTr])
            if STG < 7:
                continue
            for p in range(3):
                kb.copy("act", QT[p][:, t * 128:t * 128 + 128], psTr[:, p * 128:p * 128 + 128], [psTr], [QT[p]])
            if STG < 8:
                continue
            kb.copy("act", KT[:, t * 128:t * 128 + 128], psTr[:, 384:512], [psTr], [KT])
        import os as _os
        if _os.environ.get("ATTN_PREP_ONLY"):
            kb.barrier()
            return
        psS = [g.ps[1], g.ps[2], g.ps[3]]
        accVs, accDs = [g.ps[4], g.ps[5]], [g.ps[6], g.ps[7]]
        PTs = [kb.sbuf("PT%d" % i, [128, 512], BF16, ctx=sc) for i in range(3)]
        rcs = [kb.sbuf("rc%d" % i, [64, 512], ctx=sc) for i in range(2)]
        dens = [kb.sbuf("den%d" % i, [128, 512], ctx=sc) for i in range(2)]
        for dn_ in dens:
            kb.memset("dve", dn_[:], 0.0, [dn_])
        ots = [kb.sbuf("ot%d" % i, [64, 512], ctx=sc) for i in range(2)]
        qblocks = ([(0, 0, 256, [0, 1])] if with_ctx else []) + \
                  [(1 + i, 256 + 512 * i, 512, list(range(NT))) for i in range(nblk)]
        items = []
        bi_ = 0
        for h in range(6):
            for (blk, q0, nq, kts) in qblocks:
                for ii, kt in enumerate(kts):
                    items.append((h, blk, q0, nq, kt, ii == 0, ii == len(kts) - 1, bi_))
                bi_ += 1
        LA = 2

        def emit_qk(i):
            h, blk, q0, nq, kt, first, last, bn = items[i]
            kv, p = h // 3, h % 3
            pr = slice(64 * kv, 64 * kv + 64)
            ps, PT = psS[i % 3], PTs[i % 3]
            kb.mm(ps[:, 0:nq], KT[pr, kt * 128:kt * 128 + 128], QT[p][pr, q0:q0 + nq], True, True, [KT, QT[p]], [ps])
            kb.act(PT[:, 0:nq], ps[:, 0:nq], AF.Exp, [ps], [PT])

        def emit_pv(i):
            h, blk, q0, nq, kt, first, last, bn = items[i]
            kv = h // 3
            PT = PTs[i % 3]
            acc, dP = accVs[bn % 2], accDs[bn % 2]
            kb.mm(acc[:, 0:nq], VE[:, kt, kv, :], PT[:, 0:nq], first, last, [VE, PT], [acc])
            if last:
                rc, ot, den = rcs[bn % 2], ots[bn % 2], dens[bn % 2]
                kb.copy("act", den[64:128, 0:nq], acc[64:128, 0:nq], [acc], [den])
                kb.mm(dP[0:64, 0:nq], g.ident[:, 64:128], den[:, 0:nq], True, True, [g.ident, den], [dP])
                kb.op("dve", lambda e, o=rc[:, 0:nq], i_=dP[0:64, 0:nq]: e.reciprocal(o, i_), [dP], [rc])
                kb.tt("dve", ot[:, 0:nq], acc[0:64, 0:nq], rc[:, 0:nq], ALU.mult, [acc, rc], [ot])
                kb.dma("sp", g.MIXat[h][blk].ap, ot[:, 0:nq], [ot], [g.MIXat[h][blk]])

        n_it = len(items)
        for i in range(n_it + LA):
            if i < n_it:
                emit_qk(i)
            if i - LA >= 0:
                emit_pv(i - LA)
    kb.barrier()


def phase_sgu(g, L, tiles):
    kb = g.kb
    with ExitStack() as sc:
        WsT = kb.sbuf("WsT", [128, 4, 128], BF16, ctx=sc)
        ws32 = kb.sbuf("ws32", [128, 4, 128], ctx=sc)
        SB = kb.sbuf("SBb", [64, 512], ctx=sc)
        sgain = kb.sbuf("sgain", [128, 256], ctx=sc)
        psW = g.ps[0]
        for gi in range(4):
            kb.dma("sp", ws32[:, gi, :], g.sgu_w.ap[L, gi, :, :], [g.sgu_w], [ws32])
        for gi in range(4):
            kb.tr(psW[:, gi * 128:gi * 128 + 128], ws32[:, gi, :], g.ident[:], [ws32, g.ident], [psW])
        kb.copy("dve", WsT[:].rearrange("p g i -> p (g i)"), psW[:, :], [psW], [WsT])
        kb.dma("sp", SB[:], g.sgu_b.ap[L:L + 1, :, :].rearrange("o g i -> o (g i)").to_broadcast([64, 512]), [g.sgu_b], [SB])
        kb.dma("sp", sgain[:], g.sgu_norm_g.ap[L:L + 1, :].to_broadcast([128, 256]), [g.sgu_norm_g], [sgain])
        zvs = [kb.sbuf("zv%d" % i, [128, 256], ctx=sc) for i in range(2)]
        uts = [kb.sbuf("ut%d" % i, [64, 4, 128], ctx=sc) for i in range(2)]
        gv = kb.sbuf("gv", [128, 256], ctx=sc)
        sq = kb.sbuf("sgsq", [128, 256], ctx=sc)
        ss = kb.sbuf("sgss", [128, 4], ctx=sc)
        vb = kb.sbuf("vb", [128, 256], BF16, ctx=sc)
        tmps = [kb.sbuf("sgt%d" % i, [64, 512], ctx=sc) for i in range(2)]
        ress = [kb.sbuf("sgr%d" % i, [64, 512], ctx=sc) for i in range(2)]
        pss = [g.ps[1], g.ps[2]]
        for n, t in enumerate(tiles):
            zv, ut, tmp, res, ps = zvs[n % 2], uts[n % 2], tmps[n % 2], ress[n % 2], pss[n % 2]
            bi, boff = g.blk_of_tile(t)
            kb.dma("sp", zv[:], g.TOK[t].ap[:, TK_ZV:TK_ZV + 256], [g.TOK[t]], [zv])
            kb.dma("pool", ut[:], g.zqd[1152:1408, 128 * t:128 * t + 128].rearrange("(g d) t -> d g t", d=64),
                   [g.ZQ[9][bi], g.ZQ[10][bi]], [ut])
            kb.act(gv[:], zv[:], AF.Gelu_apprx_tanh, [zv], [gv])
            kb.tt("pool", sq[:], gv[:], gv[:], ALU.mult, [gv], [sq])
            kb.op("dve", lambda e, o=ss[:, 0:4], i=sq[:].rearrange("p (g d) -> p g d", g=4): e.reduce_sum(o, i, AX.X),
                  [sq], [ss])
            rstd_from_ssq(kb, ss[:, 0:4], ss[:, 0:4], 64, [ss], [ss])
            kb.tt("dve", gv[:].rearrange("p (g d) -> p g d", g=4), gv[:].rearrange("p (g d) -> p g d", g=4),
                  ss[:, 0:4].unsqueeze(2).to_broadcast([128, 4, 64]), ALU.mult, [gv, ss], [gv])
            kb.tt("pool", vb[:], gv[:], sgain[:], ALU.mult, [gv, sgain], [vb])
            for gi in range(4):
                kb.mm(ps[0:64, gi * 128:gi * 128 + 128], vb[:, gi * 64:gi * 64 + 64], WsT[:, gi, :], True, True, [vb, WsT], [ps])
            kb.tt("dve", tmp[:], ps[0:64, :], SB[:], ALU.add, [ps, SB], [tmp])
            kb.tt("pool", res[:], tmp[:], ut[:].rearrange("d g t -> d (g t)"), ALU.mult, [tmp, ut], [res])
            kb.dma("sp", g.mixd[768:1024, 128 * t:128 * t + 128].rearrange("(g d) t -> d g t", d=64),
                   res[:].rearrange("d (g t) -> d g t", g=4), [res], [g.MIXsg[t]])
    kb.barrier()


class _B:
    pass


def phase_dn(g, L, half_mode=False):
    kb = g.kb
    ident = g.ident
    with ExitStack() as sc:
        def S(name, shape, dt=F32):
            return kb.sbuf(name, shape, dt, ctx=sc)
        dnc = S("dnc", [128, 7, 128])
        kb.dma("sp", dnc[:], g.dncd.ap.rearrange("c p i -> p c i"), [g.dncd], [dnc])
        ones = S("ones32", [128, 128])
        kb.memset("dve", ones[:], 1.0, [ones])
        I4 = S("I4", [128, 4, 128])
        NM4 = [S("NM4%d" % d, [128, 4, 128]) for d in range(2)]
        SM4 = [S("SM4%d" % d, [128, 4, 128]) for d in range(2)]
        for c in range(4):
            kb.copy("dve", I4[:, c, :], ident[:], [ident], [I4])
            for d in range(2):
                kb.copy("dve", NM4[d][:, c, :], dnc[:, 3 + d, :], [dnc], [NM4[d]])
                kb.copy("dve", SM4[d][:, c, :], dnc[:, 5 + d, :], [dnc], [SM4[d]])
        BA = S("BA", [128, 68, 24])
        src = g.tokd[:, TK_BETA:TK_BETA + 24].rearrange("(c i) f -> i c f", i=64)
        kb.dma("sp", BA[0:64, :, :], src, g.TOK, [BA])
        kb.dma("pool", BA[64:128, :, :], src, g.TOK, [BA])
        ZB, ZA = S("ZB", [128, 6, 68]), S("ZA", [128, 6, 68])
        for d in range(2):
            for half in range(2):
                pr = slice(64 * half, 64 * half + 64)
                kb.copy("dve", ZB[pr, 3 * d:3 * d + 3, :], BA[pr, :, 6 * d + half:6 * d + 6:2].rearrange("i c p -> i p c"), [BA], [ZB])
                kb.copy("dve", ZA[pr, 3 * d:3 * d + 3, :],
                        BA[pr, :, 12 + 6 * d + half:12 + 6 * d + 6:2].rearrange("i c p -> i p c"), [BA], [ZA])
        AL, DTB, NEA = S("AL", [128, 6]), S("DTB", [128, 6]), S("NEA", [128, 6])
        for half in range(2):
            pr = slice(64 * half, 64 * half + 64)
            kb.dma("sp", AL[pr, :].rearrange("p (d q) -> p d q", d=2), g.dn_a_log.ap[L:L + 1, :, half::2].to_broadcast([64, 2, 3]),
                   [g.dn_a_log], [AL], allow_slow_non_contiguous=True)
            kb.dma("sp", DTB[pr, :].rearrange("p (d q) -> p d q", d=2), g.dn_dt_bias.ap[L:L + 1, :, half::2].to_broadcast([64, 2, 3]),
                   [g.dn_dt_bias], [DTB], allow_slow_non_contiguous=True)
        kb.act(NEA[:], AL[:], AF.Exp, [AL], [NEA])
        kb.ts("dve", NEA[:], NEA[:], -1.0, None, ALU.mult, None, [NEA], [NEA])
        NBETA, GT, GC, GL, E, KTS, EGL = [S(n, [128, 6, 68]) for n in ("NBETA", "GT", "GC", "GL", "E", "KTS", "EGL")]
        kb.act(NBETA[:], ZB[:], AF.Sigmoid, [ZB], [NBETA])
        kb.ts("dve", NBETA[:], NBETA[:], -1.0, None, ALU.mult, None, [NBETA], [NBETA])
        kb.tt("dve", GT[:], ZA[:], DTB[:].unsqueeze(2).to_broadcast([128, 6, 68]), ALU.add, [ZA, DTB], [GT])
        kb.act(GT[:], GT[:], AF.Exp, [GT], [GT])
        kb.act(GT[:], GT[:], AF.Ln, [GT], [GT], bias=1.0)
        kb.tt("dve", GT[:], GT[:], NEA[:].unsqueeze(2).to_broadcast([128, 6, 68]), ALU.mult, [GT, NEA], [GT])
        ps = g.ps[0]
        kb.mm(ps[:, 0:204], dnc[:, 1, :], GT[:, 0:3, :].rearrange("p a c -> p (a c)"), True, True, [dnc, GT], [ps])
        kb.mm(ps[:, 204:408], dnc[:, 2, :], GT[:, 3:6, :].rearrange("p a c -> p (a c)"), True, True, [dnc, GT], [ps])
        kb.copy("dve", GC[:].rearrange("p a c -> p (a c)"), ps[:, 0:408], [ps], [GC])
        kb.mm(ps[:, 0:408], dnc[:, 0, :], GT[:].rearrange("p a c -> p (a c)"), True, True, [dnc, GT], [ps])
        kb.copy("dve", GL[:].rearrange("p a c -> p (a c)"), ps[:, 0:408], [ps], [GL])
        kb.act(E[:], GC[:], AF.Exp, [GC], [E])
        kb.act(EGL[:], GL[:], AF.Exp, [GL], [EGL])
        kb.tt("dve", KTS[:], GL[:], GC[:], ALU.subtract, [GL, GC], [KTS])
        kb.act(KTS[:], KTS[:], AF.Exp, [KTS], [KTS])
        cw = S("cw", [128, 9, 3])
        for fc in range(9):
            kb.dma("sp", cw[:, fc, :], g.conv_w.ap[L, :, 128 * fc:128 * fc + 128].rearrange("k p -> p k"), [g.conv_w], [cw],
                   allow_slow_non_contiguous=True)
        dgain = S("dgain", [64, 1])
        kb.dma("sp", dgain[:], g.dn_norm_g.ap[L, :].rearrange("(e o) -> e o", o=1), [g.dn_norm_g], [dgain],
               allow_slow_non_contiguous=True)
        qn, kn, vn, zr = S("dqn", [128, T]), S("dkn", [128, T]), S("dvn", [128, T]), S("zraw", [128, T])
        sqb = S("dsqb", [128, 512])
        rsb = S("drsb", [128, 512])
        OB = S("OB", [128, 68, 64])
        bufs = []
        for d in range(2):
            B = _B()
            for n in ("kT", "qT", "vT", "kBD", "diag", "D", "attnT", "N", "NT", "P2", "PT2", "R", "kt", "tmp"):
                setattr(B, n, S("%s%d" % (n, d), [128, 4, 128]))
            B.vst = S("vst%d" % d, [128, 4, 64])
            B.ntmp, B.vnew, B.o1 = S("ntmp%d" % d, [128, 64]), S("vnew%d" % d, [128, 64]), S("o1%d" % d, [128, 64])
            B.S = [S("S%d_%d" % (d, i), [128, 64]) for i in range(2)]
            B.banks = g.ps[4 * d:4 * d + 4]
            for t_ in (B.kT, B.qT, B.vT):
                kb.memset("dve", t_[:], 0.0, [t_])
            bufs.append(B)
        gts = [S("dgt%d" % i, [128, 4, 64]) for i in range(2)]
        otb = [S("dot%d" % i, [64, 512]) for i in range(2)]
        oss = S("doss", [128, 4])
        f2 = lambda ap: ap.rearrange("p c i -> p (c i)")

        def conv_gen(pp):
                for xi, dst in enumerate((qn, kn, vn)):
                    fc = 3 * xi + pp
                    for bi, (b0, n) in enumerate(BLOCKS):
                        kb.dma("sp" if bi % 2 == 0 else "pool", zr[:, b0:b0 + n], g.ZQ[fc][bi].ap, [g.ZQ[fc][bi]], [zr])
                        yield
                    kb.act(dst[:], zr[:], AF.Copy, [zr, cw], [dst], scale=cw[:, fc, 1:2])
                    yield
                    for (a0, a1) in ((0, NCTX), (NCTX, T)):
                        kb.stt("dve", dst[:, a0 + 1:a1], zr[:, a0:a1 - 1], cw[:, fc, 0:1], dst[:, a0 + 1:a1], ALU.mult, ALU.add,
                               [zr, cw, dst], [dst])
                        yield
                        kb.stt("dve", dst[:, a0:a1 - 1], zr[:, a0 + 1:a1], cw[:, fc, 2:3], dst[:, a0:a1 - 1], ALU.mult, ALU.add,
                               [zr, cw, dst], [dst])
                        yield
                    kb.act(dst[:], dst[:], AF.Silu, [dst], [dst])
                    yield
                    if xi < 2:
                        for (b0, n) in BLOCKS:
                            kb.tt("dve", sqb[:, 0:n], dst[:, b0:b0 + n], dst[:, b0:b0 + n], ALU.mult, [dst], [sqb])
                            yield
                            kb.mm(g.ps[5][:, 0:n], dnc[:, 0, :], sqb[:, 0:n], True, True, [dnc, sqb], [g.ps[5]])
                            yield
                            sc_ = 64.0 if xi == 0 else 1.0
                            kb.act(rsb[:, 0:n], g.ps[5][:, 0:n], AF.Sqrt, [g.ps[5]], [rsb], scale=sc_, bias=sc_ * EPS)
                            yield
                            kb.op("dve", lambda e, o=rsb[:, 0:n]: e.reciprocal(o, o), [rsb], [rsb])
                            yield
                            kb.tt("dve", dst[:, b0:b0 + n], dst[:, b0:b0 + n], rsb[:, 0:n], ALU.mult, [dst, rsb], [dst])
                            yield

        def out_gen(pp):
                pO = g.ps[0]
                for grp in (range(1, 9) if half_mode else range(17)):
                    gt, ot = gts[grp % 2], otb[grp % 2]
                    c0, t0 = 4 * grp, 256 * grp
                    for ab in range(2):
                        h = 2 * pp + ab
                        kb.dma("sp" if ab == 0 else "pool", gt[64 * ab:64 * ab + 64, :, :],
                               g.tokd[t0:t0 + 256, TK_GATE + 64 * h:TK_GATE + 64 * h + 64].rearrange("(c i) e -> i c e", i=64),
                               [g.TOK[2 * grp], g.TOK[2 * grp + 1]], [gt])
                        yield
                    kb.act(gt[:], gt[:], AF.Silu, [gt], [gt])
                    yield
                    ob = OB[:, c0:c0 + 4, :]
                    tmp3 = bufs[0].tmp[:, 0:2, :].rearrange("p a (b e) -> p (a b) e", e=64)
                    kb.tt("dve", tmp3, ob, ob, ALU.mult, [OB], [bufs[0].tmp])
                    yield
                    kb.op("dve", lambda e, o=oss[:, 0:4], i=tmp3: e.reduce_sum(o, i, AX.X), [bufs[0].tmp], [oss])
                    yield
                    rstd_from_ssq(kb, oss[:, 0:4], oss[:, 0:4], 64, [oss], [oss])
                    kb.tt("dve", ob, ob, oss[:, 0:4].unsqueeze(2).to_broadcast([128, 4, 64]), ALU.mult, [OB, oss], [OB])
                    yield
                    kb.tt("dve", ob, ob, gt[:], ALU.mult, [OB, gt], [OB])
                    yield
                    for c in range(4):
                        kb.tr(pO[0:64, c * 128:c * 128 + 128], OB[:, c0 + c, :], ident[:], [OB, ident], [pO])
                        yield
                    kb.act(ot[:, :], pO[0:64, :], AF.Copy, [pO, dgain], [ot], scale=dgain[:, 0:1])
                    yield
                    for ab in range(2):
                        h = 2 * pp + ab
                        kb.dma("sp" if ab == 0 else "pool",
                               g.mixd[64 * h:64 * h + 64, t0:t0 + 256].rearrange("e (c i) -> e c i", c=4),
                               ot[:, :].rearrange("e (c ab i) -> e c ab i", c=4, ab=2)[:, :, ab, :], [ot],
                               [g.MIXdn[2 * grp], g.MIXdn[2 * grp + 1]])
                        yield

        def run_all(gen):
            for _ in gen:
                pass

        def interleave(ga, gb):
            live = [True, True]
            gs = [ga, gb]
            while any(live):
                for i_ in range(2):
                    if live[i_]:
                        try:
                            next(gs[i_])
                        except StopIteration:
                            live[i_] = False

        run_all(conv_gen(0))
        for pp in range(3):
            for d in range(2):
                kb.memset("dve", bufs[d].S[0][:], 0.0, [bufs[d].S[0]])
            written = set()
            scnt = [0, 0]

            def prep(d, grp):
                B = bufs[d]
                dp, c0, t0 = 3 * d + pp, 4 * grp, 256 * grp
                pA, pB, pC, pD = B.banks
                for dst, src_ in ((B.kT, kn), (B.qT, qn), (B.vT, vn)):
                    for half in range(2):
                        pr = slice(64 * half, 64 * half + 64)
                        kb.copy("dve", dst[pr, :, 64 * half:64 * half + 64],
                                src_[pr, t0:t0 + 256].rearrange("p (c i) -> p c i", c=4), [src_], [dst])
                        yield
                for c in range(4):
                    kb.tr(pA[:, c * 128:c * 128 + 128], B.kT[:, c, :], ident[:], [B.kT, ident], [pA])
                    yield
                kb.copy("act", f2(B.kBD[:]), pA[:, :], [pA], [B.kBD])
                yield
                for c in range(4):
                    kb.tr(pA[:, c * 128:c * 128 + 128], B.vT[:, c, :], ident[:], [B.vT, ident], [pA])
                    yield
                kb.copy("act", f2(B.tmp[:]), pA[:, :], [pA], [B.tmp])
                yield
                kb.tt("dve", B.vst[:], B.tmp[:, :, 0:64], B.tmp[:, :, 64:128], ALU.add, [B.tmp], [B.vst])
                yield
                for c in range(4):
                    kb.mm(pB[:, c * 128:c * 128 + 128], B.kT[:, c, :], B.kT[:, c, :], True, True, [B.kT], [pB])
                    yield
                for c in range(4):
                    kb.mm(pC[:, c * 128:c * 128 + 128], B.kT[:, c, :], B.qT[:, c, :], True, True, [B.kT, B.qT], [pC])
                    yield
                for c in range(4):
                    kb.ts("dve", B.diag[:, c, :], ident[:], GC[:, dp, c0 + c:c0 + c + 1], None, ALU.mult, None, [ident, GC], [B.diag])
                    yield
                kb.mm(pA[:, :], ones[:], f2(B.diag[:]), True, True, [ones, B.diag], [pA])
                yield
                for c in range(4):
                    kb.ts("dve", B.D[:, c, :], pA[:, c * 128:c * 128 + 128], GC[:, dp, c0 + c:c0 + c + 1], 0.0, ALU.subtract, ALU.min,
                          [pA, GC], [B.D])
                    yield
                kb.tt("dve", f2(B.D[:]), f2(B.D[:]), f2(NM4[d][:]), ALU.add, [B.D, NM4[d]], [B.D])
                yield
                kb.act(f2(B.D[:]), f2(B.D[:]), AF.Exp, [B.D], [B.D])
                yield
                kb.tt("dve", f2(B.attnT[:]), pC[:, :], f2(B.D[:]), ALU.mult, [pC, B.D], [B.attnT])
                yield
                kb.tt("dve", f2(B.N[:]), pB[:, :], f2(B.D[:]), ALU.mult, [pB, B.D], [B.N])
                yield
                kb.tt("dve", f2(B.N[:]), f2(B.N[:]), f2(SM4[d][:]), ALU.mult, [B.N, SM4[d]], [B.N])
                yield
                for c in range(4):
                    kb.act(B.N[:, c, :], B.N[:, c, :], AF.Copy, [B.N, NBETA], [B.N], scale=NBETA[:, dp, c0 + c:c0 + c + 1])
                    yield
                for c in range(4):
                    kb.tr(pA[:, c * 128:c * 128 + 128], B.N[:, c, :], ident[:], [B.N, ident], [pA])
                    yield
                kb.copy("act", f2(B.NT[:]), pA[:, :], [pA], [B.NT])
                yield
                kb.tt("dve", f2(B.R[:]), f2(B.N[:]), f2(I4[:]), ALU.add, [B.N, I4], [B.R])
                yield
                P, PT = B.N, B.NT
                for k in range(5):
                    Pn, PTn = (B.P2, B.PT2) if k % 2 == 0 else (B.N, B.NT)
                    for c in range(4):
                        kb.mm(pC[:, c * 128:c * 128 + 128], P[:, c, :], PT[:, c, :], True, True, [P, PT], [pC])
                        yield
                    if k < 4:
                        for c in range(4):
                            kb.mm(pB[:, c * 128:c * 128 + 128], PT[:, c, :], P[:, c, :], True, True, [P, PT], [pB])
                            yield
                    kb.copy("act", f2(PTn[:]), pC[:, :], [pC], [PTn])
                    yield
                    if k < 4:
                        kb.copy("dve", f2(Pn[:]), pB[:, :], [pB], [Pn])
                        yield
                    for c in range(4):
                        kb.mm(pA[:, c * 128:c * 128 + 128], PTn[:, c, :], B.R[:, c, :], True, True, [PTn, B.R], [pA])
                        yield
                    kb.tt("dve", f2(B.R[:]), f2(B.R[:]), pA[:, :], ALU.add, [B.R, pA], [B.R])
                    yield
                    P, PT = Pn, PTn
                for c in range(4):
                    kb.act(B.kt[:, c, :], B.kBD[:, c, :], AF.Copy, [B.kBD, KTS], [B.kt], scale=KTS[:, dp, c0 + c:c0 + c + 1])
                    yield

            def scan(d, grp):
                B = bufs[d]
                dp, c0 = 3 * d + pp, 4 * grp
                pD = B.banks[3]
                for c in (range(4) if d == 0 else range(3, -1, -1)):
                    ch = c0 + c
                    S_old, S_new = B.S[scnt[d] % 2], B.S[(scnt[d] + 1) % 2]
                    scnt[d] += 1
                    kb.mm(pD[:, 0:64], B.kT[:, c, :], S_old[:], True, True, [B.kT, S_old], [pD])
                    yield
                    kb.stt("dve", B.ntmp[:], pD[:, 0:64], E[:, dp, ch:ch + 1], B.vst[:, c, :], ALU.mult, ALU.subtract,
                           [pD, E, B.vst], [B.ntmp])
                    yield
                    kb.mm(pD[:, 64:128], B.R[:, c, :], B.ntmp[:], True, True, [B.R, B.ntmp], [pD])
                    yield
                    kb.act(B.vnew[:], pD[:, 64:128], AF.Copy, [pD, NBETA], [B.vnew], scale=NBETA[:, dp, ch:ch + 1])
                    yield
                    need_o = not (half_mode and (ch >= 36 or ch < 4))
                    if need_o:
                        kb.mm(pD[:, 128:192], B.qT[:, c, :], S_old[:], True, True, [B.qT, S_old], [pD])
                        yield
                        kb.act(B.o1[:], pD[:, 128:192], AF.Copy, [pD, E], [B.o1], scale=E[:, dp, ch:ch + 1])
                        yield
                        kb.mm(pD[:, 192:256], B.attnT[:, c, :], B.vnew[:], True, True, [B.attnT, B.vnew], [pD])
                        yield
                    if not need_o:
                        pass
                    elif ch not in written:
                        written.add(ch)
                        kb.tt("dve", OB[:, ch, :], B.o1[:], pD[:, 192:256], ALU.add, [B.o1, pD], [OB])
                        yield
                    else:
                        kb.tt("dve", B.o1[:], B.o1[:], pD[:, 192:256], ALU.add, [B.o1, pD], [B.o1])
                        yield
                        kb.tt("dve", OB[:, ch, :], OB[:, ch, :], B.o1[:], ALU.add, [OB, B.o1], [OB])
                        yield
                    kb.mm(pD[:, 256:320], B.kt[:, c, :], B.vnew[:], True, True, [B.kt, B.vnew], [pD])
                    yield
                    kb.stt("dve", S_new[:], S_old[:], EGL[:, dp, ch:ch + 1], pD[:, 256:320], ALU.mult, ALU.add,
                           [S_old, EGL, pD], [S_new])
                    yield

            order = [list(range(17)), [0] + list(range(16, 0, -1))]
            def stream(d):
                for it in range(9 if (half_mode and d == 0) else 17):
                    yield from prep(d, order[d][it])
                    yield from scan(d, order[d][it])

            gens = [stream(0), stream(1)]
            alive = [True, True]
            nstep = [1, 2] if half_mode else [1, 1]
            while any(alive):
                for d in range(2):
                    for _ in range(nstep[d]):
                        if alive[d]:
                            try:
                                next(gens[d])
                            except StopIteration:
                                alive[d] = False
            if pp < 2:
                interleave(out_gen(pp), conv_gen(pp + 1))
            else:
                run_all(out_gen(pp))
    kb.barrier()


def prep_w13(g, wa, wb, W13, nf, sc):
    kb = g.kb
    st = [kb.sbuf("w13s%d" % i, [128, 8, 256], ctx=sc) for i in range(2)]
    sb = [kb.sbuf("w13b%d" % i, [128, 8, 256], BF16, ctx=sc) for i in range(2)]
    for f in range(nf):
        s_, b_ = st[f % 2], sb[f % 2]
        kb.dma("sp", s_[:, :, 0:128], wa[0][:, 128 * f:128 * f + 128].rearrange("(k p) j -> p k j", p=128), [wa[1]], [s_])
        kb.dma("pool", s_[:, :, 128:256], wb[0][:, 128 * f:128 * f + 128].rearrange("(k p) j -> p k j", p=128), [wb[1]], [s_])
        kb.copy("dve" if f % 2 == 0 else "pool", b_[:], s_[:], [s_], [b_])
        kb.dma("sp", W13[f].ap, b_[:], [b_], [W13[f]])


def phase_out_ffn(g, L, xs_tiles, xo_tiles, do_ctx):
    kb = g.kb
    with ExitStack() as sc:
        with ExitStack() as sc2:
            prep_w13(g, (g.ffn_w1.ap[0], g.ffn_w1), (g.ffn_w3.ap[0], g.ffn_w3), g.W13, 22, sc2)
        kb.barrier()
        woutb = kb.sbuf("woutb", [128, 8, D], BF16, ctx=sc)
        w2b = kb.sbuf("w2b", [128, 22, D], BF16, ctx=sc)
        stage = [kb.sbuf("wst%d" % i, [128, D], ctx=sc) for i in range(2)]
        load_cast_weight(g, sc, "wout", g.w_out, lambda k: g.w_out.ap[L, 128 * k:128 * k + 128, :], 8, D, stage, woutb,
                         lambda k: woutb[:, k, :])
        load_cast_weight(g, sc, "w2", g.ffn_w2, lambda k: g.ffn_w2.ap[0, 128 * k:128 * k + 128, :], 22, D, stage, w2b,
                         lambda k: w2b[:, k, :])
        gmsa = kb.sbuf("gmsa", [128, 2, D], ctx=sc)
        gmlp = kb.sbuf("gmlp", [128, 2, D], ctx=sc)
        for s in range(2):
            kb.dma("sp", gmsa[:, s, :], g.gates.ap[L, 0, s:s + 1, :].to_broadcast([128, D]), [g.gates], [gmsa])
            kb.dma("sp", gmlp[:, s, :], g.gates.ap[L, 1, s:s + 1, :].to_broadcast([128, D]), [g.gates], [gmlp])
        scr = {"junk": kb.sbuf("junk", [128, 1024], ctx=sc), "ssq": kb.sbuf("ssq", [128, 1], ctx=sc),
               "xn": kb.sbuf("xn", [128, 1024], ctx=sc), "psT": [g.ps[0], g.ps[1]]}
        mix32 = [kb.sbuf("mix32_%d" % i, [128, 8, 128], ctx=sc) for i in range(2)]
        mixb = [kb.sbuf("mixb_%d" % i, [128, 8, 128], BF16, ctx=sc) for i in range(2)]
        xts = [kb.sbuf("xt%d" % i, [128, D], ctx=sc) for i in range(2)]
        tmpy = kb.sbuf("tmpy", [128, D], ctx=sc)
        x1blk = [kb.sbuf("x1b%d" % i, [128, 4, D], ctx=sc) for i in range(1)]
        h2Ts = [kb.sbuf("h2T%d" % i, [128, 8, 512], BF16, ctx=sc) for i in range(1)]
        gT = kb.sbuf("gT", [128, 22, 512], BF16, ctx=sc)
        w13s = [kb.sbuf("w13_%d" % i, [128, 8, 256], BF16, ctx=sc) for i in range(3)]
        sas = [kb.sbuf("sa%d" % i, [128, 512], ctx=sc) for i in range(2)]
        x2s = [kb.sbuf("x2_%d" % i, [128, D], ctx=sc) for i in range(2)]
        psY = [g.ps[2], g.ps[3]]
        psA, psB = [g.ps[4], g.ps[5]], [g.ps[6], g.ps[7]]
        blocks = ([(0, 0, 2)] if do_ctx else []) + [(1 + i, 2 + 4 * i, 4) for i in range(8)]
        ti = 0
        wi = 0
        for bn, (bi, t0, nt) in enumerate(blocks):
            ntok = nt * 128
            x1b, h2T = x1blk[0], h2Ts[0]
            s = 1 if bi == 0 else 0
            for j in range(nt):
                t = t0 + j
                m32, mb, xt = mix32[ti % 2], mixb[ti % 2], xts[ti % 2]
                ti += 1
                kb.dma("sp", m32[:], g.mixd[:, 128 * t:128 * t + 128].rearrange("(c p) t -> p c t", p=128),
                       [g.MIXdn[t], g.MIXsg[t]] + [g.MIXat[h][bi] for h in range(6)], [m32])
                kb.dma("pool", xt[:], xs_tiles[t].ap, [xs_tiles[t]], [xt])
                kb.copy("pool", mb[:], m32[:], [m32], [mb])
                for half in range(2):
                    for k in range(8):
                        kb.mm(psY[half][:, :], mb[:, k, :], woutb[:, k, 512 * half:512 * half + 512], k == 0, k == 7,
                              [mb, woutb], [psY[half]])
                for half in range(2):
                    kb.tt("dve", tmpy[:, 512 * half:512 * half + 512], psY[half][:, :], gmsa[:, s, 512 * half:512 * half + 512],
                          ALU.mult, [psY[half], gmsa], [tmpy])
                kb.tt("pool", x1b[:, j, :], tmpy[:], xt[:], ALU.add, [tmpy, xt], [x1b])
                norm_tile_to_hT(g, L, 1, x1b, bi == 0, [h2T[:, c, j * 128:j * 128 + 128] for c in range(8)], h2T, scr, xap=x1b[:, j, :])
            for f in range(22):
                w13 = w13s[wi % 3]
                pa, pb, sa = psA[wi % 2], psB[wi % 2], sas[wi % 2]
                wi += 1
                kb.dma("sp" if f % 2 == 0 else "pool", w13[:], g.W13[f].ap, [g.W13[f]], [w13])
                for k in range(8):
                    kb.mm(pa[:, 0:ntok], w13[:, k, 0:128], h2T[:, k, 0:ntok], k == 0, k == 7, [w13, h2T], [pa])
                for k in range(8):
                    kb.mm(pb[:, 0:ntok], w13[:, k, 128:256], h2T[:, k, 0:ntok], k == 0, k == 7, [w13, h2T], [pb])
                kb.act(sa[:, 0:ntok], pa[:, 0:ntok], AF.Silu, [pa], [sa])
                kb.tt("dve", gT[:, f, 0:ntok], sa[:, 0:ntok], pb[:, 0:ntok], ALU.mult, [sa, pb], [gT])
            for j in range(nt):
                t = t0 + j
                x2 = x2s[j % 2]
                for half in range(2):
                    for f in range(22):
                        kb.mm(psY[half][:, :], gT[:, f, j * 128:j * 128 + 128], w2b[:, f, 512 * half:512 * half + 512],
                              f == 0, f == 21, [gT, w2b], [psY[half]])
                for half in range(2):
                    kb.tt("dve", tmpy[:, 512 * half:512 * half + 512], psY[half][:, :], gmlp[:, s, 512 * half:512 * half + 512],
                          ALU.mult, [psY[half], gmlp], [tmpy])
                kb.tt("pool", x2[:], tmpy[:], x1b[:, j, :], ALU.add, [tmpy, x1b], [x2])
                kb.dma("sp", xo_tiles[t].ap, x2[:], [x2], [xo_tiles[t]])
    kb.barrier()


def phase_out_router(g, L, xs_tiles, nblk=8):
    kb = g.kb
    with ExitStack() as sc:
        woutb = kb.sbuf("woutb", [128, 8, D], BF16, ctx=sc)
        stage = [kb.sbuf("wst%d" % i, [128, D], ctx=sc) for i in range(2)]
        load_cast_weight(g, sc, "wout", g.w_out, lambda k: g.w_out.ap[L, 128 * k:128 * k + 128, :], 8, D, stage, woutb,
                         lambda k: woutb[:, k, :])
        gmsa = kb.sbuf("gmsa", [128, D], ctx=sc)
        kb.dma("sp", gmsa[:], g.gates.ap[L, 0, 0:1, :].to_broadcast([128, D]), [g.gates], [gmsa])
        rw = kb.sbuf("rw", [128, 8, NEXP], ctx=sc)
        kb.dma("sp", rw[:], g.router_w.ap[0].rearrange("(k p) e -> p k e", p=128), [g.router_w], [rw])
        rb = kb.sbuf("rb", [128, NEXP], ctx=sc)
        kb.dma("sp", rb[:], g.router_b.ap[0:1, :].to_broadcast([128, NEXP]), [g.router_b], [rb])
        scr = {"junk": kb.sbuf("junk", [128, 1024], ctx=sc), "ssq": kb.sbuf("ssq", [128, 1], ctx=sc),
               "xn": kb.sbuf("xn", [128, 1024], ctx=sc), "psT": [g.ps[0], g.ps[1]]}
        mix32 = [kb.sbuf("mix32_%d" % i, [128, 8, 128], ctx=sc) for i in range(2)]
        mixb = [kb.sbuf("mixb_%d" % i, [128, 8, 128], BF16, ctx=sc) for i in range(2)]
        xts = [kb.sbuf("xt%d" % i, [128, D], ctx=sc) for i in range(2)]
        tmpy = kb.sbuf("tmpy", [128, D], ctx=sc)
        x1s = [kb.sbuf("x1_%d" % i, [128, D], ctx=sc) for i in range(2)]
        h2Ts = [kb.sbuf("h2T%d" % i, [128, 8, 512], BF16, ctx=sc) for i in range(2)]
        h32 = kb.sbuf("h32", [128, 8, 128], ctx=sc)
        lg = kb.sbuf("lg", [128, NEXP], ctx=sc)
        mx8 = kb.sbuf("mx8", [128, 8], ctx=sc)
        msk = kb.sbuf("msk", [128, NEXP], ctx=sc)
        ex = kb.sbuf("ex", [128, NEXP], ctx=sc)
        nm1 = kb.sbuf("nm1", [128, 2], ctx=sc)
        psY = [g.ps[2], g.ps[3]]
        psR = g.ps[4]
        for bi in range(nblk):
            h2T = h2Ts[bi % 2]
            for j in range(4):
                n = 4 * bi + j
                t = 2 + n
                m32, mb, xt, x1 = mix32[n % 2], mixb[n % 2], xts[n % 2], x1s[n % 2]
                kb.dma("sp", m32[:], g.mixd[:, 128 * t:128 * t + 128].rearrange("(c p) t -> p c t", p=128),
                       [g.MIXdn[t], g.MIXsg[t]] + [g.MIXat[h][1 + bi] for h in range(6)], [m32])
                kb.dma("pool", xt[:], xs_tiles[t].ap, [xs_tiles[t]], [xt])
                kb.copy("dve", mb[:], m32[:], [m32], [mb])
                for half in range(2):
                    for k in range(8):
                        kb.mm(psY[half][:, :], mb[:, k, :], woutb[:, k, 512 * half:512 * half + 512], k == 0, k == 7,
                              [mb, woutb], [psY[half]])
                for half in range(2):
                    kb.tt("dve", tmpy[:, 512 * half:512 * half + 512], psY[half][:, :], gmsa[:, 512 * half:512 * half + 512],
                          ALU.mult, [psY[half], gmsa], [tmpy])
                kb.tt("dve", x1[:], tmpy[:], xt[:], ALU.add, [tmpy, xt], [x1])
                kb.dma("sp", g.X1S[n].ap, x1[:], [x1], [g.X1S[n]])
                norm_tile_to_hT(g, L, 1, x1, False, [h2T[:, c, j * 128:j * 128 + 128] for c in range(8)], h2T, scr,
                                also_f32=[h32[:, c, :] for c in range(8)], f32_buf=h32)
                for k in range(8):
                    kb.mm(psR[:, 0:NEXP], h32[:, k, :], rw[:, k, :], k == 0, k == 7, [h32, rw], [psR])
                kb.tt("dve", lg[:], psR[:, 0:NEXP], rb[:], ALU.add, [psR, rb], [lg])
                kb.op("dve", lambda e, o=mx8[:], i=lg[:]: e.max(out=o, in_=i), [lg], [mx8])
                kb.ts("dve", msk[:], lg[:], mx8[:, 1:2], None, ALU.is_ge, None, [lg, mx8], [msk])
                kb.ts("dve", nm1[:, 0:1], mx8[:, 0:1], -1.0, None, ALU.mult, None, [mx8], [nm1])
                kb.act(ex[:], lg[:], AF.Exp, [lg, nm1], [ex], bias=nm1[:, 0:1])
                kb.tt("dve", ex[:], ex[:], msk[:], ALU.mult, [ex, msk], [ex])
                kb.op("dve", lambda e, o=nm1[:, 1:2], i=ex[:]: e.reduce_sum(o, i, AX.X), [ex], [nm1])
                kb.op("dve", lambda e, o=nm1[:, 1:2]: e.reciprocal(o, o), [nm1], [nm1])
                kb.ts("dve", g.GATE[:, n, :], ex[:], nm1[:, 1:2], None, ALU.mult, None, [ex, nm1], [g.GATEb])
            kb.dma("sp", g.H2T[bi].ap, h2T[:], [h2T], [g.H2T[bi]])
    kb.barrier()


def phase_moe(g, L, nblk=8):
    kb = g.kb
    NF = MOE_FF // 128
    with ExitStack() as sc:
        w2b = kb.sbuf("w2b", [128, NF, D], BF16, ctx=sc)
        stage = [kb.sbuf("wst%d" % i, [128, D], ctx=sc) for i in range(2)]
        gmlp = kb.sbuf("gmlp", [128, D], ctx=sc)
        kb.dma("sp", gmlp[:], g.gates.ap[L, 1, 0:1, :].to_broadcast([128, D]), [g.gates], [gmlp])
        fgain = kb.sbuf("fgain", [128, D], ctx=sc)
        kb.dma("sp", fgain[:], g.final_norm_g.ap[0:1, :].to_broadcast([128, D]), [g.final_norm_g], [fgain])
        pst = [kb.sbuf("w13s%d" % i, [128, 8, 256], ctx=sc) for i in range(2)]
        psb = [kb.sbuf("w13b%d" % i, [128, 8, 256], BF16, ctx=sc) for i in range(2)]
        h2Ts = [kb.sbuf("h2T%d" % i, [128, 8, 512], BF16, ctx=sc) for i in range(2)]
        gT = kb.sbuf("gT", [128, NF, 512], BF16, ctx=sc)
        w13s = [kb.sbuf("w13_%d" % i, [128, 8, 256], BF16, ctx=sc) for i in range(3)]
        sas = [kb.sbuf("sa%d" % i, [128, 512], ctx=sc) for i in range(2)]
        tmpy = kb.sbuf("tmpy", [128, D], ctx=sc)
        ya = kb.sbuf("ya", [128, D], ctx=sc)
        x1 = kb.sbuf("x1", [128, D], ctx=sc)
        junk = kb.sbuf("junk", [128, D], ctx=sc)
        ssq = kb.sbuf("ssq", [128, 1], ctx=sc)
        psY = [g.ps[2], g.ps[3]]
        psA, psB = [g.ps[4], g.ps[5]], [g.ps[6], g.ps[7]]
        wi = 0
        hi = 0
        def prep_chunk(e, f):
            s_, b_ = pst[f % 2], psb[f % 2]
            kb.dma("sp", s_[:, :, 0:128], g.moe_w1.ap[0, e, :, 128 * f:128 * f + 128].rearrange("(k p) j -> p k j", p=128),
                   [g.moe_w1], [s_])
            kb.dma("sp", s_[:, :, 128:256], g.moe_w3.ap[0, e, :, 128 * f:128 * f + 128].rearrange("(k p) j -> p k j", p=128),
                   [g.moe_w3], [s_])
            kb.copy("dve", b_[:], s_[:], [s_], [b_])
            kb.dma("sp", g.W13x[e % 2][f].ap, b_[:], [b_], [g.W13x[e % 2][f]])

        def w2_chunk(e, k):
            st = stage[k % 2]
            kb.dma("sp", st[:, :], g.moe_w2.ap[0, e, 128 * k:128 * k + 128, :], [g.moe_w2], [st])
            kb.copy("dve", w2b[:, k, :], st[:, :], [st], [w2b])

        for f in range(NF):
            prep_chunk(0, f)
        for e in range(NEXP):
            for bi in range(nblk):
                h2T = h2Ts[hi % 2]
                hi += 1
                kb.dma("sp", h2T[:], g.H2T[bi].ap, [g.H2T[bi]], [h2T])
                for f in range(NF):
                    if bi == 0:
                        w2_chunk(e, f)
                    if bi == 1 and e + 1 < NEXP:
                        prep_chunk(e + 1, f)
                    w13 = w13s[wi % 3]
                    pa, pb, sa = psA[wi % 2], psB[wi % 2], sas[wi % 2]
                    wi += 1
                    kb.dma("sp", w13[:], g.W13x[e % 2][f].ap, [g.W13x[e % 2][f]], [w13])
                    for k in range(8):
                        kb.mm(pa[:, :], w13[:, k, 0:128], h2T[:, k, :], k == 0, k == 7, [w13, h2T], [pa])
                    for k in range(8):
                        kb.mm(pb[:, :], w13[:, k, 128:256], h2T[:, k, :], k == 0, k == 7, [w13, h2T], [pb])
                    kb.act(sa[:, :], pa[:, :], AF.Silu, [pa], [sa])
                    kb.tt("dve", gT[:, f, :], sa[:, :], pb[:, :], ALU.mult, [sa, pb], [gT])
                for j in range(4):
                    n = 4 * bi + j
                    for half in range(2):
                        for f in range(NF):
                            kb.mm(psY[half][:, :], gT[:, f, j * 128:j * 128 + 128], w2b[:, f, 512 * half:512 * half + 512],
                                  f == 0, f == NF - 1, [gT, w2b], [psY[half]])
                    if e > 0:
                        kb.dma("pool", ya[:], g.YACC[n].ap, [g.YACC[n]], [ya])
                    for half in range(2):
                        kb.act(tmpy[:, 512 * half:512 * half + 512], psY[half][:, :], AF.Copy, [psY[half], g.GATEb], [tmpy],
                               scale=g.GATE[:, n, e:e + 1])
                    if e == 0:
                        kb.dma("sp", g.YACC[n].ap, tmpy[:], [tmpy], [g.YACC[n]])
                        continue
                    kb.tt("dve", ya[:], ya[:], tmpy[:], ALU.add, [ya, tmpy], [ya])
                    if e < NEXP - 1:
                        kb.dma("sp", g.YACC[n].ap, ya[:], [ya], [g.YACC[n]])
                        continue
                    kb.dma("sp", x1[:], g.X1S[n].ap, [g.X1S[n]], [x1])
                    kb.tt("dve", ya[:], ya[:], gmlp[:], ALU.mult, [ya, gmlp], [ya])
                    kb.tt("dve", ya[:], ya[:], x1[:], ALU.add, [ya, x1], [ya])
                    kb.act(junk[:], ya[:], AF.Square, [ya, ssq], [junk, ssq], accum_out=ssq[:, 0:1])
                    rstd_from_ssq(kb, ssq[:, 0:1], ssq[:, 0:1], D, [ssq], [ssq])
                    kb.act(junk[:], ya[:], AF.Copy, [ya, ssq], [junk], scale=ssq[:, 0:1])
                    kb.tt("dve", junk[:], junk[:], fgain[:], ALU.mult, [junk, fgain], [junk])
                    kb.dma("sp", g.outb.ap[128 * n:128 * n + 128, :], junk[:], [junk], [g.outb])
    kb.barrier()


W_NAMES = [("mod_w", [DEPTH, D, 6 * D]), ("mod_b", [DEPTH, 6 * D]), ("norm1_g", [DEPTH, D]), ("norm2_g", [DEPTH, D]),
           ("w_in", [DEPTH, D, IN_COLS]), ("conv_w", [DEPTH, 3, 1152]), ("dn_a_log", [DEPTH, 2, 6]), ("dn_dt_bias", [DEPTH, 2, 6]),
           ("dn_norm_g", [DEPTH, 64]), ("q_norm_g", [DEPTH, 64]), ("k_norm_g", [DEPTH, 64]), ("sgu_norm_g", [DEPTH, 256]),
           ("sgu_w", [DEPTH, 4, 128, 128]), ("sgu_b", [DEPTH, 4, 128]), ("w_out", [DEPTH, D, D]),
           ("ffn_w1", [1, D, D_FF]), ("ffn_w3", [1, D, D_FF]), ("ffn_w2", [1, D_FF, D]),
           ("router_w", [1, D, NEXP]), ("router_b", [1, NEXP]), ("moe_w1", [1, NEXP, D, MOE_FF]),
           ("moe_w3", [1, NEXP, D, MOE_FF]), ("moe_w2", [1, NEXP, MOE_FF, D]), ("final_norm_g", [1, D])]
BLOCKS = [(0, 256)] + [(256 + 512 * i, 512) for i in range(8)]


def build_program(phases=None, debug=(), dbg_in=()):
    nc = bass.Bass("TRN2", target_bir_lowering=False)
    g = G()

    def ext_in(name, shape):
        return Buf(name, nc.dram_tensor(name, list(shape), F32, kind="ExternalInput").ap())

    g.xin = ext_in("xin", [T, D])
    g.cvec = ext_in("cvec", [2, D])
    g.rope = ext_in("rope", [NLAT, 64])
    g.identd = ext_in("ident", [128, 128])
    g.dncd = ext_in("dnc", [7, 128, 128])
    for name, shape in W_NAMES:
        setattr(g, name, ext_in(name, shape))
    out = Buf("out", nc.dram_tensor("out", [NLAT // 2, D], F32, kind="ExternalOutput").ap())
    dbg = {}
    for name, shape in debug:
        dbg[name] = Buf(name, nc.dram_tensor(name, list(shape), F32, kind="ExternalOutput").ap())
    for name, shape in dbg_in:
        dbg[name] = Buf(name, nc.dram_tensor(name, list(shape), F32, kind="ExternalInput").ap())
    g.dbg = dbg

    def scratch(name, shape, dt=F32):
        if name in dbg:
            return dbg[name].ap
        return nc.dram_tensor(name + "_s", list(shape), dt, kind="Internal").ap()

    with ExitStack() as ctx:
        import os as _os
        kb = KB(nc, ctx, same_engine_sync=_os.environ.get("SAMEENG", "1") == "1")
        g.kb = kb
        g.ps = [kb.psum("ps%d" % i, [128, 512]) for i in range(8)]
        g.ident = kb.sbuf("ident", [128, 128])
        kb.dma("sp", g.ident[:], g.identd.ap, [g.identd], [g.ident])
        GS = kb.sbuf("GS", [128, DEPTH, 2, 8, 2])
        SH = kb.sbuf("SH", [128, DEPTH, 2, 8, 2])
        g.GS, g.SH, g.GSb, g.SHb = GS.ap, SH.ap, GS, SH
        g.gates = Buf("gates", scratch("gates", [DEPTH, 2, 2, D]))
        tokd = scratch("TOK", [T, TK_W])
        g.tokd = tokd
        g.TOK = [Buf("TOK%d" % t, tokd[128 * t:128 * t + 128, :]) for t in range(NT)]
        g.zqd = scratch("ZQ", [1408, T])
        g.ZQ = [[Buf("ZQ%d_%d" % (fc, bi), g.zqd[128 * fc:128 * fc + 128, b0:b0 + n]) for bi, (b0, n) in enumerate(BLOCKS)]
                for fc in range(11)]
        g.blk_of_tile = lambda t: (0, t * 128) if t < 2 else (1 + (t - 2) // 4, ((t - 2) % 4) * 128)
        g.mixd = scratch("MIXT", [D, T])
        g.MIXdn = [Buf("MIXdn%d" % t, None) for t in range(NT)]
        g.MIXsg = [Buf("MIXsg%d" % t, None) for t in range(NT)]
        g.MIXat = [[Buf("MIXat%d_%d" % (h, bi), g.mixd[384 + 64 * h:384 + 64 * h + 64, b0:b0 + n])
                    for bi, (b0, n) in enumerate(BLOCKS)] for h in range(6)]
        w13d = scratch("W13", [28, 128, 8, 256], BF16)
        g.W13 = [Buf("W13_%d" % f, w13d[f]) for f in range(28)]
        w13x = scratch("W13X", [2, 28, 128, 8, 256], BF16)
        g.W13x = [[Buf("W13x%d_%d" % (i, f), w13x[i, f]) for f in range(28)] for i in range(2)]
        xs = [[Buf("xin%d" % t, g.xin.ap[128 * t:128 * t + 128, :]) for t in range(NT)]]
        for L in range(DEPTH):
            xd = scratch("XS%d" % (L + 1), [T, D])
            xs.append([Buf("xs%d_%d" % (L + 1, t), xd[128 * t:128 * t + 128, :]) for t in range(NT)])
        g.xs = xs
        g.outb = out
        x1d = scratch("X1S", [NLAT, D])
        g.X1S = [Buf("X1S%d" % n, x1d[128 * n:128 * n + 128, :]) for n in range(32)]
        yd = scratch("YACC", [NLAT, D])
        g.YACC = [Buf("YACC%d" % n, yd[128 * n:128 * n + 128, :]) for n in range(32)]
        h2d = scratch("H2T", [8, 128, 8, 512], BF16)
        g.H2T = [Buf("H2T%d" % b, h2d[b]) for b in range(8)]
        GATE = kb.sbuf("GATE", [128, 32, NEXP])
        g.GATE, g.GATEb = GATE.ap, GATE
        allp = ["mod", "a0", "attn0", "sgu0", "dn0", "ffn0", "a1", "attn1", "sgu1", "dn1", "out1", "moe1"]
        phases = allp if phases is None else phases
        if "mod" in phases:
            for L in range(DEPTH):
                phase_mod(g, L)
        if "a0" in phases:
            phase_a(g, 0, xs[0])
        if "attn0" in phases:
            phase_attn(g, 0, True)
        if "sgu0" in phases:
            phase_sgu(g, 0, list(range(NT)))
        if "dn0" in phases:
            phase_dn(g, 0)
        if "ffn0" in phases:
            phase_out_ffn(g, 0, xs[0], xs[1], True)
        if "a1" in phases:
            phase_a(g, 1, xs[1])
        if "attn1" in phases:
            phase_attn(g, 1, False, nblk=4)
        if "sgu1" in phases:
            phase_sgu(g, 1, list(range(2, 18)))
        if "dn1" in phases:
            phase_dn(g, 1, half_mode=True)
        if "out1" in phases:
            phase_out_router(g, 1, xs[1], nblk=4)
        if "moe1" in phases:
            phase_moe(g, 1, nblk=4)
        kb.barrier()
        kb.emit()
        g.n_ins = kb.n_ins
    return nc, g


def make_inputs(inputs):
    x = np.asarray(inputs["x"], np.float32)
    ctxa = np.asarray(inputs["ctx"], np.float32)
    rows = NLAT // 64
    row = np.repeat(np.arange(rows, dtype=np.float32), 64)
    col = np.tile(np.arange(64, dtype=np.float32), rows)
    inv = (10000.0 ** (-2.0 * np.arange(8, dtype=np.float32) / 32)).astype(np.float32)
    inv = (np.float32(10000.0) ** (-2.0 * np.arange(16, dtype=np.float32) / np.float32(32))).astype(np.float32)
    ang = np.stack([row[:, None] * inv, col[:, None] * inv], axis=1).astype(np.float32)
    rope = np.concatenate([np.cos(ang).reshape(NLAT, 32), np.sin(ang).reshape(NLAT, 32)], axis=1).astype(np.float32)
    shared = {k: np.ascontiguousarray(np.asarray(inputs[k], np.float32)).reshape(shp) for k, shp in W_NAMES}
    perm = np.concatenate([np.arange(C_AQ + 64 * h, C_AQ + 64 * h + 64) for h in (0, 3, 1, 4, 2, 5)])
    cols = np.arange(IN_COLS)
    cols[C_AQ:C_AQ + 384] = perm
    shared["w_in"] = np.ascontiguousarray(shared["w_in"][:, :, cols])
    shared["rope"] = rope
    shared["ident"] = np.eye(128, dtype=np.float32)
    j = np.arange(128)[:, None]
    i = np.arange(128)[None, :]
    blk = (j // 64) == (i // 64)
    dnc = np.zeros((7, 128, 128), np.float32)
    dnc[0] = blk
    dnc[1] = blk & (j <= i)
    dnc[2] = blk & (j >= i)
    dnc[3] = np.where(blk & (i >= j), 0.0, -30000.0)
    dnc[4] = np.where(blk & (i <= j), 0.0, -30000.0)
    dnc[5] = blk & (i > j)
    dnc[6] = blk & (i < j)
    shared["dnc"] = dnc
    rev = dict(shared)
    cols = np.arange(IN_COLS)
    cols[C_BETA:C_BETA + 6], cols[C_BETA + 6:C_BETA + 12] = np.arange(C_BETA + 6, C_BETA + 12), np.arange(C_BETA, C_BETA + 6)
    cols[C_ALPHA:C_ALPHA + 6], cols[C_ALPHA + 6:C_ALPHA + 12] = np.arange(C_ALPHA + 6, C_ALPHA + 12), np.arange(C_ALPHA, C_ALPHA + 6)
    rev["w_in"] = np.ascontiguousarray(shared["w_in"][:, :, cols])
    rev["conv_w"] = np.ascontiguousarray(shared["conv_w"][:, ::-1, :])
    rev["dn_a_log"] = np.ascontiguousarray(shared["dn_a_log"][:, ::-1, :])
    rev["dn_dt_bias"] = np.ascontiguousarray(shared["dn_dt_bias"][:, ::-1, :])
    rev["sgu_w"] = np.ascontiguousarray(shared["sgu_w"][:, :, ::-1, ::-1])
    rev["sgu_b"] = np.ascontiguousarray(shared["sgu_b"][:, :, ::-1])
    rev["rope"] = np.ascontiguousarray(rope[::-1])
    maps = []
    cv = np.asarray(inputs["c"], np.float32)
    cc = np.asarray(inputs["c_ctx"], np.float32)
    for b in range(4):
        for tw in range(2):
            m = dict(rev if tw else shared)
            if tw:
                m["xin"] = np.ascontiguousarray(np.concatenate([ctxa[b][::-1], x[b][::-1]], axis=0))
            else:
                m["xin"] = np.ascontiguousarray(np.concatenate([ctxa[b], x[b]], axis=0))
            m["cvec"] = np.ascontiguousarray(np.stack([cv[b], cc], 0))
            maps.append(m)
    return maps


def kernel(**inputs):
    nc, g = build_program()
    maps = make_inputs(inputs)
    res = run_bass_kernel_spmd(nc, maps, core_ids=list(range(len(maps))))
    out = np.empty((4, NLAT, D), np.float32)
    for b in range(4):
        out[b, :NLAT // 2] = res.results[2 * b]["out"]
        out[b, NLAT // 2:] = res.results[2 * b + 1]["out"][::-1]
    return out
```

```python
import numpy as np
from contextlib import ExitStack
import concourse.bass as bass
import concourse.mybir as mybir
from concourse.bass_utils import run_bass_kernel_spmd

F32 = mybir.dt.float32
BF16 = mybir.dt.bfloat16
AF = mybir.ActivationFunctionType
ALU = mybir.AluOpType
AX = mybir.AxisListType

D = 1024
NCTX = 256
NLAT = 4096
T = NCTX + NLAT
NT = T // 128
DEPTH = 2
HD = 64
IN_COLS = 2712
D_FF = 2816
MOE_FF = 3584
NEXP = 8
EPS = 1e-6
C_QKV, C_GATE, C_BETA, C_ALPHA, C_AQ, C_AK, C_AV, C_U, C_V = 0, 1152, 1536, 1548, 1560, 1944, 2072, 2200, 2456
TK_GATE, TK_BETA, TK_ALPHA, TK_AQ, TK_AK, TK_AV, TK_ZV, TK_W = 0, 384, 396, 408, 792, 920, 1048, 1304


class Buf:
    __slots__ = ("name", "ap", "last_w", "readers", "excl")

    def __init__(self, name, ap=None, excl=False):
        self.name = name
        self.ap = ap
        self.excl = excl
        self.last_w = None
        self.readers = []

    def __getitem__(self, idx):
        return self.ap[idx]


class KB:
    ENGS = ("pe", "act", "dve", "pool", "sp")
    NDMA = 12

    def __init__(self, nc, ctx, same_engine_sync=True):
        self.nc = nc
        self.ctx = ctx
        self.same = same_engine_sync
        self.ops = {e: [] for e in self.ENGS}
        self.seq = {e: 0 for e in self.ENGS}
        self.sems = {e: ctx.enter_context(nc.semaphore("c_" + e)) for e in self.ENGS}
        self.dma_sems, self.dma_cnt, self.dma_rr = {}, {}, {}
        for q in ("sp", "pool", "act"):
            self.dma_sems[q] = [ctx.enter_context(nc.semaphore("d_%s%d" % (q, i))) for i in range(self.NDMA)]
            self.dma_cnt[q] = [0] * self.NDMA
            self.dma_rr[q] = 0
        self.known = {e: {} for e in self.ENGS}
        import os as _os
        self.pool_dma = _os.environ.get("POOLDMA", "1") == "1"
        self.pool_cmp = _os.environ.get("POOLCMP", "0") == "1"
        self.n_ins = 0
        self.uid = 0

    def sbuf(self, name, shape, dt=F32, ctx=None):
        self.uid += 1
        t = (ctx or self.ctx).enter_context(self.nc.sbuf_tensor("%s_%d" % (name, self.uid), list(shape), dt))
        return Buf(name, t)

    def psum(self, name, shape, dt=F32, ctx=None):
        self.uid += 1
        t = (ctx or self.ctx).enter_context(self.nc.psum_tensor("%s_%d" % (name, self.uid), list(shape), dt))
        return Buf(name, t, excl=True)

    def dram(self, name, shape, dt=F32):
        t = self.nc.dram_tensor(name, list(shape), dt, kind="Internal")
        return Buf(name, t.ap())

    def _need(self, eng, tok, waits):
        if tok is None:
            return
        key, val = tok
        if key == eng and (eng == "pe" or not self.same):
            return
        if val > waits.get(key, 0):
            waits[key] = val

    def _sem(self, key):
        if isinstance(key, str):
            return self.sems[key]
        return self.dma_sems[key[0]][key[1]]

    def op(self, eng, fn, reads=(), writes=(), dma=False):
        if eng == "pool":
            if dma and not self.pool_dma:
                eng = "sp"
            elif not dma and not self.pool_cmp:
                eng = "dve"
        ex = [b for b in reads if b.excl]
        if ex:
            reads = [b for b in reads if not b.excl]
            writes = list(writes) + [b for b in ex if b not in writes]
        waits = {}
        for b in reads:
            self._need(eng, b.last_w, waits)
        for b in writes:
            self._need(eng, b.last_w, waits)
            for r in b.readers:
                self._need(eng, r, waits)
        if dma:
            q = eng
            i = self.dma_rr[q]
            self.dma_rr[q] = (i + 1) % self.NDMA
            key = (q, i)
            prev = self.dma_cnt[q][i]
            if prev > 0 and 16 * prev > waits.get(key, 0):
                waits[key] = 16 * prev
            self.dma_cnt[q][i] = prev + 1
            tok = (key, 16 * (prev + 1))
            inc = 16
        else:
            self.seq[eng] += 1
            tok = (eng, self.seq[eng])
            inc = 1
        kn = self.known[eng]
        wl = []
        for key, val in waits.items():
            if kn.get(key, 0) >= val:
                continue
            kn[key] = val
            wl.append((self._sem(key), val))
        self.n_ins += 1
        self.ops[eng].append((wl, fn, self._sem(tok[0]), inc))
        for b in reads:
            b.readers.append(tok)
        for b in writes:
            b.last_w = tok
            b.readers = []
        return tok

    def barrier(self):
        for e in self.ENGS:
            wl = []
            kn = self.known[e]
            for e2 in self.ENGS:
                if e2 != e and self.seq[e2] > kn.get(e2, 0):
                    kn[e2] = self.seq[e2]
                    wl.append((self.sems[e2], self.seq[e2]))
            if self.same and e != "pe" and self.seq[e] > kn.get(e, 0):
                kn[e] = self.seq[e]
                wl.append((self.sems[e], self.seq[e]))
            for q in self.dma_sems:
                for i in range(self.NDMA):
                    v = 16 * self.dma_cnt[q][i]
                    if v > kn.get((q, i), 0):
                        kn[(q, i)] = v
                        wl.append((self.dma_sems[q][i], v))
            if wl:
                self.ops[e].append((wl, None, None, 0))

    def dma(self, q, out_ap, in_ap, reads=(), writes=(), **kw):
        return self.op(q, lambda e: e.dma_start(out=out_ap, in_=in_ap, **kw), reads, writes, dma=True)

    def mm(self, out, lhsT, rhs, start, stop, reads, writes):
        return self.op("pe", lambda e: e.matmul(out, lhsT, rhs, start=start, stop=stop), reads, writes)

    def tr(self, out, in_, ident, reads, writes):
        return self.op("pe", lambda e: e.transpose(out, in_, ident), reads, writes)

    def act(self, out, in_, func, reads, writes, **kw):
        return self.op("act", lambda e: e.activation(out=out, in_=in_, func=func, **kw), reads, writes)

    def copy(self, eng, out, in_, reads, writes):
        if eng == "act":
            return self.act(out, in_, AF.Copy, reads, writes)
        return self.op(eng, lambda e: e.tensor_copy(out, in_), reads, writes)

    def tt(self, eng, out, in0, in1, op, reads, writes):
        return self.op(eng, lambda e: e.tensor_tensor(out, in0, in1, op), reads, writes)

    def ts(self, eng, out, in0, s1, s2, op0, op1, reads, writes):
        if s2 is None:
            return self.op(eng, lambda e: e.tensor_scalar(out, in0, s1, None, op0), reads, writes)
        return self.op(eng, lambda e: e.tensor_scalar(out, in0, s1, s2, op0, op1), reads, writes)

    def stt(self, eng, out, in0, scalar, in1, op0, op1, reads, writes):
        return self.op(eng, lambda e: e.scalar_tensor_tensor(out, in0, scalar, in1, op0, op1), reads, writes)

    def memset(self, eng, out, val, writes):
        return self.op(eng, lambda e: e.memset(out, val), (), writes)

    def final_wait(self, eng, bufs):
        waits = {}
        for b in bufs:
            self._need("__none__", b.last_w, waits)
        self.ops[eng].append(([(self._sem(k), v) for k, v in waits.items()], None, None, 0))

    def emit(self):
        handles = {"pe": "tensor", "act": "scalar", "dve": "vector", "pool": "gpsimd", "sp": "sync"}
        with self.nc.Block() as block:
            for e in self.ENGS:
                lst = self.ops[e]
                if not lst:
                    continue

                def body(engh, lst=lst):
                    for wl, fn, sem, inc in lst:
                        for s, v in wl:
                            engh.wait_ge(s, v)
                        if fn is not None:
                            fn(engh).then_inc(sem, inc)

                getattr(block, handles[e])(body)


class G:
    pass


def rstd_from_ssq(kb, out, ssq, n, reads, writes):
    kb.act(out, ssq, AF.Sqrt, reads, writes, scale=1.0 / n, bias=EPS)
    kb.op("dve", lambda e: e.reciprocal(out, out), writes, writes)


def phase_mod(g, L):
    kb, nc = g.kb, g.kb.nc
    with ExitStack() as sc:
        cond = kb.sbuf("cond", [128, 8, 2], ctx=sc)
        condbc = kb.sbuf("condbc", [128, 16, 128], ctx=sc)
        mbF = kb.sbuf("mbF", [128, 48], ctx=sc)
        mbrow = kb.sbuf("mbrow", [1, 6144], ctx=sc)
        gF = kb.sbuf("gF", [128, 2, 8], ctx=sc)
        modF = kb.sbuf("modF", [128, 32, 2], ctx=sc)
        grow = kb.sbuf("grow", [1, 512], ctx=sc)
        mw = [kb.sbuf("mw%d" % k, [128, 3072], ctx=sc) for k in range(8)]
        for s in range(2):
            kb.dma("sp", cond[:, :, s], g.cvec.ap[s, :].rearrange("(k p) -> p k", p=128), [g.cvec], [cond],
                   allow_slow_non_contiguous=True)
        kb.act(cond[:], cond[:], AF.Silu, [cond], [cond])
        kb.copy("dve", condbc[:], cond[:].rearrange("p k s -> p (k s)").unsqueeze(2).to_broadcast([128, 16, 128]),
                [cond], [condbc])
        kb.dma("sp", mbF[:], g.mod_b.ap[L, :].rearrange("(c p) -> p c", p=128), [g.mod_b], [mbF],
               allow_slow_non_contiguous=True)
        kb.dma("sp", mbrow[:], g.mod_b.ap[L:L + 1, :], [g.mod_b], [mbrow])
        kb.dma("sp", gF[:, 0, :], g.norm1_g.ap[L, :].rearrange("(c p) -> p c", p=128), [g.norm1_g], [gF],
               allow_slow_non_contiguous=True)
        kb.dma("sp", gF[:, 1, :], g.norm2_g.ap[L, :].rearrange("(c p) -> p c", p=128), [g.norm2_g], [gF],
               allow_slow_non_contiguous=True)
        psF, psB = g.ps[0], g.ps[1]
        for h in range(2):
            for k in range(8):
                kb.dma("sp" if k % 2 == 0 else "pool", mw[k][:],
                       g.mod_w.ap[L, 128 * k:128 * k + 128, 3072 * h:3072 * h + 3072], [g.mod_w], [mw[k]])
            for j in range(16):
                for k in range(8):
                    kb.mm(psF[:, 2 * j:2 * j + 2], mw[k][:, 128 * j:128 * j + 128], cond[:, k, :], k == 0, k == 7,
                          [mw[k], cond], [psF])
            kb.tt("dve", modF[:, 16 * h:16 * h + 16, :], psF[:, 0:32].rearrange("p (j s) -> p j s", s=2),
                  mbF[:, 24 * h:24 * h + 16].unsqueeze(2).to_broadcast([128, 16, 2]), ALU.add, [psF, mbF], [modF])
            for s in range(2):
                for cc in range(2):
                    for k in range(8):
                        kb.mm(psB[:, :], condbc[:, 2 * k + s, :], mw[k][:, 2048 + 512 * cc:2048 + 512 * cc + 512],
                              k == 0, k == 7, [mw[k], condbc], [psB])
                    c0 = 3072 * h + 2048 + 512 * cc
                    kb.tt("dve", grow[0:1, :], psB[0:1, :], mbrow[0:1, c0:c0 + 512], ALU.add, [psB, mbrow], [grow])
                    kb.dma("sp", g.gates.ap[L, h, s:s + 1, 512 * cc:512 * cc + 512], grow[0:1, :], [grow], [g.gates])
        for h in range(2):
            sc_ap = modF[:, 16 * h + 8:16 * h + 16, :]
            kb.ts("dve", g.GS[:, L, h, :, :], sc_ap, 1.0, None, ALU.add, None, [modF], [g.GSb])
            kb.tt("dve", g.GS[:, L, h, :, :], g.GS[:, L, h, :, :], gF[:, h, :].unsqueeze(2).to_broadcast([128, 8, 2]),
                  ALU.mult, [gF, g.GSb], [g.GSb])
            kb.copy("dve", g.SH[:, L, h, :, :], modF[:, 16 * h:16 * h + 8, :], [modF], [g.SHb])
    kb.barrier()


def norm_tile_to_hT(g, L, h, xt, tile_is_ctx, hT_out_aps, hT_buf, scr, also_f32=None, xap=None, f32_buf=None):
    kb = g.kb
    s = 1 if tile_is_ctx else 0
    junk, ssq, xn = scr["junk"], scr["ssq"], scr["xn"]
    if xap is None:
        xap = xt[:]
    kb.act(junk[:], xap, AF.Square, [xt, ssq], [junk, ssq], accum_out=ssq[:, 0:1])
    rstd_from_ssq(kb, ssq[:, 0:1], ssq[:, 0:1], D, [ssq], [ssq])
    kb.act(xn[:], xap, AF.Copy, [xt, ssq], [xn], scale=ssq[:, 0:1])
    pT = scr["psT"]
    for c in range(8):
        kb.tr(pT[c // 4][:, (c % 4) * 128:(c % 4) * 128 + 128], xn[:, c * 128:c * 128 + 128], g.ident[:], [xn, g.ident],
              [pT[c // 4]])
    for c in range(8):
        kb.act(hT_out_aps[c], pT[c // 4][:, (c % 4) * 128:(c % 4) * 128 + 128], AF.Identity, [pT[c // 4], g.GSb, g.SHb],
               [hT_buf], scale=g.GS[:, L, h, c, s:s + 1], bias=g.SH[:, L, h, c, s:s + 1])
        if also_f32 is not None:
            kb.act(also_f32[c], pT[c // 4][:, (c % 4) * 128:(c % 4) * 128 + 128], AF.Identity,
                   [pT[c // 4], g.GSb, g.SHb], [f32_buf], scale=g.GS[:, L, h, c, s:s + 1], bias=g.SH[:, L, h, c, s:s + 1])


def load_cast_weight(g, sc, name, dram_buf, src_ap_fn, nk, ncols, stage_bufs, dst, dst_ap_fn):
    kb = g.kb
    for k in range(nk):
        st = stage_bufs[k % len(stage_bufs)]
        kb.dma("sp" if k % 2 == 0 else "pool", st[:, 0:ncols], src_ap_fn(k), [dram_buf], [st])
        kb.copy("pool" if k % 2 == 0 else "dve", dst_ap_fn(k), st[:, 0:ncols], [st], [dst])


def phase_a(g, L, xs_tiles):
    kb = g.kb
    with ExitStack() as sc:
        winb = kb.sbuf("winb", [128, 8, IN_COLS], BF16, ctx=sc)
        stage = [kb.sbuf("wst%d" % i, [128, IN_COLS], ctx=sc) for i in range(2)]
        load_cast_weight(g, sc, "win", g.w_in, lambda k: g.w_in.ap[L, 128 * k:128 * k + 128, :], 8, IN_COLS, stage, winb,
                         lambda k: winb[:, k, :])
        scr = {"junk": kb.sbuf("junk", [128, 1024], ctx=sc), "ssq": kb.sbuf("ssq", [128, 1], ctx=sc),
               "xn": kb.sbuf("xn", [128, 1024], ctx=sc), "psT": [g.ps[0], g.ps[1]]}
        xts = [kb.sbuf("xt%d" % i, [128, 1024], ctx=sc) for i in range(2)]
        hTs = [kb.sbuf("hT%d" % i, [128, 8, 512], BF16, ctx=sc) for i in range(2)]
        toks = [kb.sbuf("tok%d" % i, [128, TK_W], ctx=sc) for i in range(2)]
        fms = [kb.sbuf("fm%d" % i, [128, 512], ctx=sc) for i in range(3)]
        psTM = [g.ps[2], g.ps[3], g.ps[4]]
        psFM = [g.ps[5], g.ps[6]]
        blocks = [(0, 2)] + [(2 + 4 * i, 4) for i in range(8)]
        ti = 0
        fi = 0
        for bi, (t0, nt) in enumerate(blocks):
            hT = hTs[bi % 2]
            ntok = nt * 128
            for j in range(nt):
                t = t0 + j
                xt = xts[ti % 2]
                tok = toks[ti % 2]
                ti += 1
                kb.dma("sp", xt[:], xs_tiles[t].ap, [xs_tiles[t]], [xt])
                norm_tile_to_hT(g, L, 0, xt, t < 2, [hT[:, c, j * 128:j * 128 + 128] for c in range(8)], hT, scr)
                segs = [(psTM[0], 0, 512, C_GATE), (psTM[1], 0, 512, C_GATE + 512), (psTM[2], 0, 24, C_GATE + 1024),
                        (psTM[2], 24, 256, C_V)]
                for ps, o0, w, c0 in segs:
                    for k in range(8):
                        kb.mm(ps[:, o0:o0 + w], hT[:, k, j * 128:j * 128 + 128], winb[:, k, c0:c0 + w], k == 0, k == 7,
                              [hT, winb], [ps])
                kb.copy("dve", tok[:, 0:512], psTM[0][:, :], [psTM[0]], [tok])
                kb.copy("act", tok[:, 512:1024], psTM[1][:, :], [psTM[1]], [tok])
                kb.copy("dve", tok[:, 1024:1304], psTM[2][:, 0:280], [psTM[2]], [tok])
                kb.dma("sp", g.TOK[t].ap, tok[:], [tok], [g.TOK[t]])
            for fc in range(11):
                c0 = C_QKV + 128 * fc if fc < 9 else C_U + 128 * (fc - 9)
                ps = psFM[fi % 2]
                fm = fms[fi % 3]
                fi += 1
                for k in range(8):
                    kb.mm(ps[:, 0:ntok], winb[:, k, c0:c0 + 128], hT[:, k, 0:ntok], k == 0, k == 7, [hT, winb], [ps])
                if fc < 9:
                    kb.copy("act" if fc % 2 == 0 else "dve", fm[:, 0:ntok], ps[:, 0:ntok], [ps], [fm])
                else:
                    kb.act(fm[:, 0:ntok], ps[:, 0:ntok], AF.Gelu, [ps], [fm])
                kb.dma("pool", g.ZQ[fc][bi].ap, fm[:, 0:ntok], [fm], [g.ZQ[fc][bi]])
    kb.barrier()


def phase_attn(g, L, with_ctx, nblk=8):
    kb = g.kb
    with ExitStack() as sc:
        QT = [kb.sbuf("QT%d" % p, [128, T], BF16, ctx=sc) for p in range(3)]
        KT = kb.sbuf("KT", [128, T], BF16, ctx=sc)
        VE = kb.sbuf("VE", [128, NT, 2, 128], BF16, ctx=sc)
        gain = kb.sbuf("gain", [128, 8, 64], ctx=sc)
        kb.memset("dve", VE[:], 1.0, [VE])
        kb.dma("sp", gain[:, 0, :], g.q_norm_g.ap[L:L + 1, :].to_broadcast([128, 64]), [g.q_norm_g], [gain])
        kb.dma("sp", gain[:, 6, :], g.k_norm_g.ap[L:L + 1, :].to_broadcast([128, 64]), [g.k_norm_g], [gain])
        kb.ts("dve", gain[:, 0, :], gain[:, 0, :], 0.125, None, ALU.mult, None, [gain], [gain])
        kb.copy("dve", gain[:, 1:6, :], gain[:, 0:1, :].to_broadcast([128, 5, 64]), [gain], [gain])
        kb.copy("dve", gain[:, 7, :], gain[:, 6, :], [gain], [gain])
        qks = [kb.sbuf("qk%d" % i, [128, 512], ctx=sc) for i in range(2)]
        v32s = [kb.sbuf("v32%d" % i, [128, 128], ctx=sc) for i in range(2)]
        css = [kb.sbuf("cs%d" % i, [128, 64], ctx=sc) for i in range(2)]
        sqt = kb.sbuf("sqt", [128, 512], ctx=sc)
        ssq = kb.sbuf("ssq8", [128, 8], ctx=sc)
        qn = kb.sbuf("qn", [128, 512], ctx=sc)
        qr = kb.sbuf("qr", [128, 512], ctx=sc)
        tm = [kb.sbuf("ropet%d" % i, [128, 256], ctx=sc) for i in range(4)]
        psTr = g.ps[0]
        import os as _os
        STG = int(_os.environ.get("ATTN_STG", "9"))
        for t in range(int(_os.environ.get("ATTN_NT", str(NT)))):
            qk, v32, cs = qks[t % 2], v32s[t % 2], css[t % 2]
            kb.dma("sp", qk[:], g.TOK[t].ap[:, TK_AQ:TK_AQ + 512], [g.TOK[t]], [qk])
            kb.dma("pool", v32[:], g.TOK[t].ap[:, TK_AV:TK_AV + 128], [g.TOK[t]], [v32])
            kb.copy("pool", VE[:, t, :, 0:64], v32[:].rearrange("p (k e) -> p k e", k=2), [v32], [VE])
            if STG < 2:
                continue
            kb.tt("pool", sqt[:], qk[:], qk[:], ALU.mult, [qk], [sqt])
            kb.op("dve", lambda e, o=ssq[:, 0:8], i=sqt[:].rearrange("p (h d) -> p h d", h=8): e.reduce_sum(o, i, AX.X),
                  [sqt], [ssq])
            rstd_from_ssq(kb, ssq[:, 0:8], ssq[:, 0:8], 64, [ssq], [ssq])
            if STG < 3:
                continue
            q3 = qn[:].rearrange("p (h d) -> p h d", h=8)
            kb.tt("dve", q3, qk[:].rearrange("p (h d) -> p h d", h=8), ssq[:, 0:8].unsqueeze(2).to_broadcast([128, 8, 64]),
                  ALU.mult, [qk, ssq], [qn])
            if STG < 4:
                continue
            if t >= 2:
                kb.tt("pool", q3, q3, gain[:], ALU.mult, [qn, gain], [qn])
                if STG < 5:
                    continue
                kb.dma("sp", cs[:], g.rope.ap[128 * (t - 2):128 * (t - 2) + 128, :], [g.rope], [cs])
                q5 = qn[:].rearrange("p (h a b r) -> p h a b r", h=8, a=2, b=2, r=16)
                o5 = qr[:].rearrange("p (h a b r) -> p h a b r", h=8, a=2, b=2, r=16)
                for ax in [int(v) for v in _os.environ.get("ROPE_AX", "0,1").split(",") if v != ""]:
                    a_, b_ = q5[:, :, ax, 0, :], q5[:, :, ax, 1, :]
                    cos = cs[:, 16 * ax:16 * ax + 16].unsqueeze(1).to_broadcast([128, 8, 16])
                    sin = cs[:, 32 + 16 * ax:32 + 16 * ax + 16].unsqueeze(1).to_broadcast([128, 8, 16])
                    tv = [x[:, 128 * ax:128 * ax + 128].rearrange("p (h r) -> p h r", h=8) for x in tm]
                    kb.tt("dve", tv[0], a_, cos, ALU.mult, [qn, cs], [tm[0]])
                    kb.tt("dve", tv[1], b_, sin, ALU.mult, [qn, cs], [tm[1]])
                    kb.tt("dve", o5[:, :, ax, 0, :], tv[0], tv[1], ALU.subtract, [tm[0], tm[1]], [qr])
                    kb.tt("dve", tv[2], a_, sin, ALU.mult, [qn, cs], [tm[2]])
                    kb.tt("dve", tv[3], b_, cos, ALU.mult, [qn, cs], [tm[3]])
                    kb.tt("dve", o5[:, :, ax, 1, :], tv[2], tv[3], ALU.add, [tm[2], tm[3]], [qr])
            else:
                kb.tt("pool", qr[:].rearrange("p (h d) -> p h d", h=8), q3, gain[:], ALU.mult, [qn, gain], [qr])
            if STG < 6:
                continue
            for p in range(3):
                kb.tr(psTr[:, p * 128:p * 128 + 128], qr[:, p * 128:p * 128 + 128], g.ident[:], [qr, g.ident], [psTr])
            kb.tr(psTr[:, 384:512], qr[:, 384:512], g.ident[:], [qr, g.ident], [psTr])
            if STG < 7:
                continue
            for p in range(3):
                kb.copy("act", QT[p][:, t * 128:t * 128 + 128], psTr[:, p * 128:p * 128 + 128], [psTr], [QT[p]])
            if STG < 8:
                continue
            kb.copy("act", KT[:, t * 128:t * 128 + 128], psTr[:, 384:512], [psTr], [KT])
        import os as _os
        if _os.environ.get("ATTN_PREP_ONLY"):
            kb.barrier()
            return
        psS = [g.ps[1], g.ps[2], g.ps[3]]
        accVs, accDs = [g.ps[4], g.ps[5]], [g.ps[6], g.ps[7]]
        PTs = [kb.sbuf("PT%d" % i, [128, 512], BF16, ctx=sc) for i in range(3)]
        rcs = [kb.sbuf("rc%d" % i, [64, 512], ctx=sc) for i in range(2)]
        dens = [kb.sbuf("den%d" % i, [128, 512], ctx=sc) for i in range(2)]
        for dn_ in dens:
            kb.memset("dve", dn_[:], 0.0, [dn_])
        ots = [kb.sbuf("ot%d" % i, [64, 512], ctx=sc) for i in range(2)]
        qblocks = ([(0, 0, 256, [0, 1])] if with_ctx else []) + \
                  [(1 + i, 256 + 512 * i, 512, list(range(NT))) for i in range(nblk)]
        items = []
        bi_ = 0
        for h in range(6):
            for (blk, q0, nq, kts) in qblocks:
                for ii, kt in enumerate(kts):
                    items.append((h, blk, q0, nq, kt, ii == 0, ii == len(kts) - 1, bi_))
                bi_ += 1
        LA = 2

        def emit_qk(i):
            h, blk, q0, nq, kt, first, last, bn = items[i]
            kv, p = h // 3, h % 3
            pr = slice(64 * kv, 64 * kv + 64)
            ps, PT = psS[i % 3], PTs[i % 3]
            kb.mm(ps[:, 0:nq], KT[pr, kt * 128:kt * 128 + 128], QT[p][pr, q0:q0 + nq], True, True, [KT, QT[p]], [ps])
            kb.act(PT[:, 0:nq], ps[:, 0:nq], AF.Exp, [ps], [PT])

        def emit_pv(i):
            h, blk, q0, nq, kt, first, last, bn = items[i]
            kv = h // 3
            PT = PTs[i % 3]
            acc, dP = accVs[bn % 2], accDs[bn % 2]
            kb.mm(acc[:, 0:nq], VE[:, kt, kv, :], PT[:, 0:nq], first, last, [VE, PT], [acc])
            if last:
                rc, ot, den = rcs[bn % 2], ots[bn % 2], dens[bn % 2]
                kb.copy("act", den[64:128, 0:nq], acc[64:128, 0:nq], [acc], [den])
                kb.mm(dP[0:64, 0:nq], g.ident[:, 64:128], den[:, 0:nq], True, True, [g.ident, den], [dP])
                kb.op("dve", lambda e, o=rc[:, 0:nq], i_=dP[0:64, 0:nq]: e.reciprocal(o, i_), [dP], [rc])
                kb.tt("dve", ot[:, 0:nq], acc[0:64, 0:nq], rc[:, 0:nq], ALU.mult, [acc, rc], [ot])
                kb.dma("sp", g.MIXat[h][blk].ap, ot[:, 0:nq], [ot], [g.MIXat[h][blk]])

        n_it = len(items)
        for i in range(n_it + LA):
            if i < n_it:
                emit_qk(i)
            if i - LA >= 0:
                emit_pv(i - LA)
    kb.barrier()


def phase_sgu(g, L, tiles):
    kb = g.kb
    with ExitStack() as sc:
        WsT = kb.sbuf("WsT", [128, 4, 128], BF16, ctx=sc)
        ws32 = kb.sbuf("ws32", [128, 4, 128], ctx=sc)
        SB = kb.sbuf("SBb", [64, 512], ctx=sc)
        sgain = kb.sbuf("sgain", [128, 256], ctx=sc)
        psW = g.ps[0]
        for gi in range(4):
            kb.dma("sp", ws32[:, gi, :], g.sgu_w.ap[L, gi, :, :], [g.sgu_w], [ws32])
        for gi in range(4):
            kb.tr(psW[:, gi * 128:gi * 128 + 128], ws32[:, gi, :], g.ident[:], [ws32, g.ident], [psW])
        kb.copy("dve", WsT[:].rearrange("p g i -> p (g i)"), psW[:, :], [psW], [WsT])
        kb.dma("sp", SB[:], g.sgu_b.ap[L:L + 1, :, :].rearrange("o g i -> o (g i)").to_broadcast([64, 512]), [g.sgu_b], [SB])
        kb.dma("sp", sgain[:], g.sgu_norm_g.ap[L:L + 1, :].to_broadcast([128, 256]), [g.sgu_norm_g], [sgain])
        zvs = [kb.sbuf("zv%d" % i, [128, 256], ctx=sc) for i in range(2)]
        uts = [kb.sbuf("ut%d" % i, [64, 4, 128], ctx=sc) for i in range(2)]
        gv = kb.sbuf("gv", [128, 256], ctx=sc)
        sq = kb.sbuf("sgsq", [128, 256], ctx=sc)
        ss = kb.sbuf("sgss", [128, 4], ctx=sc)
        vb = kb.sbuf("vb", [128, 256], BF16, ctx=sc)
        tmps = [kb.sbuf("sgt%d" % i, [64, 512], ctx=sc) for i in range(2)]
        ress = [kb.sbuf("sgr%d" % i, [64, 512], ctx=sc) for i in range(2)]
        pss = [g.ps[1], g.ps[2]]
        for n, t in enumerate(tiles):
            zv, ut, tmp, res, ps = zvs[n % 2], uts[n % 2], tmps[n % 2], ress[n % 2], pss[n % 2]
            bi, boff = g.blk_of_tile(t)
            kb.dma("sp", zv[:], g.TOK[t].ap[:, TK_ZV:TK_ZV + 256], [g.TOK[t]], [zv])
            kb.dma("pool", ut[:], g.zqd[1152:1408, 128 * t:128 * t + 128].rearrange("(g d) t -> d g t", d=64),
                   [g.ZQ[9][bi], g.ZQ[10][bi]], [ut])
            kb.act(gv[:], zv[:], AF.Gelu_apprx_tanh, [zv], [gv])
            kb.tt("pool", sq[:], gv[:], gv[:], ALU.mult, [gv], [sq])
            kb.op("dve", lambda e, o=ss[:, 0:4], i=sq[:].rearrange("p (g d) -> p g d", g=4): e.reduce_sum(o, i, AX.X),
                  [sq], [ss])
            rstd_from_ssq(kb, ss[:, 0:4], ss[:, 0:4], 64, [ss], [ss])
            kb.tt("dve", gv[:].rearrange("p (g d) -> p g d", g=4), gv[:].rearrange("p (g d) -> p g d", g=4),
                  ss[:, 0:4].unsqueeze(2).to_broadcast([128, 4, 64]), ALU.mult, [gv, ss], [gv])
            kb.tt("pool", vb[:], gv[:], sgain[:], ALU.mult, [gv, sgain], [vb])
            for gi in range(4):
                kb.mm(ps[0:64, gi * 128:gi * 128 + 128], vb[:, gi * 64:gi * 64 + 64], WsT[:, gi, :], True, True, [vb, WsT], [ps])
            kb.tt("dve", tmp[:], ps[0:64, :], SB[:], ALU.add, [ps, SB], [tmp])
            kb.tt("pool", res[:], tmp[:], ut[:].rearrange("d g t -> d (g t)"), ALU.mult, [tmp, ut], [res])
            kb.dma("sp", g.mixd[768:1024, 128 * t:128 * t + 128].rearrange("(g d) t -> d g t", d=64),
                   res[:].rearrange("d (g t) -> d g t", g=4), [res], [g.MIXsg[t]])
    kb.barrier()


class _B:
    pass


def phase_dn(g, L, half_mode=False):
    kb = g.kb
    ident = g.ident
    with ExitStack() as sc:
        def S(name, shape, dt=F32):
            return kb.sbuf(name, shape, dt, ctx=sc)
        dnc = S("dnc", [128, 7, 128])
        kb.dma("sp", dnc[:], g.dncd.ap.rearrange("c p i -> p c i"), [g.dncd], [dnc])
        ones = S("ones32", [128, 128])
        kb.memset("dve", ones[:], 1.0, [ones])
        I4 = S("I4", [128, 4, 128])
        NM4 = [S("NM4%d" % d, [128, 4, 128]) for d in range(2)]
        SM4 = [S("SM4%d" % d, [128, 4, 128]) for d in range(2)]
        for c in range(4):
            kb.copy("dve", I4[:, c, :], ident[:], [ident], [I4])
            for d in range(2):
                kb.copy("dve", NM4[d][:, c, :], dnc[:, 3 + d, :], [dnc], [NM4[d]])
                kb.copy("dve", SM4[d][:, c, :], dnc[:, 5 + d, :], [dnc], [SM4[d]])
        BA = S("BA", [128, 68, 24])
        src = g.tokd[:, TK_BETA:TK_BETA + 24].rearrange("(c i) f -> i c f", i=64)
        kb.dma("sp", BA[0:64, :, :], src, g.TOK, [BA])
        kb.dma("pool", BA[64:128, :, :], src, g.TOK, [BA])
        ZB, ZA = S("ZB", [128, 6, 68]), S("ZA", [128, 6, 68])
        for d in range(2):
            for half in range(2):
                pr = slice(64 * half, 64 * half + 64)
                kb.copy("dve", ZB[pr, 3 * d:3 * d + 3, :], BA[pr, :, 6 * d + half:6 * d + 6:2].rearrange("i c p -> i p c"), [BA], [ZB])
                kb.copy("dve", ZA[pr, 3 * d:3 * d + 3, :],
                        BA[pr, :, 12 + 6 * d + half:12 + 6 * d + 6:2].rearrange("i c p -> i p c"), [BA], [ZA])
        AL, DTB, NEA = S("AL", [128, 6]), S("DTB", [128, 6]), S("NEA", [128, 6])
        for half in range(2):
            pr = slice(64 * half, 64 * half + 64)
            kb.dma("sp", AL[pr, :].rearrange("p (d q) -> p d q", d=2), g.dn_a_log.ap[L:L + 1, :, half::2].to_broadcast([64, 2, 3]),
                   [g.dn_a_log], [AL], allow_slow_non_contiguous=True)
            kb.dma("sp", DTB[pr, :].rearrange("p (d q) -> p d q", d=2), g.dn_dt_bias.ap[L:L + 1, :, half::2].to_broadcast([64, 2, 3]),
                   [g.dn_dt_bias], [DTB], allow_slow_non_contiguous=True)
        kb.act(NEA[:], AL[:], AF.Exp, [AL], [NEA])
        kb.ts("dve", NEA[:], NEA[:], -1.0, None, ALU.mult, None, [NEA], [NEA])
        NBETA, GT, GC, GL, E, KTS, EGL = [S(n, [128, 6, 68]) for n in ("NBETA", "GT", "GC", "GL", "E", "KTS", "EGL")]
        kb.act(NBETA[:], ZB[:], AF.Sigmoid, [ZB], [NBETA])
        kb.ts("dve", NBETA[:], NBETA[:], -1.0, None, ALU.mult, None, [NBETA], [NBETA])
        kb.tt("dve", GT[:], ZA[:], DTB[:].unsqueeze(2).to_broadcast([128, 6, 68]), ALU.add, [ZA, DTB], [GT])
        kb.act(GT[:], GT[:], AF.Exp, [GT], [GT])
        kb.act(GT[:], GT[:], AF.Ln, [GT], [GT], bias=1.0)
        kb.tt("dve", GT[:], GT[:], NEA[:].unsqueeze(2).to_broadcast([128, 6, 68]), ALU.mult, [GT, NEA], [GT])
        ps = g.ps[0]
        kb.mm(ps[:, 0:204], dnc[:, 1, :], GT[:, 0:3, :].rearrange("p a c -> p (a c)"), True, True, [dnc, GT], [ps])
        kb.mm(ps[:, 204:408], dnc[:, 2, :], GT[:, 3:6, :].rearrange("p a c -> p (a c)"), True, True, [dnc, GT], [ps])
        kb.copy("dve", GC[:].rearrange("p a c -> p (a c)"), ps[:, 0:408], [ps], [GC])
        kb.mm(ps[:, 0:408], dnc[:, 0, :], GT[:].rearrange("p a c -> p (a c)"), True, True, [dnc, GT], [ps])
        kb.copy("dve", GL[:].rearrange("p a c -> p (a c)"), ps[:, 0:408], [ps], [GL])
        kb.act(E[:], GC[:], AF.Exp, [GC], [E])
        kb.act(EGL[:], GL[:], AF.Exp, [GL], [EGL])
        kb.tt("dve", KTS[:], GL[:], GC[:], ALU.subtract, [GL, GC], [KTS])
        kb.act(KTS[:], KTS[:], AF.Exp, [KTS], [KTS])
        cw = S("cw", [128, 9, 3])
        for fc in range(9):
            kb.dma("sp", cw[:, fc, :], g.conv_w.ap[L, :, 128 * fc:128 * fc + 128].rearrange("k p -> p k"), [g.conv_w], [cw],
                   allow_slow_non_contiguous=True)
        dgain = S("dgain", [64, 1])
        kb.dma("sp", dgain[:], g.dn_norm_g.ap[L, :].rearrange("(e o) -> e o", o=1), [g.dn_norm_g], [dgain],
               allow_slow_non_contiguous=True)
        qn, kn, vn, zr = S("dqn", [128, T]), S("dkn", [128, T]), S("dvn", [128, T]), S("zraw", [128, T])
        sqb = S("dsqb", [128, 512])
        rsb = S("drsb", [128, 512])
        OB = S("OB", [128, 68, 64])
        bufs = []
        for d in range(2):
            B = _B()
            for n in ("kT", "qT", "vT", "kBD", "diag", "D", "attnT", "N", "NT", "P2", "PT2", "R", "kt", "tmp"):
                setattr(B, n, S("%s%d" % (n, d), [128, 4, 128]))
            B.vst = S("vst%d" % d, [128, 4, 64])
            B.ntmp, B.vnew, B.o1 = S("ntmp%d" % d, [128, 64]), S("vnew%d" % d, [128, 64]), S("o1%d" % d, [128, 64])
            B.S = [S("S%d_%d" % (d, i), [128, 64]) for i in range(2)]
            B.banks = g.ps[4 * d:4 * d + 4]
            for t_ in (B.kT, B.qT, B.vT):
                kb.memset("dve", t_[:], 0.0, [t_])
            bufs.append(B)
        gts = [S("dgt%d" % i, [128, 4, 64]) for i in range(2)]
        otb = [S("dot%d" % i, [64, 512]) for i in range(2)]
        oss = S("doss", [128, 4])
        f2 = lambda ap: ap.rearrange("p c i -> p (c i)")

        def conv_gen(pp):
                for xi, dst in enumerate((qn, kn, vn)):
                    fc = 3 * xi + pp
                    for bi, (b0, n) in enumerate(BLOCKS):
                        kb.dma("sp" if bi % 2 == 0 else "pool", zr[:, b0:b0 + n], g.ZQ[fc][bi].ap, [g.ZQ[fc][bi]], [zr])
                        yield
                    kb.act(dst[:], zr[:], AF.Copy, [zr, cw], [dst], scale=cw[:, fc, 1:2])
                    yield
                    for (a0, a1) in ((0, NCTX), (NCTX, T)):
                        kb.stt("dve", dst[:, a0 + 1:a1], zr[:, a0:a1 - 1], cw[:, fc, 0:1], dst[:, a0 + 1:a1], ALU.mult, ALU.add,
                               [zr, cw, dst], [dst])
                        yield
                        kb.stt("dve", dst[:, a0:a1 - 1], zr[:, a0 + 1:a1], cw[:, fc, 2:3], dst[:, a0:a1 - 1], ALU.mult, ALU.add,
                               [zr, cw, dst], [dst])
                        yield
                    kb.act(dst[:], dst[:], AF.Silu, [dst], [dst])
                    yield
                    if xi < 2:
                        for (b0, n) in BLOCKS:
                            kb.tt("dve", sqb[:, 0:n], dst[:, b0:b0 + n], dst[:, b0:b0 + n], ALU.mult, [dst], [sqb])
                            yield
                            kb.mm(g.ps[5][:, 0:n], dnc[:, 0, :], sqb[:, 0:n], True, True, [dnc, sqb], [g.ps[5]])
                            yield
                            sc_ = 64.0 if xi == 0 else 1.0
                            kb.act(rsb[:, 0:n], g.ps[5][:, 0:n], AF.Sqrt, [g.ps[5]], [rsb], scale=sc_, bias=sc_ * EPS)
                            yield
                            kb.op("dve", lambda e, o=rsb[:, 0:n]: e.reciprocal(o, o), [rsb], [rsb])
                            yield
                            kb.tt("dve", dst[:, b0:b0 + n], dst[:, b0:b0 + n], rsb[:, 0:n], ALU.mult, [dst, rsb], [dst])
                            yield

        def out_gen(pp):
                pO = g.ps[0]
                for grp in (range(1, 9) if half_mode else range(17)):
                    gt, ot = gts[grp % 2], otb[grp % 2]
                    c0, t0 = 4 * grp, 256 * grp
                    for ab in range(2):
                        h = 2 * pp + ab
                        kb.dma("sp" if ab == 0 else "pool", gt[64 * ab:64 * ab + 64, :, :],
                               g.tokd[t0:t0 + 256, TK_GATE + 64 * h:TK_GATE + 64 * h + 64].rearrange("(c i) e -> i c e", i=64),
                               [g.TOK[2 * grp], g.TOK[2 * grp + 1]], [gt])
                        yield
                    kb.act(gt[:], gt[:], AF.Silu, [gt], [gt])
                    yield
                    ob = OB[:, c0:c0 + 4, :]
                    tmp3 = bufs[0].tmp[:, 0:2, :].rearrange("p a (b e) -> p (a b) e", e=64)
                    kb.tt("dve", tmp3, ob, ob, ALU.mult, [OB], [bufs[0].tmp])
                    yield
                    kb.op("dve", lambda e, o=oss[:, 0:4], i=tmp3: e.reduce_sum(o, i, AX.X), [bufs[0].tmp], [oss])
                    yield
                    rstd_from_ssq(kb, oss[:, 0:4], oss[:, 0:4], 64, [oss], [oss])
                    kb.tt("dve", ob, ob, oss[:, 0:4].unsqueeze(2).to_broadcast([128, 4, 64]), ALU.mult, [OB, oss], [OB])
                    yield
                    kb.tt("dve", ob, ob, gt[:], ALU.mult, [OB, gt], [OB])
                    yield
                    for c in range(4):
                        kb.tr(pO[0:64, c * 128:c * 128 + 128], OB[:, c0 + c, :], ident[:], [OB, ident], [pO])
                        yield
                    kb.act(ot[:, :], pO[0:64, :], AF.Copy, [pO, dgain], [ot], scale=dgain[:, 0:1])
                    yield
                    for ab in range(2):
                        h = 2 * pp + ab
                        kb.dma("sp" if ab == 0 else "pool",
                               g.mixd[64 * h:64 * h + 64, t0:t0 + 256].rearrange("e (c i) -> e c i", c=4),
                               ot[:, :].rearrange("e (c ab i) -> e c ab i", c=4, ab=2)[:, :, ab, :], [ot],
                               [g.MIXdn[2 * grp], g.MIXdn[2 * grp + 1]])
                        yield

        def run_all(gen):
            for _ in gen:
                pass

        def interleave(ga, gb):
            live = [True, True]
            gs = [ga, gb]
            while any(live):
                for i_ in range(2):
                    if live[i_]:
                        try:
                            next(gs[i_])
                        except StopIteration:
                            live[i_] = False

        run_all(conv_gen(0))
        for pp in range(3):
            for d in range(2):
                kb.memset("dve", bufs[d].S[0][:], 0.0, [bufs[d].S[0]])
            written = set()
            scnt = [0, 0]

            def prep(d, grp):
                B = bufs[d]
                dp, c0, t0 = 3 * d + pp, 4 * grp, 256 * grp
                pA, pB, pC, pD = B.banks
                for dst, src_ in ((B.kT, kn), (B.qT, qn), (B.vT, vn)):
                    for half in range(2):
                        pr = slice(64 * half, 64 * half + 64)
                        kb.copy("dve", dst[pr, :, 64 * half:64 * half + 64],
                                src_[pr, t0:t0 + 256].rearrange("p (c i) -> p c i", c=4), [src_], [dst])
                        yield
                for c in range(4):
                    kb.tr(pA[:, c * 128:c * 128 + 128], B.kT[:, c, :], ident[:], [B.kT, ident], [pA])
                    yield
                kb.copy("act", f2(B.kBD[:]), pA[:, :], [pA], [B.kBD])
                yield
                for c in range(4):
                    kb.tr(pA[:, c * 128:c * 128 + 128], B.vT[:, c, :], ident[:], [B.vT, ident], [pA])
                    yield
                kb.copy("act", f2(B.tmp[:]), pA[:, :], [pA], [B.tmp])
                yield
                kb.tt("dve", B.vst[:], B.tmp[:, :, 0:64], B.tmp[:, :, 64:128], ALU.add, [B.tmp], [B.vst])
                yield
                for c in range(4):
                    kb.mm(pB[:, c * 128:c * 128 + 128], B.kT[:, c, :], B.kT[:, c, :], True, True, [B.kT], [pB])
                    yield
                for c in range(4):
                    kb.mm(pC[:, c * 128:c * 128 + 128], B.kT[:, c, :], B.qT[:, c, :], True, True, [B.kT, B.qT], [pC])
                    yield
                for c in range(4):
                    kb.ts("dve", B.diag[:, c, :], ident[:], GC[:, dp, c0 + c:c0 + c + 1], None, ALU.mult, None, [ident, GC], [B.diag])
                    yield
                kb.mm(pA[:, :], ones[:], f2(B.diag[:]), True, True, [ones, B.diag], [pA])
                yield
                for c in range(4):
                    kb.ts("dve", B.D[:, c, :], pA[:, c * 128:c * 128 + 128], GC[:, dp, c0 + c:c0 + c + 1], 0.0, ALU.subtract, ALU.min,
                          [pA, GC], [B.D])
                    yield
                kb.tt("dve", f2(B.D[:]), f2(B.D[:]), f2(NM4[d][:]), ALU.add, [B.D, NM4[d]], [B.D])
                yield
                kb.act(f2(B.D[:]), f2(B.D[:]), AF.Exp, [B.D], [B.D])
                yield
                kb.tt("dve", f2(B.attnT[:]), pC[:, :], f2(B.D[:]), ALU.mult, [pC, B.D], [B.attnT])
                yield
                kb.tt("dve", f2(B.N[:]), pB[:, :], f2(B.D[:]), ALU.mult, [pB, B.D], [B.N])
                yield
                kb.tt("dve", f2(B.N[:]), f2(B.N[:]), f2(SM4[d][:]), ALU.mult, [B.N, SM4[d]], [B.N])
                yield
                for c in range(4):
                    kb.act(B.N[:, c, :], B.N[:, c, :], AF.Copy, [B.N, NBETA], [B.N], scale=NBETA[:, dp, c0 + c:c0 + c + 1])
                    yield
                for c in range(4):
                    kb.tr(pA[:, c * 128:c * 128 + 128], B.N[:, c, :], ident[:], [B.N, ident], [pA])
                    yield
                kb.copy("act", f2(B.NT[:]), pA[:, :], [pA], [B.NT])
                yield
                kb.tt("dve", f2(B.R[:]), f2(B.N[:]), f2(I4[:]), ALU.add, [B.N, I4], [B.R])
                yield
                P, PT = B.N, B.NT
                for k in range(5):
                    Pn, PTn = (B.P2, B.PT2) if k % 2 == 0 else (B.N, B.NT)
                    for c in range(4):
                        kb.mm(pC[:, c * 128:c * 128 + 128], P[:, c, :], PT[:, c, :], True, True, [P, PT], [pC])
                        yield
                    if k < 4:
                        for c in range(4):
                            kb.mm(pB[:, c * 128:c * 128 + 128], PT[:, c, :], P[:, c, :], True, True, [P, PT], [pB])
                            yield
                    kb.copy("act", f2(PTn[:]), pC[:, :], [pC], [PTn])
                    yield
                    if k < 4:
                        kb.copy("dve", f2(Pn[:]), pB[:, :], [pB], [Pn])
                        yield
                    for c in range(4):
                        kb.mm(pA[:, c * 128:c * 128 + 128], PTn[:, c, :], B.R[:, c, :], True, True, [PTn, B.R], [pA])
                        yield
                    kb.tt("dve", f2(B.R[:]), f2(B.R[:]), pA[:, :], ALU.add, [B.R, pA], [B.R])
                    yield
                    P, PT = Pn, PTn
                for c in range(4):
                    kb.act(B.kt[:, c, :], B.kBD[:, c, :], AF.Copy, [B.kBD, KTS], [B.kt], scale=KTS[:, dp, c0 + c:c0 + c + 1])
                    yield

            def scan(d, grp):
                B = bufs[d]
                dp, c0 = 3 * d + pp, 4 * grp
                pD = B.banks[3]
                for c in (range(4) if d == 0 else range(3, -1, -1)):
                    ch = c0 + c
                    S_old, S_new = B.S[scnt[d] % 2], B.S[(scnt[d] + 1) % 2]
                    scnt[d] += 1
                    kb.mm(pD[:, 0:64], B.kT[:, c, :], S_old[:], True, True, [B.kT, S_old], [pD])
                    yield
                    kb.stt("dve", B.ntmp[:], pD[:, 0:64], E[:, dp, ch:ch + 1], B.vst[:, c, :], ALU.mult, ALU.subtract,
                           [pD, E, B.vst], [B.ntmp])
                    yield
                    kb.mm(pD[:, 64:128], B.R[:, c, :], B.ntmp[:], True, True, [B.R, B.ntmp], [pD])
                    yield
                    kb.act(B.vnew[:], pD[:, 64:128], AF.Copy, [pD, NBETA], [B.vnew], scale=NBETA[:, dp, ch:ch + 1])
                    yield
                    need_o = not (half_mode and (ch >= 36 or ch < 4))
                    if need_o:
                        kb.mm(pD[:, 128:192], B.qT[:, c, :], S_old[:], True, True, [B.qT, S_old], [pD])
                        yield
                        kb.act(B.o1[:], pD[:, 128:192], AF.Copy, [pD, E], [B.o1], scale=E[:, dp, ch:ch + 1])
                        yield
                        kb.mm(pD[:, 192:256], B.attnT[:, c, :], B.vnew[:], True, True, [B.attnT, B.vnew], [pD])
                        yield
                    if not need_o:
                        pass
                    elif ch not in written:
                        written.add(ch)
                        kb.tt("dve", OB[:, ch, :], B.o1[:], pD[:, 192:256], ALU.add, [B.o1, pD], [OB])
                        yield
                    else:
                        kb.tt("dve", B.o1[:], B.o1[:], pD[:, 192:256], ALU.add, [B.o1, pD], [B.o1])
                        yield
                        kb.tt("dve", OB[:, ch, :], OB[:, ch, :], B.o1[:], ALU.add, [OB, B.o1], [OB])
                        yield
                    kb.mm(pD[:, 256:320], B.kt[:, c, :], B.vnew[:], True, True, [B.kt, B.vnew], [pD])
                    yield
                    kb.stt("dve", S_new[:], S_old[:], EGL[:, dp, ch:ch + 1], pD[:, 256:320], ALU.mult, ALU.add,
                           [S_old, EGL, pD], [S_new])
                    yield

            order = [list(range(17)), [0] + list(range(16, 0, -1))]
            def stream(d):
                for it in range(9 if (half_mode and d == 0) else 17):
                    yield from prep(d, order[d][it])
                    yield from scan(d, order[d][it])

            gens = [stream(0), stream(1)]
            alive = [True, True]
            nstep = [1, 2] if half_mode else [1, 1]
            while any(alive):
                for d in range(2):
                    for _ in range(nstep[d]):
                        if alive[d]:
                            try:
                                next(gens[d])
                            except StopIteration:
                                alive[d] = False
            if pp < 2:
                interleave(out_gen(pp), conv_gen(pp + 1))
            else:
                run_all(out_gen(pp))
    kb.barrier()


def prep_w13(g, wa, wb, W13, nf, sc):
    kb = g.kb
    st = [kb.sbuf("w13s%d" % i, [128, 8, 256], ctx=sc) for i in range(2)]
    sb = [kb.sbuf("w13b%d" % i, [128, 8, 256], BF16, ctx=sc) for i in range(2)]
    for f in range(nf):
        s_, b_ = st[f % 2], sb[f % 2]
        kb.dma("sp", s_[:, :, 0:128], wa[0][:, 128 * f:128 * f + 128].rearrange("(k p) j -> p k j", p=128), [wa[1]], [s_])
        kb.dma("pool", s_[:, :, 128:256], wb[0][:, 128 * f:128 * f + 128].rearrange("(k p) j -> p k j", p=128), [wb[1]], [s_])
        kb.copy("dve" if f % 2 == 0 else "pool", b_[:], s_[:], [s_], [b_])
        kb.dma("sp", W13[f].ap, b_[:], [b_], [W13[f]])


def phase_out_ffn(g, L, xs_tiles, xo_tiles, do_ctx):
    kb = g.kb
    with ExitStack() as sc:
        with ExitStack() as sc2:
            prep_w13(g, (g.ffn_w1.ap[0], g.ffn_w1), (g.ffn_w3.ap[0], g.ffn_w3), g.W13, 22, sc2)
        kb.barrier()
        woutb = kb.sbuf("woutb", [128, 8, D], BF16, ctx=sc)
        w2b = kb.sbuf("w2b", [128, 22, D], BF16, ctx=sc)
        stage = [kb.sbuf("wst%d" % i, [128, D], ctx=sc) for i in range(2)]
        load_cast_weight(g, sc, "wout", g.w_out, lambda k: g.w_out.ap[L, 128 * k:128 * k + 128, :], 8, D, stage, woutb,
                         lambda k: woutb[:, k, :])
        load_cast_weight(g, sc, "w2", g.ffn_w2, lambda k: g.ffn_w2.ap[0, 128 * k:128 * k + 128, :], 22, D, stage, w2b,
                         lambda k: w2b[:, k, :])
        gmsa = kb.sbuf("gmsa", [128, 2, D], ctx=sc)
        gmlp = kb.sbuf("gmlp", [128, 2, D], ctx=sc)
        for s in range(2):
            kb.dma("sp", gmsa[:, s, :], g.gates.ap[L, 0, s:s + 1, :].to_broadcast([128, D]), [g.gates], [gmsa])
            kb.dma("sp", gmlp[:, s, :], g.gates.ap[L, 1, s:s + 1, :].to_broadcast([128, D]), [g.gates], [gmlp])
        scr = {"junk": kb.sbuf("junk", [128, 1024], ctx=sc), "ssq": kb.sbuf("ssq", [128, 1], ctx=sc),
               "xn": kb.sbuf("xn", [128, 1024], ctx=sc), "psT": [g.ps[0], g.ps[1]]}
        mix32 = [kb.sbuf("mix32_%d" % i, [128, 8, 128], ctx=sc) for i in range(2)]
        mixb = [kb.sbuf("mixb_%d" % i, [128, 8, 128], BF16, ctx=sc) for i in range(2)]
        xts = [kb.sbuf("xt%d" % i, [128, D], ctx=sc) for i in range(2)]
        tmpy = kb.sbuf("tmpy", [128, D], ctx=sc)
        x1blk = [kb.sbuf("x1b%d" % i, [128, 4, D], ctx=sc) for i in range(1)]
        h2Ts = [kb.sbuf("h2T%d" % i, [128, 8, 512], BF16, ctx=sc) for i in range(1)]
        gT = kb.sbuf("gT", [128, 22, 512], BF16, ctx=sc)
        w13s = [kb.sbuf("w13_%d" % i, [128, 8, 256], BF16, ctx=sc) for i in range(3)]
        sas = [kb.sbuf("sa%d" % i, [128, 512], ctx=sc) for i in range(2)]
        x2s = [kb.sbuf("x2_%d" % i, [128, D], ctx=sc) for i in range(2)]
        psY = [g.ps[2], g.ps[3]]
        psA, psB = [g.ps[4], g.ps[5]], [g.ps[6], g.ps[7]]
        blocks = ([(0, 0, 2)] if do_ctx else []) + [(1 + i, 2 + 4 * i, 4) for i in range(8)]
        ti = 0
        wi = 0
        for bn, (bi, t0, nt) in enumerate(blocks):
            ntok = nt * 128
            x1b, h2T = x1blk[0], h2Ts[0]
            s = 1 if bi == 0 else 0
            for j in range(nt):
                t = t0 + j
                m32, mb, xt = mix32[ti % 2], mixb[ti % 2], xts[ti % 2]
                ti += 1
                kb.dma("sp", m32[:], g.mixd[:, 128 * t:128 * t + 128].rearrange("(c p) t -> p c t", p=128),
                       [g.MIXdn[t], g.MIXsg[t]] + [g.MIXat[h][bi] for h in range(6)], [m32])
                kb.dma("pool", xt[:], xs_tiles[t].ap, [xs_tiles[t]], [xt])
                kb.copy("pool", mb[:], m32[:], [m32], [mb])
                for half in range(2):
                    for k in range(8):
                        kb.mm(psY[half][:, :], mb[:, k, :], woutb[:, k, 512 * half:512 * half + 512], k == 0, k == 7,
                              [mb, woutb], [psY[half]])
                for half in range(2):
                    kb.tt("dve", tmpy[:, 512 * half:512 * half + 512], psY[half][:, :], gmsa[:, s, 512 * half:512 * half + 512],
                          ALU.mult, [psY[half], gmsa], [tmpy])
                kb.tt("pool", x1b[:, j, :], tmpy[:], xt[:], ALU.add, [tmpy, xt], [x1b])
                norm_tile_to_hT(g, L, 1, x1b, bi == 0, [h2T[:, c, j * 128:j * 128 + 128] for c in range(8)], h2T, scr, xap=x1b[:, j, :])
            for f in range(22):
                w13 = w13s[wi % 3]
                pa, pb, sa = psA[wi % 2], psB[wi % 2], sas[wi % 2]
                wi += 1
                kb.dma("sp" if f % 2 == 0 else "pool", w13[:], g.W13[f].ap, [g.W13[f]], [w13])
                for k in range(8):
                    kb.mm(pa[:, 0:ntok], w13[:, k, 0:128], h2T[:, k, 0:ntok], k == 0, k == 7, [w13, h2T], [pa])
                for k in range(8):
                    kb.mm(pb[:, 0:ntok], w13[:, k, 128:256], h2T[:, k, 0:ntok], k == 0, k == 7, [w13, h2T], [pb])
                kb.act(sa[:, 0:ntok], pa[:, 0:ntok], AF.Silu, [pa], [sa])
                kb.tt("dve", gT[:, f, 0:ntok], sa[:, 0:ntok], pb[:, 0:ntok], ALU.mult, [sa, pb], [gT])
            for j in range(nt):
                t = t0 + j
                x2 = x2s[j % 2]
                for half in range(2):
                    for f in range(22):
                        kb.mm(psY[half][:, :], gT[:, f, j * 128:j * 128 + 128], w2b[:, f, 512 * half:512 * half + 512],
                              f == 0, f == 21, [gT, w2b], [psY[half]])
                for half in range(2):
                    kb.tt("dve", tmpy[:, 512 * half:512 * half + 512], psY[half][:, :], gmlp[:, s, 512 * half:512 * half + 512],
                          ALU.mult, [psY[half], gmlp], [tmpy])
                kb.tt("pool", x2[:], tmpy[:], x1b[:, j, :], ALU.add, [tmpy, x1b], [x2])
                kb.dma("sp", xo_tiles[t].ap, x2[:], [x2], [xo_tiles[t]])
    kb.barrier()


def phase_out_router(g, L, xs_tiles, nblk=8):
    kb = g.kb
    with ExitStack() as sc:
        woutb = kb.sbuf("woutb", [128, 8, D], BF16, ctx=sc)
        stage = [kb.sbuf("wst%d" % i, [128, D], ctx=sc) for i in range(2)]
        load_cast_weight(g, sc, "wout", g.w_out, lambda k: g.w_out.ap[L, 128 * k:128 * k + 128, :], 8, D, stage, woutb,
                         lambda k: woutb[:, k, :])
        gmsa = kb.sbuf("gmsa", [128, D], ctx=sc)
        kb.dma("sp", gmsa[:], g.gates.ap[L, 0, 0:1, :].to_broadcast([128, D]), [g.gates], [gmsa])
        rw = kb.sbuf("rw", [128, 8, NEXP], ctx=sc)
        kb.dma("sp", rw[:], g.router_w.ap[0].rearrange("(k p) e -> p k e", p=128), [g.router_w], [rw])
        rb = kb.sbuf("rb", [128, NEXP], ctx=sc)
        kb.dma("sp", rb[:], g.router_b.ap[0:1, :].to_broadcast([128, NEXP]), [g.router_b], [rb])
        scr = {"junk": kb.sbuf("junk", [128, 1024], ctx=sc), "ssq": kb.sbuf("ssq", [128, 1], ctx=sc),
               "xn": kb.sbuf("xn", [128, 1024], ctx=sc), "psT": [g.ps[0], g.ps[1]]}
        mix32 = [kb.sbuf("mix32_%d" % i, [128, 8, 128], ctx=sc) for i in range(2)]
        mixb = [kb.sbuf("mixb_%d" % i, [128, 8, 128], BF16, ctx=sc) for i in range(2)]
        xts = [kb.sbuf("xt%d" % i, [128, D], ctx=sc) for i in range(2)]
        tmpy = kb.sbuf("tmpy", [128, D], ctx=sc)
        x1s = [kb.sbuf("x1_%d" % i, [128, D], ctx=sc) for i in range(2)]
        h2Ts = [kb.sbuf("h2T%d" % i, [128, 8, 512], BF16, ctx=sc) for i in range(2)]
        h32 = kb.sbuf("h32", [128, 8, 128], ctx=sc)
        lg = kb.sbuf("lg", [128, NEXP], ctx=sc)
        mx8 = kb.sbuf("mx8", [128, 8], ctx=sc)
        msk = kb.sbuf("msk", [128, NEXP], ctx=sc)
        ex = kb.sbuf("ex", [128, NEXP], ctx=sc)
        nm1 = kb.sbuf("nm1", [128, 2], ctx=sc)
        psY = [g.ps[2], g.ps[3]]
        psR = g.ps[4]
        for bi in range(nblk):
            h2T = h2Ts[bi % 2]
            for j in range(4):
                n = 4 * bi + j
                t = 2 + n
                m32, mb, xt, x1 = mix32[n % 2], mixb[n % 2], xts[n % 2], x1s[n % 2]
                kb.dma("sp", m32[:], g.mixd[:, 128 * t:128 * t + 128].rearrange("(c p) t -> p c t", p=128),
                       [g.MIXdn[t], g.MIXsg[t]] + [g.MIXat[h][1 + bi] for h in range(6)], [m32])
                kb.dma("pool", xt[:], xs_tiles[t].ap, [xs_tiles[t]], [xt])
                kb.copy("dve", mb[:], m32[:], [m32], [mb])
                for half in range(2):
                    for k in range(8):
                        kb.mm(psY[half][:, :], mb[:, k, :], woutb[:, k, 512 * half:512 * half + 512], k == 0, k == 7,
                              [mb, woutb], [psY[half]])
                for half in range(2):
                    kb.tt("dve", tmpy[:, 512 * half:512 * half + 512], psY[half][:, :], gmsa[:, 512 * half:512 * half + 512],
                          ALU.mult, [psY[half], gmsa], [tmpy])
                kb.tt("dve", x1[:], tmpy[:], xt[:], ALU.add, [tmpy, xt], [x1])
                kb.dma("sp", g.X1S[n].ap, x1[:], [x1], [g.X1S[n]])
                norm_tile_to_hT(g, L, 1, x1, False, [h2T[:, c, j * 128:j * 128 + 128] for c in range(8)], h2T, scr,
                                also_f32=[h32[:, c, :] for c in range(8)], f32_buf=h32)
                for k in range(8):
                    kb.mm(psR[:, 0:NEXP], h32[:, k, :], rw[:, k, :], k == 0, k == 7, [h32, rw], [psR])
                kb.tt("dve", lg[:], psR[:, 0:NEXP], rb[:], ALU.add, [psR, rb], [lg])
                kb.op("dve", lambda e, o=mx8[:], i=lg[:]: e.max(out=o, in_=i), [lg], [mx8])
                kb.ts("dve", msk[:], lg[:], mx8[:, 1:2], None, ALU.is_ge, None, [lg, mx8], [msk])
                kb.ts("dve", nm1[:, 0:1], mx8[:, 0:1], -1.0, None, ALU.mult, None, [mx8], [nm1])
                kb.act(ex[:], lg[:], AF.Exp, [lg, nm1], [ex], bias=nm1[:, 0:1])
                kb.tt("dve", ex[:], ex[:], msk[:], ALU.mult, [ex, msk], [ex])
                kb.op("dve", lambda e, o=nm1[:, 1:2], i=ex[:]: e.reduce_sum(o, i, AX.X), [ex], [nm1])
                kb.op("dve", lambda e, o=nm1[:, 1:2]: e.reciprocal(o, o), [nm1], [nm1])
                kb.ts("dve", g.GATE[:, n, :], ex[:], nm1[:, 1:2], None, ALU.mult, None, [ex, nm1], [g.GATEb])
            kb.dma("sp", g.H2T[bi].ap, h2T[:], [h2T], [g.H2T[bi]])
    kb.barrier()


def phase_moe(g, L, nblk=8):
    kb = g.kb
    NF = MOE_FF // 128
    with ExitStack() as sc:
        w2b = kb.sbuf("w2b", [128, NF, D], BF16, ctx=sc)
        stage = [kb.sbuf("wst%d" % i, [128, D], ctx=sc) for i in range(2)]
        gmlp = kb.sbuf("gmlp", [128, D], ctx=sc)
        kb.dma("sp", gmlp[:], g.gates.ap[L, 1, 0:1, :].to_broadcast([128, D]), [g.gates], [gmlp])
        fgain = kb.sbuf("fgain", [128, D], ctx=sc)
        kb.dma("sp", fgain[:], g.final_norm_g.ap[0:1, :].to_broadcast([128, D]), [g.final_norm_g], [fgain])
        pst = [kb.sbuf("w13s%d" % i, [128, 8, 256], ctx=sc) for i in range(2)]
        psb = [kb.sbuf("w13b%d" % i, [128, 8, 256], BF16, ctx=sc) for i in range(2)]
        h2Ts = [kb.sbuf("h2T%d" % i, [128, 8, 512], BF16, ctx=sc) for i in range(2)]
        gT = kb.sbuf("gT", [128, NF, 512], BF16, ctx=sc)
        w13s = [kb.sbuf("w13_%d" % i, [128, 8, 256], BF16, ctx=sc) for i in range(3)]
        sas = [kb.sbuf("sa%d" % i, [128, 512], ctx=sc) for i in range(2)]
        tmpy = kb.sbuf("tmpy", [128, D], ctx=sc)
        ya = kb.sbuf("ya", [128, D], ctx=sc)
        x1 = kb.sbuf("x1", [128, D], ctx=sc)
        junk = kb.sbuf("junk", [128, D], ctx=sc)
        ssq = kb.sbuf("ssq", [128, 1], ctx=sc)
        psY = [g.ps[2], g.ps[3]]
        psA, psB = [g.ps[4], g.ps[5]], [g.ps[6], g.ps[7]]
        wi = 0
        hi = 0
        def prep_chunk(e, f):
            s_, b_ = pst[f % 2], psb[f % 2]
            kb.dma("sp", s_[:, :, 0:128], g.moe_w1.ap[0, e, :, 128 * f:128 * f + 128].rearrange("(k p) j -> p k j", p=128),
                   [g.moe_w1], [s_])
            kb.dma("sp", s_[:, :, 128:256], g.moe_w3.ap[0, e, :, 128 * f:128 * f + 128].rearrange("(k p) j -> p k j", p=128),
                   [g.moe_w3], [s_])
            kb.copy("dve", b_[:], s_[:], [s_], [b_])
            kb.dma("sp", g.W13x[e % 2][f].ap, b_[:], [b_], [g.W13x[e % 2][f]])

        def w2_chunk(e, k):
            st = stage[k % 2]
            kb.dma("sp", st[:, :], g.moe_w2.ap[0, e, 128 * k:128 * k + 128, :], [g.moe_w2], [st])
            kb.copy("dve", w2b[:, k, :], st[:, :], [st], [w2b])

        for f in range(NF):
            prep_chunk(0, f)
        for e in range(NEXP):
            for bi in range(nblk):
                h2T = h2Ts[hi % 2]
                hi += 1
                kb.dma("sp", h2T[:], g.H2T[bi].ap, [g.H2T[bi]], [h2T])
                for f in range(NF):
                    if bi == 0:
                        w2_chunk(e, f)
                    if bi == 1 and e + 1 < NEXP:
                        prep_chunk(e + 1, f)
                    w13 = w13s[wi % 3]
                    pa, pb, sa = psA[wi % 2], psB[wi % 2], sas[wi % 2]
                    wi += 1
                    kb.dma("sp", w13[:], g.W13x[e % 2][f].ap, [g.W13x[e % 2][f]], [w13])
                    for k in range(8):
                        kb.mm(pa[:, :], w13[:, k, 0:128], h2T[:, k, :], k == 0, k == 7, [w13, h2T], [pa])
                    for k in range(8):
                        kb.mm(pb[:, :], w13[:, k, 128:256], h2T[:, k, :], k == 0, k == 7, [w13, h2T], [pb])
                    kb.act(sa[:, :], pa[:, :], AF.Silu, [pa], [sa])
                    kb.tt("dve", gT[:, f, :], sa[:, :], pb[:, :], ALU.mult, [sa, pb], [gT])
                for j in range(4):
                    n = 4 * bi + j
                    for half in range(2):
                        for f in range(NF):
                            kb.mm(psY[half][:, :], gT[:, f, j * 128:j * 128 + 128], w2b[:, f, 512 * half:512 * half + 512],
                                  f == 0, f == NF - 1, [gT, w2b], [psY[half]])
                    if e > 0:
                        kb.dma("pool", ya[:], g.YACC[n].ap, [g.YACC[n]], [ya])
                    for half in range(2):
                        kb.act(tmpy[:, 512 * half:512 * half + 512], psY[half][:, :], AF.Copy, [psY[half], g.GATEb], [tmpy],
                               scale=g.GATE[:, n, e:e + 1])
                    if e == 0:
                        kb.dma("sp", g.YACC[n].ap, tmpy[:], [tmpy], [g.YACC[n]])
                        continue
                    kb.tt("dve", ya[:], ya[:], tmpy[:], ALU.add, [ya, tmpy], [ya])
                    if e < NEXP - 1:
                        kb.dma("sp", g.YACC[n].ap, ya[:], [ya], [g.YACC[n]])
                        continue
                    kb.dma("sp", x1[:], g.X1S[n].ap, [g.X1S[n]], [x1])
                    kb.tt("dve", ya[:], ya[:], gmlp[:], ALU.mult, [ya, gmlp], [ya])
                    kb.tt("dve", ya[:], ya[:], x1[:], ALU.add, [ya, x1], [ya])
                    kb.act(junk[:], ya[:], AF.Square, [ya, ssq], [junk, ssq], accum_out=ssq[:, 0:1])
                    rstd_from_ssq(kb, ssq[:, 0:1], ssq[:, 0:1], D, [ssq], [ssq])
                    kb.act(junk[:], ya[:], AF.Copy, [ya, ssq], [junk], scale=ssq[:, 0:1])
                    kb.tt("dve", junk[:], junk[:], fgain[:], ALU.mult, [junk, fgain], [junk])
                    kb.dma("sp", g.outb.ap[128 * n:128 * n + 128, :], junk[:], [junk], [g.outb])
    kb.barrier()


W_NAMES = [("mod_w", [DEPTH, D, 6 * D]), ("mod_b", [DEPTH, 6 * D]), ("norm1_g", [DEPTH, D]), ("norm2_g", [DEPTH, D]),
           ("w_in", [DEPTH, D, IN_COLS]), ("conv_w", [DEPTH, 3, 1152]), ("dn_a_log", [DEPTH, 2, 6]), ("dn_dt_bias", [DEPTH, 2, 6]),
           ("dn_norm_g", [DEPTH, 64]), ("q_norm_g", [DEPTH, 64]), ("k_norm_g", [DEPTH, 64]), ("sgu_norm_g", [DEPTH, 256]),
           ("sgu_w", [DEPTH, 4, 128, 128]), ("sgu_b", [DEPTH, 4, 128]), ("w_out", [DEPTH, D, D]),
           ("ffn_w1", [1, D, D_FF]), ("ffn_w3", [1, D, D_FF]), ("ffn_w2", [1, D_FF, D]),
           ("router_w", [1, D, NEXP]), ("router_b", [1, NEXP]), ("moe_w1", [1, NEXP, D, MOE_FF]),
           ("moe_w3", [1, NEXP, D, MOE_FF]), ("moe_w2", [1, NEXP, MOE_FF, D]), ("final_norm_g", [1, D])]
BLOCKS = [(0, 256)] + [(256 + 512 * i, 512) for i in range(8)]


def build_program(phases=None, debug=(), dbg_in=()):
    nc = bass.Bass("TRN2", target_bir_lowering=False)
    g = G()

    def ext_in(name, shape):
        return Buf(name, nc.dram_tensor(name, list(shape), F32, kind="ExternalInput").ap())

    g.xin = ext_in("xin", [T, D])
    g.cvec = ext_in("cvec", [2, D])
    g.rope = ext_in("rope", [NLAT, 64])
    g.identd = ext_in("ident", [128, 128])
    g.dncd = ext_in("dnc", [7, 128, 128])
    for name, shape in W_NAMES:
        setattr(g, name, ext_in(name, shape))
    out = Buf("out", nc.dram_tensor("out", [NLAT // 2, D], F32, kind="ExternalOutput").ap())
    dbg = {}
    for name, shape in debug:
        dbg[name] = Buf(name, nc.dram_tensor(name, list(shape), F32, kind="ExternalOutput").ap())
    for name, shape in dbg_in:
        dbg[name] = Buf(name, nc.dram_tensor(name, list(shape), F32, kind="ExternalInput").ap())
    g.dbg = dbg

    def scratch(name, shape, dt=F32):
        if name in dbg:
            return dbg[name].ap
        return nc.dram_tensor(name + "_s", list(shape), dt, kind="Internal").ap()

    with ExitStack() as ctx:
        import os as _os
        kb = KB(nc, ctx, same_engine_sync=_os.environ.get("SAMEENG", "1") == "1")
        g.kb = kb
        g.ps = [kb.psum("ps%d" % i, [128, 512]) for i in range(8)]
        g.ident = kb.sbuf("ident", [128, 128])
        kb.dma("sp", g.ident[:], g.identd.ap, [g.identd], [g.ident])
        GS = kb.sbuf("GS", [128, DEPTH, 2, 8, 2])
        SH = kb.sbuf("SH", [128, DEPTH, 2, 8, 2])
        g.GS, g.SH, g.GSb, g.SHb = GS.ap, SH.ap, GS, SH
        g.gates = Buf("gates", scratch("gates", [DEPTH, 2, 2, D]))
        tokd = scratch("TOK", [T, TK_W])
        g.tokd = tokd
        g.TOK = [Buf("TOK%d" % t, tokd[128 * t:128 * t + 128, :]) for t in range(NT)]
        g.zqd = scratch("ZQ", [1408, T])
        g.ZQ = [[Buf("ZQ%d_%d" % (fc, bi), g.zqd[128 * fc:128 * fc + 128, b0:b0 + n]) for bi, (b0, n) in enumerate(BLOCKS)]
                for fc in range(11)]
        g.blk_of_tile = lambda t: (0, t * 128) if t < 2 else (1 + (t - 2) // 4, ((t - 2) % 4) * 128)
        g.mixd = scratch("MIXT", [D, T])
        g.MIXdn = [Buf("MIXdn%d" % t, None) for t in range(NT)]
        g.MIXsg = [Buf("MIXsg%d" % t, None) for t in range(NT)]
        g.MIXat = [[Buf("MIXat%d_%d" % (h, bi), g.mixd[384 + 64 * h:384 + 64 * h + 64, b0:b0 + n])
                    for bi, (b0, n) in enumerate(BLOCKS)] for h in range(6)]
        w13d = scratch("W13", [28, 128, 8, 256], BF16)
        g.W13 = [Buf("W13_%d" % f, w13d[f]) for f in range(28)]
        w13x = scratch("W13X", [2, 28, 128, 8, 256], BF16)
        g.W13x = [[Buf("W13x%d_%d" % (i, f), w13x[i, f]) for f in range(28)] for i in range(2)]
        xs = [[Buf("xin%d" % t, g.xin.ap[128 * t:128 * t + 128, :]) for t in range(NT)]]
        for L in range(DEPTH):
            xd = scratch("XS%d" % (L + 1), [T, D])
            xs.append([Buf("xs%d_%d" % (L + 1, t), xd[128 * t:128 * t + 128, :]) for t in range(NT)])
        g.xs = xs
        g.outb = out
        x1d = scratch("X1S", [NLAT, D])
        g.X1S = [Buf("X1S%d" % n, x1d[128 * n:128 * n + 128, :]) for n in range(32)]
        yd = scratch("YACC", [NLAT, D])
        g.YACC = [Buf("YACC%d" % n, yd[128 * n:128 * n + 128, :]) for n in range(32)]
        h2d = scratch("H2T", [8, 128, 8, 512], BF16)
        g.H2T = [Buf("H2T%d" % b, h2d[b]) for b in range(8)]
        GATE = kb.sbuf("GATE", [128, 32, NEXP])
        g.GATE, g.GATEb = GATE.ap, GATE
        allp = ["mod", "a0", "attn0", "sgu0", "dn0", "ffn0", "a1", "attn1", "sgu1", "dn1", "out1", "moe1"]
        phases = allp if phases is None else phases
        if "mod" in phases:
            for L in range(DEPTH):
                phase_mod(g, L)
        if "a0" in phases:
            phase_a(g, 0, xs[0])
        if "attn0" in phases:
            phase_attn(g, 0, True)
        if "sgu0" in phases:
            phase_sgu(g, 0, list(range(NT)))
        if "dn0" in phases:
            phase_dn(g, 0)
        if "ffn0" in phases:
            phase_out_ffn(g, 0, xs[0], xs[1], True)
        if "a1" in phases:
            phase_a(g, 1, xs[1])
        if "attn1" in phases:
            phase_attn(g, 1, False, nblk=4)
        if "sgu1" in phases:
            phase_sgu(g, 1, list(range(2, 18)))
        if "dn1" in phases:
            phase_dn(g, 1, half_mode=True)
        if "out1" in phases:
            phase_out_router(g, 1, xs[1], nblk=4)
        if "moe1" in phases:
            phase_moe(g, 1, nblk=4)
        kb.barrier()
        kb.emit()
        g.n_ins = kb.n_ins
    return nc, g


def make_inputs(inputs):
    x = np.asarray(inputs["x"], np.float32)
    ctxa = np.asarray(inputs["ctx"], np.float32)
    rows = NLAT // 64
    row = np.repeat(np.arange(rows, dtype=np.float32), 64)
    col = np.tile(np.arange(64, dtype=np.float32), rows)
    inv = (10000.0 ** (-2.0 * np.arange(8, dtype=np.float32) / 32)).astype(np.float32)
    inv = (np.float32(10000.0) ** (-2.0 * np.arange(16, dtype=np.float32) / np.float32(32))).astype(np.float32)
    ang = np.stack([row[:, None] * inv, col[:, None] * inv], axis=1).astype(np.float32)
    rope = np.concatenate([np.cos(ang).reshape(NLAT, 32), np.sin(ang).reshape(NLAT, 32)], axis=1).astype(np.float32)
    shared = {k: np.ascontiguousarray(np.asarray(inputs[k], np.float32)).reshape(shp) for k, shp in W_NAMES}
    perm = np.concatenate([np.arange(C_AQ + 64 * h, C_AQ + 64 * h + 64) for h in (0, 3, 1, 4, 2, 5)])
    cols = np.arange(IN_COLS)
    cols[C_AQ:C_AQ + 384] = perm
    shared["w_in"] = np.ascontiguousarray(shared["w_in"][:, :, cols])
    shared["rope"] = rope
    shared["ident"] = np.eye(128, dtype=np.float32)
    j = np.arange(128)[:, None]
    i = np.arange(128)[None, :]
    blk = (j // 64) == (i // 64)
    dnc = np.zeros((7, 128, 128), np.float32)
    dnc[0] = blk
    dnc[1] = blk & (j <= i)
    dnc[2] = blk & (j >= i)
    dnc[3] = np.where(blk & (i >= j), 0.0, -30000.0)
    dnc[4] = np.where(blk & (i <= j), 0.0, -30000.0)
    dnc[5] = blk & (i > j)
    dnc[6] = blk & (i < j)
    shared["dnc"] = dnc
    rev = dict(shared)
    cols = np.arange(IN_COLS)
    cols[C_BETA:C_BETA + 6], cols[C_BETA + 6:C_BETA + 12] = np.arange(C_BETA + 6, C_BETA + 12), np.arange(C_BETA, C_BETA + 6)
    cols[C_ALPHA:C_ALPHA + 6], cols[C_ALPHA + 6:C_ALPHA + 12] = np.arange(C_ALPHA + 6, C_ALPHA + 12), np.arange(C_ALPHA, C_ALPHA + 6)
    rev["w_in"] = np.ascontiguousarray(shared["w_in"][:, :, cols])
    rev["conv_w"] = np.ascontiguousarray(shared["conv_w"][:, ::-1, :])
    rev["dn_a_log"] = np.ascontiguousarray(shared["dn_a_log"][:, ::-1, :])
    rev["dn_dt_bias"] = np.ascontiguousarray(shared["dn_dt_bias"][:, ::-1, :])
    rev["sgu_w"] = np.ascontiguousarray(shared["sgu_w"][:, :, ::-1, ::-1])
    rev["sgu_b"] = np.ascontiguousarray(shared["sgu_b"][:, :, ::-1])
    rev["rope"] = np.ascontiguousarray(rope[::-1])
    maps = []
    cv = np.asarray(inputs["c"], np.float32)
    cc = np.asarray(inputs["c_ctx"], np.float32)
    for b in range(4):
        for tw in range(2):
            m = dict(rev if tw else shared)
            if tw:
                m["xin"] = np.ascontiguousarray(np.concatenate([ctxa[b][::-1], x[b][::-1]], axis=0))
            else:
                m["xin"] = np.ascontiguousarray(np.concatenate([ctxa[b], x[b]], axis=0))
            m["cvec"] = np.ascontiguousarray(np.stack([cv[b], cc], 0))
            maps.append(m)
    return maps


def kernel(**inputs):
    nc, g = build_program()
    maps = make_inputs(inputs)
    res = run_bass_kernel_spmd(nc, maps, core_ids=list(range(len(maps))))
    out = np.empty((4, NLAT, D), np.float32)
    for b in range(4):
        out[b, :NLAT // 2] = res.results[2 * b]["out"]
        out[b, NLAT // 2:] = res.results[2 * b + 1]["out"][::-1]
    return out
```
